# Optimizing a Trainium2 kernel written in Bass

```python
import jax
import jax.numpy as jnp
from jax import lax
import numpy as np

D_MODEL = 1024
BATCH = 2
SEQ = 16384
DEPTH = 2

N_MIXERS = 4
GROUP_W = D_MODEL // N_MIXERS
D_MIX = N_MIXERS * GROUP_W
HEAD_DIM = 64
CONV_WIDTH = 3
MLA_HEADS = GROUP_W // HEAD_DIM
MLA_NOPE = HEAD_DIM
MLA_ROPE = HEAD_DIM // 2
MLA_V = HEAD_DIM
MLA_QK = MLA_NOPE + MLA_ROPE
Q_LORA = 3 * D_MODEL // 16
KV_LORA = D_MODEL // 8
ATTN_BLOCK = 128
RET_HEADS = GROUP_W // HEAD_DIM
RET_DK = HEAD_DIM
RET_DV = HEAD_DIM
RET_CHUNK = 128
LRU_WIDTH = GROUP_W
LRU_BLOCKS = 4
LRU_BLOCK = LRU_WIDTH // LRU_BLOCKS
LRU_CONV = 4
LRU_C = 8.0
MOE_GROUPS = 4
EXPERTS_PER_GROUP = 8
N_EXPERTS = MOE_GROUPS * EXPERTS_PER_GROUP
MOE_TOPK = 2
D_EXPERT = D_MODEL // 4

ROPE_BASE = 10000.0
EPS = 1e-6

CONV_COLS = 3 * GROUP_W
MLA_COLS = Q_LORA + KV_LORA + MLA_ROPE
RET_COLS = 4 * GROUP_W
LRU_COLS = 2 * LRU_WIDTH
IN_COLS = CONV_COLS + MLA_COLS + RET_COLS + LRU_COLS

kernel_name = 'hymba_style_hybrid_hmoe_block'


def rms_norm(x, g):
    xf = x.astype(jnp.float32)
    y = xf * lax.rsqrt(jnp.mean(xf * xf, axis=-1, keepdims=True) + EPS)
    return (y * g.astype(jnp.float32)).astype(x.dtype)


def rope_tables(positions, dim):
    inv = 1.0 / (ROPE_BASE ** (jnp.arange(0, dim, 2, dtype=jnp.float32) / dim))
    ang = positions.astype(jnp.float32)[..., None] * inv
    return jnp.cos(ang), jnp.sin(ang)


def apply_rope(x, cos, sin):
    x1, x2 = jnp.split(x.astype(jnp.float32), 2, axis=-1)
    out = jnp.concatenate([x1 * cos - x2 * sin, x2 * cos + x1 * sin], axis=-1)
    return out.astype(x.dtype)


def causal_depthwise_conv(x, w):
    width, ch = w.shape
    return lax.conv_general_dilated(
        x, w[:, None, :].astype(x.dtype), window_strides=(1,),
        padding=[(width - 1, 0)], dimension_numbers=('NWC', 'WIO', 'NWC'),
        feature_group_count=ch)


def short_conv_mixer(u, conv_w):
    b_gate, c_gate, xin = jnp.split(u, 3, axis=-1)
    return b_gate * causal_depthwise_conv(c_gate * xin, conv_w)


def causal_block_attention(q, k, v):
    bsz, nh, s, dq = q.shape
    nb = s // ATTN_BLOCK
    scale = dq ** -0.5
    q_blocks = q.reshape(bsz, nh, nb, ATTN_BLOCK, dq).transpose(2, 0, 1, 3, 4)
    key_pos = jnp.arange(s)

    def one_block(args):
        qb, bi = args
        sc = jnp.einsum('bhqd,bhkd->bhqk', qb, k).astype(jnp.float32) * scale
        q_pos = bi * ATTN_BLOCK + jnp.arange(ATTN_BLOCK)
        sc = jnp.where(key_pos[None, :] <= q_pos[:, None], sc, -jnp.inf)
        p = jax.nn.softmax(sc, axis=-1).astype(v.dtype)
        return jnp.einsum('bhqk,bhkd->bhqd', p, v)

    out = lax.map(one_block, (q_blocks, jnp.arange(nb)))
    return out.transpose(1, 2, 0, 3, 4).reshape(bsz, nh, s, v.shape[-1])


def mla_mixer(u, cos, sin, q_norm_g, w_uq, kv_norm_g, w_ukv, q_qk_g, k_qk_g):
    bsz, s, _ = u.shape
    cq, ckv, k_rope = jnp.split(u, [Q_LORA, Q_LORA + KV_LORA], axis=-1)
    q = (rms_norm(cq, q_norm_g) @ w_uq).reshape(bsz, s, MLA_HEADS, MLA_QK)
    kv = (rms_norm(ckv, kv_norm_g) @ w_ukv).reshape(bsz, s, MLA_HEADS, MLA_NOPE + MLA_V)
    q_nope, q_rope = jnp.split(q, [MLA_NOPE], axis=-1)
    k_nope, v = jnp.split(kv, [MLA_NOPE], axis=-1)
    q_nope = rms_norm(q_nope, q_qk_g[:MLA_NOPE])
    q_rope = rms_norm(q_rope, q_qk_g[MLA_NOPE:])
    k_nope = rms_norm(k_nope, k_qk_g[:MLA_NOPE])
    k_rope = rms_norm(k_rope, k_qk_g[MLA_NOPE:])
    q_rope = apply_rope(q_rope, cos[:, :, None, :], sin[:, :, None, :])
    k_rope = apply_rope(k_rope, cos, sin)
    k_rope = jnp.broadcast_to(k_rope[:, :, None, :], (bsz, s, MLA_HEADS, MLA_ROPE))
    q = jnp.concatenate([q_nope, q_rope], axis=-1).transpose(0, 2, 1, 3)
    k = jnp.concatenate([k_nope, k_rope], axis=-1).transpose(0, 2, 1, 3)
    out = causal_block_attention(q, k, v.transpose(0, 2, 1, 3))
    return out.transpose(0, 2, 1, 3).reshape(bsz, s, MLA_HEADS * MLA_V)


def chunkwise_retention(q, k, v):
    bsz, s, nh, dk = q.shape
    dv = v.shape[-1]
    n = s // RET_CHUNK
    log_g = jnp.log(1.0 - 2.0 ** (-5.0 - jnp.arange(nh, dtype=jnp.float32)))
    idx = jnp.arange(RET_CHUNK, dtype=jnp.float32)
    rel = idx[:, None] - idx[None, :]
    inner = jnp.where(rel >= 0, jnp.exp(log_g[:, None, None] * jnp.maximum(rel, 0.0)), 0.0)
    q_dec = jnp.exp(log_g[:, None] * (idx + 1.0))[None, :, :, None]
    k_dec = jnp.exp(log_g[:, None] * (RET_CHUNK - 1.0 - idx))[None, :, :, None]
    chunk_dec = jnp.exp(log_g * RET_CHUNK)[None, :, None, None]

    def to_chunks(t):
        return t.astype(jnp.float32).reshape(bsz, n, RET_CHUNK, nh, t.shape[-1]).transpose(1, 0, 3, 2, 4)

    def step(state, xs):
        qi, ki, vi = xs
        scores = jnp.einsum('bhnd,bhmd->bhnm', qi, ki) * inner
        o = (jnp.einsum('bhnm,bhme->bhne', scores, vi)
             + jnp.einsum('bhnd,bhde->bhne', qi, state) * q_dec)
        state = chunk_dec * state + jnp.einsum('bhmd,bhme->bhde', ki * k_dec, vi)
        return state, o

    init = jnp.zeros((bsz, nh, dk, dv), jnp.float32)
    _, out = lax.scan(step, init, (to_chunks(q), to_chunks(k), to_chunks(v)))
    return out.transpose(1, 0, 3, 2, 4).reshape(bsz, s, nh, dv)


def retention_mixer(u, cos, sin):
    bsz, s, _ = u.shape
    q, k, v, g = jnp.split(u, 4, axis=-1)
    heads = lambda t: t.reshape(bsz, s, RET_HEADS, HEAD_DIM)
    q = apply_rope(heads(q), cos[:, :, None, :], sin[:, :, None, :])
    k = apply_rope(heads(k), cos[:, :, None, :], sin[:, :, None, :]) * (RET_DK ** -0.5)
    r = chunkwise_retention(q, k, heads(v))
    mu = jnp.mean(r, axis=-1, keepdims=True)
    var = jnp.mean(jnp.square(r - mu), axis=-1, keepdims=True)
    y = ((r - mu) * lax.rsqrt(var + EPS)).reshape(bsz, s, GROUP_W).astype(u.dtype)
    return jax.nn.silu(g) * y


def rglru_mixer(u, conv_w, conv_b, w_a, b_a, w_x, b_x, lam):
    bsz, s, _ = u.shape
    xb, gate = jnp.split(u, 2, axis=-1)
    xb = causal_depthwise_conv(xb, conv_w) + conv_b
    xblk = xb.reshape(bsz, s, LRU_BLOCKS, LRU_BLOCK)
    r = jax.nn.sigmoid(jnp.einsum('bsnc,ncd->bsnd', xblk, w_a).reshape(bsz, s, LRU_WIDTH) + b_a)
    i = jax.nn.sigmoid(jnp.einsum('bsnc,ncd->bsnd', xblk, w_x).reshape(bsz, s, LRU_WIDTH) + b_x)
    log_a = -LRU_C * r.astype(jnp.float32) * jax.nn.softplus(-lam.astype(jnp.float32))
    a = jnp.exp(log_a)
    b_in = jnp.sqrt(-jnp.expm1(2.0 * log_a)) * (i * xb).astype(jnp.float32)

    def combine(left, right):
        a1, b1 = left
        a2, b2 = right
        return a1 * a2, a2 * b1 + b2

    _, h = lax.associative_scan(combine, (a, b_in), axis=1)
    return h.astype(u.dtype) * jax.nn.gelu(gate)


def hierarchical_moe(h, wg_r, bg_r, we_r, be_r, w_gate, w_up, w_down):
    bsz, s, d = h.shape
    t = h.reshape(-1, d)
    group_prob = jax.nn.softmax((t @ wg_r).astype(jnp.float32), axis=-1)
    g_onehot = jax.nn.one_hot(jnp.argmax(group_prob + bg_r.astype(jnp.float32), axis=-1),
                              MOE_GROUPS, dtype=jnp.float32)
    g_weight = jnp.sum(group_prob * g_onehot, axis=-1)
    exp_logits = (t @ we_r).astype(jnp.float32).reshape(-1, MOE_GROUPS, EXPERTS_PER_GROUP)
    exp_prob = jax.nn.softmax(jnp.einsum('tge,tg->te', exp_logits, g_onehot), axis=-1)
    sel_bias = jnp.einsum('ge,tg->te', be_r.astype(jnp.float32).reshape(MOE_GROUPS, EXPERTS_PER_GROUP), g_onehot)
    _, top_idx = lax.top_k(exp_prob + sel_bias, MOE_TOPK)
    top_p = jnp.take_along_axis(exp_prob, top_idx, axis=-1)
    top_w = top_p / jnp.sum(top_p, axis=-1, keepdims=True) * g_weight[:, None]
    e_weight = jnp.sum(jax.nn.one_hot(top_idx, EXPERTS_PER_GROUP, dtype=jnp.float32) * top_w[..., None], axis=1)
    combine = (g_onehot[:, :, None] * e_weight[:, None, :]).reshape(-1, N_EXPERTS).astype(h.dtype)
    y = jnp.zeros_like(t)
    for gi in range(MOE_GROUPS):
        sl = slice(gi * EXPERTS_PER_GROUP, (gi + 1) * EXPERTS_PER_GROUP)
        hid = jax.nn.silu(jnp.einsum('td,edf->tef', t, w_gate[sl])) * jnp.einsum('td,edf->tef', t, w_up[sl])
        y = y + jnp.einsum('tef,efd->td', hid * combine[:, sl, None], w_down[sl])
    return y.reshape(bsz, s, d)


def setup_inputs(seed: int = 0) -> dict:
    key = jax.random.key(seed)
    k = jax.random.split(key, 32)
    L = DEPTH

    def nrm(kk, shape, scale):
        return jax.random.normal(kk, shape, jnp.float32) * scale

    def gain(kk, shape):
        return 1.0 + 0.02 * jax.random.normal(kk, shape, jnp.float32)

    x = nrm(k[0], (BATCH, SEQ, D_MODEL), 1.0)
    c = nrm(k[1], (BATCH, D_MODEL), 1.0)
    offset = jax.random.randint(k[2], (BATCH, 1), 0, 4096, dtype=jnp.int32)
    positions = offset + jnp.arange(SEQ, dtype=jnp.int32)[None, :]
    u = jax.random.uniform(k[3], (L, LRU_WIDTH), jnp.float32, 0.9, 0.999)
    sa = u ** (1.0 / LRU_C)
    lru_lambda = jnp.log(sa) - jnp.log1p(-sa)
    return {
        'x': x,
        'c': c,
        'positions': positions,
        'ada_w': nrm(k[4], (L, D_MODEL, 6 * D_MODEL), 0.5 * D_MODEL ** -0.5),
        'ada_b': nrm(k[5], (L, 6 * D_MODEL), 0.01),
        'norm_mix_g': gain(k[6], (L, D_MODEL)),
        'w_in': nrm(k[7], (L, D_MODEL, IN_COLS), D_MODEL ** -0.5),
        'conv_w': nrm(k[8], (L, CONV_WIDTH, GROUP_W), CONV_WIDTH ** -0.5),
        'mla_q_norm_g': gain(k[9], (L, Q_LORA)),
        'mla_w_uq': nrm(k[10], (L, Q_LORA, MLA_HEADS * MLA_QK), Q_LORA ** -0.5),
        'mla_kv_norm_g': gain(k[11], (L, KV_LORA)),
        'mla_w_ukv': nrm(k[12], (L, KV_LORA, MLA_HEADS * (MLA_NOPE + MLA_V)), KV_LORA ** -0.5),
        'mla_q_qk_g': gain(k[13], (L, MLA_QK)),
        'mla_k_qk_g': gain(k[14], (L, MLA_QK)),
        'lru_conv_w': nrm(k[15], (L, LRU_CONV, LRU_WIDTH), LRU_CONV ** -0.5),
        'lru_conv_b': nrm(k[16], (L, LRU_WIDTH), 0.01),
        'lru_w_a': nrm(k[17], (L, LRU_BLOCKS, LRU_BLOCK, LRU_BLOCK), LRU_BLOCK ** -0.5),
        'lru_b_a': nrm(k[18], (L, LRU_WIDTH), 0.01),
        'lru_w_x': nrm(k[19], (L, LRU_BLOCKS, LRU_BLOCK, LRU_BLOCK), LRU_BLOCK ** -0.5),
        'lru_b_x': nrm(k[20], (L, LRU_WIDTH), 0.01),
        'lru_lambda': lru_lambda,
        'mix_norm_g': gain(k[21], (L, D_MIX)),
        'w_out': nrm(k[22], (L, D_MIX, D_MODEL), D_MIX ** -0.5),
        'norm_ffn_g': gain(k[23], (L, D_MODEL)),
        'router_group_w': nrm(k[24], (L, D_MODEL, MOE_GROUPS), D_MODEL ** -0.5),
        'router_group_b': nrm(k[25], (L, MOE_GROUPS), 0.01),
        'router_expert_w': nrm(k[26], (L, D_MODEL, N_EXPERTS), D_MODEL ** -0.5),
        'router_expert_b': nrm(k[27], (L, N_EXPERTS), 0.01),
        'exp_w_gate': nrm(k[28], (L, N_EXPERTS, D_MODEL, D_EXPERT), D_MODEL ** -0.5),
        'exp_w_up': nrm(k[29], (L, N_EXPERTS, D_MODEL, D_EXPERT), D_MODEL ** -0.5),
        'exp_w_down': nrm(k[30], (L, N_EXPERTS, D_EXPERT, D_MODEL), D_EXPERT ** -0.5),
    }


def reference(x, c, positions, ada_w, ada_b, norm_mix_g, w_in, conv_w, mla_q_norm_g, mla_w_uq,
              mla_kv_norm_g, mla_w_ukv, mla_q_qk_g, mla_k_qk_g, lru_conv_w, lru_conv_b, lru_w_a,
              lru_b_a, lru_w_x, lru_b_x, lru_lambda, mix_norm_g, w_out, norm_ffn_g, router_group_w,
              router_group_b, router_expert_w, router_expert_b, exp_w_gate, exp_w_up, exp_w_down):
    bsz, s, _ = x.shape
    cos_mla, sin_mla = rope_tables(positions, MLA_ROPE)
    cos_ret, sin_ret = rope_tables(positions, RET_DK)
    c_act = jax.nn.silu(c)
    splits = [CONV_COLS, CONV_COLS + MLA_COLS, CONV_COLS + MLA_COLS + RET_COLS]
    for l in range(DEPTH):
        mod = c_act @ ada_w[l] + ada_b[l]
        sh_m, sc_m, gt_m, sh_f, sc_f, gt_f = [m[:, None, :] for m in jnp.split(mod, 6, axis=-1)]
        h = rms_norm(x, norm_mix_g[l]) * (1.0 + sc_m) + sh_m
        u = h @ w_in[l]
        u_conv, u_mla, u_ret, u_lru = jnp.split(u, splits, axis=-1)
        y_conv = short_conv_mixer(u_conv, conv_w[l])
        y_mla = mla_mixer(u_mla, cos_mla, sin_mla, mla_q_norm_g[l], mla_w_uq[l], mla_kv_norm_g[l],
                          mla_w_ukv[l], mla_q_qk_g[l], mla_k_qk_g[l])
        y_ret = retention_mixer(u_ret, cos_ret, sin_ret)
        y_lru = rglru_mixer(u_lru, lru_conv_w[l], lru_conv_b[l], lru_w_a[l], lru_b_a[l],
                            lru_w_x[l], lru_b_x[l], lru_lambda[l])
        y = jnp.stack([y_conv, y_mla, y_ret, y_lru], axis=2)
        y = rms_norm(y, mix_norm_g[l].reshape(N_MIXERS, GROUP_W)).reshape(bsz, s, D_MIX)
        x = x + gt_m * (y @ w_out[l])
        h = rms_norm(x, norm_ffn_g[l]) * (1.0 + sc_f) + sh_f
        x = x + gt_f * hierarchical_moe(h, router_group_w[l], router_group_b[l], router_expert_w[l],
                                        router_expert_b[l], exp_w_gate[l], exp_w_up[l], exp_w_down[l])
    return x
```

```python
import math
import numpy as np
import ml_dtypes
import concourse.bass as bass
import concourse.mybir as mybir
from concourse.bass_utils import run_bass_kernel_spmd

F32 = mybir.dt.float32
BF16 = mybir.dt.bfloat16
I32 = mybir.dt.int32
AF = mybir.ActivationFunctionType
ALU = mybir.AluOpType
AX = mybir.AxisListType

D = 1024
S = 16384
NCORE = 8
TPC = 4096
IN_COLS = 2656
EPS = 1e-6
TWO_PI = 2.0 * math.pi

ENGS = ["sync", "scalar", "vector", "gpsimd", "tensor"]
SAME_ENGINE_SYNC = {"sync": False, "scalar": True, "vector": True, "gpsimd": True, "tensor": False}
_APT = None


class Buf:
    __slots__ = ("name", "w", "r")

    def __init__(self, name=""):
        self.name = name
        self.w = None
        self.r = []


class Prog:
    NPOOL = 16

    def __init__(self, nc):
        self.nc = nc
        self.ops = {e: [] for e in ENGS}
        self.cnt = {e: 0 for e in ENGS}
        self.esem = {e: nc.alloc_semaphore("es_" + e) for e in ENGS}
        self.known = {e: {f: 0 for f in ENGS} for e in ENGS}
        self.snap = {e: [None] for e in ENGS}
        self.dq = ["sync", "scalar", "gpsimd"]
        self.pool = {q: [nc.alloc_semaphore("dp_%s_%d" % (q, i)) for i in range(self.NPOOL)] for q in self.dq}
        self.pool_val = {q: [0] * self.NPOOL for q in self.dq}
        self.pool_next = {q: 0 for q in self.dq}
        self.dknown = {e: {} for e in ENGS}
        self.bufs = {}
        self.uid = 0
        self.prefix = ""

    def sb(self, name, shape, dt=F32):
        return self.nc.alloc_sbuf_tensor(self.prefix + name, list(shape), dt)

    def ps(self, name, shape, dt=F32):
        return self.nc.alloc_psum_tensor(self.prefix + name, list(shape), dt)

    def mark(self):
        nc = self.nc
        return (nc.psum_base, nc.psum_top, nc.sbuf_base, nc.sbuf_top)

    def release(self, mk):
        self.barrier()
        nc = self.nc
        nc.psum_base, nc.psum_top, nc.sbuf_base, nc.sbuf_top = mk

    def barrier(self):
        for e in ENGS:
            waits = []
            for f in ENGS:
                if f != e and self.cnt[f] > 0:
                    self._need(e, ("E", f, self.cnt[f]), waits)
            for q in self.dq:
                for i in range(self.NPOOL):
                    v = self.pool_val[q][i]
                    if v > 0:
                        self._need(e, ("D", q, i, v, None), waits)
            self.ops[e].append((waits, None, None, None, 0))
        self.bufs = {}

    def buf_of(self, ap):
        n = ap.name
        b = self.bufs.get(n)
        if b is None:
            b = self.bufs[n] = Buf(n)
        return b

    def _merge(self, eng, sn):
        if sn is None:
            return
        kn = self.known[eng]
        for g, v in sn[0].items():
            if kn[g] < v:
                kn[g] = v
        dk = self.dknown[eng]
        for k, v in sn[1].items():
            if dk.get(k, 0) < v:
                dk[k] = v

    def _need(self, eng, ev, waits):
        if ev is None:
            return
        if ev[0] == "E":
            _, f, seq = ev
            if f == eng and not SAME_ENGINE_SYNC[eng]:
                return
            if self.known[eng][f] >= seq:
                return
            waits.append((self.esem[f], seq))
            self.known[eng][f] = seq
            self._merge(eng, self.snap[f][seq])
        else:
            _, q, i, val, sn = ev
            if self.dknown[eng].get((q, i), 0) >= val:
                return
            waits.append((self.pool[q][i], val))
            self.dknown[eng][(q, i)] = val
            self._merge(eng, sn)

    def _deps(self, eng, reads, writes, waits):
        for b in reads:
            self._need(eng, b.w, waits)
        for b in writes:
            self._need(eng, b.w, waits)
            for ev in b.r:
                self._need(eng, ev, waits)

    def _commit(self, ev, reads, writes):
        for b in reads:
            if b in writes:
                continue
            b.r.append(ev)
            if len(b.r) > 16:
                last = {}
                keep = []
                for e in b.r:
                    if e[0] == "E":
                        last[e[1]] = e
                    else:
                        keep.append(e)
                b.r = keep[-10:] + list(last.values())
        for b in writes:
            b.w = ev
            b.r = []

    def _scan(self, kwargs):
        reads, writes = [], []
        for k, v in kwargs.items():
            if isinstance(v, _APT):
                b = self.buf_of(v)
                if k in ("out", "accum_out", "out_max", "out_indices"):
                    if b not in writes:
                        writes.append(b)
                elif b not in reads:
                    reads.append(b)
        return reads, writes

    def I(self, eng, meth, **kwargs):
        xr = kwargs.pop("_reads", ())
        xw = kwargs.pop("_writes", ())
        reads, writes = self._scan(kwargs)
        reads += [self.buf_of(a) for a in xr]
        writes += [self.buf_of(a) for a in xw]
        waits = []
        self._deps(eng, reads, writes, waits)
        self.cnt[eng] += 1
        seq = self.cnt[eng]
        self.snap[eng].append((dict(self.known[eng]), dict(self.dknown[eng])))
        self.ops[eng].append((waits, meth, kwargs, self.esem[eng], 1))
        ev = ("E", eng, seq)
        self._commit(ev, reads, writes)
        return ev

    def dma(self, q, out, in_, **kw):
        reads = [self.buf_of(in_)]
        writes = [self.buf_of(out)]
        waits = []
        self._deps(q, reads, writes, waits)
        i = self.pool_next[q]
        self.pool_next[q] = (i + 1) % self.NPOOL
        prev = self.pool_val[q][i]
        if prev > 0 and self.dknown[q].get((q, i), 0) < prev:
            waits.append((self.pool[q][i], prev))
            self.dknown[q][(q, i)] = prev
        val = prev + 16
        self.pool_val[q][i] = val
        sn = (dict(self.known[q]), dict(self.dknown[q]))
        kw = dict(kw)
        kw["out"] = out
        kw["in_"] = in_
        self.ops[q].append((waits, "dma_start", kw, self.pool[q][i], 16))
        ev = ("D", q, i, val, sn)
        self._commit(ev, reads, writes)
        return ev

    def coll(self, kind, ins, outs, groups):
        q = "gpsimd"
        reads = [self.buf_of(a) for a in ins]
        writes = [self.buf_of(a) for a in outs]
        waits = []
        self._deps(q, reads, writes, waits)
        i = self.pool_next[q]
        self.pool_next[q] = (i + 1) % self.NPOOL
        prev = self.pool_val[q][i]
        if prev > 0 and self.dknown[q].get((q, i), 0) < prev:
            waits.append((self.pool[q][i], prev))
            self.dknown[q][(q, i)] = prev
        val = prev + 1
        self.pool_val[q][i] = val
        sn = (dict(self.known[q]), dict(self.dknown[q]))
        kw = dict(kind=kind, op=ALU.bypass, replica_groups=groups, ins=[a_.opt() for a_ in ins], outs=[a_.opt() for a_ in outs])
        self.ops[q].append((waits, "collective_compute", kw, self.pool[q][i], 1))
        ev = ("D", q, i, val, sn)
        self._commit(ev, reads, writes)
        return ev

    def finish(self, aps, eng="sync"):
        waits = []
        for a in aps:
            self._need(eng, self.buf_of(a).w, waits)
        self.ops[eng].append((waits, None, None, None, 0))

    def emit(self):
        nc = self.nc
        with nc.Block() as block:
            def mk(ename):
                def body(e):
                    for waits, meth, kw, sem, inc in self.ops[ename]:
                        for (s, v) in waits:
                            e.wait_ge(s, v)
                        if meth is not None:
                            getattr(e, meth)(**kw).then_inc(sem, inc)
                return body
            block.sync(mk("sync"))
            block.scalar(mk("scalar"))
            block.vector(mk("vector"))
            block.gpsimd(mk("gpsimd"))
            block.tensor(mk("tensor"))


class Rot:
    def __init__(self, P, name, shape, dt, n, psum=False):
        self.t = [(P.ps if psum else P.sb)("%s%d" % (name, i), shape, dt) for i in range(n)]
        self.i = 0

    def next(self):
        t = self.t[self.i % len(self.t)]
        self.i += 1
        return t


def new_nc():
    global _APT
    nc = bass.Bass("TRN2", target_bir_lowering=False)
    if _APT is None:
        t = nc.dram_tensor("apt_probe", [2, 2], F32).ap()
        _APT = type(t)
    return nc


class IO:
    def __init__(self, nc, m=None):
        self.nc = nc
        self.m = m or {}
        self.outs = []

    def inp(self, name, shape, dt=F32):
        if name in self.m:
            return self.m[name]
        return din(self.nc, name, shape, dt)

    def out(self, name, shape, dt=F32):
        if name in self.m:
            return self.m[name]
        ap = dout(self.nc, name, shape, dt)
        self.outs.append(ap)
        return ap


def din(nc, name, shape, dt=F32):
    return nc.dram_tensor(name, list(shape), dt, kind="ExternalInput").ap()


def dout(nc, name, shape, dt=F32):
    return nc.dram_tensor(name, list(shape), dt, kind="ExternalOutput").ap()


def make_identities(P):
    idf = P.sb("ident_f", [128, 128], F32)
    idb = P.sb("ident_b", [128, 128], BF16)
    P.I("gpsimd", "memset", ap=idf[:], constant=1.0, _writes=[idf[:]])
    P.I("gpsimd", "affine_select", out=idf[:], in_=idf[:], pattern=[[1, 128]], compare_op=ALU.is_equal,
        fill=0.0, base=0, channel_multiplier=-1)
    P.I("vector", "tensor_copy", out=idb[:], in_=idf[:])
    return idf, idb


def compute_mod(P, c_ap, adaw_ap, adab_ap, psA, psB, c0, c1, CH=256):
    n = c1 - c0
    mod = P.sb("mod_bc", [128, n], F32)
    ccol = P.sb("ccol", [128, 8], F32)
    cbc = P.sb("cbc", [128, 8, 128], F32)
    P.dma("sync", mod[:], adab_ap[c0:c1].partition_broadcast(128))
    P.dma("sync", ccol[:], c_ap.rearrange("(k p) -> p k", p=128), allow_slow_non_contiguous=True)
    P.I("scalar", "activation", out=ccol[:], in_=ccol[:], func=AF.Silu)
    P.I("vector", "tensor_copy", out=cbc[:], in_=ccol[:].unsqueeze(2).to_broadcast([128, 8, 128]))
    wr = Rot(P, "adaw_t", [128, 8, CH], F32, 2)
    awv = adaw_ap.rearrange("(k p) n -> p k n", p=128)
    for i in range(n // CH):
        wt = wr.next()
        P.dma("sync" if i % 2 == 0 else "scalar", wt[:], awv[:, :, c0 + i * CH:c0 + (i + 1) * CH])
        ps = psA if i % 2 == 0 else psB
        for k in range(8):
            P.I("tensor", "matmul", out=ps[:, 0:CH], lhsT=cbc[:, k, :], rhs=wt[:, k, :], start=(k == 0), stop=(k == 7))
        P.I("vector", "tensor_tensor", out=mod[:, i * CH:(i + 1) * CH], in0=mod[:, i * CH:(i + 1) * CH],
            in1=ps[:, 0:CH], op=ALU.add)
    return mod


def sin_of(P, out_ap, ang_ap, tmp_ap, tmpi_ap, shift):
    P.I("vector", "tensor_scalar", out=tmp_ap, in0=ang_ap, scalar1=1.0 / TWO_PI, scalar2=shift / TWO_PI, op0=ALU.mult, op1=ALU.add)
    P.I("vector", "tensor_copy", out=tmpi_ap, in_=tmp_ap)
    P.I("vector", "tensor_tensor", out=tmp_ap, in0=tmp_ap, in1=tmpi_ap, op=ALU.subtract)
    P.I("scalar", "activation", out=out_ap, in_=tmp_ap, func=AF.Sin, scale=TWO_PI)


def build_p1(ntok=TPC):
    nc = new_nc()
    P = Prog(nc)
    io = IO(nc)
    phase1(P, io, ntok)
    P.finish(io.outs)
    P.emit()
    return nc


def phase1(P, io, ntok):
    nc = P.nc
    if hasattr(P, "mla_tiles"):
        del P.mla_tiles
    NST = ntok // 512
    x_in = io.inp("x_in", [ntok, D])
    c_in = io.inp("c_in", [D])
    pos_in = io.inp("pos_in", [ntok], I32)
    adaw = io.inp("ada_w", [D, 6 * D])
    adab = io.inp("ada_b", [6 * D])
    gmix = io.inp("norm_mix_g", [D])
    w_in = io.inp("w_in", [D, IN_COLS])
    qng = io.inp("mla_q_norm_g", [192])
    wuq = io.inp("mla_w_uq", [192, 384])
    kvng = io.inp("mla_kv_norm_g", [128])
    wukv = io.inp("mla_w_ukv", [128, 512])
    qqk = io.inp("mla_q_qk_g", [96])
    kqk = io.inp("mla_k_qk_g", [96])
    inv_ret4 = io.inp("inv_ret4", [128, 1])
    inv_mla = io.inp("inv_mla", [16])
    attq = io.out("attq", [4, 96, ntok], BF16)
    attk = io.out("attk", [4, 96, ntok], BF16)
    attv = io.out("attv", [4, ntok, 64], BF16)
    retq = io.out("retq", [256, ntok], BF16)
    retk = io.out("retk", [256, ntok], BF16)
    retv = io.out("retv", [ntok, 256], BF16)
    lrux = io.out("lrux", [256, ntok], F32)
    cvx = io.out("cvx", [256, ntok], F32)
    bgate = io.out("bgate", [256, ntok], F32)
    sgate = io.out("sgate", [256, ntok], F32)
    ggate = io.out("ggate", [256, ntok], F32)

    psT = [P.ps("psT%d" % i, [128, 1024], BF16) for i in range(2)]
    psF = [P.ps("psF%d" % i, [128, 512], F32) for i in range(3)]
    psU = P.ps("psU", [128, 512], F32)
    psQ = P.ps("psQ", [128, 512], F32)
    psX = P.ps("psX", [128, 1024], BF16)
    fi = [0]

    def nextF():
        p = psF[fi[0] % 3]
        fi[0] += 1
        return p

    idf, idb = make_identities(P)
    P.negpi = P.sb("negpi", [128, 1], F32)
    P.I("vector", "memset", ap=P.negpi[:], constant=-math.pi, _writes=[P.negpi[:]])
    neghalf = P.sb("neghalf", [128, 16], F32)
    P.I("vector", "memset", ap=neghalf[:], constant=-0.5, _writes=[neghalf[:]])

    mod = compute_mod(P, c_in, adaw, adab, psF[0], psF[1], 0, 2 * D)
    gm = P.sb("gm", [128, D], F32)
    P.dma("sync", gm[:], gmix.partition_broadcast(128))
    P.I("vector", "scalar_tensor_tensor", out=gm[:], in0=mod[:, D:2 * D], scalar=1.0, in1=gm[:], op0=ALU.add, op1=ALU.mult)
    shm = mod[:, 0:D]

    w_bf = P.sb("w_bf", [128, 8, IN_COLS], BF16)
    wv = w_in.rearrange("(k p) n -> p k n", p=128)
    for k in range(8):
        for hh in range(2):
            P.dma("gpsimd", w_bf[:, k, hh * 1328:(hh + 1) * 1328], wv[:, k, hh * 1328:(hh + 1) * 1328])
    w_rot = P.sb("w_rot", [128, 8, 512], BF16)
    for k in range(8):
        src = w_bf[:, k, 1120:1632].rearrange("p (h two i) -> p h two i", two=2, i=32)
        dst = w_rot[:, k, :].rearrange("p (h two i) -> p h two i", two=2, i=32)
        P.I("vector", "tensor_scalar", out=dst[:, :, 0, :], in0=src[:, :, 1, :], scalar1=-1.0, scalar2=None, op0=ALU.mult)
        P.I("vector", "tensor_copy", out=dst[:, :, 1, :], in_=src[:, :, 0, :])
    wuq_bf = P.sb("wuq_bf", [128, 2, 384], BF16)
    P.dma("gpsimd", wuq_bf[:, 0, :], wuq[0:128, :])
    P.dma("gpsimd", wuq_bf[0:64, 1, :], wuq[128:192, :])
    wukv_bf = P.sb("wukv_bf", [128, 512], BF16)
    P.dma("gpsimd", wukv_bf[:], wukv)
    qng_bc = P.sb("qng_bc", [128, 192], F32)
    kvng_bc = P.sb("kvng_bc", [128, 128], F32)
    qqk_bc = P.sb("qqk_bc", [128, 96], F32)
    kqk_bc = P.sb("kqk_bc", [128, 96], F32)
    P.dma("sync", qng_bc[:], qng.partition_broadcast(128))
    P.dma("sync", kvng_bc[:], kvng.partition_broadcast(128))
    P.dma("sync", qqk_bc[:], qqk.partition_broadcast(128))
    P.dma("sync", kqk_bc[:], kqk.partition_broadcast(128))

    invc = P.sb("invc", [128, 1], F32)
    P.dma("sync", invc[:], inv_ret4)
    posi = P.sb("posi", [128, 512], I32)
    angt = P.sb("angt", [128, 512], F32)
    tmpa = P.sb("tmpa", [128, 512], F32)
    tmpai = P.sb("tmpai", [128, 512], I32)
    cos_r = Rot(P, "cosR", [128, 512], F32, 2)
    sin_r = Rot(P, "sinR", [128, 512], F32, 2)
    invm = P.sb("invm", [128, 16], F32)
    P.dma("sync", invm[:], inv_mla.partition_broadcast(128))
    posc_i = P.sb("posc_i", [128, 4], I32)
    posc = P.sb("posc", [128, 4], F32)
    angm = P.sb("angm", [128, 4, 16], F32)
    tmpm = P.sb("tmpm", [128, 4, 16], F32)
    tmpmi = P.sb("tmpmi", [128, 4, 16], I32)
    cosM_r = Rot(P, "cosM", [128, 4, 16], F32, 2)
    sinM_r = Rot(P, "sinM", [128, 4, 16], F32, 2)

    xt_r = Rot(P, "xt", [128, D], F32, 3)
    junk = P.sb("junk", [128, D], BF16)
    ssq = Rot(P, "ssq", [128, 4], F32, 2)
    v4 = Rot(P, "v4", [128, 4], F32, 2)
    rstd4 = Rot(P, "rstd4", [128, 4], F32, 2)
    tmp_r = Rot(P, "tmpx", [128, D], F32, 1)
    hb_r = Rot(P, "hb", [128, D], BF16, 2)
    hT_r = Rot(P, "hT", [128, 8, 512], BF16, 2)
    ev_r = Rot(P, "ev", [128, 512], F32, 4)
    evb_r = Rot(P, "evb", [128, 512], BF16, 4)
    csb_r = Rot(P, "csb", [128, 512], F32, 2)

    for st in range(NST):
        t0 = st * 512
        hT = hT_r.next()
        for j in range(4):
            xt = xt_r.next()
            P.dma("sync", xt[:], x_in[t0 + j * 128:t0 + (j + 1) * 128, :])
            sq = ssq.next()
            P.I("scalar", "activation", out=junk[:], in_=xt[:], func=AF.Square, accum_out=sq[:, 0:1])
            v = v4.next()
            rs = rstd4.next()
            P.I("vector", "tensor_scalar", out=v[:, 0:1], in0=sq[:, 0:1], scalar1=1.0 / D, scalar2=EPS, op0=ALU.mult, op1=ALU.add)
            P.I("gpsimd", "tensor_tensor", out=rs[:, 0:1], in0=v[:, 0:1], in1=neghalf[:, 0:1], op=ALU.pow)
            tm = tmp_r.next()
            hb = hb_r.next()
            P.I("vector", "scalar_tensor_tensor", out=tm[:], in0=xt[:], scalar=rs[:, 0:1], in1=gm[:],
                op0=ALU.mult, op1=ALU.mult)
            P.I("vector", "tensor_tensor", out=hb[:], in0=tm[:], in1=shm, op=ALU.add)
            pt = psT[j % 2]
            for k in range(8):
                P.I("tensor", "transpose", out=pt[:, k * 128:(k + 1) * 128], in_=hb[:, k * 128:(k + 1) * 128], identity=idb[:])
            P.I("scalar", "copy", out=hT[:, :, j * 128:(j + 1) * 128], in_=pt[:].rearrange("p (k t) -> p k t", k=8))
        cosR = cos_r.next()
        sinR = sin_r.next()
        P.dma("scalar", posi[:], pos_in[t0:t0 + 512].partition_broadcast(128))
        P.I("vector", "tensor_copy", out=angt[:], in_=posi[:])
        P.I("vector", "tensor_scalar", out=angt[:], in0=angt[:], scalar1=invc[:, 0:1], scalar2=None, op0=ALU.mult)
        sin_of(P, sinR[:], angt[:], tmpa[:], tmpai[:], 0.0)
        sin_of(P, cosR[:], angt[:], tmpa[:], tmpai[:], 0.5 * math.pi)
        cosM = cosM_r.next()
        sinM = sinM_r.next()
        P.dma("scalar", posc_i[:], pos_in[t0:t0 + 512].rearrange("(n p) -> p n", p=128), allow_slow_non_contiguous=True)
        P.I("vector", "tensor_copy", out=posc[:], in_=posc_i[:])
        P.I("vector", "tensor_tensor", out=angm[:], in0=posc[:].unsqueeze(2).to_broadcast([128, 4, 16]),
            in1=invm[:].unsqueeze(1).to_broadcast([128, 4, 16]), op=ALU.mult)
        sin_of(P, sinM[:], angm[:], tmpm[:], tmpmi[:], 0.0)
        sin_of(P, cosM[:], angm[:], tmpm[:], tmpmi[:], 0.5 * math.pi)

        def fm(wt, c0):
            ps = nextF()
            for k in range(8):
                P.I("tensor", "matmul", out=ps[:], lhsT=wt[:, k, c0:c0 + 128], rhs=hT[:, k, :], start=(k == 0), stop=(k == 7))
            return ps

        for ch in range(2):
            ps = fm(w_bf, ch * 128)
            e = ev_r.next()
            P.I("scalar", "copy", out=e[:], in_=ps[:])
            P.dma("sync", bgate[ch * 128:(ch + 1) * 128, t0:t0 + 512], e[:])
            psc = fm(w_bf, 256 + ch * 128)
            cs = csb_r.next()
            P.I("scalar", "copy", out=cs[:], in_=psc[:])
            psx = fm(w_bf, 512 + ch * 128)
            e = ev_r.next()
            P.I("vector", "tensor_tensor", out=e[:], in0=psx[:], in1=cs[:], op=ALU.mult)
            P.dma("sync", cvx[ch * 128:(ch + 1) * 128, t0:t0 + 512], e[:])
        for qk in range(2):
            for ch in range(2):
                c0 = 1120 + qk * 256 + ch * 128
                ps = fm(w_bf, c0)
                cs = csb_r.next()
                P.I("vector", "scalar_tensor_tensor", out=cs[:], in0=ps[:], scalar=(1.0 if qk == 0 else 0.125),
                    in1=cosR[:], op0=ALU.mult, op1=ALU.mult)
                psr = fm(w_rot, qk * 256 + ch * 128)
                e = ev_r.next()
                P.I("vector", "scalar_tensor_tensor", out=e[:], in0=psr[:], scalar=(1.0 if qk == 0 else 0.125),
                    in1=sinR[:], op0=ALU.mult, op1=ALU.mult)
                eb = evb_r.next()
                P.I("vector", "tensor_tensor", out=eb[:], in0=e[:], in1=cs[:], op=ALU.add)
                P.dma("sync", (retq if qk == 0 else retk)[ch * 128:(ch + 1) * 128, t0:t0 + 512], eb[:])
        for ch in range(2):
            ps = fm(w_bf, 1120 + 768 + ch * 128)
            e = ev_r.next()
            P.I("scalar", "activation", out=e[:], in_=ps[:], func=AF.Silu)
            P.dma("sync", sgate[ch * 128:(ch + 1) * 128, t0:t0 + 512], e[:])
        for ch in range(2):
            ps = fm(w_bf, 2144 + ch * 128)
            e = ev_r.next()
            P.I("scalar", "copy", out=e[:], in_=ps[:])
            P.dma("sync", lrux[ch * 128:(ch + 1) * 128, t0:t0 + 512], e[:])
        for ch in range(2):
            ps = fm(w_bf, 2144 + 256 + ch * 128)
            e = ev_r.next()
            P.I("scalar", "activation", out=e[:], in_=ps[:], func=AF.Gelu)
            P.dma("sync", ggate[ch * 128:(ch + 1) * 128, t0:t0 + 512], e[:])
        for j in range(4):
            tt = t0 + j * 128
            ti = tt // 128
            hTj = hT[:, :, j * 128:(j + 1) * 128]
            ps = nextF()
            for k in range(8):
                P.I("tensor", "matmul", out=ps[:, 0:256], lhsT=hT[:, k, j * 128:(j + 1) * 128], rhs=w_bf[:, k, 1632:1888],
                    start=(k == 0), stop=(k == 7))
            eb = evb_r.next()
            P.I("scalar", "copy", out=eb[:, 0:256], in_=ps[:, 0:256])
            P.dma("sync", retv[tt:tt + 128, :], eb[:, 0:256])
            mla_tile(P, locals(), tt, j, j)


def mla_tile(P, L, tt, ti, j):
    hT, w_bf, psU, psQ, psX = L["hT"], L["w_bf"], L["psU"], L["psQ"], L["psX"]
    idb, neghalf = L["idb"], L["neghalf"]
    qng_bc, kvng_bc, qqk_bc, kqk_bc = L["qng_bc"], L["kvng_bc"], L["qqk_bc"], L["kqk_bc"]
    wuq_bf, wukv_bf, cosM, sinM = L["wuq_bf"], L["wukv_bf"], L["cosM"], L["sinM"]
    attq, attk, attv = L["attq"], L["attk"], L["attv"]
    if not hasattr(P, "mla_tiles"):
        P.mla_tiles = dict(
            junk=P.sb("mjunk", [128, 512], F32),
            st3=Rot(P, "mst3", [128, 16], F32, 2),
            rs3=Rot(P, "mrs3", [128, 16], F32, 2),
            cqn=Rot(P, "mcqn", [128, 320], BF16, 2),
            cT=Rot(P, "mcT", [128, 384], BF16, 2),
            qsb=Rot(P, "mqsb", [128, 384], F32, 2),
            kvsb=Rot(P, "mkvsb", [128, 512], F32, 2),
            sq=Rot(P, "msq", [128, 512], F32, 4),
            Qt=Rot(P, "mQt", [128, 4, 96], BF16, 2),
            Kt=Rot(P, "mKt", [128, 4, 96], BF16, 2),
            Vt=Rot(P, "mVt", [128, 4, 64], BF16, 2),
            r1=Rot(P, "mr1", [128, 4, 32], F32, 2),
            r2=Rot(P, "mr2", [128, 4, 16], F32, 4),
            kr=Rot(P, "mkr", [128, 32], F32, 2),
            kr2=Rot(P, "mkr2", [128, 32], F32, 2),
            QT=Rot(P, "mQT", [96, 4, 128], BF16, 2),
            KT=Rot(P, "mKT", [96, 4, 128], BF16, 2),
            scl=P.sb("mscl", [128, 3], F32),
        )
        sc = P.mla_tiles["scl"]
        P.I("vector", "memset", ap=sc[:, 0:1], constant=1.0 / 192, _writes=[sc[:]])
        P.I("vector", "memset", ap=sc[:, 1:2], constant=1.0 / 128, _writes=[sc[:]])
        P.I("vector", "memset", ap=sc[:, 2:3], constant=1.0 / 32, _writes=[sc[:]])
    M = P.mla_tiles
    for k in range(8):
        P.I("tensor", "matmul", out=psU[:, 0:352], lhsT=hT[:, k, j * 128:(j + 1) * 128], rhs=w_bf[:, k, 768:1120],
            start=(k == 0), stop=(k == 7))
    st3 = M["st3"].next()
    rs3 = M["rs3"].next()
    P.I("scalar", "activation", out=M["junk"][:, 0:192], in_=psU[:, 0:192], func=AF.Square, accum_out=st3[:, 0:1])
    P.I("scalar", "activation", out=M["junk"][:, 0:128], in_=psU[:, 192:320], func=AF.Square, accum_out=st3[:, 1:2])
    P.I("scalar", "activation", out=M["junk"][:, 0:32], in_=psU[:, 320:352], func=AF.Square, accum_out=st3[:, 2:3])
    P.I("vector", "tensor_tensor", out=st3[:, 0:3], in0=st3[:, 0:3], in1=M["scl"][:], op=ALU.mult)
    P.I("vector", "tensor_scalar", out=st3[:, 0:3], in0=st3[:, 0:3], scalar1=EPS, scalar2=None, op0=ALU.add)
    P.I("gpsimd", "tensor_tensor", out=rs3[:, 0:3], in0=st3[:, 0:3], in1=neghalf[:, 0:3], op=ALU.pow)
    cqn = M["cqn"].next()
    P.I("vector", "scalar_tensor_tensor", out=cqn[:, 0:192], in0=psU[:, 0:192], scalar=rs3[:, 0:1], in1=qng_bc[:],
        op0=ALU.mult, op1=ALU.mult)
    P.I("vector", "scalar_tensor_tensor", out=cqn[:, 192:320], in0=psU[:, 192:320], scalar=rs3[:, 1:2], in1=kvng_bc[:],
        op0=ALU.mult, op1=ALU.mult)
    kr = M["kr"].next()
    P.I("vector", "scalar_tensor_tensor", out=kr[:], in0=psU[:, 320:352], scalar=rs3[:, 2:3], in1=kqk_bc[:, 64:96],
        op0=ALU.mult, op1=ALU.mult)
    P.I("tensor", "transpose", out=psX[:, 0:128], in_=cqn[:, 0:128], identity=idb[:])
    P.I("tensor", "transpose", out=psX[0:64, 128:256], in_=cqn[:, 128:192], identity=idb[:])
    P.I("tensor", "transpose", out=psX[:, 256:384], in_=cqn[:, 192:320], identity=idb[:])
    cT = M["cT"].next()
    P.I("scalar", "copy", out=cT[:, 0:128], in_=psX[:, 0:128])
    P.I("scalar", "copy", out=cT[0:64, 128:256], in_=psX[0:64, 128:256])
    P.I("scalar", "copy", out=cT[:, 256:384], in_=psX[:, 256:384])
    P.I("tensor", "matmul", out=psQ[:, 0:384], lhsT=cT[:, 0:128], rhs=wuq_bf[:, 0, :], start=True, stop=False)
    P.I("tensor", "matmul", out=psQ[:, 0:384], lhsT=cT[0:64, 128:256], rhs=wuq_bf[0:64, 1, :], start=False, stop=True)
    qsb = M["qsb"].next()
    sq = M["sq"].next()
    P.I("scalar", "copy", out=qsb[:], in_=psQ[:, 0:384])
    P.I("scalar", "activation", out=sq[:, 0:384], in_=psQ[:, 0:384], func=AF.Square)
    P.I("tensor", "matmul", out=psQ[:, 0:512], lhsT=cT[:, 256:384], rhs=wukv_bf[:], start=True, stop=True)
    kvsb = M["kvsb"].next()
    P.I("scalar", "copy", out=kvsb[:], in_=psQ[:, 0:512])
    st8 = M["st3"].next()
    rs8 = M["rs3"].next()
    sqv = sq[:, 0:384].rearrange("p (h c) -> p h c", c=96)
    P.I("vector", "tensor_reduce", out=st8[:, 0:4], in_=sqv[:, :, 0:64], axis=AX.X, op=ALU.add)
    P.I("vector", "tensor_reduce", out=st8[:, 4:8], in_=sqv[:, :, 64:96], axis=AX.X, op=ALU.add)
    sq2 = M["sq"].next()
    P.I("scalar", "activation", out=sq2[:], in_=kvsb[:], func=AF.Square)
    P.I("vector", "tensor_reduce", out=st8[:, 8:12], in_=sq2[:].rearrange("p (h c) -> p h c", c=128)[:, :, 0:64],
        axis=AX.X, op=ALU.add)
    P.I("vector", "tensor_scalar", out=st8[:, 0:4], in0=st8[:, 0:4], scalar1=1.0 / 64, scalar2=EPS, op0=ALU.mult, op1=ALU.add)
    P.I("vector", "tensor_scalar", out=st8[:, 4:8], in0=st8[:, 4:8], scalar1=1.0 / 32, scalar2=EPS, op0=ALU.mult, op1=ALU.add)
    P.I("vector", "tensor_scalar", out=st8[:, 8:12], in0=st8[:, 8:12], scalar1=1.0 / 64, scalar2=EPS, op0=ALU.mult, op1=ALU.add)
    P.I("gpsimd", "tensor_tensor", out=rs8[:, 0:12], in0=st8[:, 0:12], in1=neghalf[:, 0:12], op=ALU.pow)
    Qt = M["Qt"].next()
    Kt = M["Kt"].next()
    Vt = M["Vt"].next()
    qv = qsb[:].rearrange("p (h c) -> p h c", c=96)
    kvv = kvsb[:].rearrange("p (h c) -> p h c", c=128)
    r1 = M["r1"].next()
    tq = M["sq"].next()
    tqv = tq[:, 0:256].rearrange("p (h c) -> p h c", c=64)
    P.I("vector", "tensor_tensor", out=tqv, in0=qv[:, :, 0:64], in1=rs8[:, 0:4].unsqueeze(2).to_broadcast([128, 4, 64]), op=ALU.mult)
    P.I("vector", "tensor_tensor", out=Qt[:, :, 0:64], in0=tqv, in1=qqk_bc[:, 0:64].unsqueeze(1).to_broadcast([128, 4, 64]), op=ALU.mult)
    P.I("vector", "tensor_tensor", out=r1[:], in0=qv[:, :, 64:96], in1=rs8[:, 4:8].unsqueeze(2).to_broadcast([128, 4, 32]), op=ALU.mult)
    P.I("vector", "tensor_tensor", out=r1[:], in0=r1[:], in1=qqk_bc[:, 64:96].unsqueeze(1).to_broadcast([128, 4, 32]), op=ALU.mult)
    cb = cosM[:, ti, :].unsqueeze(1).to_broadcast([128, 4, 16])
    sb_ = sinM[:, ti, :].unsqueeze(1).to_broadcast([128, 4, 16])
    a1, a2, a3, a4 = M["r2"].next(), M["r2"].next(), M["r2"].next(), M["r2"].next()
    P.I("vector", "tensor_tensor", out=a1[:], in0=r1[:, :, 0:16], in1=cb, op=ALU.mult)
    P.I("vector", "tensor_tensor", out=a2[:], in0=r1[:, :, 16:32], in1=sb_, op=ALU.mult)
    P.I("vector", "tensor_tensor", out=a3[:], in0=r1[:, :, 16:32], in1=cb, op=ALU.mult)
    P.I("vector", "tensor_tensor", out=a4[:], in0=r1[:, :, 0:16], in1=sb_, op=ALU.mult)
    P.I("vector", "tensor_tensor", out=Qt[:, :, 64:80], in0=a1[:], in1=a2[:], op=ALU.subtract)
    P.I("vector", "tensor_tensor", out=Qt[:, :, 80:96], in0=a3[:], in1=a4[:], op=ALU.add)
    tk = M["sq"].next()
    tkv = tk[:, 0:256].rearrange("p (h c) -> p h c", c=64)
    P.I("vector", "tensor_tensor", out=tkv, in0=kvv[:, :, 0:64], in1=rs8[:, 8:12].unsqueeze(2).to_broadcast([128, 4, 64]), op=ALU.mult)
    P.I("vector", "tensor_tensor", out=Kt[:, :, 0:64], in0=tkv, in1=kqk_bc[:, 0:64].unsqueeze(1).to_broadcast([128, 4, 64]), op=ALU.mult)
    kr2 = M["kr2"].next()
    b1, b2, b3, b4 = M["r2"].next(), M["r2"].next(), M["r2"].next(), M["r2"].next()
    P.I("vector", "tensor_tensor", out=b1[:, 0, :], in0=kr[:, 0:16], in1=cosM[:, ti, :], op=ALU.mult)
    P.I("vector", "tensor_tensor", out=b2[:, 0, :], in0=kr[:, 16:32], in1=sinM[:, ti, :], op=ALU.mult)
    P.I("vector", "tensor_tensor", out=b3[:, 0, :], in0=kr[:, 16:32], in1=cosM[:, ti, :], op=ALU.mult)
    P.I("vector", "tensor_tensor", out=b4[:, 0, :], in0=kr[:, 0:16], in1=sinM[:, ti, :], op=ALU.mult)
    P.I("vector", "tensor_tensor", out=kr2[:, 0:16], in0=b1[:, 0, :], in1=b2[:, 0, :], op=ALU.subtract)
    P.I("vector", "tensor_tensor", out=kr2[:, 16:32], in0=b3[:, 0, :], in1=b4[:, 0, :], op=ALU.add)
    P.I("vector", "tensor_copy", out=Kt[:, :, 64:96], in_=kr2[:].unsqueeze(1).to_broadcast([128, 4, 32]))
    P.I("vector", "tensor_copy", out=Vt[:], in_=kvv[:, :, 64:128])
    P.dma("sync", attv[:, tt:tt + 128, :].rearrange("h t c -> t h c"), Vt[:])
    for h in range(4):
        P.I("tensor", "transpose", out=psX[0:96, 384 + h * 128:384 + (h + 1) * 128], in_=Qt[:, h, :], identity=idb[:])
    QT = M["QT"].next()
    P.I("scalar", "copy", out=QT[:], in_=psX[0:96, 384:896].rearrange("p (h t) -> p h t", h=4))
    P.dma("sync", attq[:, :, tt:tt + 128].rearrange("h c t -> c h t"), QT[:])
    for h in range(4):
        P.I("tensor", "transpose", out=psX[0:96, 384 + h * 128:384 + (h + 1) * 128], in_=Kt[:, h, :], identity=idb[:])
    KT = M["KT"].next()
    P.I("scalar", "copy", out=KT[:], in_=psX[0:96, 384:896].rearrange("p (h t) -> p h t", h=4))
    P.dma("sync", attk[:, :, tt:tt + 128].rearrange("h c t -> c h t"), KT[:])


ATT_SCALE = 96.0 ** -0.5


def build_p2(SS=S):
    nc = new_nc()
    P = Prog(nc)
    io = IO(nc)
    phase2(P, io, SS)
    P.finish(io.outs)
    P.emit()
    return nc


def phase2(P, io, SS, after_setup=None):
    nc = P.nc
    NB = SS // 128
    aq = io.inp("aq", [96, SS], BF16)
    ak = io.inp("ak", [96, SS], BF16)
    av = io.inp("av", [SS, 64], BF16)
    rq = io.inp("rq", [64, SS], BF16)
    rk = io.inp("rk", [64, SS], BF16)
    rv = io.inp("rv", [SS, 64], BF16)
    lx = io.inp("lx", [64, SS])
    cx = io.inp("cx", [64, SS])
    convw = io.inp("convw", [64, 3])
    lcw = io.inp("lcw", [64, 4])
    lcb = io.inp("lcb", [64, 1])
    wa = io.inp("wa", [64, 64])
    ba = io.inp("ba", [64, 1])
    wx = io.inp("wx", [64, 64])
    bx = io.inp("bx", [64, 1])
    lam = io.inp("lam", [64, 1])
    qqk = io.inp("qqk", [96])
    kqk = io.inp("kqk", [96])
    innerT = io.inp("innerT", [128, 128])
    qdecT = io.inp("qdecT", [64, 128])
    kdec = io.inp("kdec", [128, 1])
    cdec = io.inp("cdec", [64, 1])
    ao = io.out("ao", [64, SS])
    ro = io.out("ro", [64, SS])
    lo = io.out("lo", [64, SS])
    co = io.out("co", [64, SS])

    psS = [P.ps("psS%d" % i, [128, 512], F32) for i in range(3)]
    psO = [P.ps("psO%d" % i, [128, 512], F32) for i in range(2)]
    psB = P.ps("psB", [128, 512], F32)
    psR = [P.ps("psR%d" % i, [128, 512], F32) for i in range(2)]
    psKT = psB[:].bitcast(BF16)

    idf, idb = make_identities(P)
    half = P.sb("half", [128, 512], F32)
    P.I("vector", "memset", ap=half[:], constant=0.5, _writes=[half[:]])
    neghalf = P.sb("neghalf", [128, 512], F32)
    P.I("vector", "memset", ap=neghalf[:], constant=-0.5, _writes=[neghalf[:]])
    ones64 = P.sb("ones64", [128, 64], F32)
    P.I("vector", "memset", ap=ones64[:], constant=1.0 / 64, _writes=[ones64[:]])
    ones1 = P.sb("ones1", [128, 64], F32)
    P.I("vector", "memset", ap=ones1[:], constant=1.0, _writes=[ones1[:]])
    epsc = P.sb("epsc", [128, 1], F32)
    P.I("vector", "memset", ap=epsc[:], constant=EPS, _writes=[epsc[:]])

    if after_setup is not None:
        after_setup()
    cw = P.sb("cw", [64, 3], F32)
    P.dma("sync", cw[:], convw, allow_slow_non_contiguous=True)
    CSEG = min(2048, SS)
    cxt = P.sb("cxt", [64, 2 + CSEG], F32)
    cacc = Rot(P, "cacc", [64, CSEG], F32, 2)
    P.I("vector", "memset", ap=cxt[:, 0:2], constant=0.0, _writes=[cxt[:]])
    for sg in range(SS // CSEG):
        c0 = sg * CSEG
        if sg > 0:
            P.I("vector", "tensor_copy", out=cxt[:, 0:2], in_=cxt[:, CSEG:CSEG + 2])
        P.dma("sync", cxt[:, 2:2 + CSEG], cx[:, c0:c0 + CSEG])
        acc = cacc.next()
        P.I("vector", "tensor_scalar", out=acc[:], in0=cxt[:, 2:2 + CSEG], scalar1=cw[:, 2:3], scalar2=None, op0=ALU.mult)
        P.I("vector", "scalar_tensor_tensor", out=acc[:], in0=cxt[:, 1:1 + CSEG], scalar=cw[:, 1:2], in1=acc[:], op0=ALU.mult, op1=ALU.add)
        P.I("vector", "scalar_tensor_tensor", out=acc[:], in0=cxt[:, 0:CSEG], scalar=cw[:, 0:1], in1=acc[:], op0=ALU.mult, op1=ALU.add)
        P.dma("sync", co[:, c0:c0 + CSEG], acc[:])

    lw = P.sb("lw", [64, 4], F32)
    lb = P.sb("lb", [64, 1], F32)
    bat = P.sb("bat", [64, 1], F32)
    bxt = P.sb("bxt", [64, 1], F32)
    lamt = P.sb("lamt", [64, 1], F32)
    nsp = P.sb("nsp", [64, 1], F32)
    wa_bf = P.sb("wa_bf", [64, 64], BF16)
    wx_bf = P.sb("wx_bf", [64, 64], BF16)
    P.dma("sync", lw[:], lcw, allow_slow_non_contiguous=True)
    P.dma("sync", lb[:], lcb, allow_slow_non_contiguous=True)
    P.dma("sync", bat[:], ba, allow_slow_non_contiguous=True)
    P.dma("sync", bxt[:], bx, allow_slow_non_contiguous=True)
    P.dma("sync", lamt[:], lam, allow_slow_non_contiguous=True)
    P.dma("gpsimd", wa_bf[:], wa)
    P.dma("gpsimd", wx_bf[:], wx)
    P.I("scalar", "activation", out=nsp[:], in_=lamt[:], func=AF.Exp, scale=-1.0)
    P.I("scalar", "activation", out=nsp[:], in_=nsp[:], func=AF.Ln, bias=ones1[0:64, 0:1], scale=1.0)
    P.I("vector", "tensor_scalar", out=nsp[:], in0=nsp[:], scalar1=-8.0, scalar2=None, op0=ALU.mult)
    LSEG = min(1024, SS)
    NHF = LSEG // 512
    lxt = P.sb("lxt", [64, 3 + LSEG], F32)
    P.I("vector", "memset", ap=lxt[:, 0:3], constant=0.0, _writes=[lxt[:]])
    xc_r = Rot(P, "xc", [64, LSEG], F32, 1)
    xcb_r = Rot(P, "xcb", [64, LSEG], BF16, 1)
    rg_r = Rot(P, "rg", [64, LSEG], F32, 1)
    ig_r = Rot(P, "ig", [64, LSEG], F32, 1)
    a_r = Rot(P, "la", [64, LSEG], F32, 1)
    a2_r = Rot(P, "la2", [64, LSEG], F32, 1)
    b_r = Rot(P, "lbin", [64, LSEG], F32, 1)
    h_r = Rot(P, "lh", [64, LSEG], F32, 2)
    gbanks = [psS[0], psS[1], psS[2], psO[0]]
    hprev = None
    for sg in range(SS // LSEG):
        c0 = sg * LSEG
        if sg > 0:
            P.I("vector", "tensor_copy", out=lxt[:, 0:3], in_=lxt[:, LSEG:LSEG + 3])
        P.dma("sync", lxt[:, 3:3 + LSEG], lx[:, c0:c0 + LSEG])
        xc = xc_r.next()
        P.I("vector", "tensor_scalar", out=xc[:], in0=lxt[:, 3:3 + LSEG], scalar1=lw[:, 3:4], scalar2=lb[:, 0:1], op0=ALU.mult, op1=ALU.add)
        for kk in range(3):
            P.I("vector", "scalar_tensor_tensor", out=xc[:], in0=lxt[:, kk:kk + LSEG], scalar=lw[:, kk:kk + 1], in1=xc[:], op0=ALU.mult, op1=ALU.add)
        xcb = xcb_r.next()
        P.I("vector", "tensor_copy", out=xcb[:], in_=xc[:])
        rg = rg_r.next()
        ig = ig_r.next()
        for hf in range(NHF):
            hs = slice(hf * 512, (hf + 1) * 512)
            pr = gbanks[2 * hf]
            pi = gbanks[2 * hf + 1]
            P.I("tensor", "matmul", out=pr[0:64, :], lhsT=wa_bf[:], rhs=xcb[:, hs], start=True, stop=True)
            P.I("tensor", "matmul", out=pi[0:64, :], lhsT=wx_bf[:], rhs=xcb[:, hs], start=True, stop=True)
        for hf in range(NHF):
            hs = slice(hf * 512, (hf + 1) * 512)
            P.I("scalar", "activation", out=rg[:, hs], in_=gbanks[2 * hf][0:64, :], func=AF.Sigmoid, bias=bat[:, 0:1], scale=1.0)
            P.I("scalar", "activation", out=ig[:, hs], in_=gbanks[2 * hf + 1][0:64, :], func=AF.Sigmoid, bias=bxt[:, 0:1], scale=1.0)
        a = a_r.next()
        P.I("scalar", "activation", out=a[:], in_=rg[:], func=AF.Exp, scale=nsp[:, 0:1])
        a2 = a2_r.next()
        P.I("scalar", "activation", out=a2[:], in_=a[:], func=AF.Square)
        P.I("scalar", "activation", out=a2[:], in_=a2[:], func=AF.Sqrt, bias=ones1[0:64, 0:1], scale=-1.0)
        bb = b_r.next()
        P.I("vector", "tensor_tensor", out=bb[:], in0=ig[:], in1=xc[:], op=ALU.mult)
        P.I("vector", "tensor_tensor", out=bb[:], in0=bb[:], in1=a2[:], op=ALU.mult)
        h = h_r.next()
        P.I("vector", "tensor_tensor_scan", out=h[:], data0=a[:], data1=bb[:],
            initial=(0.0 if hprev is None else hprev[:, LSEG - 1:LSEG]), op0=ALU.mult, op1=ALU.add)
        hprev = h
        P.dma("sync", lo[:, c0:c0 + LSEG], h[:])

    inn = P.sb("inn", [128, 128], F32)
    qd_c = P.sb("qd_c", [64, 512], F32)
    kd_c = P.sb("kd_c", [128, 1], F32)
    cd_c = P.sb("cd_c", [64, 1], F32)
    P.dma("sync", inn[:], innerT)
    for r_ in range(4):
        P.dma("sync", qd_c[:, r_ * 128:(r_ + 1) * 128], qdecT)
    P.dma("sync", kd_c[:], kdec)
    P.dma("sync", cd_c[:], cdec)
    PC = min(32, NB)
    RG = 4
    KVa = P.sb("KVa", [64, PC, 64], F32)
    Sbf = P.sb("Sbf", [64, PC, 64], BF16)
    gam = P.sb("gam", [64, PC], F32)
    carry = P.sb("rcarry", [64, 64], F32)
    P.I("vector", "memset", ap=carry[:], constant=0.0, _writes=[carry[:]])
    P.I("vector", "memset", ap=gam[:], constant=1.0, _writes=[gam[:]])
    P.I("vector", "tensor_scalar", out=gam[:], in0=gam[:], scalar1=cd_c[:, 0:1], scalar2=None, op0=ALU.mult)
    rq_r = Rot(P, "rq_t", [64, RG * 128], BF16, 2)
    rk_r = Rot(P, "rk_t", [64, RG * 128], BF16, 2)
    rv_r = Rot(P, "rv_t", [128, RG, 64], BF16, 2)
    scm_r = Rot(P, "scm", [128, 128], BF16, 3)
    kd_r = Rot(P, "kdt", [128, 64], BF16, 3)
    qd_r = Rot(P, "qdt", [64, RG * 128], BF16, 2)
    oT_r = Rot(P, "oT", [64, RG * 128], F32, 2)
    cen_r = Rot(P, "cen", [64, RG * 128], F32, 2)
    sqr_r = Rot(P, "sqr", [64, RG * 128], F32, 2)
    for pc in range(NB // PC):
        c0 = pc * PC
        for g in range(PC // RG):
            t0 = (c0 + g * RG) * 128
            rkt, rvt = rk_r.next(), rv_r.next()
            P.dma("sync", rkt[:], rk[:, t0:t0 + RG * 128])
            P.dma("sync", rvt[:], rv[t0:t0 + RG * 128, :].rearrange("(n p) c -> p n c", p=128))
            for ci in range(RG):
                i = g * RG + ci
                cs = slice(ci * 128, (ci + 1) * 128)
                pT = psS[i % 3][:].bitcast(BF16)
                P.I("tensor", "transpose", out=pT[:, 0:64], in_=rkt[:, cs], identity=idb[0:64, 0:64])
                kdt = kd_r.next()
                P.I("scalar", "activation", out=kdt[:], in_=pT[:, 0:64], func=AF.Copy, scale=kd_c[:, 0:1])
                pK = psO[i % 2]
                P.I("tensor", "matmul", out=pK[0:64, 0:64], lhsT=kdt[:], rhs=rvt[:, ci, :], start=True, stop=True)
                P.I("vector", "tensor_copy", out=KVa[:, i, :], in_=pK[0:64, 0:64])
        for dv in range(64):
            P.I("vector", "tensor_tensor_scan", out=KVa[:, :, dv], data0=gam[:], data1=KVa[:, :, dv], initial=carry[:, dv:dv + 1],
                op0=ALU.mult, op1=ALU.add)
        P.I("scalar", "copy", out=Sbf[:, 0, :], in_=carry[:])
        if PC > 1:
            P.I("scalar", "copy", out=Sbf[:, 1:PC, :], in_=KVa[:, 0:PC - 1, :])
        P.I("vector", "tensor_copy", out=carry[:], in_=KVa[:, PC - 1, :])
        for g in range(PC // RG):
            t0 = (c0 + g * RG) * 128
            rqt, rkt, rvt = rq_r.next(), rk_r.next(), rv_r.next()
            P.dma("sync", rqt[:], rq[:, t0:t0 + RG * 128])
            P.dma("sync", rkt[:], rk[:, t0:t0 + RG * 128])
            P.dma("sync", rvt[:], rv[t0:t0 + RG * 128, :].rearrange("(n p) c -> p n c", p=128))
            qdt = qd_r.next()
            P.I("vector", "tensor_tensor", out=qdt[:], in0=rqt[:], in1=qd_c[:], op=ALU.mult)
            oT = oT_r.next()
            for ci in range(RG):
                i = g * RG + ci
                cs = slice(ci * 128, (ci + 1) * 128)
                pA = psS[i % 3]
                P.I("tensor", "matmul", out=pA[:, 0:128], lhsT=rkt[:, cs], rhs=rqt[:, cs], start=True, stop=True)
                scm = scm_r.next()
                P.I("vector", "tensor_tensor", out=scm[:], in0=pA[:, 0:128], in1=inn[:], op=ALU.mult)
                pBk = psO[i % 2]
                P.I("tensor", "matmul", out=pBk[0:64, 0:128], lhsT=rvt[:, ci, :], rhs=scm[:], start=True, stop=False)
                P.I("tensor", "matmul", out=pBk[0:64, 0:128], lhsT=Sbf[:, i, :], rhs=qdt[:, cs], start=False, stop=True)
                P.I("scalar", "copy", out=oT[:, cs], in_=pBk[0:64, 0:128])
            W = RG * 128
            P.I("tensor", "matmul", out=psB[0:64, 0:W], lhsT=ones64[0:64, :], rhs=oT[:], start=True, stop=True)
            cen = cen_r.next()
            P.I("vector", "tensor_tensor", out=cen[:], in0=oT[:], in1=psB[0:64, 0:W], op=ALU.subtract)
            sqr = sqr_r.next()
            P.I("scalar", "activation", out=sqr[:], in_=cen[:], func=AF.Square)
            P.I("tensor", "matmul", out=psR[0][0:64, 0:W], lhsT=ones64[0:64, :], rhs=sqr[:], start=True, stop=True)
            P.I("scalar", "activation", out=sqr[:], in_=psR[0][0:64, 0:W], func=AF.Sqrt, bias=epsc[0:64, 0:1], scale=1.0)
            P.I("vector", "reciprocal", out=sqr[:], in_=sqr[:])
            P.I("vector", "tensor_tensor", out=cen[:], in0=cen[:], in1=sqr[:], op=ALU.mult)
            P.dma("sync", ro[:, t0:t0 + W], cen[:])

    QT = P.sb("QT", [96, SS], BF16)
    KT = P.sb("KT", [96, SS], BF16)
    VA = P.sb("VA", [128, NB, 65], BF16)
    LDC = min(2048, SS)
    for i in range(SS // LDC):
        P.dma("sync", KT[:, i * LDC:(i + 1) * LDC], ak[:, i * LDC:(i + 1) * LDC])
        P.dma("scalar", QT[:, i * LDC:(i + 1) * LDC], aq[:, i * LDC:(i + 1) * LDC])
    P.I("gpsimd", "memset", ap=VA[:, :, 64:65], constant=1.0, _writes=[VA[:]])
    P.dma("sync", VA[:, :, 0:64], av.rearrange("(n p) c -> p n c", p=128))
    tri_f = P.sb("tri_f", [128, 128], F32)
    tri = P.sb("tri", [128, 128], BF16)
    P.I("gpsimd", "memset", ap=tri_f[:], constant=1.0, _writes=[tri_f[:]])
    P.I("gpsimd", "affine_select", out=tri_f[:], in_=tri_f[:], pattern=[[1, 128]], compare_op=ALU.is_ge,
        fill=0.0, base=0, channel_multiplier=-1)
    P.I("vector", "tensor_copy", out=tri[:], in_=tri_f[:])
    gq = P.sb("gq_bc", [128, 96], F32)
    gk = P.sb("gk_bc", [128, 96], F32)
    m2 = P.sb("m2", [128, 2], F32)
    negc = P.sb("negc", [128, 1], F32)
    P.dma("sync", gq[:], qqk.partition_broadcast(128))
    P.dma("sync", gk[:], kqk.partition_broadcast(128))
    P.I("vector", "tensor_tensor", out=gq[:], in0=gq[:], in1=gq[:], op=ALU.mult)
    P.I("vector", "tensor_tensor", out=gk[:], in0=gk[:], in1=gk[:], op=ALU.mult)
    P.I("vector", "tensor_reduce", out=m2[:, 0:1], in_=gq[:], axis=AX.X, op=ALU.max)
    P.I("vector", "tensor_reduce", out=m2[:, 1:2], in_=gk[:], axis=AX.X, op=ALU.max)
    P.I("vector", "tensor_tensor", out=negc[:], in0=m2[:, 0:1], in1=m2[:, 1:2], op=ALU.mult)
    P.I("gpsimd", "tensor_tensor", out=negc[:], in0=negc[:], in1=half[:, 0:1], op=ALU.pow)
    P.I("vector", "tensor_scalar", out=negc[:], in0=negc[:], scalar1=-math.sqrt(96.0), scalar2=None, op0=ALU.mult)
    PT_r = Rot(P, "PT", [128, 512], BF16, 5)
    den_r = Rot(P, "den", [128, 512], F32, 2)
    oa_r = Rot(P, "oa", [64, 512], F32, 2)
    rb_r = Rot(P, "rb", [64, 512], F32, 2)
    si = 0
    for qs in range(SS // 512):
        q0 = qs * 512
        pO = psO[qs % 2]
        nkb = 4 * qs + 4
        pend = []
        for j in range(nkb):
            d = j - 4 * qs
            lo_c = 0 if d <= 0 else d * 128
            pS = psS[si % 3]
            si += 1
            P.I("tensor", "matmul", out=pS[:, lo_c:512], lhsT=KT[:, j * 128:(j + 1) * 128], rhs=QT[:, q0 + lo_c:q0 + 512],
                start=True, stop=True)
            if len(pend) >= 2:
                pj, plo, pPT = pend.pop(0)
                P.I("tensor", "matmul", out=pO[0:65, plo:512], lhsT=VA[:, pj, :], rhs=pPT[:, plo:512], start=(pj == 0), stop=False)
            PT = PT_r.next()
            P.I("scalar", "activation", out=PT[:, lo_c:512], in_=pS[:, lo_c:512], func=AF.Exp, bias=negc[:, 0:1], scale=ATT_SCALE)
            if d >= 0:
                P.I("vector", "tensor_tensor", out=PT[:, lo_c:lo_c + 128], in0=PT[:, lo_c:lo_c + 128], in1=tri[:], op=ALU.mult)
            pend.append((j, lo_c, PT))
        while pend:
            pj, plo, pPT = pend.pop(0)
            P.I("tensor", "matmul", out=pO[0:65, plo:512], lhsT=VA[:, pj, :], rhs=pPT[:, plo:512], start=(pj == 0), stop=(len(pend) == 0))
        den = den_r.next()
        P.I("vector", "tensor_copy", out=den[64:65, :], in_=pO[64:65, :])
        P.I("tensor", "matmul", out=psB[0:64, :], lhsT=ones1[64:65, 0:64], rhs=den[64:65, :], start=True, stop=True)
        rb = rb_r.next()
        P.I("vector", "reciprocal", out=rb[:], in_=psB[0:64, :])
        oa = oa_r.next()
        P.I("vector", "tensor_tensor", out=oa[:], in0=pO[0:64, :], in1=rb[:], op=ALU.mult)
        P.dma("sync", ao[:, q0:q0 + 512], oa[:])


def build_p3(ntok=TPC, TB=1024, NEXP=32, stop=0):
    nc = new_nc()
    P = Prog(nc)
    io = IO(nc)
    phase3(P, io, ntok, TB, NEXP)
    P.finish(io.outs)
    P.emit()
    return nc


def phase3(P, io, ntok, TB=1024, NEXP=32, stop=0):
    nc = P.nc
    x_in = io.inp("x_in", [ntok, D])
    c_in = io.inp("c_in", [D])
    adaw = io.inp("ada_w", [D, 6 * D])
    adab = io.inp("ada_b", [6 * D])
    mixo = [io.inp(n, [256, ntok]) for n in ("co", "ao", "ro", "lo")]
    gates = {0: io.inp("bgate", [256, ntok]), 2: io.inp("sgate", [256, ntok]), 3: io.inp("ggate", [256, ntok])}
    mng = io.inp("mix_norm_g", [D])
    w_out = io.inp("w_out", [D, D])
    gffn = io.inp("norm_ffn_g", [D])
    wgr = io.inp("router_group_w", [D, 4])
    bgr = io.inp("router_group_b", [4])
    wer = io.inp("router_expert_w", [D, 32])
    ber = io.inp("router_expert_b", [32])
    ewg = io.inp("exp_w_gate", [32, D, 256])
    ewu = io.inp("exp_w_up", [32, D, 256])
    ewd = io.inp("exp_w_down", [32, 256, D])
    x_out = io.out("x_out", [ntok, D])
    x1s = io.out("x1s", [ntok, D])

    bank = [P.ps("bank%d" % i, [128, 512], F32) for i in range(8)]
    idf, idb = make_identities(P)
    neghalf = P.sb("neghalf", [128, 256], F32)
    P.I("vector", "memset", ap=neghalf[:], constant=-0.5, _writes=[neghalf[:]])
    onesf = P.sb("onesf", [128, 128], F32)
    P.I("vector", "memset", ap=onesf[:], constant=1.0, _writes=[onesf[:]])
    epsc = P.sb("epsc", [128, 1], F32)
    P.I("vector", "memset", ap=epsc[:], constant=EPS, _writes=[epsc[:]])

    mod = compute_mod(P, c_in, adaw, adab, bank[0], bank[1], 2 * D, 6 * D, CH=128)
    gtm = mod[:, 0:D]
    shf = mod[:, D:2 * D]
    gf = mod[:, 2 * D:3 * D]
    gtf = mod[:, 3 * D:4 * D]
    tmo_r = Rot(P, "tmo", [128, D], F32, 2)
    gtmp = tmo_r.next()
    P.dma("sync", gtmp[:], gffn.partition_broadcast(128))
    P.I("vector", "scalar_tensor_tensor", out=gf, in0=gf, scalar=1.0, in1=gtmp[:], op0=ALU.add, op1=ALU.mult)
    mngc = P.sb("mngc", [128, 8], F32)
    P.dma("sync", mngc[:], mng.rearrange("(k p) -> p k", p=128), allow_slow_non_contiguous=True)
    wout_bf = P.sb("wout_bf", [128, 8, D], BF16)
    wov = w_out.rearrange("(k p) n -> p k n", p=128)
    for k in range(8):
        P.dma("gpsimd", wout_bf[:, k, :], wov[:, k, :])
    wr = P.sb("wr", [128, 8, 36], F32)
    P.dma("sync", wr[:, :, 0:4], wgr.rearrange("(k p) n -> p k n", p=128))
    P.dma("sync", wr[:, :, 4:36], wer.rearrange("(k p) n -> p k n", p=128))
    wr_hi = P.sb("wr_hi", [128, 8, 36], BF16)
    wr_lo = P.sb("wr_lo", [128, 8, 36], BF16)
    P.I("vector", "tensor_copy", out=wr_hi[:], in_=wr[:])
    P.I("vector", "tensor_tensor", out=wr_lo[:], in0=wr[:], in1=wr_hi[:], op=ALU.subtract)
    bg_bc = P.sb("bg_bc", [128, 4], F32)
    be_bc = P.sb("be_bc", [128, 32], F32)
    P.dma("sync", bg_bc[:], bgr.partition_broadcast(128))
    P.dma("sync", be_bc[:], ber.partition_broadcast(128))

    NSUB = TB // 128
    h2T = P.sb("h2T", [128, 8, TB], BF16)
    comb_all = P.sb("comb_all", [128, NSUB, 32], F32)
    yacc = P.sb("yacc", [128, NSUB, D], F32)

    yc_r = Rot(P, "yc", [128, 256], F32, 3)
    gt_r = Rot(P, "gtl", [128, 256], F32, 2)
    ysq_r = Rot(P, "ysq", [128, 256], F32, 2)
    ych = [P.sb("ych%d" % i, [128, 256], F32) for i in range(8)]
    rsb_r = Rot(P, "rsb", [128, 256], F32, 2)
    yT = P.sb("yT", [128, 8, 256], BF16)
    xt_r = Rot(P, "xt", [128, D], F32, 1)
    x1_r = Rot(P, "x1", [128, D], F32, 2)
    junk = P.sb("junk", [128, D], BF16)
    sm_r = Rot(P, "sm", [128, 8], F32, 4)
    h2f_r = Rot(P, "h2f", [128, D], F32, 1)
    h2Tlo = P.sb("h2Tlo", [128, 8, 128], BF16)
    hsp_r = Rot(P, "hsp", [128, D], BF16, 2)
    R8 = Rot(P, "r8", [128, 8], F32, 20)
    R4 = Rot(P, "r4", [128, 4], F32, 8)
    R1 = Rot(P, "r1", [128, 1], F32, 16)
    lg_r = Rot(P, "lgs", [128, 36], F32, 2)
    wgu_r = Rot(P, "wgu", [128, 8, 512], BF16, 2)
    wd_r = Rot(P, "wd", [128, 2, D], BF16, 2)
    sg_r = Rot(P, "sg", [128, 256], BF16, 4)
    hid_r = Rot(P, "hid", [128, 2, 256], BF16, 2)

    if "wgu_s" in io.m:
        wgu_s, wd_s = io.m["wgu_s"], io.m["wd_s"]
    else:
        wgu_s, wd_s = precast_experts(P, ewg, ewu, ewd, NEXP)

    for blk in range(ntok // TB):
        b0 = blk * TB
        for tl in range(TB // 256):
            t0 = b0 + tl * 256
            for m in range(4):
                pss = bank[m % 2]
                for c in range(2):
                    ci = m * 2 + c
                    y = ych[ci]
                    if m == 1:
                        P.dma("sync", y[:], mixo[m][c * 128:(c + 1) * 128, t0:t0 + 256])
                    else:
                        yc = yc_r.next()
                        gt = gt_r.next()
                        P.dma("sync", yc[:], mixo[m][c * 128:(c + 1) * 128, t0:t0 + 256])
                        P.dma("scalar", gt[:], gates[m][c * 128:(c + 1) * 128, t0:t0 + 256])
                        P.I("vector", "tensor_tensor", out=y[:], in0=yc[:], in1=gt[:], op=ALU.mult)
                    ysq = ysq_r.next()
                    P.I("scalar", "activation", out=ysq[:], in_=y[:], func=AF.Square)
                    P.I("tensor", "matmul", out=pss[:, 0:256], lhsT=onesf[:], rhs=ysq[:], start=(c == 0), stop=(c == 1))
                rsb = rsb_r.next()
                P.I("scalar", "activation", out=rsb[:], in_=pss[:, 0:256], func=AF.Sqrt, bias=epsc[:, 0:1], scale=1.0 / 256)
                P.I("vector", "reciprocal", out=rsb[:], in_=rsb[:])
                for c in range(2):
                    ci = m * 2 + c
                    P.I("vector", "scalar_tensor_tensor", out=yT[:, ci, :], in0=ych[ci][:], scalar=mngc[:, ci:ci + 1], in1=rsb[:],
                        op0=ALU.mult, op1=ALU.mult)
            for s in range(2):
                tt = t0 + s * 128
                si = tl * 2 + s
                po = [bank[2], bank[3]]
                for hf in range(2):
                    for k in range(8):
                        P.I("tensor", "matmul", out=po[hf][:], lhsT=yT[:, k, s * 128:(s + 1) * 128], rhs=wout_bf[:, k, hf * 512:(hf + 1) * 512],
                            start=(k == 0), stop=(k == 7))
                xt = xt_r.next()
                P.dma("sync", xt[:], x_in[tt:tt + 128, :])
                tmo = tmo_r.next()
                x1 = x1_r.next()
                for hf in range(2):
                    P.I("vector", "tensor_tensor", out=tmo[:, hf * 512:(hf + 1) * 512], in0=po[hf][:], in1=gtm[:, hf * 512:(hf + 1) * 512], op=ALU.mult)
                P.I("vector", "tensor_tensor", out=x1[:], in0=tmo[:], in1=xt[:], op=ALU.add)
                P.dma("sync", x1s[tt:tt + 128, :], x1[:])
                sm = sm_r.next()
                P.I("scalar", "activation", out=junk[:], in_=x1[:], func=AF.Square, accum_out=sm[:, 0:1])
                P.I("vector", "tensor_scalar", out=sm[:, 1:2], in0=sm[:, 0:1], scalar1=1.0 / D, scalar2=EPS, op0=ALU.mult, op1=ALU.add)
                P.I("gpsimd", "tensor_tensor", out=sm[:, 2:3], in0=sm[:, 1:2], in1=neghalf[:, 0:1], op=ALU.pow)
                tm2 = tmo_r.next()
                h2f = h2f_r.next()
                P.I("vector", "scalar_tensor_tensor", out=tm2[:], in0=x1[:], scalar=sm[:, 2:3], in1=gf, op0=ALU.mult, op1=ALU.mult)
                P.I("vector", "tensor_tensor", out=h2f[:], in0=tm2[:], in1=shf, op=ALU.add)
                hhi = hsp_r.next()
                hlo = hsp_r.next()
                P.I("scalar", "copy", out=hhi[:], in_=h2f[:])
                P.I("vector", "tensor_tensor", out=hlo[:], in0=h2f[:], in1=hhi[:], op=ALU.subtract)
                b4 = bank[4][:].bitcast(BF16)
                b5 = bank[5][:].bitcast(BF16)
                for k in range(8):
                    P.I("tensor", "transpose", out=b4[:, k * 128:(k + 1) * 128], in_=hhi[:, k * 128:(k + 1) * 128], identity=idb[:])
                for k in range(8):
                    P.I("tensor", "transpose", out=b5[:, k * 128:(k + 1) * 128], in_=hlo[:, k * 128:(k + 1) * 128], identity=idb[:])
                P.I("scalar", "copy", out=h2T[:, :, si * 128:(si + 1) * 128], in_=b4.rearrange("p (k t) -> p k t", k=8))
                P.I("vector", "tensor_copy", out=h2Tlo[:], in_=b5.rearrange("p (k t) -> p k t", k=8))
                pl = bank[6]
                for k in range(8):
                    P.I("tensor", "matmul", out=pl[:, 0:36], lhsT=h2T[:, k, si * 128:(si + 1) * 128], rhs=wr_hi[:, k, :], start=(k == 0), stop=False)
                for k in range(8):
                    P.I("tensor", "matmul", out=pl[:, 0:36], lhsT=h2Tlo[:, k, :], rhs=wr_hi[:, k, :], start=False, stop=False)
                for k in range(8):
                    P.I("tensor", "matmul", out=pl[:, 0:36], lhsT=h2T[:, k, si * 128:(si + 1) * 128], rhs=wr_lo[:, k, :], start=False, stop=(k == 7))
                lg = lg_r.next()
                P.I("vector", "tensor_copy", out=lg[:], in_=pl[:, 0:36])
                router_math(P, lg, comb_all[:, si, :], bg_bc, be_bc, R8, R4, R1)
        NTL = TB // 256
        steps = [(e, tl) for e in range(NEXP) for tl in range(NTL)]
        wts = {}
        bcs = {}
        hids = {}

        def load_expert(e):
            wgu = wgu_r.next()
            wd = wd_r.next()
            P.dma("sync", wgu[:], wgu_s[e])
            P.dma("sync", wd[:], wd_s[e])
            wts[e] = (wgu, wd)

        def gu_mm(i):
            e, tl = steps[i]
            if tl == 0:
                if e not in wts:
                    load_expert(e)
            wgu, wd = wts[e]
            pg = bank[4 + 2 * (i % 2)]
            pu = bank[5 + 2 * (i % 2)]
            for fc in range(2):
                for k in range(8):
                    P.I("tensor", "matmul", out=pg[:, fc * 256:(fc + 1) * 256], lhsT=wgu[:, k, fc * 128:(fc + 1) * 128],
                        rhs=h2T[:, k, tl * 256:(tl + 1) * 256], start=(k == 0), stop=(k == 7))
                for k in range(8):
                    P.I("tensor", "matmul", out=pu[:, fc * 256:(fc + 1) * 256], lhsT=wgu[:, k, 256 + fc * 128:256 + (fc + 1) * 128],
                        rhs=h2T[:, k, tl * 256:(tl + 1) * 256], start=(k == 0), stop=(k == 7))

        def gu_ew(i):
            e, tl = steps[i]
            pg = bank[4 + 2 * (i % 2)]
            pu = bank[5 + 2 * (i % 2)]
            hid = hid_r.next()
            hids[i] = hid
            for fc in range(2):
                sg = sg_r.next()
                P.I("scalar", "activation", out=sg[:], in_=pg[:, fc * 256:(fc + 1) * 256], func=AF.Silu)
                P.I("vector", "tensor_tensor", out=hid[:, fc, :], in0=sg[:], in1=pu[:, fc * 256:(fc + 1) * 256], op=ALU.mult)

        def down(i):
            e, tl = steps[i]
            wgu, wd = wts[e]
            hid = hids.pop(i)
            for s in range(2):
                si = tl * 2 + s
                for hf in range(2):
                    py = bank[s * 2 + hf]
                    for fc in range(2):
                        P.I("tensor", "matmul", out=py[:], lhsT=hid[:, fc, s * 128:(s + 1) * 128], rhs=wd[:, fc, hf * 512:(hf + 1) * 512],
                            start=(fc == 0), stop=(fc == 1))
                    if e == 0:
                        P.I("vector", "tensor_scalar", out=yacc[:, si, hf * 512:(hf + 1) * 512], in0=py[:], scalar1=comb_all[:, si, e:e + 1],
                            scalar2=None, op0=ALU.mult)
                    else:
                        P.I("vector", "scalar_tensor_tensor", out=yacc[:, si, hf * 512:(hf + 1) * 512], in0=py[:], scalar=comb_all[:, si, e:e + 1],
                            in1=yacc[:, si, hf * 512:(hf + 1) * 512], op0=ALU.mult, op1=ALU.add)
            if tl == NTL - 1:
                wts.pop(e, None)
            if tl == 0 and e + 1 < NEXP:
                load_expert(e + 1)

        if steps:
            gu_mm(0)
            gu_ew(0)
        for i in range(len(steps)):
            if i + 1 < len(steps):
                gu_mm(i + 1)
            down(i)
            if i + 1 < len(steps):
                gu_ew(i + 1)
        for si in range(NSUB):
            tt = b0 + si * 128
            x1 = x1_r.next()
            P.dma("sync", x1[:], x1s[tt:tt + 128, :])
            tmo = tmo_r.next()
            P.I("vector", "tensor_tensor", out=tmo[:], in0=yacc[:, si, :], in1=gtf, op=ALU.mult)
            xo = xt_r.next()
            P.I("vector", "tensor_tensor", out=xo[:], in0=tmo[:], in1=x1[:], op=ALU.add)
            P.dma("sync", x_out[tt:tt + 128, :], xo[:])


RSTOP = 0


def precast_experts(P, ewg, ewu, ewd, NEXP=32):
    nc = P.nc
    wgu_s = nc.dram_tensor(P.prefix + "wgu_s", [32, 128, 8, 512], BF16).ap()
    wd_s = nc.dram_tensor(P.prefix + "wd_s", [32, 128, 2, D], BF16).ap()
    for e in range(NEXP):
        gv = ewg[e].rearrange("(k p) f -> p k f", p=128)
        uv = ewu[e].rearrange("(k p) f -> p k f", p=128)
        for k in range(8):
            P.dma("gpsimd", wgu_s[e, :, k, 0:256], gv[:, k, :])
            P.dma("gpsimd", wgu_s[e, :, k, 256:512], uv[:, k, :])
        dv = ewd[e].rearrange("(k p) n -> p k n", p=128)
        for k in range(2):
            P.dma("gpsimd", wd_s[e, :, k, :], dv[:, k, :])
    return wgu_s, wd_s


def router_math(P, lg, comb, bg_bc, be_bc, R8, R4, R1):
    V = lambda *a, **k: P.I("vector", *a, **k)
    lgg = lg[:, 0:4]
    mx = R1.next()
    V("tensor_reduce", out=mx[:], in_=lgg, axis=AX.X, op=ALU.max)
    nmx = R1.next()
    V("tensor_scalar", out=nmx[:], in0=mx[:], scalar1=-1.0, scalar2=None, op0=ALU.mult)
    eg = R4.next()
    sg = R1.next()
    P.I("scalar", "activation", out=eg[:], in_=lgg, func=AF.Exp, bias=nmx[:, 0:1], scale=1.0, accum_out=sg[:, 0:1])
    if RSTOP == 1:
        return
    rs = R1.next()
    V("reciprocal", out=rs[:], in_=sg[:])
    gp = R4.next()
    V("tensor_scalar", out=gp[:], in0=eg[:], scalar1=rs[:, 0:1], scalar2=None, op0=ALU.mult)
    sel = R4.next()
    V("tensor_tensor", out=sel[:], in0=gp[:], in1=bg_bc[:], op=ALU.add)
    m = R1.next()
    V("tensor_reduce", out=m[:], in_=sel[:], axis=AX.X, op=ALU.max)
    goh = R4.next()
    V("tensor_scalar", out=goh[:], in0=sel[:], scalar1=m[:, 0:1], scalar2=None, op0=ALU.is_equal)
    if RSTOP == 2:
        return
    gwj = R4.next()
    gw = R1.next()
    V("tensor_tensor", out=gwj[:], in0=gp[:], in1=goh[:], op=ALU.mult)
    V("tensor_reduce", out=gw[:], in_=gwj[:], axis=AX.X, op=ALU.add)
    els = R8.next()
    bes = R8.next()
    V("tensor_scalar", out=els[:], in0=lg[:, 4:12], scalar1=goh[:, 0:1], scalar2=None, op0=ALU.mult)
    V("tensor_scalar", out=bes[:], in0=be_bc[:, 0:8], scalar1=goh[:, 0:1], scalar2=None, op0=ALU.mult)
    for g in range(1, 4):
        V("scalar_tensor_tensor", out=els[:], in0=lg[:, 4 + 8 * g:12 + 8 * g], scalar=goh[:, g:g + 1], in1=els[:], op0=ALU.mult, op1=ALU.add)
        V("scalar_tensor_tensor", out=bes[:], in0=be_bc[:, 8 * g:8 * g + 8], scalar=goh[:, g:g + 1], in1=bes[:], op0=ALU.mult, op1=ALU.add)
    if RSTOP == 3:
        return
    mx8 = R1.next()
    V("tensor_reduce", out=mx8[:], in_=els[:], axis=AX.X, op=ALU.max)
    nm8 = R1.next()
    V("tensor_scalar", out=nm8[:], in0=mx8[:], scalar1=-1.0, scalar2=None, op0=ALU.mult)
    ee = R8.next()
    se = R1.next()
    P.I("scalar", "activation", out=ee[:], in_=els[:], func=AF.Exp, bias=nm8[:, 0:1], scale=1.0, accum_out=se[:, 0:1])
    rse = R1.next()
    V("reciprocal", out=rse[:], in_=se[:])
    ep = R8.next()
    V("tensor_scalar", out=ep[:], in0=ee[:], scalar1=rse[:, 0:1], scalar2=None, op0=ALU.mult)
    if RSTOP == 4:
        return
    sc = R8.next()
    V("tensor_tensor", out=sc[:], in0=ep[:], in1=bes[:], op=ALU.add)
    m1 = R1.next()
    V("tensor_reduce", out=m1[:], in_=sc[:], axis=AX.X, op=ALU.max)
    oh1 = R8.next()
    V("tensor_scalar", out=oh1[:], in0=sc[:], scalar1=m1[:, 0:1], scalar2=None, op0=ALU.is_equal)
    sc2 = R8.next()
    V("scalar_tensor_tensor", out=sc2[:], in0=oh1[:], scalar=-1e9, in1=sc[:], op0=ALU.mult, op1=ALU.add)
    m2 = R1.next()
    V("tensor_reduce", out=m2[:], in_=sc2[:], axis=AX.X, op=ALU.max)
    oh2 = R8.next()
    V("tensor_scalar", out=oh2[:], in0=sc2[:], scalar1=m2[:, 0:1], scalar2=None, op0=ALU.is_equal)
    if RSTOP == 5:
        return
    ohs = R8.next()
    V("tensor_tensor", out=ohs[:], in0=oh1[:], in1=oh2[:], op=ALU.add)
    tp = R8.next()
    V("tensor_tensor", out=tp[:], in0=ep[:], in1=ohs[:], op=ALU.mult)
    sp = R1.next()
    V("tensor_reduce", out=sp[:], in_=tp[:], axis=AX.X, op=ALU.add)
    rsp = R1.next()
    V("reciprocal", out=rsp[:], in_=sp[:])
    fac = R1.next()
    V("tensor_tensor", out=fac[:], in0=rsp[:], in1=gw[:], op=ALU.mult)
    ew = R8.next()
    V("tensor_scalar", out=ew[:], in0=tp[:], scalar1=fac[:, 0:1], scalar2=None, op0=ALU.mult)
    if RSTOP == 6:
        return
    for g in range(4):
        V("tensor_scalar", out=comb[:, g * 8:(g + 1) * 8], in0=ew[:], scalar1=goh[:, g:g + 1], scalar2=None, op0=ALU.mult)


W_SPECS = dict(
    ada_w=[2, D, 6 * D], ada_b=[2, 6 * D], norm_mix_g=[2, D], w_in=[2, D, IN_COLS], conv_w=[2, 3, 256],
    mla_q_norm_g=[2, 192], mla_w_uq=[2, 192, 384], mla_kv_norm_g=[2, 128], mla_w_ukv=[2, 128, 512],
    mla_q_qk_g=[2, 96], mla_k_qk_g=[2, 96], lru_conv_w=[2, 4, 256], lru_conv_b=[2, 256], lru_w_a=[2, 4, 64, 64],
    lru_b_a=[2, 256], lru_w_x=[2, 4, 64, 64], lru_b_x=[2, 256], lru_lambda=[2, 256], mix_norm_g=[2, D],
    w_out=[2, D, D], norm_ffn_g=[2, D], router_group_w=[2, D, 4], router_group_b=[2, 4], router_expert_w=[2, D, 32],
    router_expert_b=[2, 32], exp_w_gate=[2, 32, D, 256], exp_w_up=[2, 32, D, 256], exp_w_down=[2, 32, 256, D])


def build_fused(SS=S, NL=2, TB=1024):
    nc = new_nc()
    P = Prog(nc)
    x_in = din(nc, "x", [SS, D])
    c_in = din(nc, "c", [D])
    pos = din(nc, "positions", [SS], I32)
    W = {k: din(nc, k, shp) for k, shp in W_SPECS.items()}
    inv_ret4 = din(nc, "inv_ret4", [128, 1])
    inv_mla = din(nc, "inv_mla", [16])
    innerT = din(nc, "innerT", [4, 128, 128])
    qdecT = din(nc, "qdecT", [4, 64, 128])
    kdec = din(nc, "kdec", [4, 128, 1])
    cdec = din(nc, "cdec", [4, 64, 1])
    out = dout(nc, "out", [SS, D])

    def idr(name, shape, dt=F32):
        return nc.dram_tensor(name, list(shape), dt).ap()

    T = dict(attq=idr("i_attq", [4, 96, SS], BF16), attk=idr("i_attk", [4, 96, SS], BF16), attv=idr("i_attv", [4, SS, 64], BF16),
             retq=idr("i_retq", [256, SS], BF16), retk=idr("i_retk", [256, SS], BF16), retv=idr("i_retv", [SS, 256], BF16),
             lrux=idr("i_lrux", [256, SS]), cvx=idr("i_cvx", [256, SS]), bgate=idr("i_bgate", [256, SS]),
             sgate=idr("i_sgate", [256, SS]), ggate=idr("i_ggate", [256, SS]))
    Y = dict(co=idr("i_co", [256, SS]), ao=idr("i_ao", [256, SS]), ro=idr("i_ro", [256, SS]), lo=idr("i_lo", [256, SS]))
    x_mid = idr("i_xmid", [SS, D])
    x1s = idr("i_x1s", [SS, D])
    col = lambda ap: ap.rearrange("(c o) -> c o", o=1)
    cast = {}
    for l in range(NL):
        xs = x_in if l == 0 else x_mid
        xd = out if l == NL - 1 else x_mid
        mk = P.mark()
        P.prefix = "L%dP1_" % l
        m = dict(x_in=xs, c_in=c_in, pos_in=pos, inv_ret4=inv_ret4, inv_mla=inv_mla)
        for k in ("ada_w", "ada_b", "norm_mix_g", "w_in", "mla_q_norm_g", "mla_w_uq", "mla_kv_norm_g", "mla_w_ukv", "mla_q_qk_g", "mla_k_qk_g"):
            m[k] = W[k][l]
        m.update(T)
        phase1(P, IO(nc, m), SS)
        P.release(mk)
        for hd in range(4):
            mk = P.mark()
            P.prefix = "L%dP2h%d_" % (l, hd)
            sl = slice(hd * 64, (hd + 1) * 64)
            m = dict(aq=T["attq"][hd], ak=T["attk"][hd], av=T["attv"][hd], rq=T["retq"][sl], rk=T["retk"][sl], rv=T["retv"][:, sl],
                     lx=T["lrux"][sl], cx=T["cvx"][sl],
                     convw=W["conv_w"][l][:, sl].rearrange("k c -> c k"), lcw=W["lru_conv_w"][l][:, sl].rearrange("k c -> c k"),
                     lcb=col(W["lru_conv_b"][l][sl]), wa=W["lru_w_a"][l][hd], ba=col(W["lru_b_a"][l][sl]),
                     wx=W["lru_w_x"][l][hd], bx=col(W["lru_b_x"][l][sl]), lam=col(W["lru_lambda"][l][sl]),
                     qqk=W["mla_q_qk_g"][l], kqk=W["mla_k_qk_g"][l],
                     innerT=innerT[hd], qdecT=qdecT[hd], kdec=kdec[hd], cdec=cdec[hd],
                     ao=Y["ao"][sl], ro=Y["ro"][sl], lo=Y["lo"][sl], co=Y["co"][sl])
            if hd == 0:
                def _cast(l=l):
                    pfx = P.prefix
                    P.prefix = "L%d_" % l
                    cast[l] = precast_experts(P, W["exp_w_gate"][l], W["exp_w_up"][l], W["exp_w_down"][l])
                    P.prefix = pfx
                phase2(P, IO(nc, m), SS, after_setup=_cast)
            else:
                phase2(P, IO(nc, m), SS)
            P.release(mk)
        mk = P.mark()
        P.prefix = "L%dP3_" % l
        m = dict(x_in=xs, c_in=c_in, x_out=xd, x1s=x1s, bgate=T["bgate"], sgate=T["sgate"], ggate=T["ggate"])
        for k in ("ada_w", "ada_b", "mix_norm_g", "w_out", "norm_ffn_g", "router_group_w", "router_group_b", "router_expert_w",
                  "router_expert_b", "exp_w_gate", "exp_w_up", "exp_w_down"):
            m[k] = W[k][l]
        m.update(Y)
        m["wgu_s"], m["wd_s"] = cast[l]
        phase3(P, IO(nc, m), SS, TB, 32)
        P.release(mk)
    P.finish([out])
    P.emit()
    return nc


_NC_CACHE = {}


def _get(name, fn):
    if name not in _NC_CACHE:
        _NC_CACHE[name] = fn()
    return _NC_CACHE[name]


def _inv_freq(dim):
    return (np.float32(1.0) / (np.float32(10000.0) ** (np.arange(0, dim, 2, dtype=np.float32) / np.float32(dim)))).astype(np.float32)


def _ret_consts(hd):
    gamma = 1.0 - 2.0 ** (-5.0 - hd)
    idx = np.arange(128, dtype=np.float64)
    innerT = np.where(idx[None, :] >= idx[:, None], gamma ** np.maximum(idx[None, :] - idx[:, None], 0.0), 0.0).astype(np.float32)
    qdecT = np.ascontiguousarray(np.tile((gamma ** (idx + 1.0))[None, :], (64, 1))).astype(np.float32)
    kdec = (gamma ** (127.0 - idx)).reshape(128, 1).astype(np.float32)
    cdec = np.full((64, 1), gamma ** 128, np.float32)
    return innerT, qdecT, kdec, cdec


def kernel(**inputs):
    I = {k: np.ascontiguousarray(np.asarray(v)) for k, v in inputs.items()}
    B = I["x"].shape[0]
    nc = _get("fused", build_fused)
    rc = [_ret_consts(h) for h in range(4)]
    consts = dict(inv_ret4=np.ascontiguousarray(np.tile(_inv_freq(64), 4).reshape(128, 1)), inv_mla=_inv_freq(32),
                  innerT=np.stack([r[0] for r in rc]), qdecT=np.stack([r[1] for r in rc]),
                  kdec=np.stack([r[2] for r in rc]), cdec=np.stack([r[3] for r in rc]))
    maps = []
    for b in range(B):
        m = dict(x=np.ascontiguousarray(I["x"][b], dtype=np.float32), c=np.ascontiguousarray(I["c"][b]),
                 positions=np.ascontiguousarray(I["positions"][b]).astype(np.int32))
        for k in W_SPECS:
            m[k] = I[k]
        m.update(consts)
        maps.append(m)
    res = run_bass_kernel_spmd(nc, maps, core_ids=list(range(B))).results
    return np.stack([res[b]["out"] for b in range(B)], axis=0).astype(np.float32)
```

```python
import math
import numpy as np
import ml_dtypes
import concourse.bass as bass
import concourse.mybir as mybir
from concourse.bass_utils import run_bass_kernel_spmd

F32 = mybir.dt.float32
BF16 = mybir.dt.bfloat16
I32 = mybir.dt.int32
AF = mybir.ActivationFunctionType
ALU = mybir.AluOpType
AX = mybir.AxisListType

D = 1024
S = 16384
NCORE = 8
TPC = 4096
IN_COLS = 2656
EPS = 1e-6
TWO_PI = 2.0 * math.pi

ENGS = ["sync", "scalar", "vector", "gpsimd", "tensor"]
SAME_ENGINE_SYNC = {"sync": False, "scalar": True, "vector": True, "gpsimd": True, "tensor": False}
_APT = None


class Buf:
    __slots__ = ("name", "w", "r")

    def __init__(self, name=""):
        self.name = name
        self.w = None
        self.r = []


class Prog:
    NPOOL = 16

    def __init__(self, nc):
        self.nc = nc
        self.ops = {e: [] for e in ENGS}
        self.cnt = {e: 0 for e in ENGS}
        self.esem = {e: nc.alloc_semaphore("es_" + e) for e in ENGS}
        self.known = {e: {f: 0 for f in ENGS} for e in ENGS}
        self.snap = {e: [None] for e in ENGS}
        self.dq = ["sync", "scalar", "gpsimd"]
        self.pool = {q: [nc.alloc_semaphore("dp_%s_%d" % (q, i)) for i in range(self.NPOOL)] for q in self.dq}
        self.pool_val = {q: [0] * self.NPOOL for q in self.dq}
        self.pool_next = {q: 0 for q in self.dq}
        self.dknown = {e: {} for e in ENGS}
        self.bufs = {}
        self.uid = 0
        self.prefix = ""

    def sb(self, name, shape, dt=F32):
        return self.nc.alloc_sbuf_tensor(self.prefix + name, list(shape), dt)

    def ps(self, name, shape, dt=F32):
        return self.nc.alloc_psum_tensor(self.prefix + name, list(shape), dt)

    def mark(self):
        nc = self.nc
        return (nc.psum_base, nc.psum_top, nc.sbuf_base, nc.sbuf_top)

    def release(self, mk):
        self.barrier()
        nc = self.nc
        nc.psum_base, nc.psum_top, nc.sbuf_base, nc.sbuf_top = mk

    def barrier(self):
        for e in ENGS:
            waits = []
            for f in ENGS:
                if f != e and self.cnt[f] > 0:
                    self._need(e, ("E", f, self.cnt[f]), waits)
            for q in self.dq:
                for i in range(self.NPOOL):
                    v = self.pool_val[q][i]
                    if v > 0:
                        self._need(e, ("D", q, i, v, None), waits)
            self.ops[e].append((waits, None, None, None, 0))
        self.bufs = {}

    def buf_of(self, ap):
        n = ap.name
        b = self.bufs.get(n)
        if b is None:
            b = self.bufs[n] = Buf(n)
        return b

    def _merge(self, eng, sn):
        if sn is None:
            return
        kn = self.known[eng]
        for g, v in sn[0].items():
            if kn[g] < v:
                kn[g] = v
        dk = self.dknown[eng]
        for k, v in sn[1].items():
            if dk.get(k, 0) < v:
                dk[k] = v

    def _need(self, eng, ev, waits):
        if ev is None:
            return
        if ev[0] == "E":
            _, f, seq = ev
            if f == eng and not SAME_ENGINE_SYNC[eng]:
                return
            if self.known[eng][f] >= seq:
                return
            waits.append((self.esem[f], seq))
            self.known[eng][f] = seq
            self._merge(eng, self.snap[f][seq])
        else:
            _, q, i, val, sn = ev
            if self.dknown[eng].get((q, i), 0) >= val:
                return
            waits.append((self.pool[q][i], val))
            self.dknown[eng][(q, i)] = val
            self._merge(eng, sn)

    def _deps(self, eng, reads, writes, waits):
        for b in reads:
            self._need(eng, b.w, waits)
        for b in writes:
            self._need(eng, b.w, waits)
            for ev in b.r:
                self._need(eng, ev, waits)

    def _commit(self, ev, reads, writes):
        for b in reads:
            if b in writes:
                continue
            b.r.append(ev)
            if len(b.r) > 16:
                last = {}
                keep = []
                for e in b.r:
                    if e[0] == "E":
                        last[e[1]] = e
                    else:
                        keep.append(e)
                b.r = keep[-10:] + list(last.values())
        for b in writes:
            b.w = ev
            b.r = []

    def _scan(self, kwargs):
        reads, writes = [], []
        for k, v in kwargs.items():
            if isinstance(v, _APT):
                b = self.buf_of(v)
                if k in ("out", "accum_out", "out_max", "out_indices"):
                    if b not in writes:
                        writes.append(b)
                elif b not in reads:
                    reads.append(b)
        return reads, writes

    def I(self, eng, meth, **kwargs):
        xr = kwargs.pop("_reads", ())
        xw = kwargs.pop("_writes", ())
        reads, writes = self._scan(kwargs)
        reads += [self.buf_of(a) for a in xr]
        writes += [self.buf_of(a) for a in xw]
        waits = []
        self._deps(eng, reads, writes, waits)
        self.cnt[eng] += 1
        seq = self.cnt[eng]
        self.snap[eng].append((dict(self.known[eng]), dict(self.dknown[eng])))
        self.ops[eng].append((waits, meth, kwargs, self.esem[eng], 1))
        ev = ("E", eng, seq)
        self._commit(ev, reads, writes)
        return ev

    def dma(self, q, out, in_, **kw):
        reads = [self.buf_of(in_)]
        writes = [self.buf_of(out)]
        waits = []
        self._deps(q, reads, writes, waits)
        i = self.pool_next[q]
        self.pool_next[q] = (i + 1) % self.NPOOL
        prev = self.pool_val[q][i]
        if prev > 0 and self.dknown[q].get((q, i), 0) < prev:
            waits.append((self.pool[q][i], prev))
            self.dknown[q][(q, i)] = prev
        val = prev + 16
        self.pool_val[q][i] = val
        sn = (dict(self.known[q]), dict(self.dknown[q]))
        kw = dict(kw)
        kw["out"] = out
        kw["in_"] = in_
        self.ops[q].append((waits, "dma_start", kw, self.pool[q][i], 16))
        ev = ("D", q, i, val, sn)
        self._commit(ev, reads, writes)
        return ev

    def coll(self, kind, ins, outs, groups):
        q = "gpsimd"
        reads = [self.buf_of(a) for a in ins]
        writes = [self.buf_of(a) for a in outs]
        waits = []
        self._deps(q, reads, writes, waits)
        i = self.pool_next[q]
        self.pool_next[q] = (i + 1) % self.NPOOL
        prev = self.pool_val[q][i]
        if prev > 0 and self.dknown[q].get((q, i), 0) < prev:
            waits.append((self.pool[q][i], prev))
            self.dknown[q][(q, i)] = prev
        val = prev + 1
        self.pool_val[q][i] = val
        sn = (dict(self.known[q]), dict(self.dknown[q]))
        kw = dict(kind=kind, op=ALU.bypass, replica_groups=groups, ins=[a_.opt() for a_ in ins], outs=[a_.opt() for a_ in outs])
        self.ops[q].append((waits, "collective_compute", kw, self.pool[q][i], 1))
        ev = ("D", q, i, val, sn)
        self._commit(ev, reads, writes)
        return ev

    def finish(self, aps, eng="sync"):
        waits = []
        for a in aps:
            self._need(eng, self.buf_of(a).w, waits)
        self.ops[eng].append((waits, None, None, None, 0))

    def emit(self):
        nc = self.nc
        with nc.Block() as block:
            def mk(ename):
                def body(e):
                    for waits, meth, kw, sem, inc in self.ops[ename]:
                        for (s, v) in waits:
                            e.wait_ge(s, v)
                        if meth is not None:
                            getattr(e, meth)(**kw).then_inc(sem, inc)
                return body
            block.sync(mk("sync"))
            block.scalar(mk("scalar"))
            block.vector(mk("vector"))
            block.gpsimd(mk("gpsimd"))
            block.tensor(mk("tensor"))


class Rot:
    def __init__(self, P, name, shape, dt, n, psum=False):
        self.t = [(P.ps if psum else P.sb)("%s%d" % (name, i), shape, dt) for i in range(n)]
        self.i = 0

    def next(self):
        t = self.t[self.i % len(self.t)]
        self.i += 1
        return t


def new_nc():
    global _APT
    nc = bass.Bass("TRN2", target_bir_lowering=False)
    if _APT is None:
        t = nc.dram_tensor("apt_probe", [2, 2], F32).ap()
        _APT = type(t)
    return nc


class IO:
    def __init__(self, nc, m=None):
        self.nc = nc
        self.m = m or {}
        self.outs = []

    def inp(self, name, shape, dt=F32):
        if name in self.m:
            return self.m[name]
        return din(self.nc, name, shape, dt)

    def out(self, name, shape, dt=F32):
        if name in self.m:
            return self.m[name]
        ap = dout(self.nc, name, shape, dt)
        self.outs.append(ap)
        return ap


def din(nc, name, shape, dt=F32):
    return nc.dram_tensor(name, list(shape), dt, kind="ExternalInput").ap()


def dout(nc, name, shape, dt=F32):
    return nc.dram_tensor(name, list(shape), dt, kind="ExternalOutput").ap()


def make_identities(P):
    idf = P.sb("ident_f", [128, 128], F32)
    idb = P.sb("ident_b", [128, 128], BF16)
    P.I("gpsimd", "memset", ap=idf[:], constant=1.0, _writes=[idf[:]])
    P.I("gpsimd", "affine_select", out=idf[:], in_=idf[:], pattern=[[1, 128]], compare_op=ALU.is_equal,
        fill=0.0, base=0, channel_multiplier=-1)
    P.I("vector", "tensor_copy", out=idb[:], in_=idf[:])
    return idf, idb


def compute_mod(P, c_ap, adaw_ap, adab_ap, psA, psB, c0, c1, CH=256):
    n = c1 - c0
    mod = P.sb("mod_bc", [128, n], F32)
    ccol = P.sb("ccol", [128, 8], F32)
    cbc = P.sb("cbc", [128, 8, 128], F32)
    P.dma("sync", mod[:], adab_ap[c0:c1].partition_broadcast(128))
    P.dma("sync", ccol[:], c_ap.rearrange("(k p) -> p k", p=128), allow_slow_non_contiguous=True)
    P.I("scalar", "activation", out=ccol[:], in_=ccol[:], func=AF.Silu)
    P.I("vector", "tensor_copy", out=cbc[:], in_=ccol[:].unsqueeze(2).to_broadcast([128, 8, 128]))
    wr = Rot(P, "adaw_t", [128, 8, CH], F32, 2)
    awv = adaw_ap.rearrange("(k p) n -> p k n", p=128)
    for i in range(n // CH):
        wt = wr.next()
        P.dma("sync" if i % 2 == 0 else "scalar", wt[:], awv[:, :, c0 + i * CH:c0 + (i + 1) * CH])
        ps = psA if i % 2 == 0 else psB
        for k in range(8):
            P.I("tensor", "matmul", out=ps[:, 0:CH], lhsT=cbc[:, k, :], rhs=wt[:, k, :], start=(k == 0), stop=(k == 7))
        P.I("vector", "tensor_tensor", out=mod[:, i * CH:(i + 1) * CH], in0=mod[:, i * CH:(i + 1) * CH],
            in1=ps[:, 0:CH], op=ALU.add)
    return mod


def sin_of(P, out_ap, ang_ap, tmp_ap, tmpi_ap, shift):
    P.I("vector", "tensor_scalar", out=tmp_ap, in0=ang_ap, scalar1=1.0 / TWO_PI, scalar2=shift / TWO_PI, op0=ALU.mult, op1=ALU.add)
    P.I("vector", "tensor_copy", out=tmpi_ap, in_=tmp_ap)
    P.I("vector", "tensor_tensor", out=tmp_ap, in0=tmp_ap, in1=tmpi_ap, op=ALU.subtract)
    P.I("scalar", "activation", out=out_ap, in_=tmp_ap, func=AF.Sin, scale=TWO_PI)


def build_p1(ntok=TPC):
    nc = new_nc()
    P = Prog(nc)
    io = IO(nc)
    phase1(P, io, ntok)
    P.finish(io.outs)
    P.emit()
    return nc


def phase1(P, io, ntok):
    nc = P.nc
    if hasattr(P, "mla_tiles"):
        del P.mla_tiles
    NST = ntok // 512
    x_in = io.inp("x_in", [ntok, D])
    c_in = io.inp("c_in", [D])
    pos_in = io.inp("pos_in", [ntok], I32)
    adaw = io.inp("ada_w", [D, 6 * D])
    adab = io.inp("ada_b", [6 * D])
    gmix = io.inp("norm_mix_g", [D])
    w_in = io.inp("w_in", [D, IN_COLS])
    qng = io.inp("mla_q_norm_g", [192])
    wuq = io.inp("mla_w_uq", [192, 384])
    kvng = io.inp("mla_kv_norm_g", [128])
    wukv = io.inp("mla_w_ukv", [128, 512])
    qqk = io.inp("mla_q_qk_g", [96])
    kqk = io.inp("mla_k_qk_g", [96])
    inv_ret4 = io.inp("inv_ret4", [128, 1])
    inv_mla = io.inp("inv_mla", [16])
    attq = io.out("attq", [4, 96, ntok], BF16)
    attk = io.out("attk", [4, 96, ntok], BF16)
    attv = io.out("attv", [4, ntok, 64], BF16)
    retq = io.out("retq", [256, ntok], BF16)
    retk = io.out("retk", [256, ntok], BF16)
    retv = io.out("retv", [ntok, 256], BF16)
    lrux = io.out("lrux", [256, ntok], F32)
    cvx = io.out("cvx", [256, ntok], F32)
    bgate = io.out("bgate", [256, ntok], F32)
    sgate = io.out("sgate", [256, ntok], F32)
    ggate = io.out("ggate", [256, ntok], F32)

    psT = [P.ps("psT%d" % i, [128, 1024], BF16) for i in range(2)]
    psF = [P.ps("psF%d" % i, [128, 512], F32) for i in range(3)]
    psU = P.ps("psU", [128, 512], F32)
    psQ = P.ps("psQ", [128, 512], F32)
    psX = P.ps("psX", [128, 1024], BF16)
    fi = [0]

    def nextF():
        p = psF[fi[0] % 3]
        fi[0] += 1
        return p

    idf, idb = make_identities(P)
    P.negpi = P.sb("negpi", [128, 1], F32)
    P.I("vector", "memset", ap=P.negpi[:], constant=-math.pi, _writes=[P.negpi[:]])
    neghalf = P.sb("neghalf", [128, 16], F32)
    P.I("vector", "memset", ap=neghalf[:], constant=-0.5, _writes=[neghalf[:]])

    mod = compute_mod(P, c_in, adaw, adab, psF[0], psF[1], 0, 2 * D)
    gm = P.sb("gm", [128, D], F32)
    P.dma("sync", gm[:], gmix.partition_broadcast(128))
    P.I("vector", "scalar_tensor_tensor", out=gm[:], in0=mod[:, D:2 * D], scalar=1.0, in1=gm[:], op0=ALU.add, op1=ALU.mult)
    shm = mod[:, 0:D]

    w_bf = P.sb("w_bf", [128, 8, IN_COLS], BF16)
    wv = w_in.rearrange("(k p) n -> p k n", p=128)
    for k in range(8):
        for hh in range(2):
            P.dma("gpsimd", w_bf[:, k, hh * 1328:(hh + 1) * 1328], wv[:, k, hh * 1328:(hh + 1) * 1328])
    w_rot = P.sb("w_rot", [128, 8, 512], BF16)
    for k in range(8):
        src = w_bf[:, k, 1120:1632].rearrange("p (h two i) -> p h two i", two=2, i=32)
        dst = w_rot[:, k, :].rearrange("p (h two i) -> p h two i", two=2, i=32)
        P.I("vector", "tensor_scalar", out=dst[:, :, 0, :], in0=src[:, :, 1, :], scalar1=-1.0, scalar2=None, op0=ALU.mult)
        P.I("vector", "tensor_copy", out=dst[:, :, 1, :], in_=src[:, :, 0, :])
    wuq_bf = P.sb("wuq_bf", [128, 2, 384], BF16)
    P.dma("gpsimd", wuq_bf[:, 0, :], wuq[0:128, :])
    P.dma("gpsimd", wuq_bf[0:64, 1, :], wuq[128:192, :])
    wukv_bf = P.sb("wukv_bf", [128, 512], BF16)
    P.dma("gpsimd", wukv_bf[:], wukv)
    qng_bc = P.sb("qng_bc", [128, 192], F32)
    kvng_bc = P.sb("kvng_bc", [128, 128], F32)
    qqk_bc = P.sb("qqk_bc", [128, 96], F32)
    kqk_bc = P.sb("kqk_bc", [128, 96], F32)
    P.dma("sync", qng_bc[:], qng.partition_broadcast(128))
    P.dma("sync", kvng_bc[:], kvng.partition_broadcast(128))
    P.dma("sync", qqk_bc[:], qqk.partition_broadcast(128))
    P.dma("sync", kqk_bc[:], kqk.partition_broadcast(128))

    invc = P.sb("invc", [128, 1], F32)
    P.dma("sync", invc[:], inv_ret4)
    posi = P.sb("posi", [128, 512], I32)
    angt = P.sb("angt", [128, 512], F32)
    tmpa = P.sb("tmpa", [128, 512], F32)
    tmpai = P.sb("tmpai", [128, 512], I32)
    cos_r = Rot(P, "cosR", [128, 512], F32, 2)
    sin_r = Rot(P, "sinR", [128, 512], F32, 2)
    invm = P.sb("invm", [128, 16], F32)
    P.dma("sync", invm[:], inv_mla.partition_broadcast(128))
    posc_i = P.sb("posc_i", [128, 4], I32)
    posc = P.sb("posc", [128, 4], F32)
    angm = P.sb("angm", [128, 4, 16], F32)
    tmpm = P.sb("tmpm", [128, 4, 16], F32)
    tmpmi = P.sb("tmpmi", [128, 4, 16], I32)
    cosM_r = Rot(P, "cosM", [128, 4, 16], F32, 2)
    sinM_r = Rot(P, "sinM", [128, 4, 16], F32, 2)

    xt_r = Rot(P, "xt", [128, D], F32, 3)
    junk = P.sb("junk", [128, D], BF16)
    ssq = Rot(P, "ssq", [128, 4], F32, 2)
    v4 = Rot(P, "v4", [128, 4], F32, 2)
    rstd4 = Rot(P, "rstd4", [128, 4], F32, 2)
    tmp_r = Rot(P, "tmpx", [128, D], F32, 1)
    hb_r = Rot(P, "hb", [128, D], BF16, 2)
    hT_r = Rot(P, "hT", [128, 8, 512], BF16, 2)
    ev_r = Rot(P, "ev", [128, 512], F32, 4)
    evb_r = Rot(P, "evb", [128, 512], BF16, 4)
    csb_r = Rot(P, "csb", [128, 512], F32, 2)

    for st in range(NST):
        t0 = st * 512
        hT = hT_r.next()
        for j in range(4):
            xt = xt_r.next()
            P.dma("sync", xt[:], x_in[t0 + j * 128:t0 + (j + 1) * 128, :])
            sq = ssq.next()
            P.I("scalar", "activation", out=junk[:], in_=xt[:], func=AF.Square, accum_out=sq[:, 0:1])
            v = v4.next()
            rs = rstd4.next()
            P.I("vector", "tensor_scalar", out=v[:, 0:1], in0=sq[:, 0:1], scalar1=1.0 / D, scalar2=EPS, op0=ALU.mult, op1=ALU.add)
            P.I("gpsimd", "tensor_tensor", out=rs[:, 0:1], in0=v[:, 0:1], in1=neghalf[:, 0:1], op=ALU.pow)
            tm = tmp_r.next()
            hb = hb_r.next()
            P.I("vector", "scalar_tensor_tensor", out=tm[:], in0=xt[:], scalar=rs[:, 0:1], in1=gm[:],
                op0=ALU.mult, op1=ALU.mult)
            P.I("vector", "tensor_tensor", out=hb[:], in0=tm[:], in1=shm, op=ALU.add)
            pt = psT[j % 2]
            for k in range(8):
                P.I("tensor", "transpose", out=pt[:, k * 128:(k + 1) * 128], in_=hb[:, k * 128:(k + 1) * 128], identity=idb[:])
            P.I("scalar", "copy", out=hT[:, :, j * 128:(j + 1) * 128], in_=pt[:].rearrange("p (k t) -> p k t", k=8))
        cosR = cos_r.next()
        sinR = sin_r.next()
        P.dma("scalar", posi[:], pos_in[t0:t0 + 512].partition_broadcast(128))
        P.I("vector", "tensor_copy", out=angt[:], in_=posi[:])
        P.I("vector", "tensor_scalar", out=angt[:], in0=angt[:], scalar1=invc[:, 0:1], scalar2=None, op0=ALU.mult)
        sin_of(P, sinR[:], angt[:], tmpa[:], tmpai[:], 0.0)
        sin_of(P, cosR[:], angt[:], tmpa[:], tmpai[:], 0.5 * math.pi)
        cosM = cosM_r.next()
        sinM = sinM_r.next()
        P.dma("scalar", posc_i[:], pos_in[t0:t0 + 512].rearrange("(n p) -> p n", p=128), allow_slow_non_contiguous=True)
        P.I("vector", "tensor_copy", out=posc[:], in_=posc_i[:])
        P.I("vector", "tensor_tensor", out=angm[:], in0=posc[:].unsqueeze(2).to_broadcast([128, 4, 16]),
            in1=invm[:].unsqueeze(1).to_broadcast([128, 4, 16]), op=ALU.mult)
        sin_of(P, sinM[:], angm[:], tmpm[:], tmpmi[:], 0.0)
        sin_of(P, cosM[:], angm[:], tmpm[:], tmpmi[:], 0.5 * math.pi)

        def fm(wt, c0):
            ps = nextF()
            for k in range(8):
                P.I("tensor", "matmul", out=ps[:], lhsT=wt[:, k, c0:c0 + 128], rhs=hT[:, k, :], start=(k == 0), stop=(k == 7))
            return ps

        for ch in range(2):
            ps = fm(w_bf, ch * 128)
            e = ev_r.next()
            P.I("scalar", "copy", out=e[:], in_=ps[:])
            P.dma("sync", bgate[ch * 128:(ch + 1) * 128, t0:t0 + 512], e[:])
            psc = fm(w_bf, 256 + ch * 128)
            cs = csb_r.next()
            P.I("scalar", "copy", out=cs[:], in_=psc[:])
            psx = fm(w_bf, 512 + ch * 128)
            e = ev_r.next()
            P.I("vector", "tensor_tensor", out=e[:], in0=psx[:], in1=cs[:], op=ALU.mult)
            P.dma("sync", cvx[ch * 128:(ch + 1) * 128, t0:t0 + 512], e[:])
        for qk in range(2):
            for ch in range(2):
                c0 = 1120 + qk * 256 + ch * 128
                ps = fm(w_bf, c0)
                cs = csb_r.next()
                P.I("vector", "scalar_tensor_tensor", out=cs[:], in0=ps[:], scalar=(1.0 if qk == 0 else 0.125),
                    in1=cosR[:], op0=ALU.mult, op1=ALU.mult)
                psr = fm(w_rot, qk * 256 + ch * 128)
                e = ev_r.next()
                P.I("vector", "scalar_tensor_tensor", out=e[:], in0=psr[:], scalar=(1.0 if qk == 0 else 0.125),
                    in1=sinR[:], op0=ALU.mult, op1=ALU.mult)
                eb = evb_r.next()
                P.I("vector", "tensor_tensor", out=eb[:], in0=e[:], in1=cs[:], op=ALU.add)
                P.dma("sync", (retq if qk == 0 else retk)[ch * 128:(ch + 1) * 128, t0:t0 + 512], eb[:])
        for ch in range(2):
            ps = fm(w_bf, 1120 + 768 + ch * 128)
            e = ev_r.next()
            P.I("scalar", "activation", out=e[:], in_=ps[:], func=AF.Silu)
            P.dma("sync", sgate[ch * 128:(ch + 1) * 128, t0:t0 + 512], e[:])
        for ch in range(2):
            ps = fm(w_bf, 2144 + ch * 128)
            e = ev_r.next()
            P.I("scalar", "copy", out=e[:], in_=ps[:])
            P.dma("sync", lrux[ch * 128:(ch + 1) * 128, t0:t0 + 512], e[:])
        for ch in range(2):
            ps = fm(w_bf, 2144 + 256 + ch * 128)
            e = ev_r.next()
            P.I("scalar", "activation", out=e[:], in_=ps[:], func=AF.Gelu)
            P.dma("sync", ggate[ch * 128:(ch + 1) * 128, t0:t0 + 512], e[:])
        for j in range(4):
            tt = t0 + j * 128
            ti = tt // 128
            hTj = hT[:, :, j * 128:(j + 1) * 128]
            ps = nextF()
            for k in range(8):
                P.I("tensor", "matmul", out=ps[:, 0:256], lhsT=hT[:, k, j * 128:(j + 1) * 128], rhs=w_bf[:, k, 1632:1888],
                    start=(k == 0), stop=(k == 7))
            eb = evb_r.next()
            P.I("scalar", "copy", out=eb[:, 0:256], in_=ps[:, 0:256])
            P.dma("sync", retv[tt:tt + 128, :], eb[:, 0:256])
            mla_tile(P, locals(), tt, j, j)


def mla_tile(P, L, tt, ti, j):
    hT, w_bf, psU, psQ, psX = L["hT"], L["w_bf"], L["psU"], L["psQ"], L["psX"]
    idb, neghalf = L["idb"], L["neghalf"]
    qng_bc, kvng_bc, qqk_bc, kqk_bc = L["qng_bc"], L["kvng_bc"], L["qqk_bc"], L["kqk_bc"]
    wuq_bf, wukv_bf, cosM, sinM = L["wuq_bf"], L["wukv_bf"], L["cosM"], L["sinM"]
    attq, attk, attv = L["attq"], L["attk"], L["attv"]
    if not hasattr(P, "mla_tiles"):
        P.mla_tiles = dict(
            junk=P.sb("mjunk", [128, 512], F32),
            st3=Rot(P, "mst3", [128, 16], F32, 2),
            rs3=Rot(P, "mrs3", [128, 16], F32, 2),
            cqn=Rot(P, "mcqn", [128, 320], BF16, 2),
            cT=Rot(P, "mcT", [128, 384], BF16, 2),
            qsb=Rot(P, "mqsb", [128, 384], F32, 2),
            kvsb=Rot(P, "mkvsb", [128, 512], F32, 2),
            sq=Rot(P, "msq", [128, 512], F32, 4),
            Qt=Rot(P, "mQt", [128, 4, 96], BF16, 2),
            Kt=Rot(P, "mKt", [128, 4, 96], BF16, 2),
            Vt=Rot(P, "mVt", [128, 4, 64], BF16, 2),
            r1=Rot(P, "mr1", [128, 4, 32], F32, 2),
            r2=Rot(P, "mr2", [128, 4, 16], F32, 4),
            kr=Rot(P, "mkr", [128, 32], F32, 2),
            kr2=Rot(P, "mkr2", [128, 32], F32, 2),
            QT=Rot(P, "mQT", [96, 4, 128], BF16, 2),
            KT=Rot(P, "mKT", [96, 4, 128], BF16, 2),
            scl=P.sb("mscl", [128, 3], F32),
        )
        sc = P.mla_tiles["scl"]
        P.I("vector", "memset", ap=sc[:, 0:1], constant=1.0 / 192, _writes=[sc[:]])
        P.I("vector", "memset", ap=sc[:, 1:2], constant=1.0 / 128, _writes=[sc[:]])
        P.I("vector", "memset", ap=sc[:, 2:3], constant=1.0 / 32, _writes=[sc[:]])
    M = P.mla_tiles
    for k in range(8):
        P.I("tensor", "matmul", out=psU[:, 0:352], lhsT=hT[:, k, j * 128:(j + 1) * 128], rhs=w_bf[:, k, 768:1120],
            start=(k == 0), stop=(k == 7))
    st3 = M["st3"].next()
    rs3 = M["rs3"].next()
    P.I("scalar", "activation", out=M["junk"][:, 0:192], in_=psU[:, 0:192], func=AF.Square, accum_out=st3[:, 0:1])
    P.I("scalar", "activation", out=M["junk"][:, 0:128], in_=psU[:, 192:320], func=AF.Square, accum_out=st3[:, 1:2])
    P.I("scalar", "activation", out=M["junk"][:, 0:32], in_=psU[:, 320:352], func=AF.Square, accum_out=st3[:, 2:3])
    P.I("vector", "tensor_tensor", out=st3[:, 0:3], in0=st3[:, 0:3], in1=M["scl"][:], op=ALU.mult)
    P.I("vector", "tensor_scalar", out=st3[:, 0:3], in0=st3[:, 0:3], scalar1=EPS, scalar2=None, op0=ALU.add)
    P.I("gpsimd", "tensor_tensor", out=rs3[:, 0:3], in0=st3[:, 0:3], in1=neghalf[:, 0:3], op=ALU.pow)
    cqn = M["cqn"].next()
    P.I("vector", "scalar_tensor_tensor", out=cqn[:, 0:192], in0=psU[:, 0:192], scalar=rs3[:, 0:1], in1=qng_bc[:],
        op0=ALU.mult, op1=ALU.mult)
    P.I("vector", "scalar_tensor_tensor", out=cqn[:, 192:320], in0=psU[:, 192:320], scalar=rs3[:, 1:2], in1=kvng_bc[:],
        op0=ALU.mult, op1=ALU.mult)
    kr = M["kr"].next()
    P.I("vector", "scalar_tensor_tensor", out=kr[:], in0=psU[:, 320:352], scalar=rs3[:, 2:3], in1=kqk_bc[:, 64:96],
        op0=ALU.mult, op1=ALU.mult)
    P.I("tensor", "transpose", out=psX[:, 0:128], in_=cqn[:, 0:128], identity=idb[:])
    P.I("tensor", "transpose", out=psX[0:64, 128:256], in_=cqn[:, 128:192], identity=idb[:])
    P.I("tensor", "transpose", out=psX[:, 256:384], in_=cqn[:, 192:320], identity=idb[:])
    cT = M["cT"].next()
    P.I("scalar", "copy", out=cT[:, 0:128], in_=psX[:, 0:128])
    P.I("scalar", "copy", out=cT[0:64, 128:256], in_=psX[0:64, 128:256])
    P.I("scalar", "copy", out=cT[:, 256:384], in_=psX[:, 256:384])
    P.I("tensor", "matmul", out=psQ[:, 0:384], lhsT=cT[:, 0:128], rhs=wuq_bf[:, 0, :], start=True, stop=False)
    P.I("tensor", "matmul", out=psQ[:, 0:384], lhsT=cT[0:64, 128:256], rhs=wuq_bf[0:64, 1, :], start=False, stop=True)
    qsb = M["qsb"].next()
    sq = M["sq"].next()
    P.I("scalar", "copy", out=qsb[:], in_=psQ[:, 0:384])
    P.I("scalar", "activation", out=sq[:, 0:384], in_=psQ[:, 0:384], func=AF.Square)
    P.I("tensor", "matmul", out=psQ[:, 0:512], lhsT=cT[:, 256:384], rhs=wukv_bf[:], start=True, stop=True)
    kvsb = M["kvsb"].next()
    P.I("scalar", "copy", out=kvsb[:], in_=psQ[:, 0:512])
    st8 = M["st3"].next()
    rs8 = M["rs3"].next()
    sqv = sq[:, 0:384].rearrange("p (h c) -> p h c", c=96)
    P.I("vector", "tensor_reduce", out=st8[:, 0:4], in_=sqv[:, :, 0:64], axis=AX.X, op=ALU.add)
    P.I("vector", "tensor_reduce", out=st8[:, 4:8], in_=sqv[:, :, 64:96], axis=AX.X, op=ALU.add)
    sq2 = M["sq"].next()
    P.I("scalar", "activation", out=sq2[:], in_=kvsb[:], func=AF.Square)
    P.I("vector", "tensor_reduce", out=st8[:, 8:12], in_=sq2[:].rearrange("p (h c) -> p h c", c=128)[:, :, 0:64],
        axis=AX.X, op=ALU.add)
    P.I("vector", "tensor_scalar", out=st8[:, 0:4], in0=st8[:, 0:4], scalar1=1.0 / 64, scalar2=EPS, op0=ALU.mult, op1=ALU.add)
    P.I("vector", "tensor_scalar", out=st8[:, 4:8], in0=st8[:, 4:8], scalar1=1.0 / 32, scalar2=EPS, op0=ALU.mult, op1=ALU.add)
    P.I("vector", "tensor_scalar", out=st8[:, 8:12], in0=st8[:, 8:12], scalar1=1.0 / 64, scalar2=EPS, op0=ALU.mult, op1=ALU.add)
    P.I("gpsimd", "tensor_tensor", out=rs8[:, 0:12], in0=st8[:, 0:12], in1=neghalf[:, 0:12], op=ALU.pow)
    Qt = M["Qt"].next()
    Kt = M["Kt"].next()
    Vt = M["Vt"].next()
    qv = qsb[:].rearrange("p (h c) -> p h c", c=96)
    kvv = kvsb[:].rearrange("p (h c) -> p h c", c=128)
    r1 = M["r1"].next()
    tq = M["sq"].next()
    tqv = tq[:, 0:256].rearrange("p (h c) -> p h c", c=64)
    P.I("vector", "tensor_tensor", out=tqv, in0=qv[:, :, 0:64], in1=rs8[:, 0:4].unsqueeze(2).to_broadcast([128, 4, 64]), op=ALU.mult)
    P.I("vector", "tensor_tensor", out=Qt[:, :, 0:64], in0=tqv, in1=qqk_bc[:, 0:64].unsqueeze(1).to_broadcast([128, 4, 64]), op=ALU.mult)
    P.I("vector", "tensor_tensor", out=r1[:], in0=qv[:, :, 64:96], in1=rs8[:, 4:8].unsqueeze(2).to_broadcast([128, 4, 32]), op=ALU.mult)
    P.I("vector", "tensor_tensor", out=r1[:], in0=r1[:], in1=qqk_bc[:, 64:96].unsqueeze(1).to_broadcast([128, 4, 32]), op=ALU.mult)
    cb = cosM[:, ti, :].unsqueeze(1).to_broadcast([128, 4, 16])
    sb_ = sinM[:, ti, :].unsqueeze(1).to_broadcast([128, 4, 16])
    a1, a2, a3, a4 = M["r2"].next(), M["r2"].next(), M["r2"].next(), M["r2"].next()
    P.I("vector", "tensor_tensor", out=a1[:], in0=r1[:, :, 0:16], in1=cb, op=ALU.mult)
    P.I("vector", "tensor_tensor", out=a2[:], in0=r1[:, :, 16:32], in1=sb_, op=ALU.mult)
    P.I("vector", "tensor_tensor", out=a3[:], in0=r1[:, :, 16:32], in1=cb, op=ALU.mult)
    P.I("vector", "tensor_tensor", out=a4[:], in0=r1[:, :, 0:16], in1=sb_, op=ALU.mult)
    P.I("vector", "tensor_tensor", out=Qt[:, :, 64:80], in0=a1[:], in1=a2[:], op=ALU.subtract)
    P.I("vector", "tensor_tensor", out=Qt[:, :, 80:96], in0=a3[:], in1=a4[:], op=ALU.add)
    tk = M["sq"].next()
    tkv = tk[:, 0:256].rearrange("p (h c) -> p h c", c=64)
    P.I("vector", "tensor_tensor", out=tkv, in0=kvv[:, :, 0:64], in1=rs8[:, 8:12].unsqueeze(2).to_broadcast([128, 4, 64]), op=ALU.mult)
    P.I("vector", "tensor_tensor", out=Kt[:, :, 0:64], in0=tkv, in1=kqk_bc[:, 0:64].unsqueeze(1).to_broadcast([128, 4, 64]), op=ALU.mult)
    kr2 = M["kr2"].next()
    b1, b2, b3, b4 = M["r2"].next(), M["r2"].next(), M["r2"].next(), M["r2"].next()
    P.I("vector", "tensor_tensor", out=b1[:, 0, :], in0=kr[:, 0:16], in1=cosM[:, ti, :], op=ALU.mult)
    P.I("vector", "tensor_tensor", out=b2[:, 0, :], in0=kr[:, 16:32], in1=sinM[:, ti, :], op=ALU.mult)
    P.I("vector", "tensor_tensor", out=b3[:, 0, :], in0=kr[:, 16:32], in1=cosM[:, ti, :], op=ALU.mult)
    P.I("vector", "tensor_tensor", out=b4[:, 0, :], in0=kr[:, 0:16], in1=sinM[:, ti, :], op=ALU.mult)
    P.I("vector", "tensor_tensor", out=kr2[:, 0:16], in0=b1[:, 0, :], in1=b2[:, 0, :], op=ALU.subtract)
    P.I("vector", "tensor_tensor", out=kr2[:, 16:32], in0=b3[:, 0, :], in1=b4[:, 0, :], op=ALU.add)
    P.I("vector", "tensor_copy", out=Kt[:, :, 64:96], in_=kr2[:].unsqueeze(1).to_broadcast([128, 4, 32]))
    P.I("vector", "tensor_copy", out=Vt[:], in_=kvv[:, :, 64:128])
    P.dma("sync", attv[:, tt:tt + 128, :].rearrange("h t c -> t h c"), Vt[:])
    for h in range(4):
        P.I("tensor", "transpose", out=psX[0:96, 384 + h * 128:384 + (h + 1) * 128], in_=Qt[:, h, :], identity=idb[:])
    QT = M["QT"].next()
    P.I("scalar", "copy", out=QT[:], in_=psX[0:96, 384:896].rearrange("p (h t) -> p h t", h=4))
    P.dma("sync", attq[:, :, tt:tt + 128].rearrange("h c t -> c h t"), QT[:])
    for h in range(4):
        P.I("tensor", "transpose", out=psX[0:96, 384 + h * 128:384 + (h + 1) * 128], in_=Kt[:, h, :], identity=idb[:])
    KT = M["KT"].next()
    P.I("scalar", "copy", out=KT[:], in_=psX[0:96, 384:896].rearrange("p (h t) -> p h t", h=4))
    P.dma("sync", attk[:, :, tt:tt + 128].rearrange("h c t -> c h t"), KT[:])


ATT_SCALE = 96.0 ** -0.5


def build_p2(SS=S):
    nc = new_nc()
    P = Prog(nc)
    io = IO(nc)
    phase2(P, io, SS)
    P.finish(io.outs)
    P.emit()
    return nc


def phase2(P, io, SS, after_setup=None):
    nc = P.nc
    NB = SS // 128
    aq = io.inp("aq", [96, SS], BF16)
    ak = io.inp("ak", [96, SS], BF16)
    av = io.inp("av", [SS, 64], BF16)
    rq = io.inp("rq", [64, SS], BF16)
    rk = io.inp("rk", [64, SS], BF16)
    rv = io.inp("rv", [SS, 64], BF16)
    lx = io.inp("lx", [64, SS])
    cx = io.inp("cx", [64, SS])
    convw = io.inp("convw", [64, 3])
    lcw = io.inp("lcw", [64, 4])
    lcb = io.inp("lcb", [64, 1])
    wa = io.inp("wa", [64, 64])
    ba = io.inp("ba", [64, 1])
    wx = io.inp("wx", [64, 64])
    bx = io.inp("bx", [64, 1])
    lam = io.inp("lam", [64, 1])
    qqk = io.inp("qqk", [96])
    kqk = io.inp("kqk", [96])
    innerT = io.inp("innerT", [128, 128])
    qdecT = io.inp("qdecT", [64, 128])
    kdec = io.inp("kdec", [128, 1])
    cdec = io.inp("cdec", [64, 1])
    ao = io.out("ao", [64, SS])
    ro = io.out("ro", [64, SS])
    lo = io.out("lo", [64, SS])
    co = io.out("co", [64, SS])

    psS = [P.ps("psS%d" % i, [128, 512], F32) for i in range(3)]
    psO = [P.ps("psO%d" % i, [128, 512], F32) for i in range(2)]
    psB = P.ps("psB", [128, 512], F32)
    psR = [P.ps("psR%d" % i, [128, 512], F32) for i in range(2)]
    psKT = psB[:].bitcast(BF16)

    idf, idb = make_identities(P)
    half = P.sb("half", [128, 512], F32)
    P.I("vector", "memset", ap=half[:], constant=0.5, _writes=[half[:]])
    neghalf = P.sb("neghalf", [128, 512], F32)
    P.I("vector", "memset", ap=neghalf[:], constant=-0.5, _writes=[neghalf[:]])
    ones64 = P.sb("ones64", [128, 64], F32)
    P.I("vector", "memset", ap=ones64[:], constant=1.0 / 64, _writes=[ones64[:]])
    ones1 = P.sb("ones1", [128, 64], F32)
    P.I("vector", "memset", ap=ones1[:], constant=1.0, _writes=[ones1[:]])
    epsc = P.sb("epsc", [128, 1], F32)
    P.I("vector", "memset", ap=epsc[:], constant=EPS, _writes=[epsc[:]])

    cw = P.sb("cw", [64, 3], F32)
    P.dma("sync", cw[:], convw, allow_slow_non_contiguous=True)
    CSEG = min(2048, SS)
    cxt = P.sb("cxt", [64, 2 + CSEG], F32)
    cacc = Rot(P, "cacc", [64, CSEG], F32, 2)
    P.I("vector", "memset", ap=cxt[:, 0:2], constant=0.0, _writes=[cxt[:]])
    for sg in range(SS // CSEG):
        c0 = sg * CSEG
        if sg > 0:
            P.I("vector", "tensor_copy", out=cxt[:, 0:2], in_=cxt[:, CSEG:CSEG + 2])
        P.dma("sync", cxt[:, 2:2 + CSEG], cx[:, c0:c0 + CSEG])
        acc = cacc.next()
        P.I("vector", "tensor_scalar", out=acc[:], in0=cxt[:, 2:2 + CSEG], scalar1=cw[:, 2:3], scalar2=None, op0=ALU.mult)
        P.I("vector", "scalar_tensor_tensor", out=acc[:], in0=cxt[:, 1:1 + CSEG], scalar=cw[:, 1:2], in1=acc[:], op0=ALU.mult, op1=ALU.add)
        P.I("vector", "scalar_tensor_tensor", out=acc[:], in0=cxt[:, 0:CSEG], scalar=cw[:, 0:1], in1=acc[:], op0=ALU.mult, op1=ALU.add)
        P.dma("gpsimd", co[:, c0:c0 + CSEG], acc[:])

    lw = P.sb("lw", [64, 4], F32)
    lb = P.sb("lb", [64, 1], F32)
    bat = P.sb("bat", [64, 1], F32)
    bxt = P.sb("bxt", [64, 1], F32)
    lamt = P.sb("lamt", [64, 1], F32)
    nsp = P.sb("nsp", [64, 1], F32)
    wa_bf = P.sb("wa_bf", [64, 64], BF16)
    wx_bf = P.sb("wx_bf", [64, 64], BF16)
    P.dma("sync", lw[:], lcw, allow_slow_non_contiguous=True)
    P.dma("sync", lb[:], lcb, allow_slow_non_contiguous=True)
    P.dma("sync", bat[:], ba, allow_slow_non_contiguous=True)
    P.dma("sync", bxt[:], bx, allow_slow_non_contiguous=True)
    P.dma("sync", lamt[:], lam, allow_slow_non_contiguous=True)
    P.dma("gpsimd", wa_bf[:], wa)
    P.dma("gpsimd", wx_bf[:], wx)
    P.I("scalar", "activation", out=nsp[:], in_=lamt[:], func=AF.Exp, scale=-1.0)
    P.I("scalar", "activation", out=nsp[:], in_=nsp[:], func=AF.Ln, bias=ones1[0:64, 0:1], scale=1.0)
    P.I("vector", "tensor_scalar", out=nsp[:], in0=nsp[:], scalar1=-8.0, scalar2=None, op0=ALU.mult)
    LSEG = min(1024, SS)
    NHF = LSEG // 512
    lxt = P.sb("lxt", [64, 3 + LSEG], F32)
    P.I("vector", "memset", ap=lxt[:, 0:3], constant=0.0, _writes=[lxt[:]])
    xc_r = Rot(P, "xc", [64, LSEG], F32, 1)
    xcb_r = Rot(P, "xcb", [64, LSEG], BF16, 1)
    rg_r = Rot(P, "rg", [64, LSEG], F32, 1)
    ig_r = Rot(P, "ig", [64, LSEG], F32, 1)
    a_r = Rot(P, "la", [64, LSEG], F32, 1)
    a2_r = Rot(P, "la2", [64, LSEG], F32, 1)
    b_r = Rot(P, "lbin", [64, LSEG], F32, 1)
    h_r = Rot(P, "lh", [64, LSEG], F32, 2)
    gbanks = [psS[0], psS[1], psS[2], psO[0]]
    hprev = None
    lru_state = [None]

    def lru_gen():
      for sg in range(SS // LSEG):
        hprev = lru_state[0]
        c0 = sg * LSEG
        if sg > 0:
            P.I("vector", "tensor_copy", out=lxt[:, 0:3], in_=lxt[:, LSEG:LSEG + 3])
        P.dma("sync", lxt[:, 3:3 + LSEG], lx[:, c0:c0 + LSEG])
        xc = xc_r.next()
        P.I("vector", "tensor_scalar", out=xc[:], in0=lxt[:, 3:3 + LSEG], scalar1=lw[:, 3:4], scalar2=lb[:, 0:1], op0=ALU.mult, op1=ALU.add)
        for kk in range(3):
            P.I("vector", "scalar_tensor_tensor", out=xc[:], in0=lxt[:, kk:kk + LSEG], scalar=lw[:, kk:kk + 1], in1=xc[:], op0=ALU.mult, op1=ALU.add)
        xcb = xcb_r.next()
        P.I("vector", "tensor_copy", out=xcb[:], in_=xc[:])
        rg = rg_r.next()
        ig = ig_r.next()
        for hf in range(NHF):
            hs = slice(hf * 512, (hf + 1) * 512)
            P.I("tensor", "matmul", out=psR[0][0:64, :], lhsT=wa_bf[:], rhs=xcb[:, hs], start=True, stop=True)
            P.I("tensor", "matmul", out=psR[1][0:64, :], lhsT=wx_bf[:], rhs=xcb[:, hs], start=True, stop=True)
            P.I("scalar", "activation", out=rg[:, hs], in_=psR[0][0:64, :], func=AF.Sigmoid, bias=bat[:, 0:1], scale=1.0)
            P.I("scalar", "activation", out=ig[:, hs], in_=psR[1][0:64, :], func=AF.Sigmoid, bias=bxt[:, 0:1], scale=1.0)
        a = a_r.next()
        P.I("scalar", "activation", out=a[:], in_=rg[:], func=AF.Exp, scale=nsp[:, 0:1])
        a2 = a2_r.next()
        P.I("scalar", "activation", out=a2[:], in_=a[:], func=AF.Square)
        P.I("scalar", "activation", out=a2[:], in_=a2[:], func=AF.Sqrt, bias=ones1[0:64, 0:1], scale=-1.0)
        bb = b_r.next()
        P.I("vector", "tensor_tensor", out=bb[:], in0=ig[:], in1=xc[:], op=ALU.mult)
        P.I("vector", "tensor_tensor", out=bb[:], in0=bb[:], in1=a2[:], op=ALU.mult)
        h = h_r.next()
        P.I("vector", "tensor_tensor_scan", out=h[:], data0=a[:], data1=bb[:],
            initial=(0.0 if hprev is None else hprev[:, LSEG - 1:LSEG]), op0=ALU.mult, op1=ALU.add)
        lru_state[0] = h
        P.dma("gpsimd", lo[:, c0:c0 + LSEG], h[:])
        yield

    inn = P.sb("inn", [128, 128], F32)
    qd_c = P.sb("qd_c", [64, 512], F32)
    kd_c = P.sb("kd_c", [128, 1], F32)
    cd_c = P.sb("cd_c", [64, 1], F32)
    P.dma("sync", inn[:], innerT)
    for r_ in range(4):
        P.dma("sync", qd_c[:, r_ * 128:(r_ + 1) * 128], qdecT)
    P.dma("sync", kd_c[:], kdec)
    P.dma("sync", cd_c[:], cdec)
    PC = min(32, NB)
    RG = 4
    KVa = P.sb("KVa", [64, PC, 64], F32)
    Sbf = P.sb("Sbf", [64, PC, 64], BF16)
    gam = P.sb("gam", [64, PC], F32)
    carry = P.sb("rcarry", [64, 64], F32)
    P.I("vector", "memset", ap=carry[:], constant=0.0, _writes=[carry[:]])
    P.I("vector", "memset", ap=gam[:], constant=1.0, _writes=[gam[:]])
    P.I("vector", "tensor_scalar", out=gam[:], in0=gam[:], scalar1=cd_c[:, 0:1], scalar2=None, op0=ALU.mult)
    rq_r = Rot(P, "rq_t", [64, RG * 128], BF16, 2)
    rk_r = Rot(P, "rk_t", [64, RG * 128], BF16, 2)
    rv_r = Rot(P, "rv_t", [128, RG, 64], BF16, 2)
    scm_r = Rot(P, "scm", [128, 128], BF16, 3)
    kd_r = Rot(P, "kdt", [128, 64], BF16, 3)
    qd_r = Rot(P, "qdt", [64, RG * 128], BF16, 2)
    oT_r = Rot(P, "oT", [64, RG * 128], F32, 2)
    cen_r = Rot(P, "cen", [64, RG * 128], F32, 2)
    sqr_r = Rot(P, "sqr", [64, RG * 128], F32, 2)
    def ret_gen():
      for pc in range(NB // PC):
        c0 = pc * PC
        for g in range(PC // RG):
            t0 = (c0 + g * RG) * 128
            rkt, rvt = rk_r.next(), rv_r.next()
            P.dma("sync", rkt[:], rk[:, t0:t0 + RG * 128])
            P.dma("sync", rvt[:], rv[t0:t0 + RG * 128, :].rearrange("(n p) c -> p n c", p=128))
            for ci in range(RG):
                i = g * RG + ci
                cs = slice(ci * 128, (ci + 1) * 128)
                pT = psS[i % 3][:].bitcast(BF16)
                P.I("tensor", "transpose", out=pT[:, 0:64], in_=rkt[:, cs], identity=idb[0:64, 0:64])
                kdt = kd_r.next()
                P.I("scalar", "activation", out=kdt[:], in_=pT[:, 0:64], func=AF.Copy, scale=kd_c[:, 0:1])
                pK = psO[i % 2]
                P.I("tensor", "matmul", out=pK[0:64, 0:64], lhsT=kdt[:], rhs=rvt[:, ci, :], start=True, stop=True)
                P.I("vector", "tensor_copy", out=KVa[:, i, :], in_=pK[0:64, 0:64])
            yield
        for dv in range(64):
            P.I("vector", "tensor_tensor_scan", out=KVa[:, :, dv], data0=gam[:], data1=KVa[:, :, dv], initial=carry[:, dv:dv + 1],
                op0=ALU.mult, op1=ALU.add)
        P.I("scalar", "copy", out=Sbf[:, 0, :], in_=carry[:])
        if PC > 1:
            P.I("scalar", "copy", out=Sbf[:, 1:PC, :], in_=KVa[:, 0:PC - 1, :])
        P.I("vector", "tensor_copy", out=carry[:], in_=KVa[:, PC - 1, :])
        for g in range(PC // RG):
            t0 = (c0 + g * RG) * 128
            rqt, rkt, rvt = rq_r.next(), rk_r.next(), rv_r.next()
            P.dma("sync", rqt[:], rq[:, t0:t0 + RG * 128])
            P.dma("sync", rkt[:], rk[:, t0:t0 + RG * 128])
            P.dma("sync", rvt[:], rv[t0:t0 + RG * 128, :].rearrange("(n p) c -> p n c", p=128))
            qdt = qd_r.next()
            P.I("vector", "tensor_tensor", out=qdt[:], in0=rqt[:], in1=qd_c[:], op=ALU.mult)
            oT = oT_r.next()
            for ci in range(RG):
                i = g * RG + ci
                cs = slice(ci * 128, (ci + 1) * 128)
                pA = psS[i % 3]
                P.I("tensor", "matmul", out=pA[:, 0:128], lhsT=rkt[:, cs], rhs=rqt[:, cs], start=True, stop=True)
                scm = scm_r.next()
                P.I("vector", "tensor_tensor", out=scm[:], in0=pA[:, 0:128], in1=inn[:], op=ALU.mult)
                pBk = psO[i % 2]
                P.I("tensor", "matmul", out=pBk[0:64, 0:128], lhsT=rvt[:, ci, :], rhs=scm[:], start=True, stop=False)
                P.I("tensor", "matmul", out=pBk[0:64, 0:128], lhsT=Sbf[:, i, :], rhs=qdt[:, cs], start=False, stop=True)
                P.I("scalar", "copy", out=oT[:, cs], in_=pBk[0:64, 0:128])
            W = RG * 128
            P.I("tensor", "matmul", out=psB[0:64, 0:W], lhsT=ones64[0:64, :], rhs=oT[:], start=True, stop=True)
            cen = cen_r.next()
            P.I("vector", "tensor_tensor", out=cen[:], in0=oT[:], in1=psB[0:64, 0:W], op=ALU.subtract)
            sqr = sqr_r.next()
            P.I("scalar", "activation", out=sqr[:], in_=cen[:], func=AF.Square)
            P.I("tensor", "matmul", out=psB[0:64, 0:W], lhsT=ones64[0:64, :], rhs=sqr[:], start=True, stop=True)
            P.I("scalar", "activation", out=sqr[:], in_=psB[0:64, 0:W], func=AF.Sqrt, bias=epsc[0:64, 0:1], scale=1.0)
            P.I("vector", "reciprocal", out=sqr[:], in_=sqr[:])
            P.I("vector", "tensor_tensor", out=cen[:], in0=cen[:], in1=sqr[:], op=ALU.mult)
            P.dma("gpsimd", ro[:, t0:t0 + W], cen[:])
            yield

    g_l, g_r = lru_gen(), ret_gen()
    n_l = SS // LSEG
    n_r = (NB // PC) * 2 * (PC // RG)
    per = max(1, n_r // max(1, n_l))
    done_l = done_r = False
    while not (done_l and done_r):
        if not done_l:
            try:
                next(g_l)
            except StopIteration:
                done_l = True
        for _ in range(per):
            if not done_r:
                try:
                    next(g_r)
                except StopIteration:
                    done_r = True
    QT = P.sb("QT", [96, SS], BF16)
    KT = P.sb("KT", [96, SS], BF16)
    VA = P.sb("VA", [128, NB, 65], BF16)
    LDC = min(2048, SS)
    for i in range(SS // LDC):
        P.dma("sync", KT[:, i * LDC:(i + 1) * LDC], ak[:, i * LDC:(i + 1) * LDC])
        P.dma("scalar", QT[:, i * LDC:(i + 1) * LDC], aq[:, i * LDC:(i + 1) * LDC])
    P.I("gpsimd", "memset", ap=VA[:, :, 64:65], constant=1.0, _writes=[VA[:]])
    P.dma("sync", VA[:, :, 0:64], av.rearrange("(n p) c -> p n c", p=128))
    tri_f = P.sb("tri_f", [128, 128], F32)
    tri = P.sb("tri", [128, 128], BF16)
    P.I("gpsimd", "memset", ap=tri_f[:], constant=1.0, _writes=[tri_f[:]])
    P.I("gpsimd", "affine_select", out=tri_f[:], in_=tri_f[:], pattern=[[1, 128]], compare_op=ALU.is_ge,
        fill=0.0, base=0, channel_multiplier=-1)
    P.I("vector", "tensor_copy", out=tri[:], in_=tri_f[:])
    gq = P.sb("gq_bc", [128, 96], F32)
    gk = P.sb("gk_bc", [128, 96], F32)
    m2 = P.sb("m2", [128, 2], F32)
    negc = P.sb("negc", [128, 1], F32)
    P.dma("sync", gq[:], qqk.partition_broadcast(128))
    P.dma("sync", gk[:], kqk.partition_broadcast(128))
    P.I("vector", "tensor_tensor", out=gq[:], in0=gq[:], in1=gq[:], op=ALU.mult)
    P.I("vector", "tensor_tensor", out=gk[:], in0=gk[:], in1=gk[:], op=ALU.mult)
    P.I("vector", "tensor_reduce", out=m2[:, 0:1], in_=gq[:], axis=AX.X, op=ALU.max)
    P.I("vector", "tensor_reduce", out=m2[:, 1:2], in_=gk[:], axis=AX.X, op=ALU.max)
    P.I("vector", "tensor_tensor", out=negc[:], in0=m2[:, 0:1], in1=m2[:, 1:2], op=ALU.mult)
    P.I("gpsimd", "tensor_tensor", out=negc[:], in0=negc[:], in1=half[:, 0:1], op=ALU.pow)
    P.I("vector", "tensor_scalar", out=negc[:], in0=negc[:], scalar1=-math.sqrt(96.0), scalar2=None, op0=ALU.mult)
    if after_setup is not None:
        after_setup()
    PT_r = Rot(P, "PT", [128, 512], BF16, 5)
    den_r = Rot(P, "den", [128, 512], F32, 2)
    oa_r = Rot(P, "oa", [64, 512], F32, 2)
    rb_r = Rot(P, "rb", [64, 512], F32, 2)
    si = 0
    for qs in range(SS // 512):
        q0 = qs * 512
        pO = psO[qs % 2]
        nkb = 4 * qs + 4
        pend = []
        for j in range(nkb):
            d = j - 4 * qs
            lo_c = 0 if d <= 0 else d * 128
            pS = psS[si % 3]
            si += 1
            P.I("tensor", "matmul", out=pS[:, lo_c:512], lhsT=KT[:, j * 128:(j + 1) * 128], rhs=QT[:, q0 + lo_c:q0 + 512],
                start=True, stop=True)
            if len(pend) >= 2:
                pj, plo, pPT = pend.pop(0)
                P.I("tensor", "matmul", out=pO[0:65, plo:512], lhsT=VA[:, pj, :], rhs=pPT[:, plo:512], start=(pj == 0), stop=False)
            PT = PT_r.next()
            P.I("scalar", "activation", out=PT[:, lo_c:512], in_=pS[:, lo_c:512], func=AF.Exp, bias=negc[:, 0:1], scale=ATT_SCALE)
            if d >= 0:
                P.I("vector", "tensor_tensor", out=PT[:, lo_c:lo_c + 128], in0=PT[:, lo_c:lo_c + 128], in1=tri[:], op=ALU.mult)
            pend.append((j, lo_c, PT))
        while pend:
            pj, plo, pPT = pend.pop(0)
            P.I("tensor", "matmul", out=pO[0:65, plo:512], lhsT=VA[:, pj, :], rhs=pPT[:, plo:512], start=(pj == 0), stop=(len(pend) == 0))
        den = den_r.next()
        P.I("vector", "tensor_copy", out=den[64:65, :], in_=pO[64:65, :])
        P.I("tensor", "matmul", out=psB[0:64, :], lhsT=ones1[64:65, 0:64], rhs=den[64:65, :], start=True, stop=True)
        rb = rb_r.next()
        P.I("vector", "reciprocal", out=rb[:], in_=psB[0:64, :])
        oa = oa_r.next()
        P.I("vector", "tensor_tensor", out=oa[:], in0=pO[0:64, :], in1=rb[:], op=ALU.mult)
        P.dma("gpsimd", ao[:, q0:q0 + 512], oa[:])


def build_p3(ntok=TPC, TB=1024, NEXP=32, stop=0):
    nc = new_nc()
    P = Prog(nc)
    io = IO(nc)
    phase3(P, io, ntok, TB, NEXP)
    P.finish(io.outs)
    P.emit()
    return nc


def phase3(P, io, ntok, TB=1024, NEXP=32, stop=0):
    nc = P.nc
    x_in = io.inp("x_in", [ntok, D])
    c_in = io.inp("c_in", [D])
    adaw = io.inp("ada_w", [D, 6 * D])
    adab = io.inp("ada_b", [6 * D])
    mixo = [io.inp(n, [256, ntok]) for n in ("co", "ao", "ro", "lo")]
    gates = {0: io.inp("bgate", [256, ntok]), 2: io.inp("sgate", [256, ntok]), 3: io.inp("ggate", [256, ntok])}
    mng = io.inp("mix_norm_g", [D])
    w_out = io.inp("w_out", [D, D])
    gffn = io.inp("norm_ffn_g", [D])
    wgr = io.inp("router_group_w", [D, 4])
    bgr = io.inp("router_group_b", [4])
    wer = io.inp("router_expert_w", [D, 32])
    ber = io.inp("router_expert_b", [32])
    ewg = io.inp("exp_w_gate", [32, D, 256])
    ewu = io.inp("exp_w_up", [32, D, 256])
    ewd = io.inp("exp_w_down", [32, 256, D])
    x_out = io.out("x_out", [ntok, D])
    x1s = io.out("x1s", [ntok, D])

    bank = [P.ps("bank%d" % i, [128, 512], F32) for i in range(8)]
    idf, idb = make_identities(P)
    neghalf = P.sb("neghalf", [128, 256], F32)
    P.I("vector", "memset", ap=neghalf[:], constant=-0.5, _writes=[neghalf[:]])
    onesf = P.sb("onesf", [128, 128], F32)
    P.I("vector", "memset", ap=onesf[:], constant=1.0, _writes=[onesf[:]])
    epsc = P.sb("epsc", [128, 1], F32)
    P.I("vector", "memset", ap=epsc[:], constant=EPS, _writes=[epsc[:]])

    mod = compute_mod(P, c_in, adaw, adab, bank[0], bank[1], 2 * D, 6 * D, CH=128)
    gtm = mod[:, 0:D]
    shf = mod[:, D:2 * D]
    gf = mod[:, 2 * D:3 * D]
    gtf = mod[:, 3 * D:4 * D]
    tmo_r = Rot(P, "tmo", [128, D], F32, 2)
    gtmp = tmo_r.next()
    P.dma("sync", gtmp[:], gffn.partition_broadcast(128))
    P.I("vector", "scalar_tensor_tensor", out=gf, in0=gf, scalar=1.0, in1=gtmp[:], op0=ALU.add, op1=ALU.mult)
    mngc = P.sb("mngc", [128, 8], F32)
    P.dma("sync", mngc[:], mng.rearrange("(k p) -> p k", p=128), allow_slow_non_contiguous=True)
    wout_bf = P.sb("wout_bf", [128, 8, D], BF16)
    wov = w_out.rearrange("(k p) n -> p k n", p=128)
    for k in range(8):
        P.dma("gpsimd", wout_bf[:, k, :], wov[:, k, :])
    wr = P.sb("wr", [128, 8, 36], F32)
    P.dma("sync", wr[:, :, 0:4], wgr.rearrange("(k p) n -> p k n", p=128))
    P.dma("sync", wr[:, :, 4:36], wer.rearrange("(k p) n -> p k n", p=128))
    wr_hi = P.sb("wr_hi", [128, 8, 36], BF16)
    wr_lo = P.sb("wr_lo", [128, 8, 36], BF16)
    P.I("vector", "tensor_copy", out=wr_hi[:], in_=wr[:])
    P.I("vector", "tensor_tensor", out=wr_lo[:], in0=wr[:], in1=wr_hi[:], op=ALU.subtract)
    bg_bc = P.sb("bg_bc", [128, 4], F32)
    be_bc = P.sb("be_bc", [128, 32], F32)
    P.dma("sync", bg_bc[:], bgr.partition_broadcast(128))
    P.dma("sync", be_bc[:], ber.partition_broadcast(128))

    NSUB = TB // 128
    h2T = P.sb("h2T", [128, 8, TB], BF16)
    comb_all = P.sb("comb_all", [128, NSUB, 32], F32)
    yacc = P.sb("yacc", [128, NSUB, D], F32)

    yc_r = Rot(P, "yc", [128, 256], F32, 3)
    gt_r = Rot(P, "gtl", [128, 256], F32, 2)
    ysq_r = Rot(P, "ysq", [128, 256], F32, 2)
    ych = [P.sb("ych%d" % i, [128, 256], F32) for i in range(8)]
    rsb_r = Rot(P, "rsb", [128, 256], F32, 2)
    yT = P.sb("yT", [128, 8, 256], BF16)
    xt_r = Rot(P, "xt", [128, D], F32, 1)
    x1_r = Rot(P, "x1", [128, D], F32, 2)
    junk = P.sb("junk", [128, D], BF16)
    sm_r = Rot(P, "sm", [128, 8], F32, 4)
    h2f_r = Rot(P, "h2f", [128, D], F32, 1)
    h2Tlo = P.sb("h2Tlo", [128, 8, 128], BF16)
    hsp_r = Rot(P, "hsp", [128, D], BF16, 2)
    R8 = Rot(P, "r8", [128, 8], F32, 20)
    R4 = Rot(P, "r4", [128, 4], F32, 8)
    R1 = Rot(P, "r1", [128, 1], F32, 16)
    lg_r = Rot(P, "lgs", [128, 36], F32, 2)
    wgu_r = Rot(P, "wgu", [128, 8, 512], BF16, 2)
    wd_r = Rot(P, "wd", [128, 2, D], BF16, 2)
    sg_r = Rot(P, "sg", [128, 256], BF16, 4)
    hid_r = Rot(P, "hid", [128, 2, 256], BF16, 2)

    if "wgu_s" in io.m:
        wgu_s, wd_s = io.m["wgu_s"], io.m["wd_s"]
    else:
        wgu_s, wd_s = precast_experts(P, ewg, ewu, ewd, NEXP)

    for blk in range(ntok // TB):
        b0 = blk * TB
        for tl in range(TB // 256):
            t0 = b0 + tl * 256
            for m in range(4):
                pss = bank[m % 2]
                for c in range(2):
                    ci = m * 2 + c
                    y = ych[ci]
                    if m == 1:
                        P.dma("sync", y[:], mixo[m][c * 128:(c + 1) * 128, t0:t0 + 256])
                    else:
                        yc = yc_r.next()
                        gt = gt_r.next()
                        P.dma("sync", yc[:], mixo[m][c * 128:(c + 1) * 128, t0:t0 + 256])
                        P.dma("scalar", gt[:], gates[m][c * 128:(c + 1) * 128, t0:t0 + 256])
                        P.I("vector", "tensor_tensor", out=y[:], in0=yc[:], in1=gt[:], op=ALU.mult)
                    ysq = ysq_r.next()
                    P.I("scalar", "activation", out=ysq[:], in_=y[:], func=AF.Square)
                    P.I("tensor", "matmul", out=pss[:, 0:256], lhsT=onesf[:], rhs=ysq[:], start=(c == 0), stop=(c == 1))
                rsb = rsb_r.next()
                P.I("scalar", "activation", out=rsb[:], in_=pss[:, 0:256], func=AF.Sqrt, bias=epsc[:, 0:1], scale=1.0 / 256)
                P.I("vector", "reciprocal", out=rsb[:], in_=rsb[:])
                for c in range(2):
                    ci = m * 2 + c
                    P.I("vector", "scalar_tensor_tensor", out=yT[:, ci, :], in0=ych[ci][:], scalar=mngc[:, ci:ci + 1], in1=rsb[:],
                        op0=ALU.mult, op1=ALU.mult)
            for s in range(2):
                tt = t0 + s * 128
                si = tl * 2 + s
                po = [bank[2], bank[3]]
                for hf in range(2):
                    for k in range(8):
                        P.I("tensor", "matmul", out=po[hf][:], lhsT=yT[:, k, s * 128:(s + 1) * 128], rhs=wout_bf[:, k, hf * 512:(hf + 1) * 512],
                            start=(k == 0), stop=(k == 7))
                xt = xt_r.next()
                P.dma("sync", xt[:], x_in[tt:tt + 128, :])
                tmo = tmo_r.next()
                x1 = x1_r.next()
                for hf in range(2):
                    P.I("vector", "tensor_tensor", out=tmo[:, hf * 512:(hf + 1) * 512], in0=po[hf][:], in1=gtm[:, hf * 512:(hf + 1) * 512], op=ALU.mult)
                P.I("vector", "tensor_tensor", out=x1[:], in0=tmo[:], in1=xt[:], op=ALU.add)
                P.dma("sync", x1s[tt:tt + 128, :], x1[:])
                sm = sm_r.next()
                P.I("scalar", "activation", out=junk[:], in_=x1[:], func=AF.Square, accum_out=sm[:, 0:1])
                P.I("vector", "tensor_scalar", out=sm[:, 1:2], in0=sm[:, 0:1], scalar1=1.0 / D, scalar2=EPS, op0=ALU.mult, op1=ALU.add)
                P.I("gpsimd", "tensor_tensor", out=sm[:, 2:3], in0=sm[:, 1:2], in1=neghalf[:, 0:1], op=ALU.pow)
                tm2 = tmo_r.next()
                h2f = h2f_r.next()
                P.I("vector", "scalar_tensor_tensor", out=tm2[:], in0=x1[:], scalar=sm[:, 2:3], in1=gf, op0=ALU.mult, op1=ALU.mult)
                P.I("vector", "tensor_tensor", out=h2f[:], in0=tm2[:], in1=shf, op=ALU.add)
                hhi = hsp_r.next()
                hlo = hsp_r.next()
                P.I("scalar", "copy", out=hhi[:], in_=h2f[:])
                P.I("vector", "tensor_tensor", out=hlo[:], in0=h2f[:], in1=hhi[:], op=ALU.subtract)
                b4 = bank[4][:].bitcast(BF16)
                b5 = bank[5][:].bitcast(BF16)
                for k in range(8):
                    P.I("tensor", "transpose", out=b4[:, k * 128:(k + 1) * 128], in_=hhi[:, k * 128:(k + 1) * 128], identity=idb[:])
                for k in range(8):
                    P.I("tensor", "transpose", out=b5[:, k * 128:(k + 1) * 128], in_=hlo[:, k * 128:(k + 1) * 128], identity=idb[:])
                P.I("scalar", "copy", out=h2T[:, :, si * 128:(si + 1) * 128], in_=b4.rearrange("p (k t) -> p k t", k=8))
                P.I("vector", "tensor_copy", out=h2Tlo[:], in_=b5.rearrange("p (k t) -> p k t", k=8))
                pl = bank[6]
                for k in range(8):
                    P.I("tensor", "matmul", out=pl[:, 0:36], lhsT=h2T[:, k, si * 128:(si + 1) * 128], rhs=wr_hi[:, k, :], start=(k == 0), stop=False)
                for k in range(8):
                    P.I("tensor", "matmul", out=pl[:, 0:36], lhsT=h2Tlo[:, k, :], rhs=wr_hi[:, k, :], start=False, stop=False)
                for k in range(8):
                    P.I("tensor", "matmul", out=pl[:, 0:36], lhsT=h2T[:, k, si * 128:(si + 1) * 128], rhs=wr_lo[:, k, :], start=False, stop=(k == 7))
                lg = lg_r.next()
                P.I("vector", "tensor_copy", out=lg[:], in_=pl[:, 0:36])
                router_math(P, lg, comb_all[:, si, :], bg_bc, be_bc, R8, R4, R1)
        NTL = TB // 256
        steps = [(e, tl) for e in range(NEXP) for tl in range(NTL)]
        wts = {}
        bcs = {}
        hids = {}

        def load_expert(e):
            wgu = wgu_r.next()
            wd = wd_r.next()
            P.dma("sync", wgu[:], wgu_s[e])
            P.dma("sync", wd[:], wd_s[e])
            wts[e] = (wgu, wd)

        def gu_mm(i):
            e, tl = steps[i]
            if tl == 0:
                if e not in wts:
                    load_expert(e)
            wgu, wd = wts[e]
            pg = bank[4 + 2 * (i % 2)]
            pu = bank[5 + 2 * (i % 2)]
            for fc in range(2):
                for k in range(8):
                    P.I("tensor", "matmul", out=pg[:, fc * 256:(fc + 1) * 256], lhsT=wgu[:, k, fc * 128:(fc + 1) * 128],
                        rhs=h2T[:, k, tl * 256:(tl + 1) * 256], start=(k == 0), stop=(k == 7))
                for k in range(8):
                    P.I("tensor", "matmul", out=pu[:, fc * 256:(fc + 1) * 256], lhsT=wgu[:, k, 256 + fc * 128:256 + (fc + 1) * 128],
                        rhs=h2T[:, k, tl * 256:(tl + 1) * 256], start=(k == 0), stop=(k == 7))

        def gu_ew(i):
            e, tl = steps[i]
            pg = bank[4 + 2 * (i % 2)]
            pu = bank[5 + 2 * (i % 2)]
            hid = hid_r.next()
            hids[i] = hid
            for fc in range(2):
                sg = sg_r.next()
                P.I("scalar", "activation", out=sg[:], in_=pg[:, fc * 256:(fc + 1) * 256], func=AF.Silu)
                P.I("vector", "tensor_tensor", out=hid[:, fc, :], in0=sg[:], in1=pu[:, fc * 256:(fc + 1) * 256], op=ALU.mult)

        def down(i):
            e, tl = steps[i]
            wgu, wd = wts[e]
            hid = hids.pop(i)
            for s in range(2):
                si = tl * 2 + s
                for hf in range(2):
                    py = bank[s * 2 + hf]
                    for fc in range(2):
                        P.I("tensor", "matmul", out=py[:], lhsT=hid[:, fc, s * 128:(s + 1) * 128], rhs=wd[:, fc, hf * 512:(hf + 1) * 512],
                            start=(fc == 0), stop=(fc == 1))
                    if e == 0:
                        P.I("vector", "tensor_scalar", out=yacc[:, si, hf * 512:(hf + 1) * 512], in0=py[:], scalar1=comb_all[:, si, e:e + 1],
                            scalar2=None, op0=ALU.mult)
                    else:
                        P.I("vector", "scalar_tensor_tensor", out=yacc[:, si, hf * 512:(hf + 1) * 512], in0=py[:], scalar=comb_all[:, si, e:e + 1],
                            in1=yacc[:, si, hf * 512:(hf + 1) * 512], op0=ALU.mult, op1=ALU.add)
            if tl == NTL - 1:
                wts.pop(e, None)
            if tl == 0 and e + 1 < NEXP:
                load_expert(e + 1)

        if steps:
            gu_mm(0)
            gu_ew(0)
        for i in range(len(steps)):
            if i + 1 < len(steps):
                gu_mm(i + 1)
            down(i)
            if i + 1 < len(steps):
                gu_ew(i + 1)
        for si in range(NSUB):
            tt = b0 + si * 128
            x1 = x1_r.next()
            P.dma("sync", x1[:], x1s[tt:tt + 128, :])
            tmo = tmo_r.next()
            P.I("vector", "tensor_tensor", out=tmo[:], in0=yacc[:, si, :], in1=gtf, op=ALU.mult)
            xo = xt_r.next()
            P.I("vector", "tensor_tensor", out=xo[:], in0=tmo[:], in1=x1[:], op=ALU.add)
            P.dma("sync", x_out[tt:tt + 128, :], xo[:])


RSTOP = 0


def precast_experts(P, ewg, ewu, ewd, NEXP=32):
    nc = P.nc
    wgu_s = nc.dram_tensor(P.prefix + "wgu_s", [32, 128, 8, 512], BF16).ap()
    wd_s = nc.dram_tensor(P.prefix + "wd_s", [32, 128, 2, D], BF16).ap()
    for e in range(NEXP):
        gv = ewg[e].rearrange("(k p) f -> p k f", p=128)
        uv = ewu[e].rearrange("(k p) f -> p k f", p=128)
        for k in range(8):
            P.dma("gpsimd", wgu_s[e, :, k, 0:256], gv[:, k, :])
            P.dma("gpsimd", wgu_s[e, :, k, 256:512], uv[:, k, :])
        dv = ewd[e].rearrange("(k p) n -> p k n", p=128)
        for k in range(2):
            P.dma("gpsimd", wd_s[e, :, k, :], dv[:, k, :])
    return wgu_s, wd_s


def router_math(P, lg, comb, bg_bc, be_bc, R8, R4, R1):
    V = lambda *a, **k: P.I("vector", *a, **k)
    lgg = lg[:, 0:4]
    mx = R1.next()
    V("tensor_reduce", out=mx[:], in_=lgg, axis=AX.X, op=ALU.max)
    nmx = R1.next()
    V("tensor_scalar", out=nmx[:], in0=mx[:], scalar1=-1.0, scalar2=None, op0=ALU.mult)
    eg = R4.next()
    sg = R1.next()
    P.I("scalar", "activation", out=eg[:], in_=lgg, func=AF.Exp, bias=nmx[:, 0:1], scale=1.0, accum_out=sg[:, 0:1])
    if RSTOP == 1:
        return
    rs = R1.next()
    V("reciprocal", out=rs[:], in_=sg[:])
    gp = R4.next()
    V("tensor_scalar", out=gp[:], in0=eg[:], scalar1=rs[:, 0:1], scalar2=None, op0=ALU.mult)
    sel = R4.next()
    V("tensor_tensor", out=sel[:], in0=gp[:], in1=bg_bc[:], op=ALU.add)
    m = R1.next()
    V("tensor_reduce", out=m[:], in_=sel[:], axis=AX.X, op=ALU.max)
    goh = R4.next()
    V("tensor_scalar", out=goh[:], in0=sel[:], scalar1=m[:, 0:1], scalar2=None, op0=ALU.is_equal)
    if RSTOP == 2:
        return
    gwj = R4.next()
    gw = R1.next()
    V("tensor_tensor", out=gwj[:], in0=gp[:], in1=goh[:], op=ALU.mult)
    V("tensor_reduce", out=gw[:], in_=gwj[:], axis=AX.X, op=ALU.add)
    els = R8.next()
    bes = R8.next()
    V("tensor_scalar", out=els[:], in0=lg[:, 4:12], scalar1=goh[:, 0:1], scalar2=None, op0=ALU.mult)
    V("tensor_scalar", out=bes[:], in0=be_bc[:, 0:8], scalar1=goh[:, 0:1], scalar2=None, op0=ALU.mult)
    for g in range(1, 4):
        V("scalar_tensor_tensor", out=els[:], in0=lg[:, 4 + 8 * g:12 + 8 * g], scalar=goh[:, g:g + 1], in1=els[:], op0=ALU.mult, op1=ALU.add)
        V("scalar_tensor_tensor", out=bes[:], in0=be_bc[:, 8 * g:8 * g + 8], scalar=goh[:, g:g + 1], in1=bes[:], op0=ALU.mult, op1=ALU.add)
    if RSTOP == 3:
        return
    mx8 = R1.next()
    V("tensor_reduce", out=mx8[:], in_=els[:], axis=AX.X, op=ALU.max)
    nm8 = R1.next()
    V("tensor_scalar", out=nm8[:], in0=mx8[:], scalar1=-1.0, scalar2=None, op0=ALU.mult)
    ee = R8.next()
    se = R1.next()
    P.I("scalar", "activation", out=ee[:], in_=els[:], func=AF.Exp, bias=nm8[:, 0:1], scale=1.0, accum_out=se[:, 0:1])
    rse = R1.next()
    V("reciprocal", out=rse[:], in_=se[:])
    ep = R8.next()
    V("tensor_scalar", out=ep[:], in0=ee[:], scalar1=rse[:, 0:1], scalar2=None, op0=ALU.mult)
    if RSTOP == 4:
        return
    sc = R8.next()
    V("tensor_tensor", out=sc[:], in0=ep[:], in1=bes[:], op=ALU.add)
    m1 = R1.next()
    V("tensor_reduce", out=m1[:], in_=sc[:], axis=AX.X, op=ALU.max)
    oh1 = R8.next()
    V("tensor_scalar", out=oh1[:], in0=sc[:], scalar1=m1[:, 0:1], scalar2=None, op0=ALU.is_equal)
    sc2 = R8.next()
    V("scalar_tensor_tensor", out=sc2[:], in0=oh1[:], scalar=-1e9, in1=sc[:], op0=ALU.mult, op1=ALU.add)
    m2 = R1.next()
    V("tensor_reduce", out=m2[:], in_=sc2[:], axis=AX.X, op=ALU.max)
    oh2 = R8.next()
    V("tensor_scalar", out=oh2[:], in0=sc2[:], scalar1=m2[:, 0:1], scalar2=None, op0=ALU.is_equal)
    if RSTOP == 5:
        return
    ohs = R8.next()
    V("tensor_tensor", out=ohs[:], in0=oh1[:], in1=oh2[:], op=ALU.add)
    tp = R8.next()
    V("tensor_tensor", out=tp[:], in0=ep[:], in1=ohs[:], op=ALU.mult)
    sp = R1.next()
    V("tensor_reduce", out=sp[:], in_=tp[:], axis=AX.X, op=ALU.add)
    rsp = R1.next()
    V("reciprocal", out=rsp[:], in_=sp[:])
    fac = R1.next()
    V("tensor_tensor", out=fac[:], in0=rsp[:], in1=gw[:], op=ALU.mult)
    ew = R8.next()
    V("tensor_scalar", out=ew[:], in0=tp[:], scalar1=fac[:, 0:1], scalar2=None, op0=ALU.mult)
    if RSTOP == 6:
        return
    for g in range(4):
        V("tensor_scalar", out=comb[:, g * 8:(g + 1) * 8], in0=ew[:], scalar1=goh[:, g:g + 1], scalar2=None, op0=ALU.mult)


W_SPECS = dict(
    ada_w=[2, D, 6 * D], ada_b=[2, 6 * D], norm_mix_g=[2, D], w_in=[2, D, IN_COLS], conv_w=[2, 3, 256],
    mla_q_norm_g=[2, 192], mla_w_uq=[2, 192, 384], mla_kv_norm_g=[2, 128], mla_w_ukv=[2, 128, 512],
    mla_q_qk_g=[2, 96], mla_k_qk_g=[2, 96], lru_conv_w=[2, 4, 256], lru_conv_b=[2, 256], lru_w_a=[2, 4, 64, 64],
    lru_b_a=[2, 256], lru_w_x=[2, 4, 64, 64], lru_b_x=[2, 256], lru_lambda=[2, 256], mix_norm_g=[2, D],
    w_out=[2, D, D], norm_ffn_g=[2, D], router_group_w=[2, D, 4], router_group_b=[2, 4], router_expert_w=[2, D, 32],
    router_expert_b=[2, 32], exp_w_gate=[2, 32, D, 256], exp_w_up=[2, 32, D, 256], exp_w_down=[2, 32, 256, D])


def build_fused(SS=S, NL=2, TB=1024):
    nc = new_nc()
    P = Prog(nc)
    x_in = din(nc, "x", [SS, D])
    c_in = din(nc, "c", [D])
    pos = din(nc, "positions", [SS], I32)
    W = {k: din(nc, k, shp) for k, shp in W_SPECS.items()}
    inv_ret4 = din(nc, "inv_ret4", [128, 1])
    inv_mla = din(nc, "inv_mla", [16])
    innerT = din(nc, "innerT", [4, 128, 128])
    qdecT = din(nc, "qdecT", [4, 64, 128])
    kdec = din(nc, "kdec", [4, 128, 1])
    cdec = din(nc, "cdec", [4, 64, 1])
    out = dout(nc, "out", [SS, D])

    def idr(name, shape, dt=F32):
        return nc.dram_tensor(name, list(shape), dt).ap()

    T = dict(attq=idr("i_attq", [4, 96, SS], BF16), attk=idr("i_attk", [4, 96, SS], BF16), attv=idr("i_attv", [4, SS, 64], BF16),
             retq=idr("i_retq", [256, SS], BF16), retk=idr("i_retk", [256, SS], BF16), retv=idr("i_retv", [SS, 256], BF16),
             lrux=idr("i_lrux", [256, SS]), cvx=idr("i_cvx", [256, SS]), bgate=idr("i_bgate", [256, SS]),
             sgate=idr("i_sgate", [256, SS]), ggate=idr("i_ggate", [256, SS]))
    Y = dict(co=idr("i_co", [256, SS]), ao=idr("i_ao", [256, SS]), ro=idr("i_ro", [256, SS]), lo=idr("i_lo", [256, SS]))
    x_mid = idr("i_xmid", [SS, D])
    x1s = idr("i_x1s", [SS, D])
    col = lambda ap: ap.rearrange("(c o) -> c o", o=1)
    cast = {}
    for l in range(NL):
        xs = x_in if l == 0 else x_mid
        xd = out if l == NL - 1 else x_mid
        mk = P.mark()
        P.prefix = "L%dP1_" % l
        m = dict(x_in=xs, c_in=c_in, pos_in=pos, inv_ret4=inv_ret4, inv_mla=inv_mla)
        for k in ("ada_w", "ada_b", "norm_mix_g", "w_in", "mla_q_norm_g", "mla_w_uq", "mla_kv_norm_g", "mla_w_ukv", "mla_q_qk_g", "mla_k_qk_g"):
            m[k] = W[k][l]
        m.update(T)
        phase1(P, IO(nc, m), SS)
        P.release(mk)
        for hd in range(4):
            mk = P.mark()
            P.prefix = "L%dP2h%d_" % (l, hd)
            sl = slice(hd * 64, (hd + 1) * 64)
            m = dict(aq=T["attq"][hd], ak=T["attk"][hd], av=T["attv"][hd], rq=T["retq"][sl], rk=T["retk"][sl], rv=T["retv"][:, sl],
                     lx=T["lrux"][sl], cx=T["cvx"][sl],
                     convw=W["conv_w"][l][:, sl].rearrange("k c -> c k"), lcw=W["lru_conv_w"][l][:, sl].rearrange("k c -> c k"),
                     lcb=col(W["lru_conv_b"][l][sl]), wa=W["lru_w_a"][l][hd], ba=col(W["lru_b_a"][l][sl]),
                     wx=W["lru_w_x"][l][hd], bx=col(W["lru_b_x"][l][sl]), lam=col(W["lru_lambda"][l][sl]),
                     qqk=W["mla_q_qk_g"][l], kqk=W["mla_k_qk_g"][l],
                     innerT=innerT[hd], qdecT=qdecT[hd], kdec=kdec[hd], cdec=cdec[hd],
                     ao=Y["ao"][sl], ro=Y["ro"][sl], lo=Y["lo"][sl], co=Y["co"][sl])
            if hd == 0:
                def _cast(l=l):
                    pfx = P.prefix
                    P.prefix = "L%d_" % l
                    cast[l] = precast_experts(P, W["exp_w_gate"][l], W["exp_w_up"][l], W["exp_w_down"][l])
                    P.prefix = pfx
                phase2(P, IO(nc, m), SS, after_setup=_cast)
            else:
                phase2(P, IO(nc, m), SS)
            P.release(mk)
        mk = P.mark()
        P.prefix = "L%dP3_" % l
        m = dict(x_in=xs, c_in=c_in, x_out=xd, x1s=x1s, bgate=T["bgate"], sgate=T["sgate"], ggate=T["ggate"])
        for k in ("ada_w", "ada_b", "mix_norm_g", "w_out", "norm_ffn_g", "router_group_w", "router_group_b", "router_expert_w",
                  "router_expert_b", "exp_w_gate", "exp_w_up", "exp_w_down"):
            m[k] = W[k][l]
        m.update(Y)
        m["wgu_s"], m["wd_s"] = cast[l]
        phase3(P, IO(nc, m), SS, TB, 32)
        P.release(mk)
    P.finish([out])
    P.emit()
    return nc


_NC_CACHE = {}


def _get(name, fn):
    if name not in _NC_CACHE:
        _NC_CACHE[name] = fn()
    return _NC_CACHE[name]


def _inv_freq(dim):
    return (np.float32(1.0) / (np.float32(10000.0) ** (np.arange(0, dim, 2, dtype=np.float32) / np.float32(dim)))).astype(np.float32)


def _ret_consts(hd):
    gamma = 1.0 - 2.0 ** (-5.0 - hd)
    idx = np.arange(128, dtype=np.float64)
    innerT = np.where(idx[None, :] >= idx[:, None], gamma ** np.maximum(idx[None, :] - idx[:, None], 0.0), 0.0).astype(np.float32)
    qdecT = np.ascontiguousarray(np.tile((gamma ** (idx + 1.0))[None, :], (64, 1))).astype(np.float32)
    kdec = (gamma ** (127.0 - idx)).reshape(128, 1).astype(np.float32)
    cdec = np.full((64, 1), gamma ** 128, np.float32)
    return innerT, qdecT, kdec, cdec


def kernel(**inputs):
    I = {k: np.ascontiguousarray(np.asarray(v)) for k, v in inputs.items()}
    B = I["x"].shape[0]
    nc = _get("fused", build_fused)
    rc = [_ret_consts(h) for h in range(4)]
    consts = dict(inv_ret4=np.ascontiguousarray(np.tile(_inv_freq(64), 4).reshape(128, 1)), inv_mla=_inv_freq(32),
                  innerT=np.stack([r[0] for r in rc]), qdecT=np.stack([r[1] for r in rc]),
                  kdec=np.stack([r[2] for r in rc]), cdec=np.stack([r[3] for r in rc]))
    maps = []
    for b in range(B):
        m = dict(x=np.ascontiguousarray(I["x"][b], dtype=np.float32), c=np.ascontiguousarray(I["c"][b]),
                 positions=np.ascontiguousarray(I["positions"][b]).astype(np.int32))
        for k in W_SPECS:
            m[k] = I[k]
        m.update(consts)
        maps.append(m)
    res = run_bass_kernel_spmd(nc, maps, core_ids=list(range(B))).results
    return np.stack([res[b]["out"] for b in range(B)], axis=0).astype(np.float32)
```

```python
import math
import numpy as np
import ml_dtypes
import concourse.bass as bass
import concourse.mybir as mybir
from concourse.bass_utils import run_bass_kernel_spmd

F32 = mybir.dt.float32
BF16 = mybir.dt.bfloat16
I32 = mybir.dt.int32
AF = mybir.ActivationFunctionType
ALU = mybir.AluOpType
AX = mybir.AxisListType

D = 1024
S = 16384
NCORE = 8
TPC = 4096
IN_COLS = 2656
EPS = 1e-6
TWO_PI = 2.0 * math.pi

ENGS = ["sync", "scalar", "vector", "gpsimd", "tensor"]
SAME_ENGINE_SYNC = {"sync": False, "scalar": True, "vector": True, "gpsimd": True, "tensor": False}
_APT = None


class Buf:
    __slots__ = ("name", "w", "r")

    def __init__(self, name=""):
        self.name = name
        self.w = None
        self.r = []


class Prog:
    NPOOL = 16

    def __init__(self, nc):
        self.nc = nc
        self.ops = {e: [] for e in ENGS}
        self.cnt = {e: 0 for e in ENGS}
        self.esem = {e: nc.alloc_semaphore("es_" + e) for e in ENGS}
        self.known = {e: {f: 0 for f in ENGS} for e in ENGS}
        self.snap = {e: [None] for e in ENGS}
        self.dq = ["sync", "scalar", "gpsimd"]
        self.pool = {q: [nc.alloc_semaphore("dp_%s_%d" % (q, i)) for i in range(self.NPOOL)] for q in self.dq}
        self.pool_val = {q: [0] * self.NPOOL for q in self.dq}
        self.pool_next = {q: 0 for q in self.dq}
        self.dknown = {e: {} for e in ENGS}
        self.bufs = {}
        self.uid = 0
        self.prefix = ""

    def sb(self, name, shape, dt=F32):
        return self.nc.alloc_sbuf_tensor(self.prefix + name, list(shape), dt)

    def ps(self, name, shape, dt=F32):
        return self.nc.alloc_psum_tensor(self.prefix + name, list(shape), dt)

    def mark(self):
        nc = self.nc
        return (nc.psum_base, nc.psum_top, nc.sbuf_base, nc.sbuf_top)

    def release(self, mk):
        self.barrier()
        nc = self.nc
        nc.psum_base, nc.psum_top, nc.sbuf_base, nc.sbuf_top = mk

    def barrier(self):
        for e in ENGS:
            waits = []
            for f in ENGS:
                if f != e and self.cnt[f] > 0:
                    self._need(e, ("E", f, self.cnt[f]), waits)
            for q in self.dq:
                for i in range(self.NPOOL):
                    v = self.pool_val[q][i]
                    if v > 0:
                        self._need(e, ("D", q, i, v, None), waits)
            self.ops[e].append((waits, None, None, None, 0))
        self.bufs = {}

    def buf_of(self, ap):
        n = ap.name
        b = self.bufs.get(n)
        if b is None:
            b = self.bufs[n] = Buf(n)
        return b

    def _merge(self, eng, sn):
        if sn is None:
            return
        kn = self.known[eng]
        for g, v in sn[0].items():
            if kn[g] < v:
                kn[g] = v
        dk = self.dknown[eng]
        for k, v in sn[1].items():
            if dk.get(k, 0) < v:
                dk[k] = v

    def _need(self, eng, ev, waits):
        if ev is None:
            return
        if ev[0] == "E":
            _, f, seq = ev
            if f == eng and not SAME_ENGINE_SYNC[eng]:
                return
            if self.known[eng][f] >= seq:
                return
            waits.append((self.esem[f], seq))
            self.known[eng][f] = seq
            self._merge(eng, self.snap[f][seq])
        else:
            _, q, i, val, sn = ev
            if self.dknown[eng].get((q, i), 0) >= val:
                return
            waits.append((self.pool[q][i], val))
            self.dknown[eng][(q, i)] = val
            self._merge(eng, sn)

    def _deps(self, eng, reads, writes, waits):
        for b in reads:
            self._need(eng, b.w, waits)
        for b in writes:
            self._need(eng, b.w, waits)
            for ev in b.r:
                self._need(eng, ev, waits)

    def _commit(self, ev, reads, writes):
        for b in reads:
            if b in writes:
                continue
            b.r.append(ev)
            if len(b.r) > 16:
                last = {}
                keep = []
                for e in b.r:
                    if e[0] == "E":
                        last[e[1]] = e
                    else:
                        keep.append(e)
                b.r = keep[-10:] + list(last.values())
        for b in writes:
            b.w = ev
            b.r = []

    def _scan(self, kwargs):
        reads, writes = [], []
        for k, v in kwargs.items():
            if isinstance(v, _APT):
                b = self.buf_of(v)
                if k in ("out", "accum_out", "out_max", "out_indices"):
                    if b not in writes:
                        writes.append(b)
                elif b not in reads:
                    reads.append(b)
        return reads, writes

    def I(self, eng, meth, **kwargs):
        xr = kwargs.pop("_reads", ())
        xw = kwargs.pop("_writes", ())
        reads, writes = self._scan(kwargs)
        reads += [self.buf_of(a) for a in xr]
        writes += [self.buf_of(a) for a in xw]
        waits = []
        self._deps(eng, reads, writes, waits)
        self.cnt[eng] += 1
        seq = self.cnt[eng]
        self.snap[eng].append((dict(self.known[eng]), dict(self.dknown[eng])))
        self.ops[eng].append((waits, meth, kwargs, self.esem[eng], 1))
        ev = ("E", eng, seq)
        self._commit(ev, reads, writes)
        return ev

    def dma(self, q, out, in_, **kw):
        reads = [self.buf_of(in_)]
        writes = [self.buf_of(out)]
        waits = []
        self._deps(q, reads, writes, waits)
        i = self.pool_next[q]
        self.pool_next[q] = (i + 1) % self.NPOOL
        prev = self.pool_val[q][i]
        if prev > 0 and self.dknown[q].get((q, i), 0) < prev:
            waits.append((self.pool[q][i], prev))
            self.dknown[q][(q, i)] = prev
        val = prev + 16
        self.pool_val[q][i] = val
        sn = (dict(self.known[q]), dict(self.dknown[q]))
        kw = dict(kw)
        kw["out"] = out
        kw["in_"] = in_
        self.ops[q].append((waits, "dma_start", kw, self.pool[q][i], 16))
        ev = ("D", q, i, val, sn)
        self._commit(ev, reads, writes)
        return ev

    def coll(self, kind, ins, outs, groups):
        q = "gpsimd"
        reads = [self.buf_of(a) for a in ins]
        writes = [self.buf_of(a) for a in outs]
        waits = []
        self._deps(q, reads, writes, waits)
        i = self.pool_next[q]
        self.pool_next[q] = (i + 1) % self.NPOOL
        prev = self.pool_val[q][i]
        if prev > 0 and self.dknown[q].get((q, i), 0) < prev:
            waits.append((self.pool[q][i], prev))
            self.dknown[q][(q, i)] = prev
        val = prev + 1
        self.pool_val[q][i] = val
        sn = (dict(self.known[q]), dict(self.dknown[q]))
        kw = dict(kind=kind, op=ALU.bypass, replica_groups=groups, ins=[a_.opt() for a_ in ins], outs=[a_.opt() for a_ in outs])
        self.ops[q].append((waits, "collective_compute", kw, self.pool[q][i], 1))
        ev = ("D", q, i, val, sn)
        self._commit(ev, reads, writes)
        return ev

    def finish(self, aps, eng="sync"):
        waits = []
        for a in aps:
            self._need(eng, self.buf_of(a).w, waits)
        self.ops[eng].append((waits, None, None, None, 0))

    def emit(self):
        nc = self.nc
        with nc.Block() as block:
            def mk(ename):
                def body(e):
                    for waits, meth, kw, sem, inc in self.ops[ename]:
                        for (s, v) in waits:
                            e.wait_ge(s, v)
                        if meth is not None:
                            getattr(e, meth)(**kw).then_inc(sem, inc)
                return body
            block.sync(mk("sync"))
            block.scalar(mk("scalar"))
            block.vector(mk("vector"))
            block.gpsimd(mk("gpsimd"))
            block.tensor(mk("tensor"))


class Rot:
    def __init__(self, P, name, shape, dt, n, psum=False):
        self.t = [(P.ps if psum else P.sb)("%s%d" % (name, i), shape, dt) for i in range(n)]
        self.i = 0

    def next(self):
        t = self.t[self.i % len(self.t)]
        self.i += 1
        return t


def new_nc():
    global _APT
    nc = bass.Bass("TRN2", target_bir_lowering=False)
    if _APT is None:
        t = nc.dram_tensor("apt_probe", [2, 2], F32).ap()
        _APT = type(t)
    return nc


class IO:
    def __init__(self, nc, m=None):
        self.nc = nc
        self.m = m or {}
        self.outs = []

    def inp(self, name, shape, dt=F32):
        if name in self.m:
            return self.m[name]
        return din(self.nc, name, shape, dt)

    def out(self, name, shape, dt=F32):
        if name in self.m:
            return self.m[name]
        ap = dout(self.nc, name, shape, dt)
        self.outs.append(ap)
        return ap


def din(nc, name, shape, dt=F32):
    return nc.dram_tensor(name, list(shape), dt, kind="ExternalInput").ap()


def dout(nc, name, shape, dt=F32):
    return nc.dram_tensor(name, list(shape), dt, kind="ExternalOutput").ap()


def make_identities(P):
    idf = P.sb("ident_f", [128, 128], F32)
    idb = P.sb("ident_b", [128, 128], BF16)
    P.I("gpsimd", "memset", ap=idf[:], constant=1.0, _writes=[idf[:]])
    P.I("gpsimd", "affine_select", out=idf[:], in_=idf[:], pattern=[[1, 128]], compare_op=ALU.is_equal,
        fill=0.0, base=0, channel_multiplier=-1)
    P.I("vector", "tensor_copy", out=idb[:], in_=idf[:])
    return idf, idb


def compute_mod(P, c_ap, adaw_ap, adab_ap, psA, psB, c0, c1, CH=256):
    n = c1 - c0
    mod = P.sb("mod_bc", [128, n], F32)
    ccol = P.sb("ccol", [128, 8], F32)
    cbc = P.sb("cbc", [128, 8, 128], F32)
    P.dma("sync", mod[:], adab_ap[c0:c1].partition_broadcast(128))
    P.dma("sync", ccol[:], c_ap.rearrange("(k p) -> p k", p=128), allow_slow_non_contiguous=True)
    P.I("scalar", "activation", out=ccol[:], in_=ccol[:], func=AF.Silu)
    P.I("vector", "tensor_copy", out=cbc[:], in_=ccol[:].unsqueeze(2).to_broadcast([128, 8, 128]))
    wr = Rot(P, "adaw_t", [128, 8, CH], F32, 2)
    awv = adaw_ap.rearrange("(k p) n -> p k n", p=128)
    for i in range(n // CH):
        wt = wr.next()
        P.dma("sync" if i % 2 == 0 else "scalar", wt[:], awv[:, :, c0 + i * CH:c0 + (i + 1) * CH])
        ps = psA if i % 2 == 0 else psB
        for k in range(8):
            P.I("tensor", "matmul", out=ps[:, 0:CH], lhsT=cbc[:, k, :], rhs=wt[:, k, :], start=(k == 0), stop=(k == 7))
        P.I("vector", "tensor_tensor", out=mod[:, i * CH:(i + 1) * CH], in0=mod[:, i * CH:(i + 1) * CH],
            in1=ps[:, 0:CH], op=ALU.add)
    return mod


def sin_of(P, out_ap, ang_ap, tmp_ap, tmpi_ap, shift):
    P.I("vector", "tensor_scalar", out=tmp_ap, in0=ang_ap, scalar1=1.0 / TWO_PI, scalar2=shift / TWO_PI, op0=ALU.mult, op1=ALU.add)
    P.I("vector", "tensor_copy", out=tmpi_ap, in_=tmp_ap)
    P.I("vector", "tensor_tensor", out=tmp_ap, in0=tmp_ap, in1=tmpi_ap, op=ALU.subtract)
    P.I("scalar", "activation", out=out_ap, in_=tmp_ap, func=AF.Sin, scale=TWO_PI)


def build_p1(ntok=TPC):
    nc = new_nc()
    P = Prog(nc)
    io = IO(nc)
    phase1(P, io, ntok)
    P.finish(io.outs)
    P.emit()
    return nc


def phase1(P, io, ntok):
    nc = P.nc
    if hasattr(P, "mla_tiles"):
        del P.mla_tiles
    NST = ntok // 512
    x_in = io.inp("x_in", [ntok, D])
    c_in = io.inp("c_in", [D])
    pos_in = io.inp("pos_in", [ntok], I32)
    adaw = io.inp("ada_w", [D, 6 * D])
    adab = io.inp("ada_b", [6 * D])
    gmix = io.inp("norm_mix_g", [D])
    w_in = io.inp("w_in", [D, IN_COLS])
    qng = io.inp("mla_q_norm_g", [192])
    wuq = io.inp("mla_w_uq", [192, 384])
    kvng = io.inp("mla_kv_norm_g", [128])
    wukv = io.inp("mla_w_ukv", [128, 512])
    qqk = io.inp("mla_q_qk_g", [96])
    kqk = io.inp("mla_k_qk_g", [96])
    inv_ret4 = io.inp("inv_ret4", [128, 1])
    inv_mla = io.inp("inv_mla", [16])
    attq = io.out("attq", [4, 96, ntok], BF16)
    attk = io.out("attk", [4, 96, ntok], BF16)
    attv = io.out("attv", [4, ntok, 64], BF16)
    retq = io.out("retq", [256, ntok], BF16)
    retk = io.out("retk", [256, ntok], BF16)
    retv = io.out("retv", [ntok, 256], BF16)
    lrux = io.out("lrux", [256, ntok], F32)
    cvx = io.out("cvx", [256, ntok], F32)
    bgate = io.out("bgate", [256, ntok], F32)
    sgate = io.out("sgate", [256, ntok], F32)
    ggate = io.out("ggate", [256, ntok], F32)

    psT = [P.ps("psT%d" % i, [128, 1024], BF16) for i in range(2)]
    psF = [P.ps("psF%d" % i, [128, 512], F32) for i in range(3)]
    psU = P.ps("psU", [128, 512], F32)
    psQ = P.ps("psQ", [128, 512], F32)
    psX = P.ps("psX", [128, 1024], BF16)
    fi = [0]

    def nextF():
        p = psF[fi[0] % 3]
        fi[0] += 1
        return p

    idf, idb = make_identities(P)
    P.negpi = P.sb("negpi", [128, 1], F32)
    P.I("vector", "memset", ap=P.negpi[:], constant=-math.pi, _writes=[P.negpi[:]])
    neghalf = P.sb("neghalf", [128, 16], F32)
    P.I("vector", "memset", ap=neghalf[:], constant=-0.5, _writes=[neghalf[:]])

    mod = compute_mod(P, c_in, adaw, adab, psF[0], psF[1], 0, 2 * D)
    gm = P.sb("gm", [128, D], F32)
    P.dma("sync", gm[:], gmix.partition_broadcast(128))
    P.I("vector", "scalar_tensor_tensor", out=gm[:], in0=mod[:, D:2 * D], scalar=1.0, in1=gm[:], op0=ALU.add, op1=ALU.mult)
    shm = mod[:, 0:D]

    w_bf = P.sb("w_bf", [128, 8, IN_COLS], BF16)
    wv = w_in.rearrange("(k p) n -> p k n", p=128)
    for k in range(8):
        for hh in range(2):
            P.dma("gpsimd", w_bf[:, k, hh * 1328:(hh + 1) * 1328], wv[:, k, hh * 1328:(hh + 1) * 1328])
    w_rot = P.sb("w_rot", [128, 8, 512], BF16)
    for k in range(8):
        src = w_bf[:, k, 1120:1632].rearrange("p (h two i) -> p h two i", two=2, i=32)
        dst = w_rot[:, k, :].rearrange("p (h two i) -> p h two i", two=2, i=32)
        P.I("vector", "tensor_scalar", out=dst[:, :, 0, :], in0=src[:, :, 1, :], scalar1=-1.0, scalar2=None, op0=ALU.mult)
        P.I("vector", "tensor_copy", out=dst[:, :, 1, :], in_=src[:, :, 0, :])
    wuq_bf = P.sb("wuq_bf", [128, 2, 384], BF16)
    P.dma("gpsimd", wuq_bf[:, 0, :], wuq[0:128, :])
    P.dma("gpsimd", wuq_bf[0:64, 1, :], wuq[128:192, :])
    wukv_bf = P.sb("wukv_bf", [128, 512], BF16)
    P.dma("gpsimd", wukv_bf[:], wukv)
    qng_bc = P.sb("qng_bc", [128, 192], F32)
    kvng_bc = P.sb("kvng_bc", [128, 128], F32)
    qqk_bc = P.sb("qqk_bc", [128, 96], F32)
    kqk_bc = P.sb("kqk_bc", [128, 96], F32)
    P.dma("sync", qng_bc[:], qng.partition_broadcast(128))
    P.dma("sync", kvng_bc[:], kvng.partition_broadcast(128))
    P.dma("sync", qqk_bc[:], qqk.partition_broadcast(128))
    P.dma("sync", kqk_bc[:], kqk.partition_broadcast(128))

    invc = P.sb("invc", [128, 1], F32)
    P.dma("sync", invc[:], inv_ret4)
    posi = P.sb("posi", [128, 512], I32)
    angt = P.sb("angt", [128, 512], F32)
    tmpa = P.sb("tmpa", [128, 512], F32)
    tmpai = P.sb("tmpai", [128, 512], I32)
    cos_r = Rot(P, "cosR", [128, 512], F32, 2)
    sin_r = Rot(P, "sinR", [128, 512], F32, 2)
    invm = P.sb("invm", [128, 16], F32)
    P.dma("sync", invm[:], inv_mla.partition_broadcast(128))
    posc_i = P.sb("posc_i", [128, 4], I32)
    posc = P.sb("posc", [128, 4], F32)
    angm = P.sb("angm", [128, 4, 16], F32)
    tmpm = P.sb("tmpm", [128, 4, 16], F32)
    tmpmi = P.sb("tmpmi", [128, 4, 16], I32)
    cosM_r = Rot(P, "cosM", [128, 4, 16], F32, 2)
    sinM_r = Rot(P, "sinM", [128, 4, 16], F32, 2)

    xt_r = Rot(P, "xt", [128, D], F32, 3)
    junk = P.sb("junk", [128, D], BF16)
    ssq = Rot(P, "ssq", [128, 4], F32, 2)
    v4 = Rot(P, "v4", [128, 4], F32, 2)
    rstd4 = Rot(P, "rstd4", [128, 4], F32, 2)
    tmp_r = Rot(P, "tmpx", [128, D], F32, 1)
    hb_r = Rot(P, "hb", [128, D], BF16, 2)
    hT_r = Rot(P, "hT", [128, 8, 512], BF16, 2)
    ev_r = Rot(P, "ev", [128, 512], F32, 4)
    evb_r = Rot(P, "evb", [128, 512], BF16, 4)
    csb_r = Rot(P, "csb", [128, 512], F32, 2)

    for st in range(NST):
        t0 = st * 512
        hT = hT_r.next()
        for j in range(4):
            xt = xt_r.next()
            P.dma("sync", xt[:], x_in[t0 + j * 128:t0 + (j + 1) * 128, :])
            sq = ssq.next()
            P.I("scalar", "activation", out=junk[:], in_=xt[:], func=AF.Square, accum_out=sq[:, 0:1])
            v = v4.next()
            rs = rstd4.next()
            P.I("vector", "tensor_scalar", out=v[:, 0:1], in0=sq[:, 0:1], scalar1=1.0 / D, scalar2=EPS, op0=ALU.mult, op1=ALU.add)
            P.I("gpsimd", "tensor_tensor", out=rs[:, 0:1], in0=v[:, 0:1], in1=neghalf[:, 0:1], op=ALU.pow)
            tm = tmp_r.next()
            hb = hb_r.next()
            P.I("vector", "scalar_tensor_tensor", out=tm[:], in0=xt[:], scalar=rs[:, 0:1], in1=gm[:],
                op0=ALU.mult, op1=ALU.mult)
            P.I("vector", "tensor_tensor", out=hb[:], in0=tm[:], in1=shm, op=ALU.add)
            pt = psT[j % 2]
            for k in range(8):
                P.I("tensor", "transpose", out=pt[:, k * 128:(k + 1) * 128], in_=hb[:, k * 128:(k + 1) * 128], identity=idb[:])
            P.I("scalar", "copy", out=hT[:, :, j * 128:(j + 1) * 128], in_=pt[:].rearrange("p (k t) -> p k t", k=8))
        cosR = cos_r.next()
        sinR = sin_r.next()
        P.dma("scalar", posi[:], pos_in[t0:t0 + 512].partition_broadcast(128))
        P.I("vector", "tensor_copy", out=angt[:], in_=posi[:])
        P.I("vector", "tensor_scalar", out=angt[:], in0=angt[:], scalar1=invc[:, 0:1], scalar2=None, op0=ALU.mult)
        sin_of(P, sinR[:], angt[:], tmpa[:], tmpai[:], 0.0)
        sin_of(P, cosR[:], angt[:], tmpa[:], tmpai[:], 0.5 * math.pi)
        cosM = cosM_r.next()
        sinM = sinM_r.next()
        P.dma("scalar", posc_i[:], pos_in[t0:t0 + 512].rearrange("(n p) -> p n", p=128), allow_slow_non_contiguous=True)
        P.I("vector", "tensor_copy", out=posc[:], in_=posc_i[:])
        P.I("vector", "tensor_tensor", out=angm[:], in0=posc[:].unsqueeze(2).to_broadcast([128, 4, 16]),
            in1=invm[:].unsqueeze(1).to_broadcast([128, 4, 16]), op=ALU.mult)
        sin_of(P, sinM[:], angm[:], tmpm[:], tmpmi[:], 0.0)
        sin_of(P, cosM[:], angm[:], tmpm[:], tmpmi[:], 0.5 * math.pi)

        def fm(wt, c0):
            ps = nextF()
            for k in range(8):
                P.I("tensor", "matmul", out=ps[:], lhsT=wt[:, k, c0:c0 + 128], rhs=hT[:, k, :], start=(k == 0), stop=(k == 7))
            return ps

        for ch in range(2):
            ps = fm(w_bf, ch * 128)
            e = ev_r.next()
            P.I("scalar", "copy", out=e[:], in_=ps[:])
            P.dma("sync", bgate[ch * 128:(ch + 1) * 128, t0:t0 + 512], e[:])
            psc = fm(w_bf, 256 + ch * 128)
            cs = csb_r.next()
            P.I("scalar", "copy", out=cs[:], in_=psc[:])
            psx = fm(w_bf, 512 + ch * 128)
            e = ev_r.next()
            P.I("vector", "tensor_tensor", out=e[:], in0=psx[:], in1=cs[:], op=ALU.mult)
            P.dma("sync", cvx[ch * 128:(ch + 1) * 128, t0:t0 + 512], e[:])
        for qk in range(2):
            for ch in range(2):
                c0 = 1120 + qk * 256 + ch * 128
                ps = fm(w_bf, c0)
                cs = csb_r.next()
                P.I("vector", "scalar_tensor_tensor", out=cs[:], in0=ps[:], scalar=(1.0 if qk == 0 else 0.125),
                    in1=cosR[:], op0=ALU.mult, op1=ALU.mult)
                psr = fm(w_rot, qk * 256 + ch * 128)
                e = ev_r.next()
                P.I("vector", "scalar_tensor_tensor", out=e[:], in0=psr[:], scalar=(1.0 if qk == 0 else 0.125),
                    in1=sinR[:], op0=ALU.mult, op1=ALU.mult)
                eb = evb_r.next()
                P.I("vector", "tensor_tensor", out=eb[:], in0=e[:], in1=cs[:], op=ALU.add)
                P.dma("sync", (retq if qk == 0 else retk)[ch * 128:(ch + 1) * 128, t0:t0 + 512], eb[:])
        for ch in range(2):
            ps = fm(w_bf, 1120 + 768 + ch * 128)
            e = ev_r.next()
            P.I("scalar", "activation", out=e[:], in_=ps[:], func=AF.Silu)
            P.dma("sync", sgate[ch * 128:(ch + 1) * 128, t0:t0 + 512], e[:])
        for ch in range(2):
            ps = fm(w_bf, 2144 + ch * 128)
            e = ev_r.next()
            P.I("scalar", "copy", out=e[:], in_=ps[:])
            P.dma("sync", lrux[ch * 128:(ch + 1) * 128, t0:t0 + 512], e[:])
        for ch in range(2):
            ps = fm(w_bf, 2144 + 256 + ch * 128)
            e = ev_r.next()
            P.I("scalar", "activation", out=e[:], in_=ps[:], func=AF.Gelu)
            P.dma("sync", ggate[ch * 128:(ch + 1) * 128, t0:t0 + 512], e[:])
        for j in range(4):
            tt = t0 + j * 128
            ti = tt // 128
            hTj = hT[:, :, j * 128:(j + 1) * 128]
            ps = nextF()
            for k in range(8):
                P.I("tensor", "matmul", out=ps[:, 0:256], lhsT=hT[:, k, j * 128:(j + 1) * 128], rhs=w_bf[:, k, 1632:1888],
                    start=(k == 0), stop=(k == 7))
            eb = evb_r.next()
            P.I("scalar", "copy", out=eb[:, 0:256], in_=ps[:, 0:256])
            P.dma("sync", retv[tt:tt + 128, :], eb[:, 0:256])
            mla_tile(P, locals(), tt, j, j)


def mla_tile(P, L, tt, ti, j):
    hT, w_bf, psU, psQ, psX = L["hT"], L["w_bf"], L["psU"], L["psQ"], L["psX"]
    idb, neghalf = L["idb"], L["neghalf"]
    qng_bc, kvng_bc, qqk_bc, kqk_bc = L["qng_bc"], L["kvng_bc"], L["qqk_bc"], L["kqk_bc"]
    wuq_bf, wukv_bf, cosM, sinM = L["wuq_bf"], L["wukv_bf"], L["cosM"], L["sinM"]
    attq, attk, attv = L["attq"], L["attk"], L["attv"]
    if not hasattr(P, "mla_tiles"):
        P.mla_tiles = dict(
            junk=P.sb("mjunk", [128, 512], F32),
            st3=Rot(P, "mst3", [128, 16], F32, 2),
            rs3=Rot(P, "mrs3", [128, 16], F32, 2),
            cqn=Rot(P, "mcqn", [128, 320], BF16, 2),
            cT=Rot(P, "mcT", [128, 384], BF16, 2),
            qsb=Rot(P, "mqsb", [128, 384], F32, 2),
            kvsb=Rot(P, "mkvsb", [128, 512], F32, 2),
            sq=Rot(P, "msq", [128, 512], F32, 4),
            Qt=Rot(P, "mQt", [128, 4, 96], BF16, 2),
            Kt=Rot(P, "mKt", [128, 4, 96], BF16, 2),
            Vt=Rot(P, "mVt", [128, 4, 64], BF16, 2),
            r1=Rot(P, "mr1", [128, 4, 32], F32, 2),
            r2=Rot(P, "mr2", [128, 4, 16], F32, 4),
            kr=Rot(P, "mkr", [128, 32], F32, 2),
            kr2=Rot(P, "mkr2", [128, 32], F32, 2),
            QT=Rot(P, "mQT", [96, 4, 128], BF16, 2),
            KT=Rot(P, "mKT", [96, 4, 128], BF16, 2),
            scl=P.sb("mscl", [128, 3], F32),
        )
        sc = P.mla_tiles["scl"]
        P.I("vector", "memset", ap=sc[:, 0:1], constant=1.0 / 192, _writes=[sc[:]])
        P.I("vector", "memset", ap=sc[:, 1:2], constant=1.0 / 128, _writes=[sc[:]])
        P.I("vector", "memset", ap=sc[:, 2:3], constant=1.0 / 32, _writes=[sc[:]])
    M = P.mla_tiles
    for k in range(8):
        P.I("tensor", "matmul", out=psU[:, 0:352], lhsT=hT[:, k, j * 128:(j + 1) * 128], rhs=w_bf[:, k, 768:1120],
            start=(k == 0), stop=(k == 7))
    st3 = M["st3"].next()
    rs3 = M["rs3"].next()
    P.I("scalar", "activation", out=M["junk"][:, 0:192], in_=psU[:, 0:192], func=AF.Square, accum_out=st3[:, 0:1])
    P.I("scalar", "activation", out=M["junk"][:, 0:128], in_=psU[:, 192:320], func=AF.Square, accum_out=st3[:, 1:2])
    P.I("scalar", "activation", out=M["junk"][:, 0:32], in_=psU[:, 320:352], func=AF.Square, accum_out=st3[:, 2:3])
    P.I("vector", "tensor_tensor", out=st3[:, 0:3], in0=st3[:, 0:3], in1=M["scl"][:], op=ALU.mult)
    P.I("vector", "tensor_scalar", out=st3[:, 0:3], in0=st3[:, 0:3], scalar1=EPS, scalar2=None, op0=ALU.add)
    P.I("gpsimd", "tensor_tensor", out=rs3[:, 0:3], in0=st3[:, 0:3], in1=neghalf[:, 0:3], op=ALU.pow)
    cqn = M["cqn"].next()
    P.I("vector", "scalar_tensor_tensor", out=cqn[:, 0:192], in0=psU[:, 0:192], scalar=rs3[:, 0:1], in1=qng_bc[:],
        op0=ALU.mult, op1=ALU.mult)
    P.I("vector", "scalar_tensor_tensor", out=cqn[:, 192:320], in0=psU[:, 192:320], scalar=rs3[:, 1:2], in1=kvng_bc[:],
        op0=ALU.mult, op1=ALU.mult)
    kr = M["kr"].next()
    P.I("vector", "scalar_tensor_tensor", out=kr[:], in0=psU[:, 320:352], scalar=rs3[:, 2:3], in1=kqk_bc[:, 64:96],
        op0=ALU.mult, op1=ALU.mult)
    P.I("tensor", "transpose", out=psX[:, 0:128], in_=cqn[:, 0:128], identity=idb[:])
    P.I("tensor", "transpose", out=psX[0:64, 128:256], in_=cqn[:, 128:192], identity=idb[:])
    P.I("tensor", "transpose", out=psX[:, 256:384], in_=cqn[:, 192:320], identity=idb[:])
    cT = M["cT"].next()
    P.I("scalar", "copy", out=cT[:, 0:128], in_=psX[:, 0:128])
    P.I("scalar", "copy", out=cT[0:64, 128:256], in_=psX[0:64, 128:256])
    P.I("scalar", "copy", out=cT[:, 256:384], in_=psX[:, 256:384])
    P.I("tensor", "matmul", out=psQ[:, 0:384], lhsT=cT[:, 0:128], rhs=wuq_bf[:, 0, :], start=True, stop=False)
    P.I("tensor", "matmul", out=psQ[:, 0:384], lhsT=cT[0:64, 128:256], rhs=wuq_bf[0:64, 1, :], start=False, stop=True)
    qsb = M["qsb"].next()
    sq = M["sq"].next()
    P.I("scalar", "copy", out=qsb[:], in_=psQ[:, 0:384])
    P.I("scalar", "activation", out=sq[:, 0:384], in_=psQ[:, 0:384], func=AF.Square)
    P.I("tensor", "matmul", out=psQ[:, 0:512], lhsT=cT[:, 256:384], rhs=wukv_bf[:], start=True, stop=True)
    kvsb = M["kvsb"].next()
    P.I("scalar", "copy", out=kvsb[:], in_=psQ[:, 0:512])
    st8 = M["st3"].next()
    rs8 = M["rs3"].next()
    sqv = sq[:, 0:384].rearrange("p (h c) -> p h c", c=96)
    P.I("vector", "tensor_reduce", out=st8[:, 0:4], in_=sqv[:, :, 0:64], axis=AX.X, op=ALU.add)
    P.I("vector", "tensor_reduce", out=st8[:, 4:8], in_=sqv[:, :, 64:96], axis=AX.X, op=ALU.add)
    sq2 = M["sq"].next()
    P.I("scalar", "activation", out=sq2[:], in_=kvsb[:], func=AF.Square)
    P.I("vector", "tensor_reduce", out=st8[:, 8:12], in_=sq2[:].rearrange("p (h c) -> p h c", c=128)[:, :, 0:64],
        axis=AX.X, op=ALU.add)
    P.I("vector", "tensor_scalar", out=st8[:, 0:4], in0=st8[:, 0:4], scalar1=1.0 / 64, scalar2=EPS, op0=ALU.mult, op1=ALU.add)
    P.I("vector", "tensor_scalar", out=st8[:, 4:8], in0=st8[:, 4:8], scalar1=1.0 / 32, scalar2=EPS, op0=ALU.mult, op1=ALU.add)
    P.I("vector", "tensor_scalar", out=st8[:, 8:12], in0=st8[:, 8:12], scalar1=1.0 / 64, scalar2=EPS, op0=ALU.mult, op1=ALU.add)
    P.I("gpsimd", "tensor_tensor", out=rs8[:, 0:12], in0=st8[:, 0:12], in1=neghalf[:, 0:12], op=ALU.pow)
    Qt = M["Qt"].next()
    Kt = M["Kt"].next()
    Vt = M["Vt"].next()
    qv = qsb[:].rearrange("p (h c) -> p h c", c=96)
    kvv = kvsb[:].rearrange("p (h c) -> p h c", c=128)
    r1 = M["r1"].next()
    tq = M["sq"].next()
    tqv = tq[:, 0:256].rearrange("p (h c) -> p h c", c=64)
    P.I("vector", "tensor_tensor", out=tqv, in0=qv[:, :, 0:64], in1=rs8[:, 0:4].unsqueeze(2).to_broadcast([128, 4, 64]), op=ALU.mult)
    P.I("vector", "tensor_tensor", out=Qt[:, :, 0:64], in0=tqv, in1=qqk_bc[:, 0:64].unsqueeze(1).to_broadcast([128, 4, 64]), op=ALU.mult)
    P.I("vector", "tensor_tensor", out=r1[:], in0=qv[:, :, 64:96], in1=rs8[:, 4:8].unsqueeze(2).to_broadcast([128, 4, 32]), op=ALU.mult)
    P.I("vector", "tensor_tensor", out=r1[:], in0=r1[:], in1=qqk_bc[:, 64:96].unsqueeze(1).to_broadcast([128, 4, 32]), op=ALU.mult)
    cb = cosM[:, ti, :].unsqueeze(1).to_broadcast([128, 4, 16])
    sb_ = sinM[:, ti, :].unsqueeze(1).to_broadcast([128, 4, 16])
    a1, a2, a3, a4 = M["r2"].next(), M["r2"].next(), M["r2"].next(), M["r2"].next()
    P.I("vector", "tensor_tensor", out=a1[:], in0=r1[:, :, 0:16], in1=cb, op=ALU.mult)
    P.I("vector", "tensor_tensor", out=a2[:], in0=r1[:, :, 16:32], in1=sb_, op=ALU.mult)
    P.I("vector", "tensor_tensor", out=a3[:], in0=r1[:, :, 16:32], in1=cb, op=ALU.mult)
    P.I("vector", "tensor_tensor", out=a4[:], in0=r1[:, :, 0:16], in1=sb_, op=ALU.mult)
    P.I("vector", "tensor_tensor", out=Qt[:, :, 64:80], in0=a1[:], in1=a2[:], op=ALU.subtract)
    P.I("vector", "tensor_tensor", out=Qt[:, :, 80:96], in0=a3[:], in1=a4[:], op=ALU.add)
    tk = M["sq"].next()
    tkv = tk[:, 0:256].rearrange("p (h c) -> p h c", c=64)
    P.I("vector", "tensor_tensor", out=tkv, in0=kvv[:, :, 0:64], in1=rs8[:, 8:12].unsqueeze(2).to_broadcast([128, 4, 64]), op=ALU.mult)
    P.I("vector", "tensor_tensor", out=Kt[:, :, 0:64], in0=tkv, in1=kqk_bc[:, 0:64].unsqueeze(1).to_broadcast([128, 4, 64]), op=ALU.mult)
    kr2 = M["kr2"].next()
    b1, b2, b3, b4 = M["r2"].next(), M["r2"].next(), M["r2"].next(), M["r2"].next()
    P.I("vector", "tensor_tensor", out=b1[:, 0, :], in0=kr[:, 0:16], in1=cosM[:, ti, :], op=ALU.mult)
    P.I("vector", "tensor_tensor", out=b2[:, 0, :], in0=kr[:, 16:32], in1=sinM[:, ti, :], op=ALU.mult)
    P.I("vector", "tensor_tensor", out=b3[:, 0, :], in0=kr[:, 16:32], in1=cosM[:, ti, :], op=ALU.mult)
    P.I("vector", "tensor_tensor", out=b4[:, 0, :], in0=kr[:, 0:16], in1=sinM[:, ti, :], op=ALU.mult)
    P.I("vector", "tensor_tensor", out=kr2[:, 0:16], in0=b1[:, 0, :], in1=b2[:, 0, :], op=ALU.subtract)
    P.I("vector", "tensor_tensor", out=kr2[:, 16:32], in0=b3[:, 0, :], in1=b4[:, 0, :], op=ALU.add)
    P.I("vector", "tensor_copy", out=Kt[:, :, 64:96], in_=kr2[:].unsqueeze(1).to_broadcast([128, 4, 32]))
    P.I("vector", "tensor_copy", out=Vt[:], in_=kvv[:, :, 64:128])
    P.dma("sync", attv[:, tt:tt + 128, :].rearrange("h t c -> t h c"), Vt[:])
    for h in range(4):
        P.I("tensor", "transpose", out=psX[0:96, 384 + h * 128:384 + (h + 1) * 128], in_=Qt[:, h, :], identity=idb[:])
    QT = M["QT"].next()
    P.I("scalar", "copy", out=QT[:], in_=psX[0:96, 384:896].rearrange("p (h t) -> p h t", h=4))
    P.dma("sync", attq[:, :, tt:tt + 128].rearrange("h c t -> c h t"), QT[:])
    for h in range(4):
        P.I("tensor", "transpose", out=psX[0:96, 384 + h * 128:384 + (h + 1) * 128], in_=Kt[:, h, :], identity=idb[:])
    KT = M["KT"].next()
    P.I("scalar", "copy", out=KT[:], in_=psX[0:96, 384:896].rearrange("p (h t) -> p h t", h=4))
    P.dma("sync", attk[:, :, tt:tt + 128].rearrange("h c t -> c h t"), KT[:])


ATT_SCALE = 96.0 ** -0.5


def build_p2(SS=S):
    nc = new_nc()
    P = Prog(nc)
    io = IO(nc)
    phase2(P, io, SS)
    P.finish(io.outs)
    P.emit()
    return nc


def phase2(P, io, SS, after_setup=None):
    nc = P.nc
    NB = SS // 128
    aq = io.inp("aq", [96, SS], BF16)
    ak = io.inp("ak", [96, SS], BF16)
    av = io.inp("av", [SS, 64], BF16)
    rq = io.inp("rq", [64, SS], BF16)
    rk = io.inp("rk", [64, SS], BF16)
    rv = io.inp("rv", [SS, 64], BF16)
    lx = io.inp("lx", [64, SS])
    cx = io.inp("cx", [64, SS])
    convw = io.inp("convw", [64, 3])
    lcw = io.inp("lcw", [64, 4])
    lcb = io.inp("lcb", [64, 1])
    wa = io.inp("wa", [64, 64])
    ba = io.inp("ba", [64, 1])
    wx = io.inp("wx", [64, 64])
    bx = io.inp("bx", [64, 1])
    lam = io.inp("lam", [64, 1])
    qqk = io.inp("qqk", [96])
    kqk = io.inp("kqk", [96])
    innerT = io.inp("innerT", [128, 128])
    qdecT = io.inp("qdecT", [64, 128])
    kdec = io.inp("kdec", [128, 1])
    cdec = io.inp("cdec", [64, 1])
    ao = io.out("ao", [64, SS])
    ro = io.out("ro", [64, SS])
    lo = io.out("lo", [64, SS])
    co = io.out("co", [64, SS])

    psS = [P.ps("psS%d" % i, [128, 512], F32) for i in range(3)]
    psO = [P.ps("psO%d" % i, [128, 512], F32) for i in range(2)]
    psB = P.ps("psB", [128, 512], F32)
    psR = [P.ps("psR%d" % i, [128, 512], F32) for i in range(2)]
    psKT = psB[:].bitcast(BF16)

    idf, idb = make_identities(P)
    half = P.sb("half", [128, 512], F32)
    P.I("vector", "memset", ap=half[:], constant=0.5, _writes=[half[:]])
    neghalf = P.sb("neghalf", [128, 512], F32)
    P.I("vector", "memset", ap=neghalf[:], constant=-0.5, _writes=[neghalf[:]])
    ones64 = P.sb("ones64", [128, 64], F32)
    P.I("vector", "memset", ap=ones64[:], constant=1.0 / 64, _writes=[ones64[:]])
    ones1 = P.sb("ones1", [128, 64], F32)
    P.I("vector", "memset", ap=ones1[:], constant=1.0, _writes=[ones1[:]])
    epsc = P.sb("epsc", [128, 1], F32)
    P.I("vector", "memset", ap=epsc[:], constant=EPS, _writes=[epsc[:]])

    cw = P.sb("cw", [64, 3], F32)
    P.dma("sync", cw[:], convw, allow_slow_non_contiguous=True)
    CSEG = min(2048, SS)
    cxt = P.sb("cxt", [64, 2 + CSEG], F32)
    cacc = Rot(P, "cacc", [64, CSEG], F32, 2)
    P.I("vector", "memset", ap=cxt[:, 0:2], constant=0.0, _writes=[cxt[:]])
    for sg in range(SS // CSEG):
        c0 = sg * CSEG
        if sg > 0:
            P.I("vector", "tensor_copy", out=cxt[:, 0:2], in_=cxt[:, CSEG:CSEG + 2])
        P.dma("sync", cxt[:, 2:2 + CSEG], cx[:, c0:c0 + CSEG])
        acc = cacc.next()
        P.I("vector", "tensor_scalar", out=acc[:], in0=cxt[:, 2:2 + CSEG], scalar1=cw[:, 2:3], scalar2=None, op0=ALU.mult)
        P.I("vector", "scalar_tensor_tensor", out=acc[:], in0=cxt[:, 1:1 + CSEG], scalar=cw[:, 1:2], in1=acc[:], op0=ALU.mult, op1=ALU.add)
        P.I("vector", "scalar_tensor_tensor", out=acc[:], in0=cxt[:, 0:CSEG], scalar=cw[:, 0:1], in1=acc[:], op0=ALU.mult, op1=ALU.add)
        P.dma("gpsimd", co[:, c0:c0 + CSEG], acc[:])

    lw = P.sb("lw", [64, 4], F32)
    lb = P.sb("lb", [64, 1], F32)
    bat = P.sb("bat", [64, 1], F32)
    bxt = P.sb("bxt", [64, 1], F32)
    lamt = P.sb("lamt", [64, 1], F32)
    nsp = P.sb("nsp", [64, 1], F32)
    wa_bf = P.sb("wa_bf", [64, 64], BF16)
    wx_bf = P.sb("wx_bf", [64, 64], BF16)
    P.dma("sync", lw[:], lcw, allow_slow_non_contiguous=True)
    P.dma("sync", lb[:], lcb, allow_slow_non_contiguous=True)
    P.dma("sync", bat[:], ba, allow_slow_non_contiguous=True)
    P.dma("sync", bxt[:], bx, allow_slow_non_contiguous=True)
    P.dma("sync", lamt[:], lam, allow_slow_non_contiguous=True)
    P.dma("gpsimd", wa_bf[:], wa)
    P.dma("gpsimd", wx_bf[:], wx)
    P.I("scalar", "activation", out=nsp[:], in_=lamt[:], func=AF.Exp, scale=-1.0)
    P.I("scalar", "activation", out=nsp[:], in_=nsp[:], func=AF.Ln, bias=ones1[0:64, 0:1], scale=1.0)
    P.I("vector", "tensor_scalar", out=nsp[:], in0=nsp[:], scalar1=-8.0, scalar2=None, op0=ALU.mult)
    LSEG = min(1024, SS)
    NHF = LSEG // 512
    lxt = P.sb("lxt", [64, 3 + LSEG], F32)
    P.I("vector", "memset", ap=lxt[:, 0:3], constant=0.0, _writes=[lxt[:]])
    xc_r = Rot(P, "xc", [64, LSEG], F32, 1)
    xcb_r = Rot(P, "xcb", [64, LSEG], BF16, 1)
    rg_r = Rot(P, "rg", [64, LSEG], F32, 1)
    ig_r = Rot(P, "ig", [64, LSEG], F32, 1)
    a_r = Rot(P, "la", [64, LSEG], F32, 1)
    a2_r = Rot(P, "la2", [64, LSEG], F32, 1)
    b_r = Rot(P, "lbin", [64, LSEG], F32, 1)
    h_r = Rot(P, "lh", [64, LSEG], F32, 2)
    gbanks = [psS[0], psS[1], psS[2], psO[0]]
    hprev = None
    lru_state = [None]

    def lru_gen():
      for sg in range(SS // LSEG):
        hprev = lru_state[0]
        c0 = sg * LSEG
        if sg > 0:
            P.I("vector", "tensor_copy", out=lxt[:, 0:3], in_=lxt[:, LSEG:LSEG + 3])
        P.dma("sync", lxt[:, 3:3 + LSEG], lx[:, c0:c0 + LSEG])
        xc = xc_r.next()
        P.I("vector", "tensor_scalar", out=xc[:], in0=lxt[:, 3:3 + LSEG], scalar1=lw[:, 3:4], scalar2=lb[:, 0:1], op0=ALU.mult, op1=ALU.add)
        for kk in range(3):
            P.I("vector", "scalar_tensor_tensor", out=xc[:], in0=lxt[:, kk:kk + LSEG], scalar=lw[:, kk:kk + 1], in1=xc[:], op0=ALU.mult, op1=ALU.add)
        xcb = xcb_r.next()
        P.I("vector", "tensor_copy", out=xcb[:], in_=xc[:])
        rg = rg_r.next()
        ig = ig_r.next()
        for hf in range(NHF):
            hs = slice(hf * 512, (hf + 1) * 512)
            P.I("tensor", "matmul", out=psR[0][0:64, :], lhsT=wa_bf[:], rhs=xcb[:, hs], start=True, stop=True)
            P.I("tensor", "matmul", out=psR[1][0:64, :], lhsT=wx_bf[:], rhs=xcb[:, hs], start=True, stop=True)
            P.I("scalar", "activation", out=rg[:, hs], in_=psR[0][0:64, :], func=AF.Sigmoid, bias=bat[:, 0:1], scale=1.0)
            P.I("scalar", "activation", out=ig[:, hs], in_=psR[1][0:64, :], func=AF.Sigmoid, bias=bxt[:, 0:1], scale=1.0)
        a = a_r.next()
        P.I("scalar", "activation", out=a[:], in_=rg[:], func=AF.Exp, scale=nsp[:, 0:1])
        a2 = a2_r.next()
        P.I("scalar", "activation", out=a2[:], in_=a[:], func=AF.Square)
        P.I("scalar", "activation", out=a2[:], in_=a2[:], func=AF.Sqrt, bias=ones1[0:64, 0:1], scale=-1.0)
        bb = b_r.next()
        P.I("vector", "tensor_tensor", out=bb[:], in0=ig[:], in1=xc[:], op=ALU.mult)
        P.I("vector", "tensor_tensor", out=bb[:], in0=bb[:], in1=a2[:], op=ALU.mult)
        h = h_r.next()
        P.I("vector", "tensor_tensor_scan", out=h[:], data0=a[:], data1=bb[:],
            initial=(0.0 if hprev is None else hprev[:, LSEG - 1:LSEG]), op0=ALU.mult, op1=ALU.add)
        lru_state[0] = h
        P.dma("gpsimd", lo[:, c0:c0 + LSEG], h[:])
        yield

    inn = P.sb("inn", [128, 128], F32)
    qd_c = P.sb("qd_c", [64, 512], F32)
    kd_c = P.sb("kd_c", [128, 1], F32)
    cd_c = P.sb("cd_c", [64, 1], F32)
    P.dma("sync", inn[:], innerT)
    for r_ in range(4):
        P.dma("sync", qd_c[:, r_ * 128:(r_ + 1) * 128], qdecT)
    P.dma("sync", kd_c[:], kdec)
    P.dma("sync", cd_c[:], cdec)
    PC = min(32, NB)
    RG = 4
    KVa = P.sb("KVa", [64, PC, 64], F32)
    Sbf = P.sb("Sbf", [64, PC, 64], BF16)
    gam = P.sb("gam", [64, PC], F32)
    carry = P.sb("rcarry", [64, 64], F32)
    P.I("vector", "memset", ap=carry[:], constant=0.0, _writes=[carry[:]])
    P.I("vector", "memset", ap=gam[:], constant=1.0, _writes=[gam[:]])
    P.I("vector", "tensor_scalar", out=gam[:], in0=gam[:], scalar1=cd_c[:, 0:1], scalar2=None, op0=ALU.mult)
    rq_r = Rot(P, "rq_t", [64, RG * 128], BF16, 2)
    rk_r = Rot(P, "rk_t", [64, RG * 128], BF16, 2)
    rv_r = Rot(P, "rv_t", [128, RG, 64], BF16, 2)
    scm_r = Rot(P, "scm", [128, 128], BF16, 3)
    kd_r = Rot(P, "kdt", [128, 64], BF16, 3)
    qd_r = Rot(P, "qdt", [64, RG * 128], BF16, 2)
    oT_r = Rot(P, "oT", [64, RG * 128], F32, 2)
    cen_r = Rot(P, "cen", [64, RG * 128], F32, 2)
    sqr_r = Rot(P, "sqr", [64, RG * 128], F32, 2)
    def ret_gen():
      for pc in range(NB // PC):
        c0 = pc * PC
        for g in range(PC // RG):
            t0 = (c0 + g * RG) * 128
            rkt, rvt = rk_r.next(), rv_r.next()
            P.dma("sync", rkt[:], rk[:, t0:t0 + RG * 128])
            P.dma("sync", rvt[:], rv[t0:t0 + RG * 128, :].rearrange("(n p) c -> p n c", p=128))
            for ci in range(RG):
                i = g * RG + ci
                cs = slice(ci * 128, (ci + 1) * 128)
                pT = psS[i % 3][:].bitcast(BF16)
                P.I("tensor", "transpose", out=pT[:, 0:64], in_=rkt[:, cs], identity=idb[0:64, 0:64])
                kdt = kd_r.next()
                P.I("scalar", "activation", out=kdt[:], in_=pT[:, 0:64], func=AF.Copy, scale=kd_c[:, 0:1])
                pK = psO[i % 2]
                P.I("tensor", "matmul", out=pK[0:64, 0:64], lhsT=kdt[:], rhs=rvt[:, ci, :], start=True, stop=True)
                P.I("vector", "tensor_copy", out=KVa[:, i, :], in_=pK[0:64, 0:64])
            yield
        for dv in range(64):
            P.I("vector", "tensor_tensor_scan", out=KVa[:, :, dv], data0=gam[:], data1=KVa[:, :, dv], initial=carry[:, dv:dv + 1],
                op0=ALU.mult, op1=ALU.add)
        P.I("scalar", "copy", out=Sbf[:, 0, :], in_=carry[:])
        if PC > 1:
            P.I("scalar", "copy", out=Sbf[:, 1:PC, :], in_=KVa[:, 0:PC - 1, :])
        P.I("vector", "tensor_copy", out=carry[:], in_=KVa[:, PC - 1, :])
        for g in range(PC // RG):
            t0 = (c0 + g * RG) * 128
            rqt, rkt, rvt = rq_r.next(), rk_r.next(), rv_r.next()
            P.dma("sync", rqt[:], rq[:, t0:t0 + RG * 128])
            P.dma("sync", rkt[:], rk[:, t0:t0 + RG * 128])
            P.dma("sync", rvt[:], rv[t0:t0 + RG * 128, :].rearrange("(n p) c -> p n c", p=128))
            qdt = qd_r.next()
            P.I("vector", "tensor_tensor", out=qdt[:], in0=rqt[:], in1=qd_c[:], op=ALU.mult)
            oT = oT_r.next()
            for ci in range(RG):
                i = g * RG + ci
                cs = slice(ci * 128, (ci + 1) * 128)
                pA = psS[i % 3]
                P.I("tensor", "matmul", out=pA[:, 0:128], lhsT=rkt[:, cs], rhs=rqt[:, cs], start=True, stop=True)
                scm = scm_r.next()
                P.I("vector", "tensor_tensor", out=scm[:], in0=pA[:, 0:128], in1=inn[:], op=ALU.mult)
                pBk = psO[i % 2]
                P.I("tensor", "matmul", out=pBk[0:64, 0:128], lhsT=rvt[:, ci, :], rhs=scm[:], start=True, stop=False)
                P.I("tensor", "matmul", out=pBk[0:64, 0:128], lhsT=Sbf[:, i, :], rhs=qdt[:, cs], start=False, stop=True)
                P.I("scalar", "copy", out=oT[:, cs], in_=pBk[0:64, 0:128])
            W = RG * 128
            P.I("tensor", "matmul", out=psB[0:64, 0:W], lhsT=ones64[0:64, :], rhs=oT[:], start=True, stop=True)
            cen = cen_r.next()
            P.I("vector", "tensor_tensor", out=cen[:], in0=oT[:], in1=psB[0:64, 0:W], op=ALU.subtract)
            sqr = sqr_r.next()
            P.I("scalar", "activation", out=sqr[:], in_=cen[:], func=AF.Square)
            P.I("tensor", "matmul", out=psB[0:64, 0:W], lhsT=ones64[0:64, :], rhs=sqr[:], start=True, stop=True)
            P.I("scalar", "activation", out=sqr[:], in_=psB[0:64, 0:W], func=AF.Sqrt, bias=epsc[0:64, 0:1], scale=1.0)
            P.I("vector", "reciprocal", out=sqr[:], in_=sqr[:])
            P.I("vector", "tensor_tensor", out=cen[:], in0=cen[:], in1=sqr[:], op=ALU.mult)
            P.dma("gpsimd", ro[:, t0:t0 + W], cen[:])
            yield

    g_l, g_r = lru_gen(), ret_gen()
    n_l = SS // LSEG
    n_r = (NB // PC) * 2 * (PC // RG)
    per = max(1, n_r // max(1, n_l))
    done_l = done_r = False
    while not (done_l and done_r):
        if not done_l:
            try:
                next(g_l)
            except StopIteration:
                done_l = True
        for _ in range(per):
            if not done_r:
                try:
                    next(g_r)
                except StopIteration:
                    done_r = True
    QT = P.sb("QT", [96, SS], BF16)
    KT = P.sb("KT", [96, SS], BF16)
    VA = P.sb("VA", [128, NB, 65], BF16)
    LDC = min(2048, SS)
    for i in range(SS // LDC):
        P.dma("sync", KT[:, i * LDC:(i + 1) * LDC], ak[:, i * LDC:(i + 1) * LDC])
        P.dma("scalar", QT[:, i * LDC:(i + 1) * LDC], aq[:, i * LDC:(i + 1) * LDC])
    P.I("gpsimd", "memset", ap=VA[:, :, 64:65], constant=1.0, _writes=[VA[:]])
    P.dma("sync", VA[:, :, 0:64], av.rearrange("(n p) c -> p n c", p=128))
    tri_f = P.sb("tri_f", [128, 128], F32)
    tri = P.sb("tri", [128, 128], BF16)
    P.I("gpsimd", "memset", ap=tri_f[:], constant=1.0, _writes=[tri_f[:]])
    P.I("gpsimd", "affine_select", out=tri_f[:], in_=tri_f[:], pattern=[[1, 128]], compare_op=ALU.is_ge,
        fill=0.0, base=0, channel_multiplier=-1)
    P.I("vector", "tensor_copy", out=tri[:], in_=tri_f[:])
    gq = P.sb("gq_bc", [128, 96], F32)
    gk = P.sb("gk_bc", [128, 96], F32)
    m2 = P.sb("m2", [128, 2], F32)
    negc = P.sb("negc", [128, 1], F32)
    P.dma("sync", gq[:], qqk.partition_broadcast(128))
    P.dma("sync", gk[:], kqk.partition_broadcast(128))
    P.I("vector", "tensor_tensor", out=gq[:], in0=gq[:], in1=gq[:], op=ALU.mult)
    P.I("vector", "tensor_tensor", out=gk[:], in0=gk[:], in1=gk[:], op=ALU.mult)
    P.I("vector", "tensor_reduce", out=m2[:, 0:1], in_=gq[:], axis=AX.X, op=ALU.max)
    P.I("vector", "tensor_reduce", out=m2[:, 1:2], in_=gk[:], axis=AX.X, op=ALU.max)
    P.I("vector", "tensor_tensor", out=negc[:], in0=m2[:, 0:1], in1=m2[:, 1:2], op=ALU.mult)
    P.I("gpsimd", "tensor_tensor", out=negc[:], in0=negc[:], in1=half[:, 0:1], op=ALU.pow)
    P.I("vector", "tensor_scalar", out=negc[:], in0=negc[:], scalar1=-math.sqrt(96.0), scalar2=None, op0=ALU.mult)
    if after_setup is not None:
        after_setup()
    PT_r = Rot(P, "PT", [128, 512], BF16, 5)
    den_r = Rot(P, "den", [128, 512], F32, 2)
    oa_r = Rot(P, "oa", [64, 512], F32, 2)
    rb_r = Rot(P, "rb", [64, 512], F32, 2)
    si = 0
    for qs in range(SS // 512):
        q0 = qs * 512
        pO = psO[qs % 2]
        nkb = 4 * qs + 4
        pend = []
        for j in range(nkb):
            d = j - 4 * qs
            lo_c = 0 if d <= 0 else d * 128
            pS = psS[si % 3]
            si += 1
            P.I("tensor", "matmul", out=pS[:, lo_c:512], lhsT=KT[:, j * 128:(j + 1) * 128], rhs=QT[:, q0 + lo_c:q0 + 512],
                start=True, stop=True)
            if len(pend) >= 2:
                pj, plo, pPT = pend.pop(0)
                P.I("tensor", "matmul", out=pO[0:65, plo:512], lhsT=VA[:, pj, :], rhs=pPT[:, plo:512], start=(pj == 0), stop=False)
            PT = PT_r.next()
            P.I("scalar", "activation", out=PT[:, lo_c:512], in_=pS[:, lo_c:512], func=AF.Exp, bias=negc[:, 0:1], scale=ATT_SCALE)
            if d >= 0:
                P.I("vector", "tensor_tensor", out=PT[:, lo_c:lo_c + 128], in0=PT[:, lo_c:lo_c + 128], in1=tri[:], op=ALU.mult)
            pend.append((j, lo_c, PT))
        while pend:
            pj, plo, pPT = pend.pop(0)
            P.I("tensor", "matmul", out=pO[0:65, plo:512], lhsT=VA[:, pj, :], rhs=pPT[:, plo:512], start=(pj == 0), stop=(len(pend) == 0))
        den = den_r.next()
        P.I("vector", "tensor_copy", out=den[64:65, :], in_=pO[64:65, :])
        P.I("tensor", "matmul", out=psB[0:64, :], lhsT=ones1[64:65, 0:64], rhs=den[64:65, :], start=True, stop=True)
        rb = rb_r.next()
        P.I("vector", "reciprocal", out=rb[:], in_=psB[0:64, :])
        oa = oa_r.next()
        P.I("vector", "tensor_tensor", out=oa[:], in0=pO[0:64, :], in1=rb[:], op=ALU.mult)
        P.dma("sync", ao[:, q0:q0 + 512], oa[:])


def build_p3(ntok=TPC, TB=1024, NEXP=32, stop=0):
    nc = new_nc()
    P = Prog(nc)
    io = IO(nc)
    phase3(P, io, ntok, TB, NEXP)
    P.finish(io.outs)
    P.emit()
    return nc


def phase3(P, io, ntok, TB=1024, NEXP=32, stop=0):
    nc = P.nc
    x_in = io.inp("x_in", [ntok, D])
    c_in = io.inp("c_in", [D])
    adaw = io.inp("ada_w", [D, 6 * D])
    adab = io.inp("ada_b", [6 * D])
    mixo = [io.inp(n, [256, ntok]) for n in ("co", "ao", "ro", "lo")]
    gates = {0: io.inp("bgate", [256, ntok]), 2: io.inp("sgate", [256, ntok]), 3: io.inp("ggate", [256, ntok])}
    mng = io.inp("mix_norm_g", [D])
    w_out = io.inp("w_out", [D, D])
    gffn = io.inp("norm_ffn_g", [D])
    wgr = io.inp("router_group_w", [D, 4])
    bgr = io.inp("router_group_b", [4])
    wer = io.inp("router_expert_w", [D, 32])
    ber = io.inp("router_expert_b", [32])
    ewg = io.inp("exp_w_gate", [32, D, 256])
    ewu = io.inp("exp_w_up", [32, D, 256])
    ewd = io.inp("exp_w_down", [32, 256, D])
    x_out = io.out("x_out", [ntok, D])
    x1s = io.out("x1s", [ntok, D])

    bank = [P.ps("bank%d" % i, [128, 512], F32) for i in range(8)]
    idf, idb = make_identities(P)
    neghalf = P.sb("neghalf", [128, 256], F32)
    P.I("vector", "memset", ap=neghalf[:], constant=-0.5, _writes=[neghalf[:]])
    onesf = P.sb("onesf", [128, 128], F32)
    P.I("vector", "memset", ap=onesf[:], constant=1.0, _writes=[onesf[:]])
    epsc = P.sb("epsc", [128, 1], F32)
    P.I("vector", "memset", ap=epsc[:], constant=EPS, _writes=[epsc[:]])

    mod = compute_mod(P, c_in, adaw, adab, bank[0], bank[1], 2 * D, 6 * D, CH=128)
    gtm = mod[:, 0:D]
    shf = mod[:, D:2 * D]
    gf = mod[:, 2 * D:3 * D]
    gtf = mod[:, 3 * D:4 * D]
    tmo_r = Rot(P, "tmo", [128, D], F32, 2)
    gtmp = tmo_r.next()
    P.dma("sync", gtmp[:], gffn.partition_broadcast(128))
    P.I("vector", "scalar_tensor_tensor", out=gf, in0=gf, scalar=1.0, in1=gtmp[:], op0=ALU.add, op1=ALU.mult)
    mngc = P.sb("mngc", [128, 8], F32)
    P.dma("sync", mngc[:], mng.rearrange("(k p) -> p k", p=128), allow_slow_non_contiguous=True)
    wout_bf = P.sb("wout_bf", [128, 8, D], BF16)
    wov = w_out.rearrange("(k p) n -> p k n", p=128)
    for k in range(8):
        P.dma("gpsimd", wout_bf[:, k, :], wov[:, k, :])
    wr = P.sb("wr", [128, 8, 36], F32)
    P.dma("sync", wr[:, :, 0:4], wgr.rearrange("(k p) n -> p k n", p=128))
    P.dma("sync", wr[:, :, 4:36], wer.rearrange("(k p) n -> p k n", p=128))
    wr_hi = P.sb("wr_hi", [128, 8, 36], BF16)
    wr_lo = P.sb("wr_lo", [128, 8, 36], BF16)
    P.I("vector", "tensor_copy", out=wr_hi[:], in_=wr[:])
    P.I("vector", "tensor_tensor", out=wr_lo[:], in0=wr[:], in1=wr_hi[:], op=ALU.subtract)
    bg_bc = P.sb("bg_bc", [128, 4], F32)
    be_bc = P.sb("be_bc", [128, 32], F32)
    P.dma("sync", bg_bc[:], bgr.partition_broadcast(128))
    P.dma("sync", be_bc[:], ber.partition_broadcast(128))

    NSUB = TB // 128
    h2T = P.sb("h2T", [128, 8, TB], BF16)
    comb_all = P.sb("comb_all", [128, NSUB, 32], F32)
    yacc = P.sb("yacc", [128, NSUB, D], F32)

    yc_r = Rot(P, "yc", [128, 256], F32, 3)
    gt_r = Rot(P, "gtl", [128, 256], F32, 2)
    ysq_r = Rot(P, "ysq", [128, 256], F32, 2)
    ych = [P.sb("ych%d" % i, [128, 256], F32) for i in range(8)]
    rsb_r = Rot(P, "rsb", [128, 256], F32, 2)
    yT = P.sb("yT", [128, 8, 256], BF16)
    xt_r = Rot(P, "xt", [128, D], F32, 1)
    x1_r = Rot(P, "x1", [128, D], F32, 2)
    junk = P.sb("junk", [128, D], BF16)
    sm_r = Rot(P, "sm", [128, 8], F32, 4)
    h2f_r = Rot(P, "h2f", [128, D], F32, 1)
    h2Tlo = P.sb("h2Tlo", [128, 8, 128], BF16)
    hsp_r = Rot(P, "hsp", [128, D], BF16, 2)
    R8 = Rot(P, "r8", [128, 8], F32, 20)
    R4 = Rot(P, "r4", [128, 4], F32, 8)
    R1 = Rot(P, "r1", [128, 1], F32, 16)
    lg_r = Rot(P, "lgs", [128, 36], F32, 2)
    wgu_r = Rot(P, "wgu", [128, 8, 512], BF16, 2)
    wd_r = Rot(P, "wd", [128, 2, D], BF16, 2)
    sg_r = Rot(P, "sg", [128, 256], BF16, 4)
    hid_r = Rot(P, "hid", [128, 2, 256], BF16, 2)

    if "wgu_s" in io.m:
        wgu_s, wd_s = io.m["wgu_s"], io.m["wd_s"]
    else:
        wgu_s, wd_s = precast_experts(P, ewg, ewu, ewd, NEXP)

    for blk in range(ntok // TB):
        b0 = blk * TB
        for tl in range(TB // 256):
            t0 = b0 + tl * 256
            for m in range(4):
                pss = bank[m % 2]
                for c in range(2):
                    ci = m * 2 + c
                    y = ych[ci]
                    if m == 1:
                        P.dma("sync", y[:], mixo[m][c * 128:(c + 1) * 128, t0:t0 + 256])
                    else:
                        yc = yc_r.next()
                        gt = gt_r.next()
                        P.dma("sync", yc[:], mixo[m][c * 128:(c + 1) * 128, t0:t0 + 256])
                        P.dma("scalar", gt[:], gates[m][c * 128:(c + 1) * 128, t0:t0 + 256])
                        P.I("vector", "tensor_tensor", out=y[:], in0=yc[:], in1=gt[:], op=ALU.mult)
                    ysq = ysq_r.next()
                    P.I("scalar", "activation", out=ysq[:], in_=y[:], func=AF.Square)
                    P.I("tensor", "matmul", out=pss[:, 0:256], lhsT=onesf[:], rhs=ysq[:], start=(c == 0), stop=(c == 1))
                rsb = rsb_r.next()
                P.I("scalar", "activation", out=rsb[:], in_=pss[:, 0:256], func=AF.Sqrt, bias=epsc[:, 0:1], scale=1.0 / 256)
                P.I("vector", "reciprocal", out=rsb[:], in_=rsb[:])
                for c in range(2):
                    ci = m * 2 + c
                    P.I("vector", "scalar_tensor_tensor", out=yT[:, ci, :], in0=ych[ci][:], scalar=mngc[:, ci:ci + 1], in1=rsb[:],
                        op0=ALU.mult, op1=ALU.mult)
            for s in range(2):
                tt = t0 + s * 128
                si = tl * 2 + s
                po = [bank[2], bank[3]]
                for hf in range(2):
                    for k in range(8):
                        P.I("tensor", "matmul", out=po[hf][:], lhsT=yT[:, k, s * 128:(s + 1) * 128], rhs=wout_bf[:, k, hf * 512:(hf + 1) * 512],
                            start=(k == 0), stop=(k == 7))
                xt = xt_r.next()
                P.dma("sync", xt[:], x_in[tt:tt + 128, :])
                tmo = tmo_r.next()
                x1 = x1_r.next()
                for hf in range(2):
                    P.I("vector", "tensor_tensor", out=tmo[:, hf * 512:(hf + 1) * 512], in0=po[hf][:], in1=gtm[:, hf * 512:(hf + 1) * 512], op=ALU.mult)
                P.I("vector", "tensor_tensor", out=x1[:], in0=tmo[:], in1=xt[:], op=ALU.add)
                P.dma("sync", x1s[tt:tt + 128, :], x1[:])
                sm = sm_r.next()
                P.I("scalar", "activation", out=junk[:], in_=x1[:], func=AF.Square, accum_out=sm[:, 0:1])
                P.I("vector", "tensor_scalar", out=sm[:, 1:2], in0=sm[:, 0:1], scalar1=1.0 / D, scalar2=EPS, op0=ALU.mult, op1=ALU.add)
                P.I("gpsimd", "tensor_tensor", out=sm[:, 2:3], in0=sm[:, 1:2], in1=neghalf[:, 0:1], op=ALU.pow)
                tm2 = tmo_r.next()
                h2f = h2f_r.next()
                P.I("vector", "scalar_tensor_tensor", out=tm2[:], in0=x1[:], scalar=sm[:, 2:3], in1=gf, op0=ALU.mult, op1=ALU.mult)
                P.I("vector", "tensor_tensor", out=h2f[:], in0=tm2[:], in1=shf, op=ALU.add)
                hhi = hsp_r.next()
                hlo = hsp_r.next()
                P.I("scalar", "copy", out=hhi[:], in_=h2f[:])
                P.I("vector", "tensor_tensor", out=hlo[:], in0=h2f[:], in1=hhi[:], op=ALU.subtract)
                b4 = bank[4][:].bitcast(BF16)
                b5 = bank[5][:].bitcast(BF16)
                for k in range(8):
                    P.I("tensor", "transpose", out=b4[:, k * 128:(k + 1) * 128], in_=hhi[:, k * 128:(k + 1) * 128], identity=idb[:])
                for k in range(8):
                    P.I("tensor", "transpose", out=b5[:, k * 128:(k + 1) * 128], in_=hlo[:, k * 128:(k + 1) * 128], identity=idb[:])
                P.I("scalar", "copy", out=h2T[:, :, si * 128:(si + 1) * 128], in_=b4.rearrange("p (k t) -> p k t", k=8))
                P.I("vector", "tensor_copy", out=h2Tlo[:], in_=b5.rearrange("p (k t) -> p k t", k=8))
                pl = bank[6]
                for k in range(8):
                    P.I("tensor", "matmul", out=pl[:, 0:36], lhsT=h2T[:, k, si * 128:(si + 1) * 128], rhs=wr_hi[:, k, :], start=(k == 0), stop=False)
                for k in range(8):
                    P.I("tensor", "matmul", out=pl[:, 0:36], lhsT=h2Tlo[:, k, :], rhs=wr_hi[:, k, :], start=False, stop=False)
                for k in range(8):
                    P.I("tensor", "matmul", out=pl[:, 0:36], lhsT=h2T[:, k, si * 128:(si + 1) * 128], rhs=wr_lo[:, k, :], start=False, stop=(k == 7))
                lg = lg_r.next()
                P.I("vector", "tensor_copy", out=lg[:], in_=pl[:, 0:36])
                router_math(P, lg, comb_all[:, si, :], bg_bc, be_bc, R8, R4, R1)
        NTL = TB // 256
        steps = [(e, tl) for e in range(NEXP) for tl in range(NTL)]
        wts = {}
        bcs = {}
        hids = {}

        def load_expert(e):
            wgu = wgu_r.next()
            wd = wd_r.next()
            P.dma("sync", wgu[:], wgu_s[e])
            P.dma("sync", wd[:], wd_s[e])
            wts[e] = (wgu, wd)

        def gu_mm(i):
            e, tl = steps[i]
            if tl == 0:
                if e not in wts:
                    load_expert(e)
            wgu, wd = wts[e]
            pg = bank[4 + 2 * (i % 2)]
            pu = bank[5 + 2 * (i % 2)]
            for fc in range(2):
                for k in range(8):
                    P.I("tensor", "matmul", out=pg[:, fc * 256:(fc + 1) * 256], lhsT=wgu[:, k, fc * 128:(fc + 1) * 128],
                        rhs=h2T[:, k, tl * 256:(tl + 1) * 256], start=(k == 0), stop=(k == 7))
                for k in range(8):
                    P.I("tensor", "matmul", out=pu[:, fc * 256:(fc + 1) * 256], lhsT=wgu[:, k, 256 + fc * 128:256 + (fc + 1) * 128],
                        rhs=h2T[:, k, tl * 256:(tl + 1) * 256], start=(k == 0), stop=(k == 7))

        def gu_ew(i):
            e, tl = steps[i]
            pg = bank[4 + 2 * (i % 2)]
            pu = bank[5 + 2 * (i % 2)]
            hid = hid_r.next()
            hids[i] = hid
            for fc in range(2):
                sg = sg_r.next()
                P.I("scalar", "activation", out=sg[:], in_=pg[:, fc * 256:(fc + 1) * 256], func=AF.Silu)
                P.I("vector", "tensor_tensor", out=hid[:, fc, :], in0=sg[:], in1=pu[:, fc * 256:(fc + 1) * 256], op=ALU.mult)

        def down(i):
            e, tl = steps[i]
            wgu, wd = wts[e]
            hid = hids.pop(i)
            for s in range(2):
                si = tl * 2 + s
                for hf in range(2):
                    py = bank[s * 2 + hf]
                    for fc in range(2):
                        P.I("tensor", "matmul", out=py[:], lhsT=hid[:, fc, s * 128:(s + 1) * 128], rhs=wd[:, fc, hf * 512:(hf + 1) * 512],
                            start=(fc == 0), stop=(fc == 1))
                    if e == 0:
                        P.I("vector", "tensor_scalar", out=yacc[:, si, hf * 512:(hf + 1) * 512], in0=py[:], scalar1=comb_all[:, si, e:e + 1],
                            scalar2=None, op0=ALU.mult)
                    else:
                        P.I("vector", "scalar_tensor_tensor", out=yacc[:, si, hf * 512:(hf + 1) * 512], in0=py[:], scalar=comb_all[:, si, e:e + 1],
                            in1=yacc[:, si, hf * 512:(hf + 1) * 512], op0=ALU.mult, op1=ALU.add)
            if tl == NTL - 1:
                wts.pop(e, None)
            if tl == 0 and e + 1 < NEXP:
                load_expert(e + 1)

        if steps:
            gu_mm(0)
            gu_ew(0)
        for i in range(len(steps)):
            if i + 1 < len(steps):
                gu_mm(i + 1)
            down(i)
            if i + 1 < len(steps):
                gu_ew(i + 1)
        for si in range(NSUB):
            tt = b0 + si * 128
            x1 = x1_r.next()
            P.dma("sync", x1[:], x1s[tt:tt + 128, :])
            tmo = tmo_r.next()
            P.I("vector", "tensor_tensor", out=tmo[:], in0=yacc[:, si, :], in1=gtf, op=ALU.mult)
            xo = xt_r.next()
            P.I("vector", "tensor_tensor", out=xo[:], in0=tmo[:], in1=x1[:], op=ALU.add)
            P.dma("sync", x_out[tt:tt + 128, :], xo[:])


RSTOP = 0


def precast_experts(P, ewg, ewu, ewd, NEXP=32):
    nc = P.nc
    wgu_s = nc.dram_tensor(P.prefix + "wgu_s", [32, 128, 8, 512], BF16).ap()
    wd_s = nc.dram_tensor(P.prefix + "wd_s", [32, 128, 2, D], BF16).ap()
    for e in range(NEXP):
        gv = ewg[e].rearrange("(k p) f -> p k f", p=128)
        uv = ewu[e].rearrange("(k p) f -> p k f", p=128)
        for k in range(8):
            P.dma("gpsimd", wgu_s[e, :, k, 0:256], gv[:, k, :])
            P.dma("gpsimd", wgu_s[e, :, k, 256:512], uv[:, k, :])
        dv = ewd[e].rearrange("(k p) n -> p k n", p=128)
        for k in range(2):
            P.dma("gpsimd", wd_s[e, :, k, :], dv[:, k, :])
    return wgu_s, wd_s


def router_math(P, lg, comb, bg_bc, be_bc, R8, R4, R1):
    V = lambda *a, **k: P.I("vector", *a, **k)
    lgg = lg[:, 0:4]
    mx = R1.next()
    V("tensor_reduce", out=mx[:], in_=lgg, axis=AX.X, op=ALU.max)
    nmx = R1.next()
    V("tensor_scalar", out=nmx[:], in0=mx[:], scalar1=-1.0, scalar2=None, op0=ALU.mult)
    eg = R4.next()
    sg = R1.next()
    P.I("scalar", "activation", out=eg[:], in_=lgg, func=AF.Exp, bias=nmx[:, 0:1], scale=1.0, accum_out=sg[:, 0:1])
    if RSTOP == 1:
        return
    rs = R1.next()
    V("reciprocal", out=rs[:], in_=sg[:])
    gp = R4.next()
    V("tensor_scalar", out=gp[:], in0=eg[:], scalar1=rs[:, 0:1], scalar2=None, op0=ALU.mult)
    sel = R4.next()
    V("tensor_tensor", out=sel[:], in0=gp[:], in1=bg_bc[:], op=ALU.add)
    m = R1.next()
    V("tensor_reduce", out=m[:], in_=sel[:], axis=AX.X, op=ALU.max)
    goh = R4.next()
    V("tensor_scalar", out=goh[:], in0=sel[:], scalar1=m[:, 0:1], scalar2=None, op0=ALU.is_equal)
    if RSTOP == 2:
        return
    gwj = R4.next()
    gw = R1.next()
    V("tensor_tensor", out=gwj[:], in0=gp[:], in1=goh[:], op=ALU.mult)
    V("tensor_reduce", out=gw[:], in_=gwj[:], axis=AX.X, op=ALU.add)
    els = R8.next()
    bes = R8.next()
    V("tensor_scalar", out=els[:], in0=lg[:, 4:12], scalar1=goh[:, 0:1], scalar2=None, op0=ALU.mult)
    V("tensor_scalar", out=bes[:], in0=be_bc[:, 0:8], scalar1=goh[:, 0:1], scalar2=None, op0=ALU.mult)
    for g in range(1, 4):
        V("scalar_tensor_tensor", out=els[:], in0=lg[:, 4 + 8 * g:12 + 8 * g], scalar=goh[:, g:g + 1], in1=els[:], op0=ALU.mult, op1=ALU.add)
        V("scalar_tensor_tensor", out=bes[:], in0=be_bc[:, 8 * g:8 * g + 8], scalar=goh[:, g:g + 1], in1=bes[:], op0=ALU.mult, op1=ALU.add)
    if RSTOP == 3:
        return
    mx8 = R1.next()
    V("tensor_reduce", out=mx8[:], in_=els[:], axis=AX.X, op=ALU.max)
    nm8 = R1.next()
    V("tensor_scalar", out=nm8[:], in0=mx8[:], scalar1=-1.0, scalar2=None, op0=ALU.mult)
    ee = R8.next()
    se = R1.next()
    P.I("scalar", "activation", out=ee[:], in_=els[:], func=AF.Exp, bias=nm8[:, 0:1], scale=1.0, accum_out=se[:, 0:1])
    rse = R1.next()
    V("reciprocal", out=rse[:], in_=se[:])
    ep = R8.next()
    V("tensor_scalar", out=ep[:], in0=ee[:], scalar1=rse[:, 0:1], scalar2=None, op0=ALU.mult)
    if RSTOP == 4:
        return
    sc = R8.next()
    V("tensor_tensor", out=sc[:], in0=ep[:], in1=bes[:], op=ALU.add)
    m1 = R1.next()
    V("tensor_reduce", out=m1[:], in_=sc[:], axis=AX.X, op=ALU.max)
    oh1 = R8.next()
    V("tensor_scalar", out=oh1[:], in0=sc[:], scalar1=m1[:, 0:1], scalar2=None, op0=ALU.is_equal)
    sc2 = R8.next()
    V("scalar_tensor_tensor", out=sc2[:], in0=oh1[:], scalar=-1e9, in1=sc[:], op0=ALU.mult, op1=ALU.add)
    m2 = R1.next()
    V("tensor_reduce", out=m2[:], in_=sc2[:], axis=AX.X, op=ALU.max)
    oh2 = R8.next()
    V("tensor_scalar", out=oh2[:], in0=sc2[:], scalar1=m2[:, 0:1], scalar2=None, op0=ALU.is_equal)
    if RSTOP == 5:
        return
    ohs = R8.next()
    V("tensor_tensor", out=ohs[:], in0=oh1[:], in1=oh2[:], op=ALU.add)
    tp = R8.next()
    V("tensor_tensor", out=tp[:], in0=ep[:], in1=ohs[:], op=ALU.mult)
    sp = R1.next()
    V("tensor_reduce", out=sp[:], in_=tp[:], axis=AX.X, op=ALU.add)
    rsp = R1.next()
    V("reciprocal", out=rsp[:], in_=sp[:])
    fac = R1.next()
    V("tensor_tensor", out=fac[:], in0=rsp[:], in1=gw[:], op=ALU.mult)
    ew = R8.next()
    V("tensor_scalar", out=ew[:], in0=tp[:], scalar1=fac[:, 0:1], scalar2=None, op0=ALU.mult)
    if RSTOP == 6:
        return
    for g in range(4):
        V("tensor_scalar", out=comb[:, g * 8:(g + 1) * 8], in0=ew[:], scalar1=goh[:, g:g + 1], scalar2=None, op0=ALU.mult)


W_SPECS = dict(
    ada_w=[2, D, 6 * D], ada_b=[2, 6 * D], norm_mix_g=[2, D], w_in=[2, D, IN_COLS], conv_w=[2, 3, 256],
    mla_q_norm_g=[2, 192], mla_w_uq=[2, 192, 384], mla_kv_norm_g=[2, 128], mla_w_ukv=[2, 128, 512],
    mla_q_qk_g=[2, 96], mla_k_qk_g=[2, 96], lru_conv_w=[2, 4, 256], lru_conv_b=[2, 256], lru_w_a=[2, 4, 64, 64],
    lru_b_a=[2, 256], lru_w_x=[2, 4, 64, 64], lru_b_x=[2, 256], lru_lambda=[2, 256], mix_norm_g=[2, D],
    w_out=[2, D, D], norm_ffn_g=[2, D], router_group_w=[2, D, 4], router_group_b=[2, 4], router_expert_w=[2, D, 32],
    router_expert_b=[2, 32], exp_w_gate=[2, 32, D, 256], exp_w_up=[2, 32, D, 256], exp_w_down=[2, 32, 256, D])


def build_fused(SS=S, NL=2, TB=1024):
    nc = new_nc()
    P = Prog(nc)
    x_in = din(nc, "x", [SS, D])
    c_in = din(nc, "c", [D])
    pos = din(nc, "positions", [SS], I32)
    W = {k: din(nc, k, shp) for k, shp in W_SPECS.items()}
    inv_ret4 = din(nc, "inv_ret4", [128, 1])
    inv_mla = din(nc, "inv_mla", [16])
    innerT = din(nc, "innerT", [4, 128, 128])
    qdecT = din(nc, "qdecT", [4, 64, 128])
    kdec = din(nc, "kdec", [4, 128, 1])
    cdec = din(nc, "cdec", [4, 64, 1])
    out = dout(nc, "out", [SS, D])

    def idr(name, shape, dt=F32):
        return nc.dram_tensor(name, list(shape), dt).ap()

    T = dict(attq=idr("i_attq", [4, 96, SS], BF16), attk=idr("i_attk", [4, 96, SS], BF16), attv=idr("i_attv", [4, SS, 64], BF16),
             retq=idr("i_retq", [256, SS], BF16), retk=idr("i_retk", [256, SS], BF16), retv=idr("i_retv", [SS, 256], BF16),
             lrux=idr("i_lrux", [256, SS]), cvx=idr("i_cvx", [256, SS]), bgate=idr("i_bgate", [256, SS]),
             sgate=idr("i_sgate", [256, SS]), ggate=idr("i_ggate", [256, SS]))
    Y = dict(co=idr("i_co", [256, SS]), ao=idr("i_ao", [256, SS]), ro=idr("i_ro", [256, SS]), lo=idr("i_lo", [256, SS]))
    x_mid = idr("i_xmid", [SS, D])
    x1s = idr("i_x1s", [SS, D])
    col = lambda ap: ap.rearrange("(c o) -> c o", o=1)
    cast = {}
    for l in range(NL):
        xs = x_in if l == 0 else x_mid
        xd = out if l == NL - 1 else x_mid
        mk = P.mark()
        P.prefix = "L%dP1_" % l
        m = dict(x_in=xs, c_in=c_in, pos_in=pos, inv_ret4=inv_ret4, inv_mla=inv_mla)
        for k in ("ada_w", "ada_b", "norm_mix_g", "w_in", "mla_q_norm_g", "mla_w_uq", "mla_kv_norm_g", "mla_w_ukv", "mla_q_qk_g", "mla_k_qk_g"):
            m[k] = W[k][l]
        m.update(T)
        phase1(P, IO(nc, m), SS)
        P.release(mk)
        for hd in range(4):
            mk = P.mark()
            P.prefix = "L%dP2h%d_" % (l, hd)
            sl = slice(hd * 64, (hd + 1) * 64)
            m = dict(aq=T["attq"][hd], ak=T["attk"][hd], av=T["attv"][hd], rq=T["retq"][sl], rk=T["retk"][sl], rv=T["retv"][:, sl],
                     lx=T["lrux"][sl], cx=T["cvx"][sl],
                     convw=W["conv_w"][l][:, sl].rearrange("k c -> c k"), lcw=W["lru_conv_w"][l][:, sl].rearrange("k c -> c k"),
                     lcb=col(W["lru_conv_b"][l][sl]), wa=W["lru_w_a"][l][hd], ba=col(W["lru_b_a"][l][sl]),
                     wx=W["lru_w_x"][l][hd], bx=col(W["lru_b_x"][l][sl]), lam=col(W["lru_lambda"][l][sl]),
                     qqk=W["mla_q_qk_g"][l], kqk=W["mla_k_qk_g"][l],
                     innerT=innerT[hd], qdecT=qdecT[hd], kdec=kdec[hd], cdec=cdec[hd],
                     ao=Y["ao"][sl], ro=Y["ro"][sl], lo=Y["lo"][sl], co=Y["co"][sl])
            if hd == 0:
                def _cast(l=l):
                    pfx = P.prefix
                    P.prefix = "L%d_" % l
                    cast[l] = precast_experts(P, W["exp_w_gate"][l], W["exp_w_up"][l], W["exp_w_down"][l])
                    P.prefix = pfx
                phase2(P, IO(nc, m), SS, after_setup=_cast)
            else:
                phase2(P, IO(nc, m), SS)
            P.release(mk)
        mk = P.mark()
        P.prefix = "L%dP3_" % l
        m = dict(x_in=xs, c_in=c_in, x_out=xd, x1s=x1s, bgate=T["bgate"], sgate=T["sgate"], ggate=T["ggate"])
        for k in ("ada_w", "ada_b", "mix_norm_g", "w_out", "norm_ffn_g", "router_group_w", "router_group_b", "router_expert_w",
                  "router_expert_b", "exp_w_gate", "exp_w_up", "exp_w_down"):
            m[k] = W[k][l]
        m.update(Y)
        m["wgu_s"], m["wd_s"] = cast[l]
        phase3(P, IO(nc, m), SS, TB, 32)
        P.release(mk)
    P.finish([out])
    P.emit()
    return nc


_NC_CACHE = {}


def _get(name, fn):
    if name not in _NC_CACHE:
        _NC_CACHE[name] = fn()
    return _NC_CACHE[name]


def _inv_freq(dim):
    return (np.float32(1.0) / (np.float32(10000.0) ** (np.arange(0, dim, 2, dtype=np.float32) / np.float32(dim)))).astype(np.float32)


def _ret_consts(hd):
    gamma = 1.0 - 2.0 ** (-5.0 - hd)
    idx = np.arange(128, dtype=np.float64)
    innerT = np.where(idx[None, :] >= idx[:, None], gamma ** np.maximum(idx[None, :] - idx[:, None], 0.0), 0.0).astype(np.float32)
    qdecT = np.ascontiguousarray(np.tile((gamma ** (idx + 1.0))[None, :], (64, 1))).astype(np.float32)
    kdec = (gamma ** (127.0 - idx)).reshape(128, 1).astype(np.float32)
    cdec = np.full((64, 1), gamma ** 128, np.float32)
    return innerT, qdecT, kdec, cdec


def kernel(**inputs):
    I = {k: np.ascontiguousarray(np.asarray(v)) for k, v in inputs.items()}
    B = I["x"].shape[0]
    nc = _get("fused", build_fused)
    rc = [_ret_consts(h) for h in range(4)]
    consts = dict(inv_ret4=np.ascontiguousarray(np.tile(_inv_freq(64), 4).reshape(128, 1)), inv_mla=_inv_freq(32),
                  innerT=np.stack([r[0] for r in rc]), qdecT=np.stack([r[1] for r in rc]),
                  kdec=np.stack([r[2] for r in rc]), cdec=np.stack([r[3] for r in rc]))
    maps = []
    for b in range(B):
        m = dict(x=np.ascontiguousarray(I["x"][b], dtype=np.float32), c=np.ascontiguousarray(I["c"][b]),
                 positions=np.ascontiguousarray(I["positions"][b]).astype(np.int32))
        for k in W_SPECS:
            m[k] = I[k]
        m.update(consts)
        maps.append(m)
    res = run_bass_kernel_spmd(nc, maps, core_ids=list(range(B))).results
    return np.stack([res[b]["out"] for b in range(B)], axis=0).astype(np.float32)
```

```python
import math
import numpy as np
import ml_dtypes
import concourse.bass as bass
import concourse.mybir as mybir
from concourse.bass_utils import run_bass_kernel_spmd

F32 = mybir.dt.float32
BF16 = mybir.dt.bfloat16
I32 = mybir.dt.int32
AF = mybir.ActivationFunctionType
ALU = mybir.AluOpType
AX = mybir.AxisListType

D = 1024
S = 16384
NCORE = 8
TPC = 4096
IN_COLS = 2656
EPS = 1e-6
TWO_PI = 2.0 * math.pi

ENGS = ["sync", "scalar", "vector", "gpsimd", "tensor"]
SAME_ENGINE_SYNC = {"sync": False, "scalar": True, "vector": True, "gpsimd": True, "tensor": False}
_APT = None


class Buf:
    __slots__ = ("name", "w", "r")

    def __init__(self, name=""):
        self.name = name
        self.w = None
        self.r = []


class Prog:
    NPOOL = 16

    def __init__(self, nc):
        self.nc = nc
        self.ops = {e: [] for e in ENGS}
        self.cnt = {e: 0 for e in ENGS}
        self.esem = {e: nc.alloc_semaphore("es_" + e) for e in ENGS}
        self.known = {e: {f: 0 for f in ENGS} for e in ENGS}
        self.snap = {e: [None] for e in ENGS}
        self.dq = ["sync", "scalar", "gpsimd"]
        self.pool = {q: [nc.alloc_semaphore("dp_%s_%d" % (q, i)) for i in range(self.NPOOL)] for q in self.dq}
        self.pool_val = {q: [0] * self.NPOOL for q in self.dq}
        self.pool_next = {q: 0 for q in self.dq}
        self.dknown = {e: {} for e in ENGS}
        self.bufs = {}
        self.uid = 0
        self.prefix = ""

    def sb(self, name, shape, dt=F32):
        return self.nc.alloc_sbuf_tensor(self.prefix + name, list(shape), dt)

    def ps(self, name, shape, dt=F32):
        return self.nc.alloc_psum_tensor(self.prefix + name, list(shape), dt)

    def mark(self):
        nc = self.nc
        return (nc.psum_base, nc.psum_top, nc.sbuf_base, nc.sbuf_top)

    def release(self, mk):
        self.barrier()
        nc = self.nc
        nc.psum_base, nc.psum_top, nc.sbuf_base, nc.sbuf_top = mk

    def barrier(self):
        for e in ENGS:
            waits = []
            for f in ENGS:
                if f != e and self.cnt[f] > 0:
                    self._need(e, ("E", f, self.cnt[f]), waits)
            for q in self.dq:
                for i in range(self.NPOOL):
                    v = self.pool_val[q][i]
                    if v > 0:
                        self._need(e, ("D", q, i, v, None), waits)
            self.ops[e].append((waits, None, None, None, 0))
        self.bufs = {}

    def buf_of(self, ap):
        n = ap.name
        b = self.bufs.get(n)
        if b is None:
            b = self.bufs[n] = Buf(n)
        return b

    def _merge(self, eng, sn):
        if sn is None:
            return
        kn = self.known[eng]
        for g, v in sn[0].items():
            if kn[g] < v:
                kn[g] = v
        dk = self.dknown[eng]
        for k, v in sn[1].items():
            if dk.get(k, 0) < v:
                dk[k] = v

    def _need(self, eng, ev, waits):
        if ev is None:
            return
        if ev[0] == "E":
            _, f, seq = ev
            if f == eng and not SAME_ENGINE_SYNC[eng]:
                return
            if self.known[eng][f] >= seq:
                return
            waits.append((self.esem[f], seq))
            self.known[eng][f] = seq
            self._merge(eng, self.snap[f][seq])
        else:
            _, q, i, val, sn = ev
            if self.dknown[eng].get((q, i), 0) >= val:
                return
            waits.append((self.pool[q][i], val))
            self.dknown[eng][(q, i)] = val
            self._merge(eng, sn)

    def _deps(self, eng, reads, writes, waits):
        for b in reads:
            self._need(eng, b.w, waits)
        for b in writes:
            self._need(eng, b.w, waits)
            for ev in b.r:
                self._need(eng, ev, waits)

    def _commit(self, ev, reads, writes):
        for b in reads:
            if b in writes:
                continue
            b.r.append(ev)
            if len(b.r) > 16:
                last = {}
                keep = []
                for e in b.r:
                    if e[0] == "E":
                        last[e[1]] = e
                    else:
                        keep.append(e)
                b.r = keep[-10:] + list(last.values())
        for b in writes:
            b.w = ev
            b.r = []

    def _scan(self, kwargs):
        reads, writes = [], []
        for k, v in kwargs.items():
            if isinstance(v, _APT):
                b = self.buf_of(v)
                if k in ("out", "accum_out", "out_max", "out_indices"):
                    if b not in writes:
                        writes.append(b)
                elif b not in reads:
                    reads.append(b)
        return reads, writes

    def I(self, eng, meth, **kwargs):
        xr = kwargs.pop("_reads", ())
        xw = kwargs.pop("_writes", ())
        reads, writes = self._scan(kwargs)
        reads += [self.buf_of(a) for a in xr]
        writes += [self.buf_of(a) for a in xw]
        waits = []
        self._deps(eng, reads, writes, waits)
        self.cnt[eng] += 1
        seq = self.cnt[eng]
        self.snap[eng].append((dict(self.known[eng]), dict(self.dknown[eng])))
        self.ops[eng].append((waits, meth, kwargs, self.esem[eng], 1))
        ev = ("E", eng, seq)
        self._commit(ev, reads, writes)
        return ev

    def dma(self, q, out, in_, **kw):
        reads = [self.buf_of(in_)]
        writes = [self.buf_of(out)]
        waits = []
        self._deps(q, reads, writes, waits)
        i = self.pool_next[q]
        self.pool_next[q] = (i + 1) % self.NPOOL
        prev = self.pool_val[q][i]
        if prev > 0 and self.dknown[q].get((q, i), 0) < prev:
            waits.append((self.pool[q][i], prev))
            self.dknown[q][(q, i)] = prev
        val = prev + 16
        self.pool_val[q][i] = val
        sn = (dict(self.known[q]), dict(self.dknown[q]))
        kw = dict(kw)
        kw["out"] = out
        kw["in_"] = in_
        self.ops[q].append((waits, "dma_start", kw, self.pool[q][i], 16))
        ev = ("D", q, i, val, sn)
        self._commit(ev, reads, writes)
        return ev

    def coll(self, kind, ins, outs, groups):
        q = "gpsimd"
        reads = [self.buf_of(a) for a in ins]
        writes = [self.buf_of(a) for a in outs]
        waits = []
        self._deps(q, reads, writes, waits)
        i = self.pool_next[q]
        self.pool_next[q] = (i + 1) % self.NPOOL
        prev = self.pool_val[q][i]
        if prev > 0 and self.dknown[q].get((q, i), 0) < prev:
            waits.append((self.pool[q][i], prev))
            self.dknown[q][(q, i)] = prev
        val = prev + 1
        self.pool_val[q][i] = val
        sn = (dict(self.known[q]), dict(self.dknown[q]))
        kw = dict(kind=kind, op=ALU.bypass, replica_groups=groups, ins=[a_.opt() for a_ in ins], outs=[a_.opt() for a_ in outs])
        self.ops[q].append((waits, "collective_compute", kw, self.pool[q][i], 1))
        ev = ("D", q, i, val, sn)
        self._commit(ev, reads, writes)
        return ev

    def finish(self, aps, eng="sync"):
        waits = []
        for a in aps:
            self._need(eng, self.buf_of(a).w, waits)
        self.ops[eng].append((waits, None, None, None, 0))

    def emit(self):
        nc = self.nc
        with nc.Block() as block:
            def mk(ename):
                def body(e):
                    for waits, meth, kw, sem, inc in self.ops[ename]:
                        for (s, v) in waits:
                            e.wait_ge(s, v)
                        if meth is not None:
                            getattr(e, meth)(**kw).then_inc(sem, inc)
                return body
            block.sync(mk("sync"))
            block.scalar(mk("scalar"))
            block.vector(mk("vector"))
            block.gpsimd(mk("gpsimd"))
            block.tensor(mk("tensor"))


class Rot:
    def __init__(self, P, name, shape, dt, n, psum=False):
        self.t = [(P.ps if psum else P.sb)("%s%d" % (name, i), shape, dt) for i in range(n)]
        self.i = 0

    def next(self):
        t = self.t[self.i % len(self.t)]
        self.i += 1
        return t


def new_nc():
    global _APT
    nc = bass.Bass("TRN2", target_bir_lowering=False)
    if _APT is None:
        t = nc.dram_tensor("apt_probe", [2, 2], F32).ap()
        _APT = type(t)
    return nc


class IO:
    def __init__(self, nc, m=None):
        self.nc = nc
        self.m = m or {}
        self.outs = []

    def inp(self, name, shape, dt=F32):
        if name in self.m:
            return self.m[name]
        return din(self.nc, name, shape, dt)

    def out(self, name, shape, dt=F32):
        if name in self.m:
            return self.m[name]
        ap = dout(self.nc, name, shape, dt)
        self.outs.append(ap)
        return ap


def din(nc, name, shape, dt=F32):
    return nc.dram_tensor(name, list(shape), dt, kind="ExternalInput").ap()


def dout(nc, name, shape, dt=F32):
    return nc.dram_tensor(name, list(shape), dt, kind="ExternalOutput").ap()


def make_identities(P):
    idf = P.sb("ident_f", [128, 128], F32)
    idb = P.sb("ident_b", [128, 128], BF16)
    P.I("gpsimd", "memset", ap=idf[:], constant=1.0, _writes=[idf[:]])
    P.I("gpsimd", "affine_select", out=idf[:], in_=idf[:], pattern=[[1, 128]], compare_op=ALU.is_equal,
        fill=0.0, base=0, channel_multiplier=-1)
    P.I("vector", "tensor_copy", out=idb[:], in_=idf[:])
    return idf, idb


def compute_mod(P, c_ap, adaw_ap, adab_ap, psA, psB, c0, c1, CH=256):
    n = c1 - c0
    mod = P.sb("mod_bc", [128, n], F32)
    ccol = P.sb("ccol", [128, 8], F32)
    cbc = P.sb("cbc", [128, 8, 128], F32)
    P.dma("sync", mod[:], adab_ap[c0:c1].partition_broadcast(128))
    P.dma("sync", ccol[:], c_ap.rearrange("(k p) -> p k", p=128), allow_slow_non_contiguous=True)
    P.I("scalar", "activation", out=ccol[:], in_=ccol[:], func=AF.Silu)
    P.I("vector", "tensor_copy", out=cbc[:], in_=ccol[:].unsqueeze(2).to_broadcast([128, 8, 128]))
    wr = Rot(P, "adaw_t", [128, 8, CH], F32, 2)
    awv = adaw_ap.rearrange("(k p) n -> p k n", p=128)
    for i in range(n // CH):
        wt = wr.next()
        P.dma("sync" if i % 2 == 0 else "scalar", wt[:], awv[:, :, c0 + i * CH:c0 + (i + 1) * CH])
        ps = psA if i % 2 == 0 else psB
        for k in range(8):
            P.I("tensor", "matmul", out=ps[:, 0:CH], lhsT=cbc[:, k, :], rhs=wt[:, k, :], start=(k == 0), stop=(k == 7))
        P.I("vector", "tensor_tensor", out=mod[:, i * CH:(i + 1) * CH], in0=mod[:, i * CH:(i + 1) * CH],
            in1=ps[:, 0:CH], op=ALU.add)
    return mod


def sin_of(P, out_ap, ang_ap, tmp_ap, tmpi_ap, shift):
    P.I("vector", "tensor_scalar", out=tmp_ap, in0=ang_ap, scalar1=1.0 / TWO_PI, scalar2=shift / TWO_PI, op0=ALU.mult, op1=ALU.add)
    P.I("vector", "tensor_copy", out=tmpi_ap, in_=tmp_ap)
    P.I("vector", "tensor_tensor", out=tmp_ap, in0=tmp_ap, in1=tmpi_ap, op=ALU.subtract)
    P.I("scalar", "activation", out=out_ap, in_=tmp_ap, func=AF.Sin, scale=TWO_PI)


def build_p1(ntok=TPC):
    nc = new_nc()
    P = Prog(nc)
    io = IO(nc)
    phase1(P, io, ntok)
    P.finish(io.outs)
    P.emit()
    return nc


def phase1(P, io, ntok):
    nc = P.nc
    if hasattr(P, "mla_tiles"):
        del P.mla_tiles
    NST = ntok // 512
    x_in = io.inp("x_in", [ntok, D])
    c_in = io.inp("c_in", [D])
    pos_in = io.inp("pos_in", [ntok], I32)
    adaw = io.inp("ada_w", [D, 6 * D])
    adab = io.inp("ada_b", [6 * D])
    gmix = io.inp("norm_mix_g", [D])
    w_in = io.inp("w_in", [D, IN_COLS])
    qng = io.inp("mla_q_norm_g", [192])
    wuq = io.inp("mla_w_uq", [192, 384])
    kvng = io.inp("mla_kv_norm_g", [128])
    wukv = io.inp("mla_w_ukv", [128, 512])
    qqk = io.inp("mla_q_qk_g", [96])
    kqk = io.inp("mla_k_qk_g", [96])
    inv_ret4 = io.inp("inv_ret4", [128, 1])
    inv_mla = io.inp("inv_mla", [16])
    attq = io.out("attq", [4, 96, ntok], BF16)
    attk = io.out("attk", [4, 96, ntok], BF16)
    attv = io.out("attv", [4, ntok, 64], BF16)
    retq = io.out("retq", [256, ntok], BF16)
    retk = io.out("retk", [256, ntok], BF16)
    retv = io.out("retv", [ntok, 256], BF16)
    lrux = io.out("lrux", [256, ntok], F32)
    cvx = io.out("cvx", [256, ntok], F32)
    bgate = io.out("bgate", [256, ntok], F32)
    sgate = io.out("sgate", [256, ntok], F32)
    ggate = io.out("ggate", [256, ntok], F32)

    psT = [P.ps("psT%d" % i, [128, 1024], BF16) for i in range(2)]
    psF = [P.ps("psF%d" % i, [128, 512], F32) for i in range(3)]
    psU = P.ps("psU", [128, 512], F32)
    psQ = P.ps("psQ", [128, 512], F32)
    psX = P.ps("psX", [128, 1024], BF16)
    fi = [0]

    def nextF():
        p = psF[fi[0] % 3]
        fi[0] += 1
        return p

    idf, idb = make_identities(P)
    P.negpi = P.sb("negpi", [128, 1], F32)
    P.I("vector", "memset", ap=P.negpi[:], constant=-math.pi, _writes=[P.negpi[:]])
    neghalf = P.sb("neghalf", [128, 16], F32)
    P.I("vector", "memset", ap=neghalf[:], constant=-0.5, _writes=[neghalf[:]])

    mod = compute_mod(P, c_in, adaw, adab, psF[0], psF[1], 0, 2 * D)
    gm = P.sb("gm", [128, D], F32)
    P.dma("sync", gm[:], gmix.partition_broadcast(128))
    P.I("vector", "scalar_tensor_tensor", out=gm[:], in0=mod[:, D:2 * D], scalar=1.0, in1=gm[:], op0=ALU.add, op1=ALU.mult)
    shm = mod[:, 0:D]

    w_bf = P.sb("w_bf", [128, 8, IN_COLS], BF16)
    wv = w_in.rearrange("(k p) n -> p k n", p=128)
    for k in range(8):
        for hh in range(2):
            P.dma("gpsimd", w_bf[:, k, hh * 1328:(hh + 1) * 1328], wv[:, k, hh * 1328:(hh + 1) * 1328])
    w_rot = P.sb("w_rot", [128, 8, 512], BF16)
    for k in range(8):
        src = w_bf[:, k, 1120:1632].rearrange("p (h two i) -> p h two i", two=2, i=32)
        dst = w_rot[:, k, :].rearrange("p (h two i) -> p h two i", two=2, i=32)
        P.I("vector", "tensor_scalar", out=dst[:, :, 0, :], in0=src[:, :, 1, :], scalar1=-1.0, scalar2=None, op0=ALU.mult)
        P.I("vector", "tensor_copy", out=dst[:, :, 1, :], in_=src[:, :, 0, :])
    wuq_bf = P.sb("wuq_bf", [128, 2, 384], BF16)
    P.dma("gpsimd", wuq_bf[:, 0, :], wuq[0:128, :])
    P.dma("gpsimd", wuq_bf[0:64, 1, :], wuq[128:192, :])
    wukv_bf = P.sb("wukv_bf", [128, 512], BF16)
    P.dma("gpsimd", wukv_bf[:], wukv)
    qng_bc = P.sb("qng_bc", [128, 192], F32)
    kvng_bc = P.sb("kvng_bc", [128, 128], F32)
    qqk_bc = P.sb("qqk_bc", [128, 96], F32)
    kqk_bc = P.sb("kqk_bc", [128, 96], F32)
    P.dma("sync", qng_bc[:], qng.partition_broadcast(128))
    P.dma("sync", kvng_bc[:], kvng.partition_broadcast(128))
    P.dma("sync", qqk_bc[:], qqk.partition_broadcast(128))
    P.dma("sync", kqk_bc[:], kqk.partition_broadcast(128))

    invc = P.sb("invc", [128, 1], F32)
    P.dma("sync", invc[:], inv_ret4)
    posi = P.sb("posi", [128, 512], I32)
    angt = P.sb("angt", [128, 512], F32)
    tmpa = P.sb("tmpa", [128, 512], F32)
    tmpai = P.sb("tmpai", [128, 512], I32)
    cos_r = Rot(P, "cosR", [128, 512], F32, 2)
    sin_r = Rot(P, "sinR", [128, 512], F32, 2)
    invm = P.sb("invm", [128, 16], F32)
    P.dma("sync", invm[:], inv_mla.partition_broadcast(128))
    posc_i = P.sb("posc_i", [128, 4], I32)
    posc = P.sb("posc", [128, 4], F32)
    angm = P.sb("angm", [128, 4, 16], F32)
    tmpm = P.sb("tmpm", [128, 4, 16], F32)
    tmpmi = P.sb("tmpmi", [128, 4, 16], I32)
    cosM_r = Rot(P, "cosM", [128, 4, 16], F32, 2)
    sinM_r = Rot(P, "sinM", [128, 4, 16], F32, 2)

    xt_r = Rot(P, "xt", [128, D], F32, 3)
    junk = P.sb("junk", [128, D], BF16)
    ssq = Rot(P, "ssq", [128, 4], F32, 2)
    v4 = Rot(P, "v4", [128, 4], F32, 2)
    rstd4 = Rot(P, "rstd4", [128, 4], F32, 2)
    tmp_r = Rot(P, "tmpx", [128, D], F32, 1)
    hb_r = Rot(P, "hb", [128, D], BF16, 2)
    hT_r = Rot(P, "hT", [128, 8, 512], BF16, 2)
    ev_r = Rot(P, "ev", [128, 512], F32, 4)
    evb_r = Rot(P, "evb", [128, 512], BF16, 4)
    csb_r = Rot(P, "csb", [128, 512], F32, 2)

    for st in range(NST):
        t0 = st * 512
        hT = hT_r.next()
        for j in range(4):
            xt = xt_r.next()
            P.dma("sync", xt[:], x_in[t0 + j * 128:t0 + (j + 1) * 128, :])
            sq = ssq.next()
            P.I("scalar", "activation", out=junk[:], in_=xt[:], func=AF.Square, accum_out=sq[:, 0:1])
            v = v4.next()
            rs = rstd4.next()
            P.I("vector", "tensor_scalar", out=v[:, 0:1], in0=sq[:, 0:1], scalar1=1.0 / D, scalar2=EPS, op0=ALU.mult, op1=ALU.add)
            P.I("gpsimd", "tensor_tensor", out=rs[:, 0:1], in0=v[:, 0:1], in1=neghalf[:, 0:1], op=ALU.pow)
            tm = tmp_r.next()
            hb = hb_r.next()
            P.I("vector", "scalar_tensor_tensor", out=tm[:], in0=xt[:], scalar=rs[:, 0:1], in1=gm[:],
                op0=ALU.mult, op1=ALU.mult)
            P.I("vector", "tensor_tensor", out=hb[:], in0=tm[:], in1=shm, op=ALU.add)
            pt = psT[j % 2]
            for k in range(8):
                P.I("tensor", "transpose", out=pt[:, k * 128:(k + 1) * 128], in_=hb[:, k * 128:(k + 1) * 128], identity=idb[:])
            P.I("scalar", "copy", out=hT[:, :, j * 128:(j + 1) * 128], in_=pt[:].rearrange("p (k t) -> p k t", k=8))
        cosR = cos_r.next()
        sinR = sin_r.next()
        P.dma("scalar", posi[:], pos_in[t0:t0 + 512].partition_broadcast(128))
        P.I("vector", "tensor_copy", out=angt[:], in_=posi[:])
        P.I("vector", "tensor_scalar", out=angt[:], in0=angt[:], scalar1=invc[:, 0:1], scalar2=None, op0=ALU.mult)
        sin_of(P, sinR[:], angt[:], tmpa[:], tmpai[:], 0.0)
        sin_of(P, cosR[:], angt[:], tmpa[:], tmpai[:], 0.5 * math.pi)
        cosM = cosM_r.next()
        sinM = sinM_r.next()
        P.dma("scalar", posc_i[:], pos_in[t0:t0 + 512].rearrange("(n p) -> p n", p=128), allow_slow_non_contiguous=True)
        P.I("vector", "tensor_copy", out=posc[:], in_=posc_i[:])
        P.I("vector", "tensor_tensor", out=angm[:], in0=posc[:].unsqueeze(2).to_broadcast([128, 4, 16]),
            in1=invm[:].unsqueeze(1).to_broadcast([128, 4, 16]), op=ALU.mult)
        sin_of(P, sinM[:], angm[:], tmpm[:], tmpmi[:], 0.0)
        sin_of(P, cosM[:], angm[:], tmpm[:], tmpmi[:], 0.5 * math.pi)

        def fm(wt, c0):
            ps = nextF()
            for k in range(8):
                P.I("tensor", "matmul", out=ps[:], lhsT=wt[:, k, c0:c0 + 128], rhs=hT[:, k, :], start=(k == 0), stop=(k == 7))
            return ps

        for ch in range(2):
            ps = fm(w_bf, ch * 128)
            e = ev_r.next()
            P.I("scalar", "copy", out=e[:], in_=ps[:])
            P.dma("sync", bgate[ch * 128:(ch + 1) * 128, t0:t0 + 512], e[:])
            psc = fm(w_bf, 256 + ch * 128)
            cs = csb_r.next()
            P.I("scalar", "copy", out=cs[:], in_=psc[:])
            psx = fm(w_bf, 512 + ch * 128)
            e = ev_r.next()
            P.I("vector", "tensor_tensor", out=e[:], in0=psx[:], in1=cs[:], op=ALU.mult)
            P.dma("sync", cvx[ch * 128:(ch + 1) * 128, t0:t0 + 512], e[:])
        for qk in range(2):
            for ch in range(2):
                c0 = 1120 + qk * 256 + ch * 128
                ps = fm(w_bf, c0)
                cs = csb_r.next()
                P.I("vector", "scalar_tensor_tensor", out=cs[:], in0=ps[:], scalar=(1.0 if qk == 0 else 0.125),
                    in1=cosR[:], op0=ALU.mult, op1=ALU.mult)
                psr = fm(w_rot, qk * 256 + ch * 128)
                e = ev_r.next()
                P.I("vector", "scalar_tensor_tensor", out=e[:], in0=psr[:], scalar=(1.0 if qk == 0 else 0.125),
                    in1=sinR[:], op0=ALU.mult, op1=ALU.mult)
                eb = evb_r.next()
                P.I("vector", "tensor_tensor", out=eb[:], in0=e[:], in1=cs[:], op=ALU.add)
                P.dma("sync", (retq if qk == 0 else retk)[ch * 128:(ch + 1) * 128, t0:t0 + 512], eb[:])
        for ch in range(2):
            ps = fm(w_bf, 1120 + 768 + ch * 128)
            e = ev_r.next()
            P.I("scalar", "activation", out=e[:], in_=ps[:], func=AF.Silu)
            P.dma("sync", sgate[ch * 128:(ch + 1) * 128, t0:t0 + 512], e[:])
        for ch in range(2):
            ps = fm(w_bf, 2144 + ch * 128)
            e = ev_r.next()
            P.I("scalar", "copy", out=e[:], in_=ps[:])
            P.dma("sync", lrux[ch * 128:(ch + 1) * 128, t0:t0 + 512], e[:])
        for ch in range(2):
            ps = fm(w_bf, 2144 + 256 + ch * 128)
            e = ev_r.next()
            P.I("scalar", "activation", out=e[:], in_=ps[:], func=AF.Gelu)
            P.dma("sync", ggate[ch * 128:(ch + 1) * 128, t0:t0 + 512], e[:])
        for j in range(4):
            tt = t0 + j * 128
            ti = tt // 128
            hTj = hT[:, :, j * 128:(j + 1) * 128]
            ps = nextF()
            for k in range(8):
                P.I("tensor", "matmul", out=ps[:, 0:256], lhsT=hT[:, k, j * 128:(j + 1) * 128], rhs=w_bf[:, k, 1632:1888],
                    start=(k == 0), stop=(k == 7))
            eb = evb_r.next()
            P.I("scalar", "copy", out=eb[:, 0:256], in_=ps[:, 0:256])
            P.dma("sync", retv[tt:tt + 128, :], eb[:, 0:256])
            mla_tile(P, locals(), tt, j, j)


def mla_tile(P, L, tt, ti, j):
    hT, w_bf, psU, psQ, psX = L["hT"], L["w_bf"], L["psU"], L["psQ"], L["psX"]
    idb, neghalf = L["idb"], L["neghalf"]
    qng_bc, kvng_bc, qqk_bc, kqk_bc = L["qng_bc"], L["kvng_bc"], L["qqk_bc"], L["kqk_bc"]
    wuq_bf, wukv_bf, cosM, sinM = L["wuq_bf"], L["wukv_bf"], L["cosM"], L["sinM"]
    attq, attk, attv = L["attq"], L["attk"], L["attv"]
    if not hasattr(P, "mla_tiles"):
        P.mla_tiles = dict(
            junk=P.sb("mjunk", [128, 512], F32),
            st3=Rot(P, "mst3", [128, 16], F32, 2),
            rs3=Rot(P, "mrs3", [128, 16], F32, 2),
            cqn=Rot(P, "mcqn", [128, 320], BF16, 2),
            cT=Rot(P, "mcT", [128, 384], BF16, 2),
            qsb=Rot(P, "mqsb", [128, 384], F32, 2),
            kvsb=Rot(P, "mkvsb", [128, 512], F32, 2),
            sq=Rot(P, "msq", [128, 512], F32, 4),
            Qt=Rot(P, "mQt", [128, 4, 96], BF16, 2),
            Kt=Rot(P, "mKt", [128, 4, 96], BF16, 2),
            Vt=Rot(P, "mVt", [128, 4, 64], BF16, 2),
            r1=Rot(P, "mr1", [128, 4, 32], F32, 2),
            r2=Rot(P, "mr2", [128, 4, 16], F32, 4),
            kr=Rot(P, "mkr", [128, 32], F32, 2),
            kr2=Rot(P, "mkr2", [128, 32], F32, 2),
            QT=Rot(P, "mQT", [96, 4, 128], BF16, 2),
            KT=Rot(P, "mKT", [96, 4, 128], BF16, 2),
            scl=P.sb("mscl", [128, 3], F32),
        )
        sc = P.mla_tiles["scl"]
        P.I("vector", "memset", ap=sc[:, 0:1], constant=1.0 / 192, _writes=[sc[:]])
        P.I("vector", "memset", ap=sc[:, 1:2], constant=1.0 / 128, _writes=[sc[:]])
        P.I("vector", "memset", ap=sc[:, 2:3], constant=1.0 / 32, _writes=[sc[:]])
    M = P.mla_tiles
    for k in range(8):
        P.I("tensor", "matmul", out=psU[:, 0:352], lhsT=hT[:, k, j * 128:(j + 1) * 128], rhs=w_bf[:, k, 768:1120],
            start=(k == 0), stop=(k == 7))
    st3 = M["st3"].next()
    rs3 = M["rs3"].next()
    P.I("scalar", "activation", out=M["junk"][:, 0:192], in_=psU[:, 0:192], func=AF.Square, accum_out=st3[:, 0:1])
    P.I("scalar", "activation", out=M["junk"][:, 0:128], in_=psU[:, 192:320], func=AF.Square, accum_out=st3[:, 1:2])
    P.I("scalar", "activation", out=M["junk"][:, 0:32], in_=psU[:, 320:352], func=AF.Square, accum_out=st3[:, 2:3])
    P.I("vector", "tensor_tensor", out=st3[:, 0:3], in0=st3[:, 0:3], in1=M["scl"][:], op=ALU.mult)
    P.I("vector", "tensor_scalar", out=st3[:, 0:3], in0=st3[:, 0:3], scalar1=EPS, scalar2=None, op0=ALU.add)
    P.I("gpsimd", "tensor_tensor", out=rs3[:, 0:3], in0=st3[:, 0:3], in1=neghalf[:, 0:3], op=ALU.pow)
    cqn = M["cqn"].next()
    P.I("vector", "scalar_tensor_tensor", out=cqn[:, 0:192], in0=psU[:, 0:192], scalar=rs3[:, 0:1], in1=qng_bc[:],
        op0=ALU.mult, op1=ALU.mult)
    P.I("vector", "scalar_tensor_tensor", out=cqn[:, 192:320], in0=psU[:, 192:320], scalar=rs3[:, 1:2], in1=kvng_bc[:],
        op0=ALU.mult, op1=ALU.mult)
    kr = M["kr"].next()
    P.I("vector", "scalar_tensor_tensor", out=kr[:], in0=psU[:, 320:352], scalar=rs3[:, 2:3], in1=kqk_bc[:, 64:96],
        op0=ALU.mult, op1=ALU.mult)
    P.I("tensor", "transpose", out=psX[:, 0:128], in_=cqn[:, 0:128], identity=idb[:])
    P.I("tensor", "transpose", out=psX[0:64, 128:256], in_=cqn[:, 128:192], identity=idb[:])
    P.I("tensor", "transpose", out=psX[:, 256:384], in_=cqn[:, 192:320], identity=idb[:])
    cT = M["cT"].next()
    P.I("scalar", "copy", out=cT[:, 0:128], in_=psX[:, 0:128])
    P.I("scalar", "copy", out=cT[0:64, 128:256], in_=psX[0:64, 128:256])
    P.I("scalar", "copy", out=cT[:, 256:384], in_=psX[:, 256:384])
    P.I("tensor", "matmul", out=psQ[:, 0:384], lhsT=cT[:, 0:128], rhs=wuq_bf[:, 0, :], start=True, stop=False)
    P.I("tensor", "matmul", out=psQ[:, 0:384], lhsT=cT[0:64, 128:256], rhs=wuq_bf[0:64, 1, :], start=False, stop=True)
    qsb = M["qsb"].next()
    sq = M["sq"].next()
    P.I("scalar", "copy", out=qsb[:], in_=psQ[:, 0:384])
    P.I("scalar", "activation", out=sq[:, 0:384], in_=psQ[:, 0:384], func=AF.Square)
    P.I("tensor", "matmul", out=psQ[:, 0:512], lhsT=cT[:, 256:384], rhs=wukv_bf[:], start=True, stop=True)
    kvsb = M["kvsb"].next()
    P.I("scalar", "copy", out=kvsb[:], in_=psQ[:, 0:512])
    st8 = M["st3"].next()
    rs8 = M["rs3"].next()
    sqv = sq[:, 0:384].rearrange("p (h c) -> p h c", c=96)
    P.I("vector", "tensor_reduce", out=st8[:, 0:4], in_=sqv[:, :, 0:64], axis=AX.X, op=ALU.add)
    P.I("vector", "tensor_reduce", out=st8[:, 4:8], in_=sqv[:, :, 64:96], axis=AX.X, op=ALU.add)
    sq2 = M["sq"].next()
    P.I("scalar", "activation", out=sq2[:], in_=kvsb[:], func=AF.Square)
    P.I("vector", "tensor_reduce", out=st8[:, 8:12], in_=sq2[:].rearrange("p (h c) -> p h c", c=128)[:, :, 0:64],
        axis=AX.X, op=ALU.add)
    P.I("vector", "tensor_scalar", out=st8[:, 0:4], in0=st8[:, 0:4], scalar1=1.0 / 64, scalar2=EPS, op0=ALU.mult, op1=ALU.add)
    P.I("vector", "tensor_scalar", out=st8[:, 4:8], in0=st8[:, 4:8], scalar1=1.0 / 32, scalar2=EPS, op0=ALU.mult, op1=ALU.add)
    P.I("vector", "tensor_scalar", out=st8[:, 8:12], in0=st8[:, 8:12], scalar1=1.0 / 64, scalar2=EPS, op0=ALU.mult, op1=ALU.add)
    P.I("gpsimd", "tensor_tensor", out=rs8[:, 0:12], in0=st8[:, 0:12], in1=neghalf[:, 0:12], op=ALU.pow)
    Qt = M["Qt"].next()
    Kt = M["Kt"].next()
    Vt = M["Vt"].next()
    qv = qsb[:].rearrange("p (h c) -> p h c", c=96)
    kvv = kvsb[:].rearrange("p (h c) -> p h c", c=128)
    r1 = M["r1"].next()
    tq = M["sq"].next()
    tqv = tq[:, 0:256].rearrange("p (h c) -> p h c", c=64)
    P.I("vector", "tensor_tensor", out=tqv, in0=qv[:, :, 0:64], in1=rs8[:, 0:4].unsqueeze(2).to_broadcast([128, 4, 64]), op=ALU.mult)
    P.I("vector", "tensor_tensor", out=Qt[:, :, 0:64], in0=tqv, in1=qqk_bc[:, 0:64].unsqueeze(1).to_broadcast([128, 4, 64]), op=ALU.mult)
    P.I("vector", "tensor_tensor", out=r1[:], in0=qv[:, :, 64:96], in1=rs8[:, 4:8].unsqueeze(2).to_broadcast([128, 4, 32]), op=ALU.mult)
    P.I("vector", "tensor_tensor", out=r1[:], in0=r1[:], in1=qqk_bc[:, 64:96].unsqueeze(1).to_broadcast([128, 4, 32]), op=ALU.mult)
    cb = cosM[:, ti, :].unsqueeze(1).to_broadcast([128, 4, 16])
    sb_ = sinM[:, ti, :].unsqueeze(1).to_broadcast([128, 4, 16])
    a1, a2, a3, a4 = M["r2"].next(), M["r2"].next(), M["r2"].next(), M["r2"].next()
    P.I("vector", "tensor_tensor", out=a1[:], in0=r1[:, :, 0:16], in1=cb, op=ALU.mult)
    P.I("vector", "tensor_tensor", out=a2[:], in0=r1[:, :, 16:32], in1=sb_, op=ALU.mult)
    P.I("vector", "tensor_tensor", out=a3[:], in0=r1[:, :, 16:32], in1=cb, op=ALU.mult)
    P.I("vector", "tensor_tensor", out=a4[:], in0=r1[:, :, 0:16], in1=sb_, op=ALU.mult)
    P.I("vector", "tensor_tensor", out=Qt[:, :, 64:80], in0=a1[:], in1=a2[:], op=ALU.subtract)
    P.I("vector", "tensor_tensor", out=Qt[:, :, 80:96], in0=a3[:], in1=a4[:], op=ALU.add)
    tk = M["sq"].next()
    tkv = tk[:, 0:256].rearrange("p (h c) -> p h c", c=64)
    P.I("vector", "tensor_tensor", out=tkv, in0=kvv[:, :, 0:64], in1=rs8[:, 8:12].unsqueeze(2).to_broadcast([128, 4, 64]), op=ALU.mult)
    P.I("vector", "tensor_tensor", out=Kt[:, :, 0:64], in0=tkv, in1=kqk_bc[:, 0:64].unsqueeze(1).to_broadcast([128, 4, 64]), op=ALU.mult)
    kr2 = M["kr2"].next()
    b1, b2, b3, b4 = M["r2"].next(), M["r2"].next(), M["r2"].next(), M["r2"].next()
    P.I("vector", "tensor_tensor", out=b1[:, 0, :], in0=kr[:, 0:16], in1=cosM[:, ti, :], op=ALU.mult)
    P.I("vector", "tensor_tensor", out=b2[:, 0, :], in0=kr[:, 16:32], in1=sinM[:, ti, :], op=ALU.mult)
    P.I("vector", "tensor_tensor", out=b3[:, 0, :], in0=kr[:, 16:32], in1=cosM[:, ti, :], op=ALU.mult)
    P.I("vector", "tensor_tensor", out=b4[:, 0, :], in0=kr[:, 0:16], in1=sinM[:, ti, :], op=ALU.mult)
    P.I("vector", "tensor_tensor", out=kr2[:, 0:16], in0=b1[:, 0, :], in1=b2[:, 0, :], op=ALU.subtract)
    P.I("vector", "tensor_tensor", out=kr2[:, 16:32], in0=b3[:, 0, :], in1=b4[:, 0, :], op=ALU.add)
    P.I("vector", "tensor_copy", out=Kt[:, :, 64:96], in_=kr2[:].unsqueeze(1).to_broadcast([128, 4, 32]))
    P.I("vector", "tensor_copy", out=Vt[:], in_=kvv[:, :, 64:128])
    P.dma("sync", attv[:, tt:tt + 128, :].rearrange("h t c -> t h c"), Vt[:])
    for h in range(4):
        P.I("tensor", "transpose", out=psX[0:96, 384 + h * 128:384 + (h + 1) * 128], in_=Qt[:, h, :], identity=idb[:])
    QT = M["QT"].next()
    P.I("scalar", "copy", out=QT[:], in_=psX[0:96, 384:896].rearrange("p (h t) -> p h t", h=4))
    P.dma("sync", attq[:, :, tt:tt + 128].rearrange("h c t -> c h t"), QT[:])
    for h in range(4):
        P.I("tensor", "transpose", out=psX[0:96, 384 + h * 128:384 + (h + 1) * 128], in_=Kt[:, h, :], identity=idb[:])
    KT = M["KT"].next()
    P.I("scalar", "copy", out=KT[:], in_=psX[0:96, 384:896].rearrange("p (h t) -> p h t", h=4))
    P.dma("sync", attk[:, :, tt:tt + 128].rearrange("h c t -> c h t"), KT[:])


ATT_SCALE = 96.0 ** -0.5


def build_p2(SS=S):
    nc = new_nc()
    P = Prog(nc)
    io = IO(nc)
    phase2(P, io, SS)
    P.finish(io.outs)
    P.emit()
    return nc


def phase2(P, io, SS, after_setup=None):
    nc = P.nc
    NB = SS // 128
    aq = io.inp("aq", [96, SS], BF16)
    ak = io.inp("ak", [96, SS], BF16)
    av = io.inp("av", [SS, 64], BF16)
    rq = io.inp("rq", [64, SS], BF16)
    rk = io.inp("rk", [64, SS], BF16)
    rv = io.inp("rv", [SS, 64], BF16)
    lx = io.inp("lx", [64, SS])
    cx = io.inp("cx", [64, SS])
    convw = io.inp("convw", [64, 3])
    lcw = io.inp("lcw", [64, 4])
    lcb = io.inp("lcb", [64, 1])
    wa = io.inp("wa", [64, 64])
    ba = io.inp("ba", [64, 1])
    wx = io.inp("wx", [64, 64])
    bx = io.inp("bx", [64, 1])
    lam = io.inp("lam", [64, 1])
    qqk = io.inp("qqk", [96])
    kqk = io.inp("kqk", [96])
    innerT = io.inp("innerT", [128, 128])
    qdecT = io.inp("qdecT", [64, 128])
    kdec = io.inp("kdec", [128, 1])
    cdec = io.inp("cdec", [64, 1])
    ao = io.out("ao", [64, SS])
    ro = io.out("ro", [64, SS])
    lo = io.out("lo", [64, SS])
    co = io.out("co", [64, SS])

    psS = [P.ps("psS%d" % i, [128, 512], F32) for i in range(3)]
    psO = [P.ps("psO%d" % i, [128, 512], F32) for i in range(2)]
    psB = P.ps("psB", [128, 512], F32)
    psR = [P.ps("psR%d" % i, [128, 512], F32) for i in range(2)]
    psKT = psB[:].bitcast(BF16)

    idf, idb = make_identities(P)
    half = P.sb("half", [128, 512], F32)
    P.I("vector", "memset", ap=half[:], constant=0.5, _writes=[half[:]])
    neghalf = P.sb("neghalf", [128, 512], F32)
    P.I("vector", "memset", ap=neghalf[:], constant=-0.5, _writes=[neghalf[:]])
    ones64 = P.sb("ones64", [128, 64], F32)
    P.I("vector", "memset", ap=ones64[:], constant=1.0 / 64, _writes=[ones64[:]])
    ones1 = P.sb("ones1", [128, 64], F32)
    P.I("vector", "memset", ap=ones1[:], constant=1.0, _writes=[ones1[:]])
    epsc = P.sb("epsc", [128, 1], F32)
    P.I("vector", "memset", ap=epsc[:], constant=EPS, _writes=[epsc[:]])

    cw = P.sb("cw", [64, 3], F32)
    P.dma("sync", cw[:], convw, allow_slow_non_contiguous=True)
    CSEG = min(2048, SS)
    cxt = P.sb("cxt", [64, 2 + CSEG], F32)
    cacc = Rot(P, "cacc", [64, CSEG], F32, 2)
    P.I("vector", "memset", ap=cxt[:, 0:2], constant=0.0, _writes=[cxt[:]])
    for sg in range(SS // CSEG):
        c0 = sg * CSEG
        if sg > 0:
            P.I("vector", "tensor_copy", out=cxt[:, 0:2], in_=cxt[:, CSEG:CSEG + 2])
        P.dma("sync", cxt[:, 2:2 + CSEG], cx[:, c0:c0 + CSEG])
        acc = cacc.next()
        P.I("vector", "tensor_scalar", out=acc[:], in0=cxt[:, 2:2 + CSEG], scalar1=cw[:, 2:3], scalar2=None, op0=ALU.mult)
        P.I("vector", "scalar_tensor_tensor", out=acc[:], in0=cxt[:, 1:1 + CSEG], scalar=cw[:, 1:2], in1=acc[:], op0=ALU.mult, op1=ALU.add)
        P.I("vector", "scalar_tensor_tensor", out=acc[:], in0=cxt[:, 0:CSEG], scalar=cw[:, 0:1], in1=acc[:], op0=ALU.mult, op1=ALU.add)
        P.dma("gpsimd", co[:, c0:c0 + CSEG], acc[:])

    lw = P.sb("lw", [64, 4], F32)
    lb = P.sb("lb", [64, 1], F32)
    bat = P.sb("bat", [64, 1], F32)
    bxt = P.sb("bxt", [64, 1], F32)
    lamt = P.sb("lamt", [64, 1], F32)
    nsp = P.sb("nsp", [64, 1], F32)
    wa_bf = P.sb("wa_bf", [64, 64], BF16)
    wx_bf = P.sb("wx_bf", [64, 64], BF16)
    P.dma("sync", lw[:], lcw, allow_slow_non_contiguous=True)
    P.dma("sync", lb[:], lcb, allow_slow_non_contiguous=True)
    P.dma("sync", bat[:], ba, allow_slow_non_contiguous=True)
    P.dma("sync", bxt[:], bx, allow_slow_non_contiguous=True)
    P.dma("sync", lamt[:], lam, allow_slow_non_contiguous=True)
    P.dma("gpsimd", wa_bf[:], wa)
    P.dma("gpsimd", wx_bf[:], wx)
    P.I("scalar", "activation", out=nsp[:], in_=lamt[:], func=AF.Exp, scale=-1.0)
    P.I("scalar", "activation", out=nsp[:], in_=nsp[:], func=AF.Ln, bias=ones1[0:64, 0:1], scale=1.0)
    P.I("vector", "tensor_scalar", out=nsp[:], in0=nsp[:], scalar1=-8.0, scalar2=None, op0=ALU.mult)
    LSEG = min(1024, SS)
    NHF = LSEG // 512
    lxt = P.sb("lxt", [64, 3 + LSEG], F32)
    P.I("vector", "memset", ap=lxt[:, 0:3], constant=0.0, _writes=[lxt[:]])
    xc_r = Rot(P, "xc", [64, LSEG], F32, 1)
    xcb_r = Rot(P, "xcb", [64, LSEG], BF16, 1)
    rg_r = Rot(P, "rg", [64, LSEG], F32, 1)
    ig_r = Rot(P, "ig", [64, LSEG], F32, 1)
    a_r = Rot(P, "la", [64, LSEG], F32, 1)
    a2_r = Rot(P, "la2", [64, LSEG], F32, 1)
    b_r = Rot(P, "lbin", [64, LSEG], F32, 1)
    h_r = Rot(P, "lh", [64, LSEG], F32, 2)
    gbanks = [psS[0], psS[1], psS[2], psO[0]]
    hprev = None
    lru_state = [None]

    def lru_gen():
      for sg in range(SS // LSEG):
        hprev = lru_state[0]
        c0 = sg * LSEG
        if sg > 0:
            P.I("vector", "tensor_copy", out=lxt[:, 0:3], in_=lxt[:, LSEG:LSEG + 3])
        P.dma("sync", lxt[:, 3:3 + LSEG], lx[:, c0:c0 + LSEG])
        xc = xc_r.next()
        P.I("vector", "tensor_scalar", out=xc[:], in0=lxt[:, 3:3 + LSEG], scalar1=lw[:, 3:4], scalar2=lb[:, 0:1], op0=ALU.mult, op1=ALU.add)
        for kk in range(3):
            P.I("vector", "scalar_tensor_tensor", out=xc[:], in0=lxt[:, kk:kk + LSEG], scalar=lw[:, kk:kk + 1], in1=xc[:], op0=ALU.mult, op1=ALU.add)
        xcb = xcb_r.next()
        P.I("vector", "tensor_copy", out=xcb[:], in_=xc[:])
        rg = rg_r.next()
        ig = ig_r.next()
        for hf in range(NHF):
            hs = slice(hf * 512, (hf + 1) * 512)
            P.I("tensor", "matmul", out=psR[0][0:64, :], lhsT=wa_bf[:], rhs=xcb[:, hs], start=True, stop=True)
            P.I("tensor", "matmul", out=psR[1][0:64, :], lhsT=wx_bf[:], rhs=xcb[:, hs], start=True, stop=True)
            P.I("scalar", "activation", out=rg[:, hs], in_=psR[0][0:64, :], func=AF.Sigmoid, bias=bat[:, 0:1], scale=1.0)
            P.I("scalar", "activation", out=ig[:, hs], in_=psR[1][0:64, :], func=AF.Sigmoid, bias=bxt[:, 0:1], scale=1.0)
        a = a_r.next()
        P.I("scalar", "activation", out=a[:], in_=rg[:], func=AF.Exp, scale=nsp[:, 0:1])
        a2 = a2_r.next()
        P.I("scalar", "activation", out=a2[:], in_=a[:], func=AF.Square)
        P.I("scalar", "activation", out=a2[:], in_=a2[:], func=AF.Sqrt, bias=ones1[0:64, 0:1], scale=-1.0)
        bb = b_r.next()
        P.I("vector", "tensor_tensor", out=bb[:], in0=ig[:], in1=xc[:], op=ALU.mult)
        P.I("vector", "tensor_tensor", out=bb[:], in0=bb[:], in1=a2[:], op=ALU.mult)
        h = h_r.next()
        P.I("vector", "tensor_tensor_scan", out=h[:], data0=a[:], data1=bb[:],
            initial=(0.0 if hprev is None else hprev[:, LSEG - 1:LSEG]), op0=ALU.mult, op1=ALU.add)
        lru_state[0] = h
        P.dma("gpsimd", lo[:, c0:c0 + LSEG], h[:])
        yield

    inn = P.sb("inn", [128, 128], F32)
    qd_c = P.sb("qd_c", [64, 512], F32)
    kd_c = P.sb("kd_c", [128, 1], F32)
    cd_c = P.sb("cd_c", [64, 1], F32)
    P.dma("sync", inn[:], innerT)
    for r_ in range(4):
        P.dma("sync", qd_c[:, r_ * 128:(r_ + 1) * 128], qdecT)
    P.dma("sync", kd_c[:], kdec)
    P.dma("sync", cd_c[:], cdec)
    PC = min(32, NB)
    RG = 4
    KVa = P.sb("KVa", [64, PC, 64], F32)
    Sbf = P.sb("Sbf", [64, PC, 64], BF16)
    gam = P.sb("gam", [64, PC], F32)
    carry = P.sb("rcarry", [64, 64], F32)
    P.I("vector", "memset", ap=carry[:], constant=0.0, _writes=[carry[:]])
    P.I("vector", "memset", ap=gam[:], constant=1.0, _writes=[gam[:]])
    P.I("vector", "tensor_scalar", out=gam[:], in0=gam[:], scalar1=cd_c[:, 0:1], scalar2=None, op0=ALU.mult)
    rq_r = Rot(P, "rq_t", [64, RG * 128], BF16, 2)
    rk_r = Rot(P, "rk_t", [64, RG * 128], BF16, 2)
    rv_r = Rot(P, "rv_t", [128, RG, 64], BF16, 2)
    scm_r = Rot(P, "scm", [128, 128], BF16, 3)
    kd_r = Rot(P, "kdt", [128, 64], BF16, 3)
    qd_r = Rot(P, "qdt", [64, RG * 128], BF16, 2)
    oT_r = Rot(P, "oT", [64, RG * 128], F32, 2)
    cen_r = Rot(P, "cen", [64, RG * 128], F32, 2)
    sqr_r = Rot(P, "sqr", [64, RG * 128], F32, 2)
    def ret_gen():
      for pc in range(NB // PC):
        c0 = pc * PC
        for g in range(PC // RG):
            t0 = (c0 + g * RG) * 128
            rkt, rvt = rk_r.next(), rv_r.next()
            P.dma("sync", rkt[:], rk[:, t0:t0 + RG * 128])
            P.dma("sync", rvt[:], rv[t0:t0 + RG * 128, :].rearrange("(n p) c -> p n c", p=128))
            for ci in range(RG):
                i = g * RG + ci
                cs = slice(ci * 128, (ci + 1) * 128)
                pT = psS[i % 3][:].bitcast(BF16)
                P.I("tensor", "transpose", out=pT[:, 0:64], in_=rkt[:, cs], identity=idb[0:64, 0:64])
                kdt = kd_r.next()
                P.I("scalar", "activation", out=kdt[:], in_=pT[:, 0:64], func=AF.Copy, scale=kd_c[:, 0:1])
                pK = psO[i % 2]
                P.I("tensor", "matmul", out=pK[0:64, 0:64], lhsT=kdt[:], rhs=rvt[:, ci, :], start=True, stop=True)
                P.I("vector", "tensor_copy", out=KVa[:, i, :], in_=pK[0:64, 0:64])
            yield
        for dv in range(64):
            P.I("vector", "tensor_tensor_scan", out=KVa[:, :, dv], data0=gam[:], data1=KVa[:, :, dv], initial=carry[:, dv:dv + 1],
                op0=ALU.mult, op1=ALU.add)
        P.I("scalar", "copy", out=Sbf[:, 0, :], in_=carry[:])
        if PC > 1:
            P.I("scalar", "copy", out=Sbf[:, 1:PC, :], in_=KVa[:, 0:PC - 1, :])
        P.I("vector", "tensor_copy", out=carry[:], in_=KVa[:, PC - 1, :])
        for g in range(PC // RG):
            t0 = (c0 + g * RG) * 128
            rqt, rkt, rvt = rq_r.next(), rk_r.next(), rv_r.next()
            P.dma("sync", rqt[:], rq[:, t0:t0 + RG * 128])
            P.dma("sync", rkt[:], rk[:, t0:t0 + RG * 128])
            P.dma("sync", rvt[:], rv[t0:t0 + RG * 128, :].rearrange("(n p) c -> p n c", p=128))
            qdt = qd_r.next()
            P.I("vector", "tensor_tensor", out=qdt[:], in0=rqt[:], in1=qd_c[:], op=ALU.mult)
            oT = oT_r.next()
            for ci in range(RG):
                i = g * RG + ci
                cs = slice(ci * 128, (ci + 1) * 128)
                pA = psS[i % 3]
                P.I("tensor", "matmul", out=pA[:, 0:128], lhsT=rkt[:, cs], rhs=rqt[:, cs], start=True, stop=True)
                scm = scm_r.next()
                P.I("vector", "tensor_tensor", out=scm[:], in0=pA[:, 0:128], in1=inn[:], op=ALU.mult)
                pBk = psO[i % 2]
                P.I("tensor", "matmul", out=pBk[0:64, 0:128], lhsT=rvt[:, ci, :], rhs=scm[:], start=True, stop=False)
                P.I("tensor", "matmul", out=pBk[0:64, 0:128], lhsT=Sbf[:, i, :], rhs=qdt[:, cs], start=False, stop=True)
                P.I("scalar", "copy", out=oT[:, cs], in_=pBk[0:64, 0:128])
            W = RG * 128
            P.I("tensor", "matmul", out=psB[0:64, 0:W], lhsT=ones64[0:64, :], rhs=oT[:], start=True, stop=True)
            cen = cen_r.next()
            P.I("vector", "tensor_tensor", out=cen[:], in0=oT[:], in1=psB[0:64, 0:W], op=ALU.subtract)
            sqr = sqr_r.next()
            P.I("scalar", "activation", out=sqr[:], in_=cen[:], func=AF.Square)
            P.I("tensor", "matmul", out=psB[0:64, 0:W], lhsT=ones64[0:64, :], rhs=sqr[:], start=True, stop=True)
            P.I("scalar", "activation", out=sqr[:], in_=psB[0:64, 0:W], func=AF.Sqrt, bias=epsc[0:64, 0:1], scale=1.0)
            P.I("vector", "reciprocal", out=sqr[:], in_=sqr[:])
            P.I("vector", "tensor_tensor", out=cen[:], in0=cen[:], in1=sqr[:], op=ALU.mult)
            P.dma("gpsimd", ro[:, t0:t0 + W], cen[:])
            yield

    g_l, g_r = lru_gen(), ret_gen()
    n_l = SS // LSEG
    n_r = (NB // PC) * 2 * (PC // RG)
    per = max(1, n_r // max(1, n_l))
    done_l = done_r = False
    while not (done_l and done_r):
        if not done_l:
            try:
                next(g_l)
            except StopIteration:
                done_l = True
        for _ in range(per):
            if not done_r:
                try:
                    next(g_r)
                except StopIteration:
                    done_r = True
    QT = P.sb("QT", [96, SS], BF16)
    KT = P.sb("KT", [96, SS], BF16)
    VA = P.sb("VA", [128, NB, 65], BF16)
    LDC = min(2048, SS)
    for i in range(SS // LDC):
        P.dma("sync", KT[:, i * LDC:(i + 1) * LDC], ak[:, i * LDC:(i + 1) * LDC])
        P.dma("scalar", QT[:, i * LDC:(i + 1) * LDC], aq[:, i * LDC:(i + 1) * LDC])
    P.I("gpsimd", "memset", ap=VA[:, :, 64:65], constant=1.0, _writes=[VA[:]])
    P.dma("sync", VA[:, :, 0:64], av.rearrange("(n p) c -> p n c", p=128))
    tri_f = P.sb("tri_f", [128, 128], F32)
    tri = P.sb("tri", [128, 128], BF16)
    P.I("gpsimd", "memset", ap=tri_f[:], constant=1.0, _writes=[tri_f[:]])
    P.I("gpsimd", "affine_select", out=tri_f[:], in_=tri_f[:], pattern=[[1, 128]], compare_op=ALU.is_ge,
        fill=0.0, base=0, channel_multiplier=-1)
    P.I("vector", "tensor_copy", out=tri[:], in_=tri_f[:])
    gq = P.sb("gq_bc", [128, 96], F32)
    gk = P.sb("gk_bc", [128, 96], F32)
    m2 = P.sb("m2", [128, 2], F32)
    negc = P.sb("negc", [128, 1], F32)
    P.dma("sync", gq[:], qqk.partition_broadcast(128))
    P.dma("sync", gk[:], kqk.partition_broadcast(128))
    P.I("vector", "tensor_tensor", out=gq[:], in0=gq[:], in1=gq[:], op=ALU.mult)
    P.I("vector", "tensor_tensor", out=gk[:], in0=gk[:], in1=gk[:], op=ALU.mult)
    P.I("vector", "tensor_reduce", out=m2[:, 0:1], in_=gq[:], axis=AX.X, op=ALU.max)
    P.I("vector", "tensor_reduce", out=m2[:, 1:2], in_=gk[:], axis=AX.X, op=ALU.max)
    P.I("vector", "tensor_tensor", out=negc[:], in0=m2[:, 0:1], in1=m2[:, 1:2], op=ALU.mult)
    P.I("gpsimd", "tensor_tensor", out=negc[:], in0=negc[:], in1=half[:, 0:1], op=ALU.pow)
    P.I("vector", "tensor_scalar", out=negc[:], in0=negc[:], scalar1=-math.sqrt(96.0), scalar2=None, op0=ALU.mult)
    if after_setup is not None:
        after_setup()
    PT_r = Rot(P, "PT", [128, 512], BF16, 5)
    den_r = Rot(P, "den", [128, 512], F32, 2)
    oa_r = Rot(P, "oa", [64, 512], F32, 2)
    rb_r = Rot(P, "rb", [64, 512], F32, 2)
    si = 0
    for qs in range(SS // 512):
        q0 = qs * 512
        pO = psO[qs % 2]
        nkb = 4 * qs + 4
        pend = []
        for j in range(nkb):
            d = j - 4 * qs
            lo_c = 0 if d <= 0 else d * 128
            pS = psS[si % 3]
            si += 1
            P.I("tensor", "matmul", out=pS[:, lo_c:512], lhsT=KT[:, j * 128:(j + 1) * 128], rhs=QT[:, q0 + lo_c:q0 + 512],
                start=True, stop=True)
            if len(pend) >= 2:
                pj, plo, pPT = pend.pop(0)
                P.I("tensor", "matmul", out=pO[0:65, plo:512], lhsT=VA[:, pj, :], rhs=pPT[:, plo:512], start=(pj == 0), stop=False)
            PT = PT_r.next()
            P.I("scalar", "activation", out=PT[:, lo_c:512], in_=pS[:, lo_c:512], func=AF.Exp, bias=negc[:, 0:1], scale=ATT_SCALE)
            if d >= 0:
                P.I("vector", "tensor_tensor", out=PT[:, lo_c:lo_c + 128], in0=PT[:, lo_c:lo_c + 128], in1=tri[:], op=ALU.mult)
            pend.append((j, lo_c, PT))
        while pend:
            pj, plo, pPT = pend.pop(0)
            P.I("tensor", "matmul", out=pO[0:65, plo:512], lhsT=VA[:, pj, :], rhs=pPT[:, plo:512], start=(pj == 0), stop=(len(pend) == 0))
        den = den_r.next()
        P.I("vector", "tensor_copy", out=den[64:65, :], in_=pO[64:65, :])
        P.I("tensor", "matmul", out=psB[0:64, :], lhsT=ones1[64:65, 0:64], rhs=den[64:65, :], start=True, stop=True)
        rb = rb_r.next()
        P.I("vector", "reciprocal", out=rb[:], in_=psB[0:64, :])
        oa = oa_r.next()
        P.I("vector", "tensor_tensor", out=oa[:], in0=pO[0:64, :], in1=rb[:], op=ALU.mult)
        P.dma("sync", ao[:, q0:q0 + 512], oa[:])


def build_p3(ntok=TPC, TB=1024, NEXP=32, stop=0):
    nc = new_nc()
    P = Prog(nc)
    io = IO(nc)
    phase3(P, io, ntok, TB, NEXP)
    P.finish(io.outs)
    P.emit()
    return nc


def phase3(P, io, ntok, TB=1024, NEXP=32, stop=0):
    nc = P.nc
    x_in = io.inp("x_in", [ntok, D])
    c_in = io.inp("c_in", [D])
    adaw = io.inp("ada_w", [D, 6 * D])
    adab = io.inp("ada_b", [6 * D])
    mixo = [io.inp(n, [256, ntok]) for n in ("co", "ao", "ro", "lo")]
    gates = {0: io.inp("bgate", [256, ntok]), 2: io.inp("sgate", [256, ntok]), 3: io.inp("ggate", [256, ntok])}
    mng = io.inp("mix_norm_g", [D])
    w_out = io.inp("w_out", [D, D])
    gffn = io.inp("norm_ffn_g", [D])
    wgr = io.inp("router_group_w", [D, 4])
    bgr = io.inp("router_group_b", [4])
    wer = io.inp("router_expert_w", [D, 32])
    ber = io.inp("router_expert_b", [32])
    ewg = io.inp("exp_w_gate", [32, D, 256])
    ewu = io.inp("exp_w_up", [32, D, 256])
    ewd = io.inp("exp_w_down", [32, 256, D])
    x_out = io.out("x_out", [ntok, D])
    x1s = io.out("x1s", [ntok, D])

    bank = [P.ps("bank%d" % i, [128, 512], F32) for i in range(8)]
    idf, idb = make_identities(P)
    neghalf = P.sb("neghalf", [128, 256], F32)
    P.I("vector", "memset", ap=neghalf[:], constant=-0.5, _writes=[neghalf[:]])
    onesf = P.sb("onesf", [128, 128], F32)
    P.I("vector", "memset", ap=onesf[:], constant=1.0, _writes=[onesf[:]])
    epsc = P.sb("epsc", [128, 1], F32)
    P.I("vector", "memset", ap=epsc[:], constant=EPS, _writes=[epsc[:]])

    mod = compute_mod(P, c_in, adaw, adab, bank[0], bank[1], 2 * D, 6 * D, CH=128)
    gtm = mod[:, 0:D]
    shf = mod[:, D:2 * D]
    gf = mod[:, 2 * D:3 * D]
    gtf = mod[:, 3 * D:4 * D]
    tmo_r = Rot(P, "tmo", [128, D], F32, 2)
    gtmp = tmo_r.next()
    P.dma("sync", gtmp[:], gffn.partition_broadcast(128))
    P.I("vector", "scalar_tensor_tensor", out=gf, in0=gf, scalar=1.0, in1=gtmp[:], op0=ALU.add, op1=ALU.mult)
    mngc = P.sb("mngc", [128, 8], F32)
    P.dma("sync", mngc[:], mng.rearrange("(k p) -> p k", p=128), allow_slow_non_contiguous=True)
    wout_bf = P.sb("wout_bf", [128, 8, D], BF16)
    wov = w_out.rearrange("(k p) n -> p k n", p=128)
    for k in range(8):
        P.dma("gpsimd", wout_bf[:, k, :], wov[:, k, :])
    wr = P.sb("wr", [128, 8, 36], F32)
    P.dma("sync", wr[:, :, 0:4], wgr.rearrange("(k p) n -> p k n", p=128))
    P.dma("sync", wr[:, :, 4:36], wer.rearrange("(k p) n -> p k n", p=128))
    wr_hi = P.sb("wr_hi", [128, 8, 36], BF16)
    wr_lo = P.sb("wr_lo", [128, 8, 36], BF16)
    P.I("vector", "tensor_copy", out=wr_hi[:], in_=wr[:])
    P.I("vector", "tensor_tensor", out=wr_lo[:], in0=wr[:], in1=wr_hi[:], op=ALU.subtract)
    bg_bc = P.sb("bg_bc", [128, 4], F32)
    be_bc = P.sb("be_bc", [128, 32], F32)
    P.dma("sync", bg_bc[:], bgr.partition_broadcast(128))
    P.dma("sync", be_bc[:], ber.partition_broadcast(128))

    NSUB = TB // 128
    h2T = P.sb("h2T", [128, 8, TB], BF16)
    comb_all = P.sb("comb_all", [128, NSUB, 32], F32)
    yacc = P.sb("yacc", [128, NSUB, D], F32)

    yc_r = Rot(P, "yc", [128, 256], F32, 6)
    gt_r = Rot(P, "gtl", [128, 256], F32, 6)
    ysq_r = Rot(P, "ysq", [128, 256], F32, 8)
    ych = [P.sb("ych%d" % i, [128, 256], F32) for i in range(8)]
    rsb_r = Rot(P, "rsb", [128, 256], F32, 4)
    yT = P.sb("yT", [128, 8, 256], BF16)
    xt_r = Rot(P, "xt", [128, D], F32, 1)
    x1_r = Rot(P, "x1", [128, D], F32, 2)
    junk = P.sb("junk", [128, D], BF16)
    sm_r = Rot(P, "sm", [128, 8], F32, 4)
    h2f_r = Rot(P, "h2f", [128, D], F32, 1)
    h2Tlo = P.sb("h2Tlo", [128, 8, 128], BF16)
    hsp_r = Rot(P, "hsp", [128, D], BF16, 2)
    R8 = Rot(P, "r8", [128, 8], F32, 20)
    R4 = Rot(P, "r4", [128, 4], F32, 8)
    R1 = Rot(P, "r1", [128, 1], F32, 16)
    lg_r = Rot(P, "lgs", [128, 36], F32, 2)
    wgu_r = Rot(P, "wgu", [128, 8, 512], BF16, 2)
    wd_r = Rot(P, "wd", [128, 2, D], BF16, 2)
    sg_r = Rot(P, "sg", [128, 256], BF16, 4)
    hid_r = Rot(P, "hid", [128, 2, 256], BF16, 2)

    if "wgu_s" in io.m:
        wgu_s, wd_s = io.m["wgu_s"], io.m["wd_s"]
    else:
        wgu_s, wd_s = precast_experts(P, ewg, ewu, ewd, NEXP)

    for blk in range(ntok // TB):
        b0 = blk * TB
        for tl in range(TB // 256):
            t0 = b0 + tl * 256
            for m in range(4):
                for c in range(2):
                    ci = m * 2 + c
                    y = ych[ci]
                    if m == 1:
                        P.dma("sync", y[:], mixo[m][c * 128:(c + 1) * 128, t0:t0 + 256])
                    else:
                        yc = yc_r.next()
                        gt = gt_r.next()
                        P.dma("sync", yc[:], mixo[m][c * 128:(c + 1) * 128, t0:t0 + 256])
                        P.dma("scalar", gt[:], gates[m][c * 128:(c + 1) * 128, t0:t0 + 256])
                        P.I("vector", "tensor_tensor", out=y[:], in0=yc[:], in1=gt[:], op=ALU.mult)
            ysqs = []
            for ci in range(8):
                ysq = ysq_r.next()
                ysqs.append(ysq)
                P.I("scalar", "activation", out=ysq[:], in_=ych[ci][:], func=AF.Square)
            psss = []
            for m in range(4):
                pss = bank[m // 2][:, (m % 2) * 256:(m % 2 + 1) * 256]
                psss.append(pss)
                for c in range(2):
                    P.I("tensor", "matmul", out=pss, lhsT=onesf[:], rhs=ysqs[m * 2 + c][:], start=(c == 0), stop=(c == 1))
            rsbs = []
            for m in range(4):
                rsb = rsb_r.next()
                rsbs.append(rsb)
                P.I("scalar", "activation", out=rsb[:], in_=psss[m], func=AF.Ln, bias=epsc[:, 0:1], scale=1.0 / 256)
            for m in range(4):
                P.I("scalar", "activation", out=rsbs[m][:], in_=rsbs[m][:], func=AF.Exp, scale=-0.5)
            for m in range(4):
                for c in range(2):
                    ci = m * 2 + c
                    P.I("vector", "scalar_tensor_tensor", out=yT[:, ci, :], in0=ych[ci][:], scalar=mngc[:, ci:ci + 1], in1=rsbs[m][:],
                        op0=ALU.mult, op1=ALU.mult)
            for s in range(2):
                tt = t0 + s * 128
                si = tl * 2 + s
                po = [bank[2], bank[3]]
                for hf in range(2):
                    for k in range(8):
                        P.I("tensor", "matmul", out=po[hf][:], lhsT=yT[:, k, s * 128:(s + 1) * 128], rhs=wout_bf[:, k, hf * 512:(hf + 1) * 512],
                            start=(k == 0), stop=(k == 7))
                xt = xt_r.next()
                P.dma("sync", xt[:], x_in[tt:tt + 128, :])
                tmo = tmo_r.next()
                x1 = x1_r.next()
                for hf in range(2):
                    P.I("vector", "tensor_tensor", out=tmo[:, hf * 512:(hf + 1) * 512], in0=po[hf][:], in1=gtm[:, hf * 512:(hf + 1) * 512], op=ALU.mult)
                P.I("vector", "tensor_tensor", out=x1[:], in0=tmo[:], in1=xt[:], op=ALU.add)
                P.dma("sync", x1s[tt:tt + 128, :], x1[:])
                sm = sm_r.next()
                P.I("scalar", "activation", out=junk[:], in_=x1[:], func=AF.Square, accum_out=sm[:, 0:1])
                P.I("vector", "tensor_scalar", out=sm[:, 1:2], in0=sm[:, 0:1], scalar1=1.0 / D, scalar2=EPS, op0=ALU.mult, op1=ALU.add)
                P.I("gpsimd", "tensor_tensor", out=sm[:, 2:3], in0=sm[:, 1:2], in1=neghalf[:, 0:1], op=ALU.pow)
                tm2 = tmo_r.next()
                h2f = h2f_r.next()
                P.I("vector", "scalar_tensor_tensor", out=tm2[:], in0=x1[:], scalar=sm[:, 2:3], in1=gf, op0=ALU.mult, op1=ALU.mult)
                P.I("vector", "tensor_tensor", out=h2f[:], in0=tm2[:], in1=shf, op=ALU.add)
                hhi = hsp_r.next()
                hlo = hsp_r.next()
                P.I("scalar", "copy", out=hhi[:], in_=h2f[:])
                P.I("vector", "tensor_tensor", out=hlo[:], in0=h2f[:], in1=hhi[:], op=ALU.subtract)
                b4 = bank[4][:].bitcast(BF16)
                b5 = bank[5][:].bitcast(BF16)
                for k in range(8):
                    P.I("tensor", "transpose", out=b4[:, k * 128:(k + 1) * 128], in_=hhi[:, k * 128:(k + 1) * 128], identity=idb[:])
                for k in range(8):
                    P.I("tensor", "transpose", out=b5[:, k * 128:(k + 1) * 128], in_=hlo[:, k * 128:(k + 1) * 128], identity=idb[:])
                P.I("scalar", "copy", out=h2T[:, :, si * 128:(si + 1) * 128], in_=b4.rearrange("p (k t) -> p k t", k=8))
                P.I("vector", "tensor_copy", out=h2Tlo[:], in_=b5.rearrange("p (k t) -> p k t", k=8))
                pl = bank[6]
                for k in range(8):
                    P.I("tensor", "matmul", out=pl[:, 0:36], lhsT=h2T[:, k, si * 128:(si + 1) * 128], rhs=wr_hi[:, k, :], start=(k == 0), stop=False)
                for k in range(8):
                    P.I("tensor", "matmul", out=pl[:, 0:36], lhsT=h2Tlo[:, k, :], rhs=wr_hi[:, k, :], start=False, stop=False)
                for k in range(8):
                    P.I("tensor", "matmul", out=pl[:, 0:36], lhsT=h2T[:, k, si * 128:(si + 1) * 128], rhs=wr_lo[:, k, :], start=False, stop=(k == 7))
                lg = lg_r.next()
                P.I("vector", "tensor_copy", out=lg[:], in_=pl[:, 0:36])
                router_math(P, lg, comb_all[:, si, :], bg_bc, be_bc, R8, R4, R1)
        NTL = TB // 256
        steps = [(e, tl) for e in range(NEXP) for tl in range(NTL)]
        wts = {}
        bcs = {}
        hids = {}

        def load_expert(e):
            wgu = wgu_r.next()
            wd = wd_r.next()
            P.dma("sync", wgu[:], wgu_s[e])
            P.dma("sync", wd[:], wd_s[e])
            wts[e] = (wgu, wd)

        def gu_mm(i):
            e, tl = steps[i]
            if tl == 0:
                if e not in wts:
                    load_expert(e)
            wgu, wd = wts[e]
            pg = bank[4 + 2 * (i % 2)]
            pu = bank[5 + 2 * (i % 2)]
            for fc in range(2):
                for k in range(8):
                    P.I("tensor", "matmul", out=pg[:, fc * 256:(fc + 1) * 256], lhsT=wgu[:, k, fc * 128:(fc + 1) * 128],
                        rhs=h2T[:, k, tl * 256:(tl + 1) * 256], start=(k == 0), stop=(k == 7))
                for k in range(8):
                    P.I("tensor", "matmul", out=pu[:, fc * 256:(fc + 1) * 256], lhsT=wgu[:, k, 256 + fc * 128:256 + (fc + 1) * 128],
                        rhs=h2T[:, k, tl * 256:(tl + 1) * 256], start=(k == 0), stop=(k == 7))

        def gu_ew(i):
            e, tl = steps[i]
            pg = bank[4 + 2 * (i % 2)]
            pu = bank[5 + 2 * (i % 2)]
            hid = hid_r.next()
            hids[i] = hid
            for fc in range(2):
                sg = sg_r.next()
                P.I("scalar", "activation", out=sg[:], in_=pg[:, fc * 256:(fc + 1) * 256], func=AF.Silu)
                P.I("vector", "tensor_tensor", out=hid[:, fc, :], in0=sg[:], in1=pu[:, fc * 256:(fc + 1) * 256], op=ALU.mult)

        def down(i):
            e, tl = steps[i]
            wgu, wd = wts[e]
            hid = hids.pop(i)
            for s in range(2):
                si = tl * 2 + s
                for hf in range(2):
                    py = bank[s * 2 + hf]
                    for fc in range(2):
                        P.I("tensor", "matmul", out=py[:], lhsT=hid[:, fc, s * 128:(s + 1) * 128], rhs=wd[:, fc, hf * 512:(hf + 1) * 512],
                            start=(fc == 0), stop=(fc == 1))
                    if e == 0:
                        P.I("vector", "tensor_scalar", out=yacc[:, si, hf * 512:(hf + 1) * 512], in0=py[:], scalar1=comb_all[:, si, e:e + 1],
                            scalar2=None, op0=ALU.mult)
                    else:
                        P.I("vector", "scalar_tensor_tensor", out=yacc[:, si, hf * 512:(hf + 1) * 512], in0=py[:], scalar=comb_all[:, si, e:e + 1],
                            in1=yacc[:, si, hf * 512:(hf + 1) * 512], op0=ALU.mult, op1=ALU.add)
            if tl == NTL - 1:
                wts.pop(e, None)
            if tl == 0 and e + 1 < NEXP:
                load_expert(e + 1)

        if steps:
            gu_mm(0)
            gu_ew(0)
        for i in range(len(steps)):
            if i + 1 < len(steps):
                gu_mm(i + 1)
            down(i)
            if i + 1 < len(steps):
                gu_ew(i + 1)
        for si in range(NSUB):
            tt = b0 + si * 128
            x1 = x1_r.next()
            P.dma("sync", x1[:], x1s[tt:tt + 128, :])
            tmo = tmo_r.next()
            P.I("vector", "tensor_tensor", out=tmo[:], in0=yacc[:, si, :], in1=gtf, op=ALU.mult)
            xo = xt_r.next()
            P.I("vector", "tensor_tensor", out=xo[:], in0=tmo[:], in1=x1[:], op=ALU.add)
            P.dma("sync", x_out[tt:tt + 128, :], xo[:])


RSTOP = 0


def precast_experts(P, ewg, ewu, ewd, NEXP=32):
    nc = P.nc
    wgu_s = nc.dram_tensor(P.prefix + "wgu_s", [32, 128, 8, 512], BF16).ap()
    wd_s = nc.dram_tensor(P.prefix + "wd_s", [32, 128, 2, D], BF16).ap()
    for e in range(NEXP):
        gv = ewg[e].rearrange("(k p) f -> p k f", p=128)
        uv = ewu[e].rearrange("(k p) f -> p k f", p=128)
        for k in range(8):
            P.dma("gpsimd", wgu_s[e, :, k, 0:256], gv[:, k, :])
            P.dma("gpsimd", wgu_s[e, :, k, 256:512], uv[:, k, :])
        dv = ewd[e].rearrange("(k p) n -> p k n", p=128)
        for k in range(2):
            P.dma("gpsimd", wd_s[e, :, k, :], dv[:, k, :])
    return wgu_s, wd_s


def router_math(P, lg, comb, bg_bc, be_bc, R8, R4, R1):
    V = lambda *a, **k: P.I("vector", *a, **k)
    lgg = lg[:, 0:4]
    mx = R1.next()
    V("tensor_reduce", out=mx[:], in_=lgg, axis=AX.X, op=ALU.max)
    nmx = R1.next()
    V("tensor_scalar", out=nmx[:], in0=mx[:], scalar1=-1.0, scalar2=None, op0=ALU.mult)
    eg = R4.next()
    sg = R1.next()
    P.I("scalar", "activation", out=eg[:], in_=lgg, func=AF.Exp, bias=nmx[:, 0:1], scale=1.0, accum_out=sg[:, 0:1])
    if RSTOP == 1:
        return
    rs = R1.next()
    V("reciprocal", out=rs[:], in_=sg[:])
    gp = R4.next()
    V("tensor_scalar", out=gp[:], in0=eg[:], scalar1=rs[:, 0:1], scalar2=None, op0=ALU.mult)
    sel = R4.next()
    V("tensor_tensor", out=sel[:], in0=gp[:], in1=bg_bc[:], op=ALU.add)
    m = R1.next()
    V("tensor_reduce", out=m[:], in_=sel[:], axis=AX.X, op=ALU.max)
    goh = R4.next()
    V("tensor_scalar", out=goh[:], in0=sel[:], scalar1=m[:, 0:1], scalar2=None, op0=ALU.is_equal)
    if RSTOP == 2:
        return
    gwj = R4.next()
    gw = R1.next()
    V("tensor_tensor", out=gwj[:], in0=gp[:], in1=goh[:], op=ALU.mult)
    V("tensor_reduce", out=gw[:], in_=gwj[:], axis=AX.X, op=ALU.add)
    els = R8.next()
    bes = R8.next()
    V("tensor_scalar", out=els[:], in0=lg[:, 4:12], scalar1=goh[:, 0:1], scalar2=None, op0=ALU.mult)
    V("tensor_scalar", out=bes[:], in0=be_bc[:, 0:8], scalar1=goh[:, 0:1], scalar2=None, op0=ALU.mult)
    for g in range(1, 4):
        V("scalar_tensor_tensor", out=els[:], in0=lg[:, 4 + 8 * g:12 + 8 * g], scalar=goh[:, g:g + 1], in1=els[:], op0=ALU.mult, op1=ALU.add)
        V("scalar_tensor_tensor", out=bes[:], in0=be_bc[:, 8 * g:8 * g + 8], scalar=goh[:, g:g + 1], in1=bes[:], op0=ALU.mult, op1=ALU.add)
    if RSTOP == 3:
        return
    mx8 = R1.next()
    V("tensor_reduce", out=mx8[:], in_=els[:], axis=AX.X, op=ALU.max)
    nm8 = R1.next()
    V("tensor_scalar", out=nm8[:], in0=mx8[:], scalar1=-1.0, scalar2=None, op0=ALU.mult)
    ee = R8.next()
    se = R1.next()
    P.I("scalar", "activation", out=ee[:], in_=els[:], func=AF.Exp, bias=nm8[:, 0:1], scale=1.0, accum_out=se[:, 0:1])
    rse = R1.next()
    V("reciprocal", out=rse[:], in_=se[:])
    ep = R8.next()
    V("tensor_scalar", out=ep[:], in0=ee[:], scalar1=rse[:, 0:1], scalar2=None, op0=ALU.mult)
    if RSTOP == 4:
        return
    sc = R8.next()
    V("tensor_tensor", out=sc[:], in0=ep[:], in1=bes[:], op=ALU.add)
    m1 = R1.next()
    V("tensor_reduce", out=m1[:], in_=sc[:], axis=AX.X, op=ALU.max)
    oh1 = R8.next()
    V("tensor_scalar", out=oh1[:], in0=sc[:], scalar1=m1[:, 0:1], scalar2=None, op0=ALU.is_equal)
    sc2 = R8.next()
    V("scalar_tensor_tensor", out=sc2[:], in0=oh1[:], scalar=-1e9, in1=sc[:], op0=ALU.mult, op1=ALU.add)
    m2 = R1.next()
    V("tensor_reduce", out=m2[:], in_=sc2[:], axis=AX.X, op=ALU.max)
    oh2 = R8.next()
    V("tensor_scalar", out=oh2[:], in0=sc2[:], scalar1=m2[:, 0:1], scalar2=None, op0=ALU.is_equal)
    if RSTOP == 5:
        return
    ohs = R8.next()
    V("tensor_tensor", out=ohs[:], in0=oh1[:], in1=oh2[:], op=ALU.add)
    tp = R8.next()
    V("tensor_tensor", out=tp[:], in0=ep[:], in1=ohs[:], op=ALU.mult)
    sp = R1.next()
    V("tensor_reduce", out=sp[:], in_=tp[:], axis=AX.X, op=ALU.add)
    rsp = R1.next()
    V("reciprocal", out=rsp[:], in_=sp[:])
    fac = R1.next()
    V("tensor_tensor", out=fac[:], in0=rsp[:], in1=gw[:], op=ALU.mult)
    ew = R8.next()
    V("tensor_scalar", out=ew[:], in0=tp[:], scalar1=fac[:, 0:1], scalar2=None, op0=ALU.mult)
    if RSTOP == 6:
        return
    for g in range(4):
        V("tensor_scalar", out=comb[:, g * 8:(g + 1) * 8], in0=ew[:], scalar1=goh[:, g:g + 1], scalar2=None, op0=ALU.mult)


W_SPECS = dict(
    ada_w=[2, D, 6 * D], ada_b=[2, 6 * D], norm_mix_g=[2, D], w_in=[2, D, IN_COLS], conv_w=[2, 3, 256],
    mla_q_norm_g=[2, 192], mla_w_uq=[2, 192, 384], mla_kv_norm_g=[2, 128], mla_w_ukv=[2, 128, 512],
    mla_q_qk_g=[2, 96], mla_k_qk_g=[2, 96], lru_conv_w=[2, 4, 256], lru_conv_b=[2, 256], lru_w_a=[2, 4, 64, 64],
    lru_b_a=[2, 256], lru_w_x=[2, 4, 64, 64], lru_b_x=[2, 256], lru_lambda=[2, 256], mix_norm_g=[2, D],
    w_out=[2, D, D], norm_ffn_g=[2, D], router_group_w=[2, D, 4], router_group_b=[2, 4], router_expert_w=[2, D, 32],
    router_expert_b=[2, 32], exp_w_gate=[2, 32, D, 256], exp_w_up=[2, 32, D, 256], exp_w_down=[2, 32, 256, D])


def build_fused(SS=S, NL=2, TB=1024):
    nc = new_nc()
    P = Prog(nc)
    x_in = din(nc, "x", [SS, D])
    c_in = din(nc, "c", [D])
    pos = din(nc, "positions", [SS], I32)
    W = {k: din(nc, k, shp) for k, shp in W_SPECS.items()}
    inv_ret4 = din(nc, "inv_ret4", [128, 1])
    inv_mla = din(nc, "inv_mla", [16])
    innerT = din(nc, "innerT", [4, 128, 128])
    qdecT = din(nc, "qdecT", [4, 64, 128])
    kdec = din(nc, "kdec", [4, 128, 1])
    cdec = din(nc, "cdec", [4, 64, 1])
    out = dout(nc, "out", [SS, D])

    def idr(name, shape, dt=F32):
        return nc.dram_tensor(name, list(shape), dt).ap()

    T = dict(attq=idr("i_attq", [4, 96, SS], BF16), attk=idr("i_attk", [4, 96, SS], BF16), attv=idr("i_attv", [4, SS, 64], BF16),
             retq=idr("i_retq", [256, SS], BF16), retk=idr("i_retk", [256, SS], BF16), retv=idr("i_retv", [SS, 256], BF16),
             lrux=idr("i_lrux", [256, SS]), cvx=idr("i_cvx", [256, SS]), bgate=idr("i_bgate", [256, SS]),
             sgate=idr("i_sgate", [256, SS]), ggate=idr("i_ggate", [256, SS]))
    Y = dict(co=idr("i_co", [256, SS]), ao=idr("i_ao", [256, SS]), ro=idr("i_ro", [256, SS]), lo=idr("i_lo", [256, SS]))
    x_mid = idr("i_xmid", [SS, D])
    x1s = idr("i_x1s", [SS, D])
    col = lambda ap: ap.rearrange("(c o) -> c o", o=1)
    cast = {}
    for l in range(NL):
        xs = x_in if l == 0 else x_mid
        xd = out if l == NL - 1 else x_mid
        mk = P.mark()
        P.prefix = "L%dP1_" % l
        m = dict(x_in=xs, c_in=c_in, pos_in=pos, inv_ret4=inv_ret4, inv_mla=inv_mla)
        for k in ("ada_w", "ada_b", "norm_mix_g", "w_in", "mla_q_norm_g", "mla_w_uq", "mla_kv_norm_g", "mla_w_ukv", "mla_q_qk_g", "mla_k_qk_g"):
            m[k] = W[k][l]
        m.update(T)
        phase1(P, IO(nc, m), SS)
        P.release(mk)
        for hd in range(4):
            mk = P.mark()
            P.prefix = "L%dP2h%d_" % (l, hd)
            sl = slice(hd * 64, (hd + 1) * 64)
            m = dict(aq=T["attq"][hd], ak=T["attk"][hd], av=T["attv"][hd], rq=T["retq"][sl], rk=T["retk"][sl], rv=T["retv"][:, sl],
                     lx=T["lrux"][sl], cx=T["cvx"][sl],
                     convw=W["conv_w"][l][:, sl].rearrange("k c -> c k"), lcw=W["lru_conv_w"][l][:, sl].rearrange("k c -> c k"),
                     lcb=col(W["lru_conv_b"][l][sl]), wa=W["lru_w_a"][l][hd], ba=col(W["lru_b_a"][l][sl]),
                     wx=W["lru_w_x"][l][hd], bx=col(W["lru_b_x"][l][sl]), lam=col(W["lru_lambda"][l][sl]),
                     qqk=W["mla_q_qk_g"][l], kqk=W["mla_k_qk_g"][l],
                     innerT=innerT[hd], qdecT=qdecT[hd], kdec=kdec[hd], cdec=cdec[hd],
                     ao=Y["ao"][sl], ro=Y["ro"][sl], lo=Y["lo"][sl], co=Y["co"][sl])
            if hd == 0:
                def _cast(l=l):
                    pfx = P.prefix
                    P.prefix = "L%d_" % l
                    cast[l] = precast_experts(P, W["exp_w_gate"][l], W["exp_w_up"][l], W["exp_w_down"][l])
                    P.prefix = pfx
                phase2(P, IO(nc, m), SS, after_setup=_cast)
            else:
                phase2(P, IO(nc, m), SS)
            P.release(mk)
        mk = P.mark()
        P.prefix = "L%dP3_" % l
        m = dict(x_in=xs, c_in=c_in, x_out=xd, x1s=x1s, bgate=T["bgate"], sgate=T["sgate"], ggate=T["ggate"])
        for k in ("ada_w", "ada_b", "mix_norm_g", "w_out", "norm_ffn_g", "router_group_w", "router_group_b", "router_expert_w",
                  "router_expert_b", "exp_w_gate", "exp_w_up", "exp_w_down"):
            m[k] = W[k][l]
        m.update(Y)
        m["wgu_s"], m["wd_s"] = cast[l]
        phase3(P, IO(nc, m), SS, TB, 32)
        P.release(mk)
    P.finish([out])
    P.emit()
    return nc


_NC_CACHE = {}


def _get(name, fn):
    if name not in _NC_CACHE:
        _NC_CACHE[name] = fn()
    return _NC_CACHE[name]


def _inv_freq(dim):
    return (np.float32(1.0) / (np.float32(10000.0) ** (np.arange(0, dim, 2, dtype=np.float32) / np.float32(dim)))).astype(np.float32)


def _ret_consts(hd):
    gamma = 1.0 - 2.0 ** (-5.0 - hd)
    idx = np.arange(128, dtype=np.float64)
    innerT = np.where(idx[None, :] >= idx[:, None], gamma ** np.maximum(idx[None, :] - idx[:, None], 0.0), 0.0).astype(np.float32)
    qdecT = np.ascontiguousarray(np.tile((gamma ** (idx + 1.0))[None, :], (64, 1))).astype(np.float32)
    kdec = (gamma ** (127.0 - idx)).reshape(128, 1).astype(np.float32)
    cdec = np.full((64, 1), gamma ** 128, np.float32)
    return innerT, qdecT, kdec, cdec


def kernel(**inputs):
    I = {k: np.ascontiguousarray(np.asarray(v)) for k, v in inputs.items()}
    B = I["x"].shape[0]
    nc = _get("fused", build_fused)
    rc = [_ret_consts(h) for h in range(4)]
    consts = dict(inv_ret4=np.ascontiguousarray(np.tile(_inv_freq(64), 4).reshape(128, 1)), inv_mla=_inv_freq(32),
                  innerT=np.stack([r[0] for r in rc]), qdecT=np.stack([r[1] for r in rc]),
                  kdec=np.stack([r[2] for r in rc]), cdec=np.stack([r[3] for r in rc]))
    maps = []
    for b in range(B):
        m = dict(x=np.ascontiguousarray(I["x"][b], dtype=np.float32), c=np.ascontiguousarray(I["c"][b]),
                 positions=np.ascontiguousarray(I["positions"][b]).astype(np.int32))
        for k in W_SPECS:
            m[k] = I[k]
        m.update(consts)
        maps.append(m)
    res = run_bass_kernel_spmd(nc, maps, core_ids=list(range(B))).results
    return np.stack([res[b]["out"] for b in range(B)], axis=0).astype(np.float32)
```

```python
import math
import numpy as np
import ml_dtypes
import concourse.bass as bass
import concourse.mybir as mybir
from concourse.bass_utils import run_bass_kernel_spmd

F32 = mybir.dt.float32
BF16 = mybir.dt.bfloat16
I32 = mybir.dt.int32
AF = mybir.ActivationFunctionType
ALU = mybir.AluOpType
AX = mybir.AxisListType

D = 1024
S = 16384
NCORE = 8
TPC = 4096
IN_COLS = 2656
EPS = 1e-6
TWO_PI = 2.0 * math.pi

ENGS = ["sync", "scalar", "vector", "gpsimd", "tensor"]
SAME_ENGINE_SYNC = {"sync": False, "scalar": True, "vector": True, "gpsimd": True, "tensor": False}
_APT = None


class Buf:
    __slots__ = ("name", "w", "r")

    def __init__(self, name=""):
        self.name = name
        self.w = None
        self.r = []


class Prog:
    NPOOL = 16

    def __init__(self, nc):
        self.nc = nc
        self.ops = {e: [] for e in ENGS}
        self.cnt = {e: 0 for e in ENGS}
        self.esem = {e: nc.alloc_semaphore("es_" + e) for e in ENGS}
        self.known = {e: {f: 0 for f in ENGS} for e in ENGS}
        self.snap = {e: [None] for e in ENGS}
        self.dq = ["sync", "scalar", "gpsimd"]
        self.pool = {q: [nc.alloc_semaphore("dp_%s_%d" % (q, i)) for i in range(self.NPOOL)] for q in self.dq}
        self.pool_val = {q: [0] * self.NPOOL for q in self.dq}
        self.pool_next = {q: 0 for q in self.dq}
        self.dknown = {e: {} for e in ENGS}
        self.bufs = {}
        self.uid = 0
        self.prefix = ""

    def sb(self, name, shape, dt=F32):
        return self.nc.alloc_sbuf_tensor(self.prefix + name, list(shape), dt)

    def ps(self, name, shape, dt=F32):
        return self.nc.alloc_psum_tensor(self.prefix + name, list(shape), dt)

    def mark(self):
        nc = self.nc
        return (nc.psum_base, nc.psum_top, nc.sbuf_base, nc.sbuf_top)

    def release(self, mk):
        self.barrier()
        nc = self.nc
        nc.psum_base, nc.psum_top, nc.sbuf_base, nc.sbuf_top = mk

    def barrier(self):
        for e in ENGS:
            waits = []
            for f in ENGS:
                if f != e and self.cnt[f] > 0:
                    self._need(e, ("E", f, self.cnt[f]), waits)
            for q in self.dq:
                for i in range(self.NPOOL):
                    v = self.pool_val[q][i]
                    if v > 0:
                        self._need(e, ("D", q, i, v, None), waits)
            self.ops[e].append((waits, None, None, None, 0))
        self.bufs = {}

    def buf_of(self, ap):
        n = ap.name
        b = self.bufs.get(n)
        if b is None:
            b = self.bufs[n] = Buf(n)
        return b

    def _merge(self, eng, sn):
        if sn is None:
            return
        kn = self.known[eng]
        for g, v in sn[0].items():
            if kn[g] < v:
                kn[g] = v
        dk = self.dknown[eng]
        for k, v in sn[1].items():
            if dk.get(k, 0) < v:
                dk[k] = v

    def _need(self, eng, ev, waits):
        if ev is None:
            return
        if ev[0] == "E":
            _, f, seq = ev
            if f == eng and not SAME_ENGINE_SYNC[eng]:
                return
            if self.known[eng][f] >= seq:
                return
            waits.append((self.esem[f], seq))
            self.known[eng][f] = seq
            self._merge(eng, self.snap[f][seq])
        else:
            _, q, i, val, sn = ev
            if self.dknown[eng].get((q, i), 0) >= val:
                return
            waits.append((self.pool[q][i], val))
            self.dknown[eng][(q, i)] = val
            self._merge(eng, sn)

    def _deps(self, eng, reads, writes, waits):
        for b in reads:
            self._need(eng, b.w, waits)
        for b in writes:
            self._need(eng, b.w, waits)
            for ev in b.r:
                self._need(eng, ev, waits)

    def _commit(self, ev, reads, writes):
        for b in reads:
            if b in writes:
                continue
            b.r.append(ev)
            if len(b.r) > 16:
                last = {}
                keep = []
                for e in b.r:
                    if e[0] == "E":
                        last[e[1]] = e
                    else:
                        keep.append(e)
                b.r = keep[-10:] + list(last.values())
        for b in writes:
            b.w = ev
            b.r = []

    def _scan(self, kwargs):
        reads, writes = [], []
        for k, v in kwargs.items():
            if isinstance(v, _APT):
                b = self.buf_of(v)
                if k in ("out", "accum_out", "out_max", "out_indices"):
                    if b not in writes:
                        writes.append(b)
                elif b not in reads:
                    reads.append(b)
        return reads, writes

    def I(self, eng, meth, **kwargs):
        xr = kwargs.pop("_reads", ())
        xw = kwargs.pop("_writes", ())
        reads, writes = self._scan(kwargs)
        reads += [self.buf_of(a) for a in xr]
        writes += [self.buf_of(a) for a in xw]
        waits = []
        self._deps(eng, reads, writes, waits)
        self.cnt[eng] += 1
        seq = self.cnt[eng]
        self.snap[eng].append((dict(self.known[eng]), dict(self.dknown[eng])))
        self.ops[eng].append((waits, meth, kwargs, self.esem[eng], 1))
        ev = ("E", eng, seq)
        self._commit(ev, reads, writes)
        return ev

    def dma(self, q, out, in_, **kw):
        reads = [self.buf_of(in_)]
        writes = [self.buf_of(out)]
        waits = []
        self._deps(q, reads, writes, waits)
        i = self.pool_next[q]
        self.pool_next[q] = (i + 1) % self.NPOOL
        prev = self.pool_val[q][i]
        if prev > 0 and self.dknown[q].get((q, i), 0) < prev:
            waits.append((self.pool[q][i], prev))
            self.dknown[q][(q, i)] = prev
        val = prev + 16
        self.pool_val[q][i] = val
        sn = (dict(self.known[q]), dict(self.dknown[q]))
        kw = dict(kw)
        kw["out"] = out
        kw["in_"] = in_
        self.ops[q].append((waits, "dma_start", kw, self.pool[q][i], 16))
        ev = ("D", q, i, val, sn)
        self._commit(ev, reads, writes)
        return ev

    def coll(self, kind, ins, outs, groups):
        q = "gpsimd"
        reads = [self.buf_of(a) for a in ins]
        writes = [self.buf_of(a) for a in outs]
        waits = []
        self._deps(q, reads, writes, waits)
        i = self.pool_next[q]
        self.pool_next[q] = (i + 1) % self.NPOOL
        prev = self.pool_val[q][i]
        if prev > 0 and self.dknown[q].get((q, i), 0) < prev:
            waits.append((self.pool[q][i], prev))
            self.dknown[q][(q, i)] = prev
        val = prev + 1
        self.pool_val[q][i] = val
        sn = (dict(self.known[q]), dict(self.dknown[q]))
        kw = dict(kind=kind, op=ALU.bypass, replica_groups=groups, ins=[a_.opt() for a_ in ins], outs=[a_.opt() for a_ in outs])
        self.ops[q].append((waits, "collective_compute", kw, self.pool[q][i], 1))
        ev = ("D", q, i, val, sn)
        self._commit(ev, reads, writes)
        return ev

    def finish(self, aps, eng="sync"):
        waits = []
        for a in aps:
            self._need(eng, self.buf_of(a).w, waits)
        self.ops[eng].append((waits, None, None, None, 0))

    def emit(self):
        nc = self.nc
        with nc.Block() as block:
            def mk(ename):
                def body(e):
                    for waits, meth, kw, sem, inc in self.ops[ename]:
                        for (s, v) in waits:
                            e.wait_ge(s, v)
                        if meth is not None:
                            getattr(e, meth)(**kw).then_inc(sem, inc)
                return body
            block.sync(mk("sync"))
            block.scalar(mk("scalar"))
            block.vector(mk("vector"))
            block.gpsimd(mk("gpsimd"))
            block.tensor(mk("tensor"))


class Rot:
    def __init__(self, P, name, shape, dt, n, psum=False):
        self.t = [(P.ps if psum else P.sb)("%s%d" % (name, i), shape, dt) for i in range(n)]
        self.i = 0

    def next(self):
        t = self.t[self.i % len(self.t)]
        self.i += 1
        return t


def new_nc():
    global _APT
    nc = bass.Bass("TRN2", target_bir_lowering=False)
    if _APT is None:
        t = nc.dram_tensor("apt_probe", [2, 2], F32).ap()
        _APT = type(t)
    return nc


class IO:
    def __init__(self, nc, m=None):
        self.nc = nc
        self.m = m or {}
        self.outs = []

    def inp(self, name, shape, dt=F32):
        if name in self.m:
            return self.m[name]
        return din(self.nc, name, shape, dt)

    def out(self, name, shape, dt=F32):
        if name in self.m:
            return self.m[name]
        ap = dout(self.nc, name, shape, dt)
        self.outs.append(ap)
        return ap


def din(nc, name, shape, dt=F32):
    return nc.dram_tensor(name, list(shape), dt, kind="ExternalInput").ap()


def dout(nc, name, shape, dt=F32):
    return nc.dram_tensor(name, list(shape), dt, kind="ExternalOutput").ap()


def make_identities(P):
    idf = P.sb("ident_f", [128, 128], F32)
    idb = P.sb("ident_b", [128, 128], BF16)
    P.I("gpsimd", "memset", ap=idf[:], constant=1.0, _writes=[idf[:]])
    P.I("gpsimd", "affine_select", out=idf[:], in_=idf[:], pattern=[[1, 128]], compare_op=ALU.is_equal,
        fill=0.0, base=0, channel_multiplier=-1)
    P.I("vector", "tensor_copy", out=idb[:], in_=idf[:])
    return idf, idb


def compute_mod(P, c_ap, adaw_ap, adab_ap, psA, psB, c0, c1, CH=256):
    n = c1 - c0
    mod = P.sb("mod_bc", [128, n], F32)
    ccol = P.sb("ccol", [128, 8], F32)
    cbc = P.sb("cbc", [128, 8, 128], F32)
    P.dma("sync", mod[:], adab_ap[c0:c1].partition_broadcast(128))
    P.dma("sync", ccol[:], c_ap.rearrange("(k p) -> p k", p=128), allow_slow_non_contiguous=True)
    P.I("scalar", "activation", out=ccol[:], in_=ccol[:], func=AF.Silu)
    P.I("vector", "tensor_copy", out=cbc[:], in_=ccol[:].unsqueeze(2).to_broadcast([128, 8, 128]))
    wr = Rot(P, "adaw_t", [128, 8, CH], F32, 2)
    awv = adaw_ap.rearrange("(k p) n -> p k n", p=128)
    for i in range(n // CH):
        wt = wr.next()
        P.dma("sync" if i % 2 == 0 else "scalar", wt[:], awv[:, :, c0 + i * CH:c0 + (i + 1) * CH])
        ps = psA if i % 2 == 0 else psB
        for k in range(8):
            P.I("tensor", "matmul", out=ps[:, 0:CH], lhsT=cbc[:, k, :], rhs=wt[:, k, :], start=(k == 0), stop=(k == 7))
        P.I("vector", "tensor_tensor", out=mod[:, i * CH:(i + 1) * CH], in0=mod[:, i * CH:(i + 1) * CH],
            in1=ps[:, 0:CH], op=ALU.add)
    return mod


def sin_of(P, out_ap, ang_ap, tmp_ap, tmpi_ap, shift):
    P.I("vector", "tensor_scalar", out=tmp_ap, in0=ang_ap, scalar1=1.0 / TWO_PI, scalar2=shift / TWO_PI, op0=ALU.mult, op1=ALU.add)
    P.I("vector", "tensor_copy", out=tmpi_ap, in_=tmp_ap)
    P.I("vector", "tensor_tensor", out=tmp_ap, in0=tmp_ap, in1=tmpi_ap, op=ALU.subtract)
    P.I("scalar", "activation", out=out_ap, in_=tmp_ap, func=AF.Sin, scale=TWO_PI)


def build_p1(ntok=TPC):
    nc = new_nc()
    P = Prog(nc)
    io = IO(nc)
    phase1(P, io, ntok)
    P.finish(io.outs)
    P.emit()
    return nc


def phase1(P, io, ntok):
    nc = P.nc
    if hasattr(P, "mla_tiles"):
        del P.mla_tiles
    NST = ntok // 512
    x_in = io.inp("x_in", [ntok, D])
    c_in = io.inp("c_in", [D])
    pos_in = io.inp("pos_in", [ntok], I32)
    adaw = io.inp("ada_w", [D, 6 * D])
    adab = io.inp("ada_b", [6 * D])
    gmix = io.inp("norm_mix_g", [D])
    w_in = io.inp("w_in", [D, IN_COLS])
    qng = io.inp("mla_q_norm_g", [192])
    wuq = io.inp("mla_w_uq", [192, 384])
    kvng = io.inp("mla_kv_norm_g", [128])
    wukv = io.inp("mla_w_ukv", [128, 512])
    qqk = io.inp("mla_q_qk_g", [96])
    kqk = io.inp("mla_k_qk_g", [96])
    inv_ret4 = io.inp("inv_ret4", [128, 1])
    inv_mla = io.inp("inv_mla", [16])
    attq = io.out("attq", [4, 96, ntok], BF16)
    attk = io.out("attk", [4, 96, ntok], BF16)
    attv = io.out("attv", [4, ntok, 64], BF16)
    retq = io.out("retq", [256, ntok], BF16)
    retk = io.out("retk", [256, ntok], BF16)
    retv = io.out("retv", [ntok, 256], BF16)
    lrux = io.out("lrux", [256, ntok], F32)
    cvx = io.out("cvx", [256, ntok], F32)
    bgate = io.out("bgate", [256, ntok], F32)
    sgate = io.out("sgate", [256, ntok], F32)
    ggate = io.out("ggate", [256, ntok], F32)

    psT = [P.ps("psT%d" % i, [128, 1024], BF16) for i in range(2)]
    psF = [P.ps("psF%d" % i, [128, 512], F32) for i in range(3)]
    psU = P.ps("psU", [128, 512], F32)
    psQ = P.ps("psQ", [128, 512], F32)
    psX = P.ps("psX", [128, 1024], BF16)
    fi = [0]

    def nextF():
        p = psF[fi[0] % 3]
        fi[0] += 1
        return p

    idf, idb = make_identities(P)
    P.negpi = P.sb("negpi", [128, 1], F32)
    P.I("vector", "memset", ap=P.negpi[:], constant=-math.pi, _writes=[P.negpi[:]])
    neghalf = P.sb("neghalf", [128, 16], F32)
    P.I("vector", "memset", ap=neghalf[:], constant=-0.5, _writes=[neghalf[:]])

    mod = compute_mod(P, c_in, adaw, adab, psF[0], psF[1], 0, 2 * D)
    gm = P.sb("gm", [128, D], F32)
    P.dma("sync", gm[:], gmix.partition_broadcast(128))
    P.I("vector", "scalar_tensor_tensor", out=gm[:], in0=mod[:, D:2 * D], scalar=1.0, in1=gm[:], op0=ALU.add, op1=ALU.mult)
    shm = mod[:, 0:D]

    w_bf = P.sb("w_bf", [128, 8, IN_COLS], BF16)
    wv = w_in.rearrange("(k p) n -> p k n", p=128)
    for k in range(8):
        for hh in range(2):
            P.dma("gpsimd", w_bf[:, k, hh * 1328:(hh + 1) * 1328], wv[:, k, hh * 1328:(hh + 1) * 1328])
    w_rot = P.sb("w_rot", [128, 8, 512], BF16)
    for k in range(8):
        src = w_bf[:, k, 1120:1632].rearrange("p (h two i) -> p h two i", two=2, i=32)
        dst = w_rot[:, k, :].rearrange("p (h two i) -> p h two i", two=2, i=32)
        P.I("vector", "tensor_scalar", out=dst[:, :, 0, :], in0=src[:, :, 1, :], scalar1=-1.0, scalar2=None, op0=ALU.mult)
        P.I("vector", "tensor_copy", out=dst[:, :, 1, :], in_=src[:, :, 0, :])
    wuq_bf = P.sb("wuq_bf", [128, 2, 384], BF16)
    P.dma("gpsimd", wuq_bf[:, 0, :], wuq[0:128, :])
    P.dma("gpsimd", wuq_bf[0:64, 1, :], wuq[128:192, :])
    wukv_bf = P.sb("wukv_bf", [128, 512], BF16)
    P.dma("gpsimd", wukv_bf[:], wukv)
    qng_bc = P.sb("qng_bc", [128, 192], F32)
    kvng_bc = P.sb("kvng_bc", [128, 128], F32)
    qqk_bc = P.sb("qqk_bc", [128, 96], F32)
    kqk_bc = P.sb("kqk_bc", [128, 96], F32)
    P.dma("sync", qng_bc[:], qng.partition_broadcast(128))
    P.dma("sync", kvng_bc[:], kvng.partition_broadcast(128))
    P.dma("sync", qqk_bc[:], qqk.partition_broadcast(128))
    P.dma("sync", kqk_bc[:], kqk.partition_broadcast(128))

    invc = P.sb("invc", [128, 1], F32)
    P.dma("sync", invc[:], inv_ret4)
    posi = P.sb("posi", [128, 512], I32)
    angt = P.sb("angt", [128, 512], F32)
    tmpa = P.sb("tmpa", [128, 512], F32)
    tmpai = P.sb("tmpai", [128, 512], I32)
    cos_r = Rot(P, "cosR", [128, 512], F32, 2)
    sin_r = Rot(P, "sinR", [128, 512], F32, 2)
    invm = P.sb("invm", [128, 16], F32)
    P.dma("sync", invm[:], inv_mla.partition_broadcast(128))
    posc_i = P.sb("posc_i", [128, 4], I32)
    posc = P.sb("posc", [128, 4], F32)
    angm = P.sb("angm", [128, 4, 16], F32)
    tmpm = P.sb("tmpm", [128, 4, 16], F32)
    tmpmi = P.sb("tmpmi", [128, 4, 16], I32)
    cosM_r = Rot(P, "cosM", [128, 4, 16], F32, 2)
    sinM_r = Rot(P, "sinM", [128, 4, 16], F32, 2)

    xt_r = Rot(P, "xt", [128, D], F32, 3)
    junk = P.sb("junk", [128, D], BF16)
    ssq = Rot(P, "ssq", [128, 4], F32, 2)
    v4 = Rot(P, "v4", [128, 4], F32, 2)
    rstd4 = Rot(P, "rstd4", [128, 4], F32, 2)
    tmp_r = Rot(P, "tmpx", [128, D], F32, 1)
    hb_r = Rot(P, "hb", [128, D], BF16, 2)
    hT_r = Rot(P, "hT", [128, 8, 512], BF16, 2)
    ev_r = Rot(P, "ev", [128, 512], F32, 4)
    evb_r = Rot(P, "evb", [128, 512], BF16, 4)
    csb_r = Rot(P, "csb", [128, 512], F32, 2)

    for st in range(NST):
        t0 = st * 512
        hT = hT_r.next()
        for j in range(4):
            xt = xt_r.next()
            P.dma("sync", xt[:], x_in[t0 + j * 128:t0 + (j + 1) * 128, :])
            sq = ssq.next()
            P.I("scalar", "activation", out=junk[:], in_=xt[:], func=AF.Square, accum_out=sq[:, 0:1])
            v = v4.next()
            rs = rstd4.next()
            P.I("vector", "tensor_scalar", out=v[:, 0:1], in0=sq[:, 0:1], scalar1=1.0 / D, scalar2=EPS, op0=ALU.mult, op1=ALU.add)
            P.I("gpsimd", "tensor_tensor", out=rs[:, 0:1], in0=v[:, 0:1], in1=neghalf[:, 0:1], op=ALU.pow)
            tm = tmp_r.next()
            hb = hb_r.next()
            P.I("vector", "scalar_tensor_tensor", out=tm[:], in0=xt[:], scalar=rs[:, 0:1], in1=gm[:],
                op0=ALU.mult, op1=ALU.mult)
            P.I("vector", "tensor_tensor", out=hb[:], in0=tm[:], in1=shm, op=ALU.add)
            pt = psT[j % 2]
            for k in range(8):
                P.I("tensor", "transpose", out=pt[:, k * 128:(k + 1) * 128], in_=hb[:, k * 128:(k + 1) * 128], identity=idb[:])
            P.I("scalar", "copy", out=hT[:, :, j * 128:(j + 1) * 128], in_=pt[:].rearrange("p (k t) -> p k t", k=8))
        cosR = cos_r.next()
        sinR = sin_r.next()
        P.dma("scalar", posi[:], pos_in[t0:t0 + 512].partition_broadcast(128))
        P.I("vector", "tensor_copy", out=angt[:], in_=posi[:])
        P.I("vector", "tensor_scalar", out=angt[:], in0=angt[:], scalar1=invc[:, 0:1], scalar2=None, op0=ALU.mult)
        sin_of(P, sinR[:], angt[:], tmpa[:], tmpai[:], 0.0)
        sin_of(P, cosR[:], angt[:], tmpa[:], tmpai[:], 0.5 * math.pi)
        cosM = cosM_r.next()
        sinM = sinM_r.next()
        P.dma("scalar", posc_i[:], pos_in[t0:t0 + 512].rearrange("(n p) -> p n", p=128), allow_slow_non_contiguous=True)
        P.I("vector", "tensor_copy", out=posc[:], in_=posc_i[:])
        P.I("vector", "tensor_tensor", out=angm[:], in0=posc[:].unsqueeze(2).to_broadcast([128, 4, 16]),
            in1=invm[:].unsqueeze(1).to_broadcast([128, 4, 16]), op=ALU.mult)
        sin_of(P, sinM[:], angm[:], tmpm[:], tmpmi[:], 0.0)
        sin_of(P, cosM[:], angm[:], tmpm[:], tmpmi[:], 0.5 * math.pi)

        def fm(wt, c0):
            ps = nextF()
            for k in range(8):
                P.I("tensor", "matmul", out=ps[:], lhsT=wt[:, k, c0:c0 + 128], rhs=hT[:, k, :], start=(k == 0), stop=(k == 7))
            return ps

        for ch in range(2):
            ps = fm(w_bf, ch * 128)
            e = ev_r.next()
            P.I("scalar", "copy", out=e[:], in_=ps[:])
            P.dma("sync", bgate[ch * 128:(ch + 1) * 128, t0:t0 + 512], e[:])
            psc = fm(w_bf, 256 + ch * 128)
            cs = csb_r.next()
            P.I("scalar", "copy", out=cs[:], in_=psc[:])
            psx = fm(w_bf, 512 + ch * 128)
            e = ev_r.next()
            P.I("vector", "tensor_tensor", out=e[:], in0=psx[:], in1=cs[:], op=ALU.mult)
            P.dma("sync", cvx[ch * 128:(ch + 1) * 128, t0:t0 + 512], e[:])
        for qk in range(2):
            for ch in range(2):
                c0 = 1120 + qk * 256 + ch * 128
                ps = fm(w_bf, c0)
                cs = csb_r.next()
                P.I("vector", "scalar_tensor_tensor", out=cs[:], in0=ps[:], scalar=(1.0 if qk == 0 else 0.125),
                    in1=cosR[:], op0=ALU.mult, op1=ALU.mult)
                psr = fm(w_rot, qk * 256 + ch * 128)
                e = ev_r.next()
                P.I("vector", "scalar_tensor_tensor", out=e[:], in0=psr[:], scalar=(1.0 if qk == 0 else 0.125),
                    in1=sinR[:], op0=ALU.mult, op1=ALU.mult)
                eb = evb_r.next()
                P.I("vector", "tensor_tensor", out=eb[:], in0=e[:], in1=cs[:], op=ALU.add)
                P.dma("sync", (retq if qk == 0 else retk)[ch * 128:(ch + 1) * 128, t0:t0 + 512], eb[:])
        for ch in range(2):
            ps = fm(w_bf, 1120 + 768 + ch * 128)
            e = ev_r.next()
            P.I("scalar", "activation", out=e[:], in_=ps[:], func=AF.Silu)
            P.dma("sync", sgate[ch * 128:(ch + 1) * 128, t0:t0 + 512], e[:])
        for ch in range(2):
            ps = fm(w_bf, 2144 + ch * 128)
            e = ev_r.next()
            P.I("scalar", "copy", out=e[:], in_=ps[:])
            P.dma("sync", lrux[ch * 128:(ch + 1) * 128, t0:t0 + 512], e[:])
        for ch in range(2):
            ps = fm(w_bf, 2144 + 256 + ch * 128)
            e = ev_r.next()
            P.I("scalar", "activation", out=e[:], in_=ps[:], func=AF.Gelu)
            P.dma("sync", ggate[ch * 128:(ch + 1) * 128, t0:t0 + 512], e[:])
        for j in range(4):
            tt = t0 + j * 128
            ti = tt // 128
            hTj = hT[:, :, j * 128:(j + 1) * 128]
            ps = nextF()
            for k in range(8):
                P.I("tensor", "matmul", out=ps[:, 0:256], lhsT=hT[:, k, j * 128:(j + 1) * 128], rhs=w_bf[:, k, 1632:1888],
                    start=(k == 0), stop=(k == 7))
            eb = evb_r.next()
            P.I("scalar", "copy", out=eb[:, 0:256], in_=ps[:, 0:256])
            P.dma("sync", retv[tt:tt + 128, :], eb[:, 0:256])
            mla_tile(P, locals(), tt, j, j)


def mla_tile(P, L, tt, ti, j):
    hT, w_bf, psU, psQ, psX = L["hT"], L["w_bf"], L["psU"], L["psQ"], L["psX"]
    idb, neghalf = L["idb"], L["neghalf"]
    qng_bc, kvng_bc, qqk_bc, kqk_bc = L["qng_bc"], L["kvng_bc"], L["qqk_bc"], L["kqk_bc"]
    wuq_bf, wukv_bf, cosM, sinM = L["wuq_bf"], L["wukv_bf"], L["cosM"], L["sinM"]
    attq, attk, attv = L["attq"], L["attk"], L["attv"]
    if not hasattr(P, "mla_tiles"):
        P.mla_tiles = dict(
            junk=P.sb("mjunk", [128, 512], F32),
            st3=Rot(P, "mst3", [128, 16], F32, 2),
            rs3=Rot(P, "mrs3", [128, 16], F32, 2),
            cqn=Rot(P, "mcqn", [128, 320], BF16, 2),
            cT=Rot(P, "mcT", [128, 384], BF16, 2),
            qsb=Rot(P, "mqsb", [128, 384], F32, 2),
            kvsb=Rot(P, "mkvsb", [128, 512], F32, 2),
            sq=Rot(P, "msq", [128, 512], F32, 4),
            Qt=Rot(P, "mQt", [128, 4, 96], BF16, 2),
            Kt=Rot(P, "mKt", [128, 4, 96], BF16, 2),
            Vt=Rot(P, "mVt", [128, 4, 64], BF16, 2),
            r1=Rot(P, "mr1", [128, 4, 32], F32, 2),
            r2=Rot(P, "mr2", [128, 4, 16], F32, 4),
            kr=Rot(P, "mkr", [128, 32], F32, 2),
            kr2=Rot(P, "mkr2", [128, 32], F32, 2),
            QT=Rot(P, "mQT", [96, 4, 128], BF16, 2),
            KT=Rot(P, "mKT", [96, 4, 128], BF16, 2),
            scl=P.sb("mscl", [128, 3], F32),
        )
        sc = P.mla_tiles["scl"]
        P.I("vector", "memset", ap=sc[:, 0:1], constant=1.0 / 192, _writes=[sc[:]])
        P.I("vector", "memset", ap=sc[:, 1:2], constant=1.0 / 128, _writes=[sc[:]])
        P.I("vector", "memset", ap=sc[:, 2:3], constant=1.0 / 32, _writes=[sc[:]])
    M = P.mla_tiles
    for k in range(8):
        P.I("tensor", "matmul", out=psU[:, 0:352], lhsT=hT[:, k, j * 128:(j + 1) * 128], rhs=w_bf[:, k, 768:1120],
            start=(k == 0), stop=(k == 7))
    st3 = M["st3"].next()
    rs3 = M["rs3"].next()
    P.I("scalar", "activation", out=M["junk"][:, 0:192], in_=psU[:, 0:192], func=AF.Square, accum_out=st3[:, 0:1])
    P.I("scalar", "activation", out=M["junk"][:, 0:128], in_=psU[:, 192:320], func=AF.Square, accum_out=st3[:, 1:2])
    P.I("scalar", "activation", out=M["junk"][:, 0:32], in_=psU[:, 320:352], func=AF.Square, accum_out=st3[:, 2:3])
    P.I("vector", "tensor_tensor", out=st3[:, 0:3], in0=st3[:, 0:3], in1=M["scl"][:], op=ALU.mult)
    P.I("vector", "tensor_scalar", out=st3[:, 0:3], in0=st3[:, 0:3], scalar1=EPS, scalar2=None, op0=ALU.add)
    P.I("gpsimd", "tensor_tensor", out=rs3[:, 0:3], in0=st3[:, 0:3], in1=neghalf[:, 0:3], op=ALU.pow)
    cqn = M["cqn"].next()
    P.I("vector", "scalar_tensor_tensor", out=cqn[:, 0:192], in0=psU[:, 0:192], scalar=rs3[:, 0:1], in1=qng_bc[:],
        op0=ALU.mult, op1=ALU.mult)
    P.I("vector", "scalar_tensor_tensor", out=cqn[:, 192:320], in0=psU[:, 192:320], scalar=rs3[:, 1:2], in1=kvng_bc[:],
        op0=ALU.mult, op1=ALU.mult)
    kr = M["kr"].next()
    P.I("vector", "scalar_tensor_tensor", out=kr[:], in0=psU[:, 320:352], scalar=rs3[:, 2:3], in1=kqk_bc[:, 64:96],
        op0=ALU.mult, op1=ALU.mult)
    P.I("tensor", "transpose", out=psX[:, 0:128], in_=cqn[:, 0:128], identity=idb[:])
    P.I("tensor", "transpose", out=psX[0:64, 128:256], in_=cqn[:, 128:192], identity=idb[:])
    P.I("tensor", "transpose", out=psX[:, 256:384], in_=cqn[:, 192:320], identity=idb[:])
    cT = M["cT"].next()
    P.I("scalar", "copy", out=cT[:, 0:128], in_=psX[:, 0:128])
    P.I("scalar", "copy", out=cT[0:64, 128:256], in_=psX[0:64, 128:256])
    P.I("scalar", "copy", out=cT[:, 256:384], in_=psX[:, 256:384])
    P.I("tensor", "matmul", out=psQ[:, 0:384], lhsT=cT[:, 0:128], rhs=wuq_bf[:, 0, :], start=True, stop=False)
    P.I("tensor", "matmul", out=psQ[:, 0:384], lhsT=cT[0:64, 128:256], rhs=wuq_bf[0:64, 1, :], start=False, stop=True)
    qsb = M["qsb"].next()
    sq = M["sq"].next()
    P.I("scalar", "copy", out=qsb[:], in_=psQ[:, 0:384])
    P.I("scalar", "activation", out=sq[:, 0:384], in_=psQ[:, 0:384], func=AF.Square)
    P.I("tensor", "matmul", out=psQ[:, 0:512], lhsT=cT[:, 256:384], rhs=wukv_bf[:], start=True, stop=True)
    kvsb = M["kvsb"].next()
    P.I("scalar", "copy", out=kvsb[:], in_=psQ[:, 0:512])
    st8 = M["st3"].next()
    rs8 = M["rs3"].next()
    sqv = sq[:, 0:384].rearrange("p (h c) -> p h c", c=96)
    P.I("vector", "tensor_reduce", out=st8[:, 0:4], in_=sqv[:, :, 0:64], axis=AX.X, op=ALU.add)
    P.I("vector", "tensor_reduce", out=st8[:, 4:8], in_=sqv[:, :, 64:96], axis=AX.X, op=ALU.add)
    sq2 = M["sq"].next()
    P.I("scalar", "activation", out=sq2[:], in_=kvsb[:], func=AF.Square)
    P.I("vector", "tensor_reduce", out=st8[:, 8:12], in_=sq2[:].rearrange("p (h c) -> p h c", c=128)[:, :, 0:64],
        axis=AX.X, op=ALU.add)
    P.I("vector", "tensor_scalar", out=st8[:, 0:4], in0=st8[:, 0:4], scalar1=1.0 / 64, scalar2=EPS, op0=ALU.mult, op1=ALU.add)
    P.I("vector", "tensor_scalar", out=st8[:, 4:8], in0=st8[:, 4:8], scalar1=1.0 / 32, scalar2=EPS, op0=ALU.mult, op1=ALU.add)
    P.I("vector", "tensor_scalar", out=st8[:, 8:12], in0=st8[:, 8:12], scalar1=1.0 / 64, scalar2=EPS, op0=ALU.mult, op1=ALU.add)
    P.I("gpsimd", "tensor_tensor", out=rs8[:, 0:12], in0=st8[:, 0:12], in1=neghalf[:, 0:12], op=ALU.pow)
    Qt = M["Qt"].next()
    Kt = M["Kt"].next()
    Vt = M["Vt"].next()
    qv = qsb[:].rearrange("p (h c) -> p h c", c=96)
    kvv = kvsb[:].rearrange("p (h c) -> p h c", c=128)
    r1 = M["r1"].next()
    tq = M["sq"].next()
    tqv = tq[:, 0:256].rearrange("p (h c) -> p h c", c=64)
    P.I("vector", "tensor_tensor", out=tqv, in0=qv[:, :, 0:64], in1=rs8[:, 0:4].unsqueeze(2).to_broadcast([128, 4, 64]), op=ALU.mult)
    P.I("vector", "tensor_tensor", out=Qt[:, :, 0:64], in0=tqv, in1=qqk_bc[:, 0:64].unsqueeze(1).to_broadcast([128, 4, 64]), op=ALU.mult)
    P.I("vector", "tensor_tensor", out=r1[:], in0=qv[:, :, 64:96], in1=rs8[:, 4:8].unsqueeze(2).to_broadcast([128, 4, 32]), op=ALU.mult)
    P.I("vector", "tensor_tensor", out=r1[:], in0=r1[:], in1=qqk_bc[:, 64:96].unsqueeze(1).to_broadcast([128, 4, 32]), op=ALU.mult)
    cb = cosM[:, ti, :].unsqueeze(1).to_broadcast([128, 4, 16])
    sb_ = sinM[:, ti, :].unsqueeze(1).to_broadcast([128, 4, 16])
    a1, a2, a3, a4 = M["r2"].next(), M["r2"].next(), M["r2"].next(), M["r2"].next()
    P.I("vector", "tensor_tensor", out=a1[:], in0=r1[:, :, 0:16], in1=cb, op=ALU.mult)
    P.I("vector", "tensor_tensor", out=a2[:], in0=r1[:, :, 16:32], in1=sb_, op=ALU.mult)
    P.I("vector", "tensor_tensor", out=a3[:], in0=r1[:, :, 16:32], in1=cb, op=ALU.mult)
    P.I("vector", "tensor_tensor", out=a4[:], in0=r1[:, :, 0:16], in1=sb_, op=ALU.mult)
    P.I("vector", "tensor_tensor", out=Qt[:, :, 64:80], in0=a1[:], in1=a2[:], op=ALU.subtract)
    P.I("vector", "tensor_tensor", out=Qt[:, :, 80:96], in0=a3[:], in1=a4[:], op=ALU.add)
    tk = M["sq"].next()
    tkv = tk[:, 0:256].rearrange("p (h c) -> p h c", c=64)
    P.I("vector", "tensor_tensor", out=tkv, in0=kvv[:, :, 0:64], in1=rs8[:, 8:12].unsqueeze(2).to_broadcast([128, 4, 64]), op=ALU.mult)
    P.I("vector", "tensor_tensor", out=Kt[:, :, 0:64], in0=tkv, in1=kqk_bc[:, 0:64].unsqueeze(1).to_broadcast([128, 4, 64]), op=ALU.mult)
    kr2 = M["kr2"].next()
    b1, b2, b3, b4 = M["r2"].next(), M["r2"].next(), M["r2"].next(), M["r2"].next()
    P.I("vector", "tensor_tensor", out=b1[:, 0, :], in0=kr[:, 0:16], in1=cosM[:, ti, :], op=ALU.mult)
    P.I("vector", "tensor_tensor", out=b2[:, 0, :], in0=kr[:, 16:32], in1=sinM[:, ti, :], op=ALU.mult)
    P.I("vector", "tensor_tensor", out=b3[:, 0, :], in0=kr[:, 16:32], in1=cosM[:, ti, :], op=ALU.mult)
    P.I("vector", "tensor_tensor", out=b4[:, 0, :], in0=kr[:, 0:16], in1=sinM[:, ti, :], op=ALU.mult)
    P.I("vector", "tensor_tensor", out=kr2[:, 0:16], in0=b1[:, 0, :], in1=b2[:, 0, :], op=ALU.subtract)
    P.I("vector", "tensor_tensor", out=kr2[:, 16:32], in0=b3[:, 0, :], in1=b4[:, 0, :], op=ALU.add)
    P.I("vector", "tensor_copy", out=Kt[:, :, 64:96], in_=kr2[:].unsqueeze(1).to_broadcast([128, 4, 32]))
    P.I("vector", "tensor_copy", out=Vt[:], in_=kvv[:, :, 64:128])
    P.dma("sync", attv[:, tt:tt + 128, :].rearrange("h t c -> t h c"), Vt[:])
    for h in range(4):
        P.I("tensor", "transpose", out=psX[0:96, 384 + h * 128:384 + (h + 1) * 128], in_=Qt[:, h, :], identity=idb[:])
    QT = M["QT"].next()
    P.I("scalar", "copy", out=QT[:], in_=psX[0:96, 384:896].rearrange("p (h t) -> p h t", h=4))
    P.dma("sync", attq[:, :, tt:tt + 128].rearrange("h c t -> c h t"), QT[:])
    for h in range(4):
        P.I("tensor", "transpose", out=psX[0:96, 384 + h * 128:384 + (h + 1) * 128], in_=Kt[:, h, :], identity=idb[:])
    KT = M["KT"].next()
    P.I("scalar", "copy", out=KT[:], in_=psX[0:96, 384:896].rearrange("p (h t) -> p h t", h=4))
    P.dma("sync", attk[:, :, tt:tt + 128].rearrange("h c t -> c h t"), KT[:])


ATT_SCALE = 96.0 ** -0.5


def build_p2(SS=S):
    nc = new_nc()
    P = Prog(nc)
    io = IO(nc)
    phase2(P, io, SS)
    P.finish(io.outs)
    P.emit()
    return nc


def phase2(P, io, SS, after_setup=None):
    nc = P.nc
    NB = SS // 128
    aq = io.inp("aq", [96, SS], BF16)
    ak = io.inp("ak", [96, SS], BF16)
    av = io.inp("av", [SS, 64], BF16)
    rq = io.inp("rq", [64, SS], BF16)
    rk = io.inp("rk", [64, SS], BF16)
    rv = io.inp("rv", [SS, 64], BF16)
    lx = io.inp("lx", [64, SS])
    cx = io.inp("cx", [64, SS])
    convw = io.inp("convw", [64, 3])
    lcw = io.inp("lcw", [64, 4])
    lcb = io.inp("lcb", [64, 1])
    wa = io.inp("wa", [64, 64])
    ba = io.inp("ba", [64, 1])
    wx = io.inp("wx", [64, 64])
    bx = io.inp("bx", [64, 1])
    lam = io.inp("lam", [64, 1])
    qqk = io.inp("qqk", [96])
    kqk = io.inp("kqk", [96])
    innerT = io.inp("innerT", [128, 128])
    qdecT = io.inp("qdecT", [64, 128])
    kdec = io.inp("kdec", [128, 1])
    cdec = io.inp("cdec", [64, 1])
    ao = io.out("ao", [64, SS])
    ro = io.out("ro", [64, SS])
    lo = io.out("lo", [64, SS])
    co = io.out("co", [64, SS])

    psS = [P.ps("psS%d" % i, [128, 512], F32) for i in range(3)]
    psO = [P.ps("psO%d" % i, [128, 512], F32) for i in range(2)]
    psB = P.ps("psB", [128, 512], F32)
    psR = [P.ps("psR%d" % i, [128, 512], F32) for i in range(2)]
    psKT = psB[:].bitcast(BF16)

    idf, idb = make_identities(P)
    half = P.sb("half", [128, 512], F32)
    P.I("vector", "memset", ap=half[:], constant=0.5, _writes=[half[:]])
    neghalf = P.sb("neghalf", [128, 512], F32)
    P.I("vector", "memset", ap=neghalf[:], constant=-0.5, _writes=[neghalf[:]])
    ones64 = P.sb("ones64", [128, 64], F32)
    P.I("vector", "memset", ap=ones64[:], constant=1.0 / 64, _writes=[ones64[:]])
    ones1 = P.sb("ones1", [128, 64], F32)
    P.I("vector", "memset", ap=ones1[:], constant=1.0, _writes=[ones1[:]])
    epsc = P.sb("epsc", [128, 1], F32)
    P.I("vector", "memset", ap=epsc[:], constant=EPS, _writes=[epsc[:]])

    cw = P.sb("cw", [64, 3], F32)
    P.dma("sync", cw[:], convw, allow_slow_non_contiguous=True)
    CSEG = min(2048, SS)
    cxt = P.sb("cxt", [64, 2 + CSEG], F32)
    cacc = Rot(P, "cacc", [64, CSEG], F32, 2)
    P.I("vector", "memset", ap=cxt[:, 0:2], constant=0.0, _writes=[cxt[:]])
    for sg in range(SS // CSEG):
        c0 = sg * CSEG
        if sg > 0:
            P.I("vector", "tensor_copy", out=cxt[:, 0:2], in_=cxt[:, CSEG:CSEG + 2])
        P.dma("sync", cxt[:, 2:2 + CSEG], cx[:, c0:c0 + CSEG])
        acc = cacc.next()
        P.I("vector", "tensor_scalar", out=acc[:], in0=cxt[:, 2:2 + CSEG], scalar1=cw[:, 2:3], scalar2=None, op0=ALU.mult)
        P.I("vector", "scalar_tensor_tensor", out=acc[:], in0=cxt[:, 1:1 + CSEG], scalar=cw[:, 1:2], in1=acc[:], op0=ALU.mult, op1=ALU.add)
        P.I("vector", "scalar_tensor_tensor", out=acc[:], in0=cxt[:, 0:CSEG], scalar=cw[:, 0:1], in1=acc[:], op0=ALU.mult, op1=ALU.add)
        P.dma("gpsimd", co[:, c0:c0 + CSEG], acc[:])

    lw = P.sb("lw", [64, 4], F32)
    lb = P.sb("lb", [64, 1], F32)
    bat = P.sb("bat", [64, 1], F32)
    bxt = P.sb("bxt", [64, 1], F32)
    lamt = P.sb("lamt", [64, 1], F32)
    nsp = P.sb("nsp", [64, 1], F32)
    wa_bf = P.sb("wa_bf", [64, 64], BF16)
    wx_bf = P.sb("wx_bf", [64, 64], BF16)
    P.dma("sync", lw[:], lcw, allow_slow_non_contiguous=True)
    P.dma("sync", lb[:], lcb, allow_slow_non_contiguous=True)
    P.dma("sync", bat[:], ba, allow_slow_non_contiguous=True)
    P.dma("sync", bxt[:], bx, allow_slow_non_contiguous=True)
    P.dma("sync", lamt[:], lam, allow_slow_non_contiguous=True)
    P.dma("gpsimd", wa_bf[:], wa)
    P.dma("gpsimd", wx_bf[:], wx)
    P.I("scalar", "activation", out=nsp[:], in_=lamt[:], func=AF.Exp, scale=-1.0)
    P.I("scalar", "activation", out=nsp[:], in_=nsp[:], func=AF.Ln, bias=ones1[0:64, 0:1], scale=1.0)
    P.I("vector", "tensor_scalar", out=nsp[:], in0=nsp[:], scalar1=-8.0, scalar2=None, op0=ALU.mult)
    LSEG = min(1024, SS)
    NHF = LSEG // 512
    lxt = P.sb("lxt", [64, 3 + LSEG], F32)
    P.I("vector", "memset", ap=lxt[:, 0:3], constant=0.0, _writes=[lxt[:]])
    xc_r = Rot(P, "xc", [64, LSEG], F32, 1)
    xcb_r = Rot(P, "xcb", [64, LSEG], BF16, 1)
    rg_r = Rot(P, "rg", [64, LSEG], F32, 1)
    ig_r = Rot(P, "ig", [64, LSEG], F32, 1)
    a_r = Rot(P, "la", [64, LSEG], F32, 1)
    a2_r = Rot(P, "la2", [64, LSEG], F32, 1)
    b_r = Rot(P, "lbin", [64, LSEG], F32, 1)
    h_r = Rot(P, "lh", [64, LSEG], F32, 2)
    gbanks = [psS[0], psS[1], psS[2], psO[0]]
    hprev = None
    lru_state = [None]

    def lru_gen():
      for sg in range(SS // LSEG):
        hprev = lru_state[0]
        c0 = sg * LSEG
        if sg > 0:
            P.I("vector", "tensor_copy", out=lxt[:, 0:3], in_=lxt[:, LSEG:LSEG + 3])
        P.dma("sync", lxt[:, 3:3 + LSEG], lx[:, c0:c0 + LSEG])
        xc = xc_r.next()
        P.I("vector", "tensor_scalar", out=xc[:], in0=lxt[:, 3:3 + LSEG], scalar1=lw[:, 3:4], scalar2=lb[:, 0:1], op0=ALU.mult, op1=ALU.add)
        for kk in range(3):
            P.I("vector", "scalar_tensor_tensor", out=xc[:], in0=lxt[:, kk:kk + LSEG], scalar=lw[:, kk:kk + 1], in1=xc[:], op0=ALU.mult, op1=ALU.add)
        xcb = xcb_r.next()
        P.I("vector", "tensor_copy", out=xcb[:], in_=xc[:])
        rg = rg_r.next()
        ig = ig_r.next()
        for hf in range(NHF):
            hs = slice(hf * 512, (hf + 1) * 512)
            P.I("tensor", "matmul", out=psR[0][0:64, :], lhsT=wa_bf[:], rhs=xcb[:, hs], start=True, stop=True)
            P.I("tensor", "matmul", out=psR[1][0:64, :], lhsT=wx_bf[:], rhs=xcb[:, hs], start=True, stop=True)
            P.I("scalar", "activation", out=rg[:, hs], in_=psR[0][0:64, :], func=AF.Sigmoid, bias=bat[:, 0:1], scale=1.0)
            P.I("scalar", "activation", out=ig[:, hs], in_=psR[1][0:64, :], func=AF.Sigmoid, bias=bxt[:, 0:1], scale=1.0)
        a = a_r.next()
        P.I("scalar", "activation", out=a[:], in_=rg[:], func=AF.Exp, scale=nsp[:, 0:1])
        a2 = a2_r.next()
        P.I("scalar", "activation", out=a2[:], in_=a[:], func=AF.Square)
        P.I("scalar", "activation", out=a2[:], in_=a2[:], func=AF.Sqrt, bias=ones1[0:64, 0:1], scale=-1.0)
        bb = b_r.next()
        P.I("vector", "tensor_tensor", out=bb[:], in0=ig[:], in1=xc[:], op=ALU.mult)
        P.I("vector", "tensor_tensor", out=bb[:], in0=bb[:], in1=a2[:], op=ALU.mult)
        h = h_r.next()
        P.I("vector", "tensor_tensor_scan", out=h[:], data0=a[:], data1=bb[:],
            initial=(0.0 if hprev is None else hprev[:, LSEG - 1:LSEG]), op0=ALU.mult, op1=ALU.add)
        lru_state[0] = h
        P.dma("gpsimd", lo[:, c0:c0 + LSEG], h[:])
        yield

    inn = P.sb("inn", [128, 128], F32)
    qd_c = P.sb("qd_c", [64, 512], F32)
    kd_c = P.sb("kd_c", [128, 1], F32)
    cd_c = P.sb("cd_c", [64, 1], F32)
    P.dma("sync", inn[:], innerT)
    for r_ in range(4):
        P.dma("sync", qd_c[:, r_ * 128:(r_ + 1) * 128], qdecT)
    P.dma("sync", kd_c[:], kdec)
    P.dma("sync", cd_c[:], cdec)
    PC = min(32, NB)
    RG = 4
    KVa = P.sb("KVa", [64, PC, 64], F32)
    Sbf = P.sb("Sbf", [64, PC, 64], BF16)
    gam = P.sb("gam", [64, PC], F32)
    carry = P.sb("rcarry", [64, 64], F32)
    P.I("vector", "memset", ap=carry[:], constant=0.0, _writes=[carry[:]])
    P.I("vector", "memset", ap=gam[:], constant=1.0, _writes=[gam[:]])
    P.I("vector", "tensor_scalar", out=gam[:], in0=gam[:], scalar1=cd_c[:, 0:1], scalar2=None, op0=ALU.mult)
    rq_r = Rot(P, "rq_t", [64, RG * 128], BF16, 2)
    rk_r = Rot(P, "rk_t", [64, RG * 128], BF16, 2)
    rv_r = Rot(P, "rv_t", [128, RG, 64], BF16, 2)
    scm_r = Rot(P, "scm", [128, 128], BF16, 3)
    kd_r = Rot(P, "kdt", [128, 64], BF16, 3)
    qd_r = Rot(P, "qdt", [64, RG * 128], BF16, 2)
    oT_r = Rot(P, "oT", [64, RG * 128], F32, 2)
    cen_r = Rot(P, "cen", [64, RG * 128], F32, 2)
    sqr_r = Rot(P, "sqr", [64, RG * 128], F32, 2)
    def ret_gen():
      for pc in range(NB // PC):
        c0 = pc * PC
        for g in range(PC // RG):
            t0 = (c0 + g * RG) * 128
            rkt, rvt = rk_r.next(), rv_r.next()
            P.dma("sync", rkt[:], rk[:, t0:t0 + RG * 128])
            P.dma("sync", rvt[:], rv[t0:t0 + RG * 128, :].rearrange("(n p) c -> p n c", p=128))
            for ci in range(RG):
                i = g * RG + ci
                cs = slice(ci * 128, (ci + 1) * 128)
                pT = psS[i % 3][:].bitcast(BF16)
                P.I("tensor", "transpose", out=pT[:, 0:64], in_=rkt[:, cs], identity=idb[0:64, 0:64])
                kdt = kd_r.next()
                P.I("scalar", "activation", out=kdt[:], in_=pT[:, 0:64], func=AF.Copy, scale=kd_c[:, 0:1])
                pK = psO[i % 2]
                P.I("tensor", "matmul", out=pK[0:64, 0:64], lhsT=kdt[:], rhs=rvt[:, ci, :], start=True, stop=True)
                P.I("vector", "tensor_copy", out=KVa[:, i, :], in_=pK[0:64, 0:64])
            yield
        for dv in range(64):
            P.I("vector", "tensor_tensor_scan", out=KVa[:, :, dv], data0=gam[:], data1=KVa[:, :, dv], initial=carry[:, dv:dv + 1],
                op0=ALU.mult, op1=ALU.add)
        P.I("scalar", "copy", out=Sbf[:, 0, :], in_=carry[:])
        if PC > 1:
            P.I("scalar", "copy", out=Sbf[:, 1:PC, :], in_=KVa[:, 0:PC - 1, :])
        P.I("vector", "tensor_copy", out=carry[:], in_=KVa[:, PC - 1, :])
        for g in range(PC // RG):
            t0 = (c0 + g * RG) * 128
            rqt, rkt, rvt = rq_r.next(), rk_r.next(), rv_r.next()
            P.dma("sync", rqt[:], rq[:, t0:t0 + RG * 128])
            P.dma("sync", rkt[:], rk[:, t0:t0 + RG * 128])
            P.dma("sync", rvt[:], rv[t0:t0 + RG * 128, :].rearrange("(n p) c -> p n c", p=128))
            qdt = qd_r.next()
            P.I("vector", "tensor_tensor", out=qdt[:], in0=rqt[:], in1=qd_c[:], op=ALU.mult)
            oT = oT_r.next()
            for ci in range(RG):
                i = g * RG + ci
                cs = slice(ci * 128, (ci + 1) * 128)
                pA = psS[i % 3]
                P.I("tensor", "matmul", out=pA[:, 0:128], lhsT=rkt[:, cs], rhs=rqt[:, cs], start=True, stop=True)
                scm = scm_r.next()
                P.I("vector", "tensor_tensor", out=scm[:], in0=pA[:, 0:128], in1=inn[:], op=ALU.mult)
                pBk = psO[i % 2]
                P.I("tensor", "matmul", out=pBk[0:64, 0:128], lhsT=rvt[:, ci, :], rhs=scm[:], start=True, stop=False)
                P.I("tensor", "matmul", out=pBk[0:64, 0:128], lhsT=Sbf[:, i, :], rhs=qdt[:, cs], start=False, stop=True)
                P.I("scalar", "copy", out=oT[:, cs], in_=pBk[0:64, 0:128])
            W = RG * 128
            P.I("tensor", "matmul", out=psB[0:64, 0:W], lhsT=ones64[0:64, :], rhs=oT[:], start=True, stop=True)
            cen = cen_r.next()
            P.I("vector", "tensor_tensor", out=cen[:], in0=oT[:], in1=psB[0:64, 0:W], op=ALU.subtract)
            sqr = sqr_r.next()
            P.I("scalar", "activation", out=sqr[:], in_=cen[:], func=AF.Square)
            P.I("tensor", "matmul", out=psB[0:64, 0:W], lhsT=ones64[0:64, :], rhs=sqr[:], start=True, stop=True)
            P.I("scalar", "activation", out=sqr[:], in_=psB[0:64, 0:W], func=AF.Sqrt, bias=epsc[0:64, 0:1], scale=1.0)
            P.I("vector", "reciprocal", out=sqr[:], in_=sqr[:])
            P.I("vector", "tensor_tensor", out=cen[:], in0=cen[:], in1=sqr[:], op=ALU.mult)
            P.dma("gpsimd", ro[:, t0:t0 + W], cen[:])
            yield

    g_l, g_r = lru_gen(), ret_gen()
    n_l = SS // LSEG
    n_r = (NB // PC) * 2 * (PC // RG)
    per = max(1, n_r // max(1, n_l))
    done_l = done_r = False
    while not (done_l and done_r):
        if not done_l:
            try:
                next(g_l)
            except StopIteration:
                done_l = True
        for _ in range(per):
            if not done_r:
                try:
                    next(g_r)
                except StopIteration:
                    done_r = True
    QT = P.sb("QT", [96, SS], BF16)
    KT = P.sb("KT", [96, SS], BF16)
    VA = P.sb("VA", [128, NB, 65], BF16)
    LDC = min(2048, SS)
    for i in range(SS // LDC):
        P.dma("sync", KT[:, i * LDC:(i + 1) * LDC], ak[:, i * LDC:(i + 1) * LDC])
        P.dma("scalar", QT[:, i * LDC:(i + 1) * LDC], aq[:, i * LDC:(i + 1) * LDC])
    P.I("gpsimd", "memset", ap=VA[:, :, 64:65], constant=1.0, _writes=[VA[:]])
    P.dma("sync", VA[:, :, 0:64], av.rearrange("(n p) c -> p n c", p=128))
    tri_f = P.sb("tri_f", [128, 128], F32)
    tri = P.sb("tri", [128, 128], BF16)
    P.I("gpsimd", "memset", ap=tri_f[:], constant=1.0, _writes=[tri_f[:]])
    P.I("gpsimd", "affine_select", out=tri_f[:], in_=tri_f[:], pattern=[[1, 128]], compare_op=ALU.is_ge,
        fill=0.0, base=0, channel_multiplier=-1)
    P.I("vector", "tensor_copy", out=tri[:], in_=tri_f[:])
    gq = P.sb("gq_bc", [128, 96], F32)
    gk = P.sb("gk_bc", [128, 96], F32)
    m2 = P.sb("m2", [128, 2], F32)
    negc = P.sb("negc", [128, 1], F32)
    P.dma("sync", gq[:], qqk.partition_broadcast(128))
    P.dma("sync", gk[:], kqk.partition_broadcast(128))
    P.I("vector", "tensor_tensor", out=gq[:], in0=gq[:], in1=gq[:], op=ALU.mult)
    P.I("vector", "tensor_tensor", out=gk[:], in0=gk[:], in1=gk[:], op=ALU.mult)
    P.I("vector", "tensor_reduce", out=m2[:, 0:1], in_=gq[:], axis=AX.X, op=ALU.max)
    P.I("vector", "tensor_reduce", out=m2[:, 1:2], in_=gk[:], axis=AX.X, op=ALU.max)
    P.I("vector", "tensor_tensor", out=negc[:], in0=m2[:, 0:1], in1=m2[:, 1:2], op=ALU.mult)
    P.I("gpsimd", "tensor_tensor", out=negc[:], in0=negc[:], in1=half[:, 0:1], op=ALU.pow)
    P.I("vector", "tensor_scalar", out=negc[:], in0=negc[:], scalar1=-math.sqrt(96.0), scalar2=None, op0=ALU.mult)
    if after_setup is not None:
        after_setup()
    PT_r = Rot(P, "PT", [128, 512], BF16, 5)
    den_r = Rot(P, "den", [128, 512], F32, 2)
    oa_r = Rot(P, "oa", [64, 512], F32, 2)
    rb_r = Rot(P, "rb", [64, 512], F32, 2)
    si = 0
    for qs in range(SS // 512):
        q0 = qs * 512
        pO = psO[qs % 2]
        nkb = 4 * qs + 4
        pend = []
        for j in range(nkb):
            d = j - 4 * qs
            lo_c = 0 if d <= 0 else d * 128
            pS = psS[si % 3]
            si += 1
            P.I("tensor", "matmul", out=pS[:, lo_c:512], lhsT=KT[:, j * 128:(j + 1) * 128], rhs=QT[:, q0 + lo_c:q0 + 512],
                start=True, stop=True)
            if len(pend) >= 2:
                pj, plo, pPT = pend.pop(0)
                P.I("tensor", "matmul", out=pO[0:65, plo:512], lhsT=VA[:, pj, :], rhs=pPT[:, plo:512], start=(pj == 0), stop=False)
            PT = PT_r.next()
            P.I("scalar", "activation", out=PT[:, lo_c:512], in_=pS[:, lo_c:512], func=AF.Exp, bias=negc[:, 0:1], scale=ATT_SCALE)
            if d >= 0:
                P.I("vector", "tensor_tensor", out=PT[:, lo_c:lo_c + 128], in0=PT[:, lo_c:lo_c + 128], in1=tri[:], op=ALU.mult)
            pend.append((j, lo_c, PT))
        while pend:
            pj, plo, pPT = pend.pop(0)
            P.I("tensor", "matmul", out=pO[0:65, plo:512], lhsT=VA[:, pj, :], rhs=pPT[:, plo:512], start=(pj == 0), stop=(len(pend) == 0))
        den = den_r.next()
        P.I("vector", "tensor_copy", out=den[64:65, :], in_=pO[64:65, :])
        P.I("tensor", "matmul", out=psB[0:64, :], lhsT=ones1[64:65, 0:64], rhs=den[64:65, :], start=True, stop=True)
        rb = rb_r.next()
        P.I("vector", "reciprocal", out=rb[:], in_=psB[0:64, :])
        oa = oa_r.next()
        P.I("vector", "tensor_tensor", out=oa[:], in0=pO[0:64, :], in1=rb[:], op=ALU.mult)
        P.dma("sync", ao[:, q0:q0 + 512], oa[:])


def build_p3(ntok=TPC, TB=1024, NEXP=32, stop=0):
    nc = new_nc()
    P = Prog(nc)
    io = IO(nc)
    phase3(P, io, ntok, TB, NEXP)
    P.finish(io.outs)
    P.emit()
    return nc


def phase3(P, io, ntok, TB=1024, NEXP=32, stop=0):
    nc = P.nc
    x_in = io.inp("x_in", [ntok, D])
    c_in = io.inp("c_in", [D])
    adaw = io.inp("ada_w", [D, 6 * D])
    adab = io.inp("ada_b", [6 * D])
    mixo = [io.inp(n, [256, ntok]) for n in ("co", "ao", "ro", "lo")]
    gates = {0: io.inp("bgate", [256, ntok]), 2: io.inp("sgate", [256, ntok]), 3: io.inp("ggate", [256, ntok])}
    mng = io.inp("mix_norm_g", [D])
    w_out = io.inp("w_out", [D, D])
    gffn = io.inp("norm_ffn_g", [D])
    wgr = io.inp("router_group_w", [D, 4])
    bgr = io.inp("router_group_b", [4])
    wer = io.inp("router_expert_w", [D, 32])
    ber = io.inp("router_expert_b", [32])
    ewg = io.inp("exp_w_gate", [32, D, 256])
    ewu = io.inp("exp_w_up", [32, D, 256])
    ewd = io.inp("exp_w_down", [32, 256, D])
    x_out = io.out("x_out", [ntok, D])
    x1s = io.out("x1s", [ntok, D])

    bank = [P.ps("bank%d" % i, [128, 512], F32) for i in range(8)]
    idf, idb = make_identities(P)
    neghalf = P.sb("neghalf", [128, 256], F32)
    P.I("vector", "memset", ap=neghalf[:], constant=-0.5, _writes=[neghalf[:]])
    onesf = P.sb("onesf", [128, 128], F32)
    P.I("vector", "memset", ap=onesf[:], constant=1.0, _writes=[onesf[:]])
    epsc = P.sb("epsc", [128, 1], F32)
    P.I("vector", "memset", ap=epsc[:], constant=EPS, _writes=[epsc[:]])

    mod = compute_mod(P, c_in, adaw, adab, bank[0], bank[1], 2 * D, 6 * D, CH=128)
    gtm = mod[:, 0:D]
    shf = mod[:, D:2 * D]
    gf = mod[:, 2 * D:3 * D]
    gtf = mod[:, 3 * D:4 * D]
    tmo_r = Rot(P, "tmo", [128, D], F32, 2)
    gtmp = tmo_r.next()
    P.dma("sync", gtmp[:], gffn.partition_broadcast(128))
    P.I("vector", "scalar_tensor_tensor", out=gf, in0=gf, scalar=1.0, in1=gtmp[:], op0=ALU.add, op1=ALU.mult)
    mngc = P.sb("mngc", [128, 8], F32)
    P.dma("sync", mngc[:], mng.rearrange("(k p) -> p k", p=128), allow_slow_non_contiguous=True)
    wout_bf = P.sb("wout_bf", [128, 8, D], BF16)
    wov = w_out.rearrange("(k p) n -> p k n", p=128)
    for k in range(8):
        P.dma("gpsimd", wout_bf[:, k, :], wov[:, k, :])
    wr = P.sb("wr", [128, 8, 36], F32)
    P.dma("sync", wr[:, :, 0:4], wgr.rearrange("(k p) n -> p k n", p=128))
    P.dma("sync", wr[:, :, 4:36], wer.rearrange("(k p) n -> p k n", p=128))
    wr_hi = P.sb("wr_hi", [128, 8, 36], BF16)
    wr_lo = P.sb("wr_lo", [128, 8, 36], BF16)
    P.I("vector", "tensor_copy", out=wr_hi[:], in_=wr[:])
    P.I("vector", "tensor_tensor", out=wr_lo[:], in0=wr[:], in1=wr_hi[:], op=ALU.subtract)
    bg_bc = P.sb("bg_bc", [128, 4], F32)
    be_bc = P.sb("be_bc", [128, 32], F32)
    P.dma("sync", bg_bc[:], bgr.partition_broadcast(128))
    P.dma("sync", be_bc[:], ber.partition_broadcast(128))

    NSUB = TB // 128
    h2T = P.sb("h2T", [128, 8, TB], BF16)
    comb_all = P.sb("comb_all", [128, NSUB, 32], F32)
    yacc = P.sb("yacc", [128, NSUB, D], F32)

    yc_r = Rot(P, "yc", [128, 256], F32, 6)
    gt_r = Rot(P, "gtl", [128, 256], F32, 6)
    ysq_r = Rot(P, "ysq", [128, 256], F32, 8)
    ych = [P.sb("ych%d" % i, [128, 256], F32) for i in range(8)]
    rsb_r = Rot(P, "rsb", [128, 256], F32, 4)
    yT = P.sb("yT", [128, 8, 256], BF16)
    xt_r = Rot(P, "xt", [128, D], F32, 1)
    x1_r = Rot(P, "x1", [128, D], F32, 2)
    junk = P.sb("junk", [128, D], BF16)
    sm_r = Rot(P, "sm", [128, 8], F32, 4)
    h2f_r = Rot(P, "h2f", [128, D], F32, 1)
    h2Tlo = P.sb("h2Tlo", [128, 8, 128], BF16)
    hsp_r = Rot(P, "hsp", [128, D], BF16, 2)
    R8 = Rot(P, "r8", [128, 8], F32, 44)
    R4 = Rot(P, "r4", [128, 4], F32, 20)
    R1 = Rot(P, "r1", [128, 1], F32, 40)
    lg_r = Rot(P, "lgs", [128, 36], F32, 3)
    wgu_r = Rot(P, "wgu", [128, 8, 512], BF16, 2)
    wd_r = Rot(P, "wd", [128, 2, D], BF16, 2)
    sg_r = Rot(P, "sg", [128, 256], BF16, 4)
    hid_r = Rot(P, "hid", [128, 2, 256], BF16, 2)

    if "wgu_s" in io.m:
        wgu_s, wd_s = io.m["wgu_s"], io.m["wd_s"]
    else:
        wgu_s, wd_s = precast_experts(P, ewg, ewu, ewd, NEXP)

    for blk in range(ntok // TB):
        b0 = blk * TB
        for tl in range(TB // 256):
            t0 = b0 + tl * 256
            for m in range(4):
                for c in range(2):
                    ci = m * 2 + c
                    y = ych[ci]
                    if m == 1:
                        P.dma("sync", y[:], mixo[m][c * 128:(c + 1) * 128, t0:t0 + 256])
                    else:
                        yc = yc_r.next()
                        gt = gt_r.next()
                        P.dma("sync", yc[:], mixo[m][c * 128:(c + 1) * 128, t0:t0 + 256])
                        P.dma("scalar", gt[:], gates[m][c * 128:(c + 1) * 128, t0:t0 + 256])
                        P.I("vector", "tensor_tensor", out=y[:], in0=yc[:], in1=gt[:], op=ALU.mult)
            ysqs = []
            for ci in range(8):
                ysq = ysq_r.next()
                ysqs.append(ysq)
                P.I("scalar", "activation", out=ysq[:], in_=ych[ci][:], func=AF.Square)
            psss = []
            for m in range(4):
                pss = bank[m // 2][:, (m % 2) * 256:(m % 2 + 1) * 256]
                psss.append(pss)
                for c in range(2):
                    P.I("tensor", "matmul", out=pss, lhsT=onesf[:], rhs=ysqs[m * 2 + c][:], start=(c == 0), stop=(c == 1))
            rsbs = []
            for m in range(4):
                rsb = rsb_r.next()
                rsbs.append(rsb)
                P.I("scalar", "activation", out=rsb[:], in_=psss[m], func=AF.Ln, bias=epsc[:, 0:1], scale=1.0 / 256)
            for m in range(4):
                P.I("scalar", "activation", out=rsbs[m][:], in_=rsbs[m][:], func=AF.Exp, scale=-0.5)
            for m in range(4):
                for c in range(2):
                    ci = m * 2 + c
                    P.I("vector", "scalar_tensor_tensor", out=yT[:, ci, :], in0=ych[ci][:], scalar=mngc[:, ci:ci + 1], in1=rsbs[m][:],
                        op0=ALU.mult, op1=ALU.mult)
            rgens = []
            for s in range(2):
                tt = t0 + s * 128
                si = tl * 2 + s
                po = [bank[2], bank[3]]
                for hf in range(2):
                    for k in range(8):
                        P.I("tensor", "matmul", out=po[hf][:], lhsT=yT[:, k, s * 128:(s + 1) * 128], rhs=wout_bf[:, k, hf * 512:(hf + 1) * 512],
                            start=(k == 0), stop=(k == 7))
                xt = xt_r.next()
                P.dma("sync", xt[:], x_in[tt:tt + 128, :])
                tmo = tmo_r.next()
                x1 = x1_r.next()
                for hf in range(2):
                    P.I("vector", "tensor_tensor", out=tmo[:, hf * 512:(hf + 1) * 512], in0=po[hf][:], in1=gtm[:, hf * 512:(hf + 1) * 512], op=ALU.mult)
                P.I("vector", "tensor_tensor", out=x1[:], in0=tmo[:], in1=xt[:], op=ALU.add)
                P.dma("sync", x1s[tt:tt + 128, :], x1[:])
                sm = sm_r.next()
                P.I("scalar", "activation", out=junk[:], in_=x1[:], func=AF.Square, accum_out=sm[:, 0:1])
                P.I("vector", "tensor_scalar", out=sm[:, 1:2], in0=sm[:, 0:1], scalar1=1.0 / D, scalar2=EPS, op0=ALU.mult, op1=ALU.add)
                P.I("gpsimd", "tensor_tensor", out=sm[:, 2:3], in0=sm[:, 1:2], in1=neghalf[:, 0:1], op=ALU.pow)
                tm2 = tmo_r.next()
                h2f = h2f_r.next()
                P.I("vector", "scalar_tensor_tensor", out=tm2[:], in0=x1[:], scalar=sm[:, 2:3], in1=gf, op0=ALU.mult, op1=ALU.mult)
                P.I("vector", "tensor_tensor", out=h2f[:], in0=tm2[:], in1=shf, op=ALU.add)
                hhi = hsp_r.next()
                hlo = hsp_r.next()
                P.I("scalar", "copy", out=hhi[:], in_=h2f[:])
                P.I("vector", "tensor_tensor", out=hlo[:], in0=h2f[:], in1=hhi[:], op=ALU.subtract)
                b4 = bank[4][:].bitcast(BF16)
                b5 = bank[5][:].bitcast(BF16)
                for k in range(8):
                    P.I("tensor", "transpose", out=b4[:, k * 128:(k + 1) * 128], in_=hhi[:, k * 128:(k + 1) * 128], identity=idb[:])
                for k in range(8):
                    P.I("tensor", "transpose", out=b5[:, k * 128:(k + 1) * 128], in_=hlo[:, k * 128:(k + 1) * 128], identity=idb[:])
                P.I("scalar", "copy", out=h2T[:, :, si * 128:(si + 1) * 128], in_=b4.rearrange("p (k t) -> p k t", k=8))
                P.I("vector", "tensor_copy", out=h2Tlo[:], in_=b5.rearrange("p (k t) -> p k t", k=8))
                pl = bank[6]
                for k in range(8):
                    P.I("tensor", "matmul", out=pl[:, 0:36], lhsT=h2T[:, k, si * 128:(si + 1) * 128], rhs=wr_hi[:, k, :], start=(k == 0), stop=False)
                for k in range(8):
                    P.I("tensor", "matmul", out=pl[:, 0:36], lhsT=h2Tlo[:, k, :], rhs=wr_hi[:, k, :], start=False, stop=False)
                for k in range(8):
                    P.I("tensor", "matmul", out=pl[:, 0:36], lhsT=h2T[:, k, si * 128:(si + 1) * 128], rhs=wr_lo[:, k, :], start=False, stop=(k == 7))
                lg = lg_r.next()
                P.I("vector", "tensor_copy", out=lg[:], in_=pl[:, 0:36])
                rgens.append(router_math(P, lg, comb_all[:, si, :], bg_bc, be_bc, R8, R4, R1))
            live = list(rgens)
            while live:
                nxt = []
                for g_ in live:
                    try:
                        next(g_)
                        nxt.append(g_)
                    except StopIteration:
                        pass
                live = nxt
        NTL = TB // 256
        steps = [(e, tl) for e in range(NEXP) for tl in range(NTL)]
        wts = {}
        bcs = {}
        hids = {}

        def load_expert(e):
            wgu = wgu_r.next()
            wd = wd_r.next()
            P.dma("sync", wgu[:], wgu_s[e])
            P.dma("sync", wd[:], wd_s[e])
            wts[e] = (wgu, wd)

        def gu_mm(i):
            e, tl = steps[i]
            if tl == 0:
                if e not in wts:
                    load_expert(e)
            wgu, wd = wts[e]
            pg = bank[4 + 2 * (i % 2)]
            pu = bank[5 + 2 * (i % 2)]
            for fc in range(2):
                for k in range(8):
                    P.I("tensor", "matmul", out=pg[:, fc * 256:(fc + 1) * 256], lhsT=wgu[:, k, fc * 128:(fc + 1) * 128],
                        rhs=h2T[:, k, tl * 256:(tl + 1) * 256], start=(k == 0), stop=(k == 7))
                for k in range(8):
                    P.I("tensor", "matmul", out=pu[:, fc * 256:(fc + 1) * 256], lhsT=wgu[:, k, 256 + fc * 128:256 + (fc + 1) * 128],
                        rhs=h2T[:, k, tl * 256:(tl + 1) * 256], start=(k == 0), stop=(k == 7))

        def gu_ew(i):
            e, tl = steps[i]
            pg = bank[4 + 2 * (i % 2)]
            pu = bank[5 + 2 * (i % 2)]
            hid = hid_r.next()
            hids[i] = hid
            for fc in range(2):
                sg = sg_r.next()
                P.I("scalar", "activation", out=sg[:], in_=pg[:, fc * 256:(fc + 1) * 256], func=AF.Silu)
                P.I("vector", "tensor_tensor", out=hid[:, fc, :], in0=sg[:], in1=pu[:, fc * 256:(fc + 1) * 256], op=ALU.mult)

        def down(i):
            e, tl = steps[i]
            wgu, wd = wts[e]
            hid = hids.pop(i)
            for s in range(2):
                si = tl * 2 + s
                for hf in range(2):
                    py = bank[s * 2 + hf]
                    for fc in range(2):
                        P.I("tensor", "matmul", out=py[:], lhsT=hid[:, fc, s * 128:(s + 1) * 128], rhs=wd[:, fc, hf * 512:(hf + 1) * 512],
                            start=(fc == 0), stop=(fc == 1))
                    if e == 0:
                        P.I("vector", "tensor_scalar", out=yacc[:, si, hf * 512:(hf + 1) * 512], in0=py[:], scalar1=comb_all[:, si, e:e + 1],
                            scalar2=None, op0=ALU.mult)
                    else:
                        P.I("vector", "scalar_tensor_tensor", out=yacc[:, si, hf * 512:(hf + 1) * 512], in0=py[:], scalar=comb_all[:, si, e:e + 1],
                            in1=yacc[:, si, hf * 512:(hf + 1) * 512], op0=ALU.mult, op1=ALU.add)
            if tl == NTL - 1:
                wts.pop(e, None)
            if tl == 0 and e + 1 < NEXP:
                load_expert(e + 1)

        if steps:
            gu_mm(0)
            gu_ew(0)
        for i in range(len(steps)):
            if i + 1 < len(steps):
                gu_mm(i + 1)
            down(i)
            if i + 1 < len(steps):
                gu_ew(i + 1)
        for si in range(NSUB):
            tt = b0 + si * 128
            x1 = x1_r.next()
            P.dma("sync", x1[:], x1s[tt:tt + 128, :])
            tmo = tmo_r.next()
            P.I("vector", "tensor_tensor", out=tmo[:], in0=yacc[:, si, :], in1=gtf, op=ALU.mult)
            xo = xt_r.next()
            P.I("vector", "tensor_tensor", out=xo[:], in0=tmo[:], in1=x1[:], op=ALU.add)
            P.dma("sync", x_out[tt:tt + 128, :], xo[:])


RSTOP = 0


def precast_experts(P, ewg, ewu, ewd, NEXP=32):
    nc = P.nc
    wgu_s = nc.dram_tensor(P.prefix + "wgu_s", [32, 128, 8, 512], BF16).ap()
    wd_s = nc.dram_tensor(P.prefix + "wd_s", [32, 128, 2, D], BF16).ap()
    for e in range(NEXP):
        gv = ewg[e].rearrange("(k p) f -> p k f", p=128)
        uv = ewu[e].rearrange("(k p) f -> p k f", p=128)
        for k in range(8):
            P.dma("gpsimd", wgu_s[e, :, k, 0:256], gv[:, k, :])
            P.dma("gpsimd", wgu_s[e, :, k, 256:512], uv[:, k, :])
        dv = ewd[e].rearrange("(k p) n -> p k n", p=128)
        for k in range(2):
            P.dma("gpsimd", wd_s[e, :, k, :], dv[:, k, :])
    return wgu_s, wd_s


def router_math(P, lg, comb, bg_bc, be_bc, R8, R4, R1):
    V = lambda *a, **k: P.I("vector", *a, **k)
    yield
    lgg = lg[:, 0:4]
    mx = R1.next()
    V("tensor_reduce", out=mx[:], in_=lgg, axis=AX.X, op=ALU.max)
    yield
    nmx = R1.next()
    V("tensor_scalar", out=nmx[:], in0=mx[:], scalar1=-1.0, scalar2=None, op0=ALU.mult)
    yield
    eg = R4.next()
    sg = R1.next()
    P.I("scalar", "activation", out=eg[:], in_=lgg, func=AF.Exp, bias=nmx[:, 0:1], scale=1.0, accum_out=sg[:, 0:1])
    yield
    if RSTOP == 1:
        return
    rs = R1.next()
    V("reciprocal", out=rs[:], in_=sg[:])
    yield
    gp = R4.next()
    V("tensor_scalar", out=gp[:], in0=eg[:], scalar1=rs[:, 0:1], scalar2=None, op0=ALU.mult)
    yield
    sel = R4.next()
    V("tensor_tensor", out=sel[:], in0=gp[:], in1=bg_bc[:], op=ALU.add)
    yield
    m = R1.next()
    V("tensor_reduce", out=m[:], in_=sel[:], axis=AX.X, op=ALU.max)
    yield
    goh = R4.next()
    V("tensor_scalar", out=goh[:], in0=sel[:], scalar1=m[:, 0:1], scalar2=None, op0=ALU.is_equal)
    yield
    if RSTOP == 2:
        return
    gwj = R4.next()
    gw = R1.next()
    V("tensor_tensor", out=gwj[:], in0=gp[:], in1=goh[:], op=ALU.mult)
    yield
    V("tensor_reduce", out=gw[:], in_=gwj[:], axis=AX.X, op=ALU.add)
    yield
    els = R8.next()
    bes = R8.next()
    V("tensor_scalar", out=els[:], in0=lg[:, 4:12], scalar1=goh[:, 0:1], scalar2=None, op0=ALU.mult)
    yield
    V("tensor_scalar", out=bes[:], in0=be_bc[:, 0:8], scalar1=goh[:, 0:1], scalar2=None, op0=ALU.mult)
    yield
    for g in range(1, 4):
        V("scalar_tensor_tensor", out=els[:], in0=lg[:, 4 + 8 * g:12 + 8 * g], scalar=goh[:, g:g + 1], in1=els[:], op0=ALU.mult, op1=ALU.add)
        yield
        V("scalar_tensor_tensor", out=bes[:], in0=be_bc[:, 8 * g:8 * g + 8], scalar=goh[:, g:g + 1], in1=bes[:], op0=ALU.mult, op1=ALU.add)
        yield
    if RSTOP == 3:
        return
    mx8 = R1.next()
    V("tensor_reduce", out=mx8[:], in_=els[:], axis=AX.X, op=ALU.max)
    yield
    nm8 = R1.next()
    V("tensor_scalar", out=nm8[:], in0=mx8[:], scalar1=-1.0, scalar2=None, op0=ALU.mult)
    yield
    ee = R8.next()
    se = R1.next()
    P.I("scalar", "activation", out=ee[:], in_=els[:], func=AF.Exp, bias=nm8[:, 0:1], scale=1.0, accum_out=se[:, 0:1])
    yield
    rse = R1.next()
    V("reciprocal", out=rse[:], in_=se[:])
    yield
    ep = R8.next()
    V("tensor_scalar", out=ep[:], in0=ee[:], scalar1=rse[:, 0:1], scalar2=None, op0=ALU.mult)
    yield
    if RSTOP == 4:
        return
    sc = R8.next()
    V("tensor_tensor", out=sc[:], in0=ep[:], in1=bes[:], op=ALU.add)
    yield
    m1 = R1.next()
    V("tensor_reduce", out=m1[:], in_=sc[:], axis=AX.X, op=ALU.max)
    yield
    oh1 = R8.next()
    V("tensor_scalar", out=oh1[:], in0=sc[:], scalar1=m1[:, 0:1], scalar2=None, op0=ALU.is_equal)
    yield
    sc2 = R8.next()
    V("scalar_tensor_tensor", out=sc2[:], in0=oh1[:], scalar=-1e9, in1=sc[:], op0=ALU.mult, op1=ALU.add)
    yield
    m2 = R1.next()
    V("tensor_reduce", out=m2[:], in_=sc2[:], axis=AX.X, op=ALU.max)
    yield
    oh2 = R8.next()
    V("tensor_scalar", out=oh2[:], in0=sc2[:], scalar1=m2[:, 0:1], scalar2=None, op0=ALU.is_equal)
    yield
    if RSTOP == 5:
        return
    ohs = R8.next()
    V("tensor_tensor", out=ohs[:], in0=oh1[:], in1=oh2[:], op=ALU.add)
    yield
    tp = R8.next()
    V("tensor_tensor", out=tp[:], in0=ep[:], in1=ohs[:], op=ALU.mult)
    yield
    sp = R1.next()
    V("tensor_reduce", out=sp[:], in_=tp[:], axis=AX.X, op=ALU.add)
    yield
    rsp = R1.next()
    V("reciprocal", out=rsp[:], in_=sp[:])
    yield
    fac = R1.next()
    V("tensor_tensor", out=fac[:], in0=rsp[:], in1=gw[:], op=ALU.mult)
    yield
    ew = R8.next()
    V("tensor_scalar", out=ew[:], in0=tp[:], scalar1=fac[:, 0:1], scalar2=None, op0=ALU.mult)
    yield
    if RSTOP == 6:
        return
    for g in range(4):
        V("tensor_scalar", out=comb[:, g * 8:(g + 1) * 8], in0=ew[:], scalar1=goh[:, g:g + 1], scalar2=None, op0=ALU.mult)
        yield


W_SPECS = dict(
    ada_w=[2, D, 6 * D], ada_b=[2, 6 * D], norm_mix_g=[2, D], w_in=[2, D, IN_COLS], conv_w=[2, 3, 256],
    mla_q_norm_g=[2, 192], mla_w_uq=[2, 192, 384], mla_kv_norm_g=[2, 128], mla_w_ukv=[2, 128, 512],
    mla_q_qk_g=[2, 96], mla_k_qk_g=[2, 96], lru_conv_w=[2, 4, 256], lru_conv_b=[2, 256], lru_w_a=[2, 4, 64, 64],
    lru_b_a=[2, 256], lru_w_x=[2, 4, 64, 64], lru_b_x=[2, 256], lru_lambda=[2, 256], mix_norm_g=[2, D],
    w_out=[2, D, D], norm_ffn_g=[2, D], router_group_w=[2, D, 4], router_group_b=[2, 4], router_expert_w=[2, D, 32],
    router_expert_b=[2, 32], exp_w_gate=[2, 32, D, 256], exp_w_up=[2, 32, D, 256], exp_w_down=[2, 32, 256, D])


def build_fused(SS=S, NL=2, TB=1024):
    nc = new_nc()
    P = Prog(nc)
    x_in = din(nc, "x", [SS, D])
    c_in = din(nc, "c", [D])
    pos = din(nc, "positions", [SS], I32)
    W = {k: din(nc, k, shp) for k, shp in W_SPECS.items()}
    inv_ret4 = din(nc, "inv_ret4", [128, 1])
    inv_mla = din(nc, "inv_mla", [16])
    innerT = din(nc, "innerT", [4, 128, 128])
    qdecT = din(nc, "qdecT", [4, 64, 128])
    kdec = din(nc, "kdec", [4, 128, 1])
    cdec = din(nc, "cdec", [4, 64, 1])
    out = dout(nc, "out", [SS, D])

    def idr(name, shape, dt=F32):
        return nc.dram_tensor(name, list(shape), dt).ap()

    T = dict(attq=idr("i_attq", [4, 96, SS], BF16), attk=idr("i_attk", [4, 96, SS], BF16), attv=idr("i_attv", [4, SS, 64], BF16),
             retq=idr("i_retq", [256, SS], BF16), retk=idr("i_retk", [256, SS], BF16), retv=idr("i_retv", [SS, 256], BF16),
             lrux=idr("i_lrux", [256, SS]), cvx=idr("i_cvx", [256, SS]), bgate=idr("i_bgate", [256, SS]),
             sgate=idr("i_sgate", [256, SS]), ggate=idr("i_ggate", [256, SS]))
    Y = dict(co=idr("i_co", [256, SS]), ao=idr("i_ao", [256, SS]), ro=idr("i_ro", [256, SS]), lo=idr("i_lo", [256, SS]))
    x_mid = idr("i_xmid", [SS, D])
    x1s = idr("i_x1s", [SS, D])
    col = lambda ap: ap.rearrange("(c o) -> c o", o=1)
    cast = {}
    for l in range(NL):
        xs = x_in if l == 0 else x_mid
        xd = out if l == NL - 1 else x_mid
        mk = P.mark()
        P.prefix = "L%dP1_" % l
        m = dict(x_in=xs, c_in=c_in, pos_in=pos, inv_ret4=inv_ret4, inv_mla=inv_mla)
        for k in ("ada_w", "ada_b", "norm_mix_g", "w_in", "mla_q_norm_g", "mla_w_uq", "mla_kv_norm_g", "mla_w_ukv", "mla_q_qk_g", "mla_k_qk_g"):
            m[k] = W[k][l]
        m.update(T)
        phase1(P, IO(nc, m), SS)
        P.release(mk)
        for hd in range(4):
            mk = P.mark()
            P.prefix = "L%dP2h%d_" % (l, hd)
            sl = slice(hd * 64, (hd + 1) * 64)
            m = dict(aq=T["attq"][hd], ak=T["attk"][hd], av=T["attv"][hd], rq=T["retq"][sl], rk=T["retk"][sl], rv=T["retv"][:, sl],
                     lx=T["lrux"][sl], cx=T["cvx"][sl],
                     convw=W["conv_w"][l][:, sl].rearrange("k c -> c k"), lcw=W["lru_conv_w"][l][:, sl].rearrange("k c -> c k"),
                     lcb=col(W["lru_conv_b"][l][sl]), wa=W["lru_w_a"][l][hd], ba=col(W["lru_b_a"][l][sl]),
                     wx=W["lru_w_x"][l][hd], bx=col(W["lru_b_x"][l][sl]), lam=col(W["lru_lambda"][l][sl]),
                     qqk=W["mla_q_qk_g"][l], kqk=W["mla_k_qk_g"][l],
                     innerT=innerT[hd], qdecT=qdecT[hd], kdec=kdec[hd], cdec=cdec[hd],
                     ao=Y["ao"][sl], ro=Y["ro"][sl], lo=Y["lo"][sl], co=Y["co"][sl])
            if hd == 0:
                def _cast(l=l):
                    pfx = P.prefix
                    P.prefix = "L%d_" % l
                    cast[l] = precast_experts(P, W["exp_w_gate"][l], W["exp_w_up"][l], W["exp_w_down"][l])
                    P.prefix = pfx
                phase2(P, IO(nc, m), SS, after_setup=_cast)
            else:
                phase2(P, IO(nc, m), SS)
            P.release(mk)
        mk = P.mark()
        P.prefix = "L%dP3_" % l
        m = dict(x_in=xs, c_in=c_in, x_out=xd, x1s=x1s, bgate=T["bgate"], sgate=T["sgate"], ggate=T["ggate"])
        for k in ("ada_w", "ada_b", "mix_norm_g", "w_out", "norm_ffn_g", "router_group_w", "router_group_b", "router_expert_w",
                  "router_expert_b", "exp_w_gate", "exp_w_up", "exp_w_down"):
            m[k] = W[k][l]
        m.update(Y)
        m["wgu_s"], m["wd_s"] = cast[l]
        phase3(P, IO(nc, m), SS, TB, 32)
        P.release(mk)
    P.finish([out])
    P.emit()
    return nc


_NC_CACHE = {}


def _get(name, fn):
    if name not in _NC_CACHE:
        _NC_CACHE[name] = fn()
    return _NC_CACHE[name]


def _inv_freq(dim):
    return (np.float32(1.0) / (np.float32(10000.0) ** (np.arange(0, dim, 2, dtype=np.float32) / np.float32(dim)))).astype(np.float32)


def _ret_consts(hd):
    gamma = 1.0 - 2.0 ** (-5.0 - hd)
    idx = np.arange(128, dtype=np.float64)
    innerT = np.where(idx[None, :] >= idx[:, None], gamma ** np.maximum(idx[None, :] - idx[:, None], 0.0), 0.0).astype(np.float32)
    qdecT = np.ascontiguousarray(np.tile((gamma ** (idx + 1.0))[None, :], (64, 1))).astype(np.float32)
    kdec = (gamma ** (127.0 - idx)).reshape(128, 1).astype(np.float32)
    cdec = np.full((64, 1), gamma ** 128, np.float32)
    return innerT, qdecT, kdec, cdec


def kernel(**inputs):
    I = {k: np.ascontiguousarray(np.asarray(v)) for k, v in inputs.items()}
    B = I["x"].shape[0]
    nc = _get("fused", build_fused)
    rc = [_ret_consts(h) for h in range(4)]
    consts = dict(inv_ret4=np.ascontiguousarray(np.tile(_inv_freq(64), 4).reshape(128, 1)), inv_mla=_inv_freq(32),
                  innerT=np.stack([r[0] for r in rc]), qdecT=np.stack([r[1] for r in rc]),
                  kdec=np.stack([r[2] for r in rc]), cdec=np.stack([r[3] for r in rc]))
    maps = []
    for b in range(B):
        m = dict(x=np.ascontiguousarray(I["x"][b], dtype=np.float32), c=np.ascontiguousarray(I["c"][b]),
                 positions=np.ascontiguousarray(I["positions"][b]).astype(np.int32))
        for k in W_SPECS:
            m[k] = I[k]
        m.update(consts)
        maps.append(m)
    res = run_bass_kernel_spmd(nc, maps, core_ids=list(range(B))).results
    return np.stack([res[b]["out"] for b in range(B)], axis=0).astype(np.float32)
```

```python
import math
import numpy as np
import ml_dtypes
import concourse.bass as bass
import concourse.mybir as mybir
from concourse.bass_utils import run_bass_kernel_spmd

F32 = mybir.dt.float32
BF16 = mybir.dt.bfloat16
I32 = mybir.dt.int32
AF = mybir.ActivationFunctionType
ALU = mybir.AluOpType
AX = mybir.AxisListType

D = 1024
S = 16384
NCORE = 8
TPC = 4096
IN_COLS = 2656
EPS = 1e-6
TWO_PI = 2.0 * math.pi

ENGS = ["sync", "scalar", "vector", "gpsimd", "tensor"]
SAME_ENGINE_SYNC = {"sync": False, "scalar": True, "vector": True, "gpsimd": True, "tensor": False}
_APT = None


class Buf:
    __slots__ = ("name", "w", "r")

    def __init__(self, name=""):
        self.name = name
        self.w = None
        self.r = []


class Prog:
    NPOOL = 16

    def __init__(self, nc):
        self.nc = nc
        self.ops = {e: [] for e in ENGS}
        self.cnt = {e: 0 for e in ENGS}
        self.esem = {e: nc.alloc_semaphore("es_" + e) for e in ENGS}
        self.known = {e: {f: 0 for f in ENGS} for e in ENGS}
        self.snap = {e: [None] for e in ENGS}
        self.dq = ["sync", "scalar", "gpsimd"]
        self.pool = {q: [nc.alloc_semaphore("dp_%s_%d" % (q, i)) for i in range(self.NPOOL)] for q in self.dq}
        self.pool_val = {q: [0] * self.NPOOL for q in self.dq}
        self.pool_next = {q: 0 for q in self.dq}
        self.dknown = {e: {} for e in ENGS}
        self.bufs = {}
        self.uid = 0
        self.prefix = ""

    def sb(self, name, shape, dt=F32):
        return self.nc.alloc_sbuf_tensor(self.prefix + name, list(shape), dt)

    def ps(self, name, shape, dt=F32):
        return self.nc.alloc_psum_tensor(self.prefix + name, list(shape), dt)

    def mark(self):
        nc = self.nc
        return (nc.psum_base, nc.psum_top, nc.sbuf_base, nc.sbuf_top)

    def release(self, mk):
        self.barrier()
        nc = self.nc
        nc.psum_base, nc.psum_top, nc.sbuf_base, nc.sbuf_top = mk

    def barrier(self):
        for e in ENGS:
            waits = []
            for f in ENGS:
                if f != e and self.cnt[f] > 0:
                    self._need(e, ("E", f, self.cnt[f]), waits)
            for q in self.dq:
                for i in range(self.NPOOL):
                    v = self.pool_val[q][i]
                    if v > 0:
                        self._need(e, ("D", q, i, v, None), waits)
            self.ops[e].append((waits, None, None, None, 0))
        self.bufs = {}

    def buf_of(self, ap):
        n = ap.name
        b = self.bufs.get(n)
        if b is None:
            b = self.bufs[n] = Buf(n)
        return b

    def _merge(self, eng, sn):
        if sn is None:
            return
        kn = self.known[eng]
        for g, v in sn[0].items():
            if kn[g] < v:
                kn[g] = v
        dk = self.dknown[eng]
        for k, v in sn[1].items():
            if dk.get(k, 0) < v:
                dk[k] = v

    def _need(self, eng, ev, waits):
        if ev is None:
            return
        if ev[0] == "E":
            _, f, seq = ev
            if f == eng and not SAME_ENGINE_SYNC[eng]:
                return
            if self.known[eng][f] >= seq:
                return
            waits.append((self.esem[f], seq))
            self.known[eng][f] = seq
            self._merge(eng, self.snap[f][seq])
        else:
            _, q, i, val, sn = ev
            if self.dknown[eng].get((q, i), 0) >= val:
                return
            waits.append((self.pool[q][i], val))
            self.dknown[eng][(q, i)] = val
            self._merge(eng, sn)

    def _deps(self, eng, reads, writes, waits):
        for b in reads:
            self._need(eng, b.w, waits)
        for b in writes:
            self._need(eng, b.w, waits)
            for ev in b.r:
                self._need(eng, ev, waits)

    def _commit(self, ev, reads, writes):
        for b in reads:
            if b in writes:
                continue
            b.r.append(ev)
            if len(b.r) > 16:
                last = {}
                keep = []
                for e in b.r:
                    if e[0] == "E":
                        last[e[1]] = e
                    else:
                        keep.append(e)
                b.r = keep[-10:] + list(last.values())
        for b in writes:
            b.w = ev
            b.r = []

    def _scan(self, kwargs):
        reads, writes = [], []
        for k, v in kwargs.items():
            if isinstance(v, _APT):
                b = self.buf_of(v)
                if k in ("out", "accum_out", "out_max", "out_indices"):
                    if b not in writes:
                        writes.append(b)
                elif b not in reads:
                    reads.append(b)
        return reads, writes

    def I(self, eng, meth, **kwargs):
        xr = kwargs.pop("_reads", ())
        xw = kwargs.pop("_writes", ())
        reads, writes = self._scan(kwargs)
        reads += [self.buf_of(a) for a in xr]
        writes += [self.buf_of(a) for a in xw]
        waits = []
        self._deps(eng, reads, writes, waits)
        self.cnt[eng] += 1
        seq = self.cnt[eng]
        self.snap[eng].append((dict(self.known[eng]), dict(self.dknown[eng])))
        self.ops[eng].append((waits, meth, kwargs, self.esem[eng], 1))
        ev = ("E", eng, seq)
        self._commit(ev, reads, writes)
        return ev

    def dma(self, q, out, in_, **kw):
        reads = [self.buf_of(in_)]
        writes = [self.buf_of(out)]
        waits = []
        self._deps(q, reads, writes, waits)
        i = self.pool_next[q]
        self.pool_next[q] = (i + 1) % self.NPOOL
        prev = self.pool_val[q][i]
        if prev > 0 and self.dknown[q].get((q, i), 0) < prev:
            waits.append((self.pool[q][i], prev))
            self.dknown[q][(q, i)] = prev
        val = prev + 16
        self.pool_val[q][i] = val
        sn = (dict(self.known[q]), dict(self.dknown[q]))
        kw = dict(kw)
        kw["out"] = out
        kw["in_"] = in_
        self.ops[q].append((waits, "dma_start", kw, self.pool[q][i], 16))
        ev = ("D", q, i, val, sn)
        self._commit(ev, reads, writes)
        return ev

    def coll(self, kind, ins, outs, groups):
        q = "gpsimd"
        reads = [self.buf_of(a) for a in ins]
        writes = [self.buf_of(a) for a in outs]
        waits = []
        self._deps(q, reads, writes, waits)
        i = self.pool_next[q]
        self.pool_next[q] = (i + 1) % self.NPOOL
        prev = self.pool_val[q][i]
        if prev > 0 and self.dknown[q].get((q, i), 0) < prev:
            waits.append((self.pool[q][i], prev))
            self.dknown[q][(q, i)] = prev
        val = prev + 1
        self.pool_val[q][i] = val
        sn = (dict(self.known[q]), dict(self.dknown[q]))
        kw = dict(kind=kind, op=ALU.bypass, replica_groups=groups, ins=[a_.opt() for a_ in ins], outs=[a_.opt() for a_ in outs])
        self.ops[q].append((waits, "collective_compute", kw, self.pool[q][i], 1))
        ev = ("D", q, i, val, sn)
        self._commit(ev, reads, writes)
        return ev

    def finish(self, aps, eng="sync"):
        waits = []
        for a in aps:
            self._need(eng, self.buf_of(a).w, waits)
        self.ops[eng].append((waits, None, None, None, 0))

    def emit(self):
        nc = self.nc
        with nc.Block() as block:
            def mk(ename):
                def body(e):
                    for waits, meth, kw, sem, inc in self.ops[ename]:
                        for (s, v) in waits:
                            e.wait_ge(s, v)
                        if meth is not None:
                            getattr(e, meth)(**kw).then_inc(sem, inc)
                return body
            block.sync(mk("sync"))
            block.scalar(mk("scalar"))
            block.vector(mk("vector"))
            block.gpsimd(mk("gpsimd"))
            block.tensor(mk("tensor"))


class Rot:
    def __init__(self, P, name, shape, dt, n, psum=False):
        self.t = [(P.ps if psum else P.sb)("%s%d" % (name, i), shape, dt) for i in range(n)]
        self.i = 0

    def next(self):
        t = self.t[self.i % len(self.t)]
        self.i += 1
        return t


def new_nc():
    global _APT
    nc = bass.Bass("TRN2", target_bir_lowering=False)
    if _APT is None:
        t = nc.dram_tensor("apt_probe", [2, 2], F32).ap()
        _APT = type(t)
    return nc


class IO:
    def __init__(self, nc, m=None):
        self.nc = nc
        self.m = m or {}
        self.outs = []

    def inp(self, name, shape, dt=F32):
        if name in self.m:
            return self.m[name]
        return din(self.nc, name, shape, dt)

    def out(self, name, shape, dt=F32):
        if name in self.m:
            return self.m[name]
        ap = dout(self.nc, name, shape, dt)
        self.outs.append(ap)
        return ap


def din(nc, name, shape, dt=F32):
    return nc.dram_tensor(name, list(shape), dt, kind="ExternalInput").ap()


def dout(nc, name, shape, dt=F32):
    return nc.dram_tensor(name, list(shape), dt, kind="ExternalOutput").ap()


def make_identities(P):
    idf = P.sb("ident_f", [128, 128], F32)
    idb = P.sb("ident_b", [128, 128], BF16)
    P.I("gpsimd", "memset", ap=idf[:], constant=1.0, _writes=[idf[:]])
    P.I("gpsimd", "affine_select", out=idf[:], in_=idf[:], pattern=[[1, 128]], compare_op=ALU.is_equal,
        fill=0.0, base=0, channel_multiplier=-1)
    P.I("vector", "tensor_copy", out=idb[:], in_=idf[:])
    return idf, idb


def compute_mod(P, c_ap, adaw_ap, adab_ap, psA, psB, c0, c1, CH=256):
    n = c1 - c0
    mod = P.sb("mod_bc", [128, n], F32)
    ccol = P.sb("ccol", [128, 8], F32)
    cbc = P.sb("cbc", [128, 8, 128], F32)
    P.dma("sync", mod[:], adab_ap[c0:c1].partition_broadcast(128))
    P.dma("sync", ccol[:], c_ap.rearrange("(k p) -> p k", p=128), allow_slow_non_contiguous=True)
    P.I("scalar", "activation", out=ccol[:], in_=ccol[:], func=AF.Silu)
    P.I("vector", "tensor_copy", out=cbc[:], in_=ccol[:].unsqueeze(2).to_broadcast([128, 8, 128]))
    wr = Rot(P, "adaw_t", [128, 8, CH], F32, 2)
    awv = adaw_ap.rearrange("(k p) n -> p k n", p=128)
    for i in range(n // CH):
        wt = wr.next()
        P.dma("sync" if i % 2 == 0 else "scalar", wt[:], awv[:, :, c0 + i * CH:c0 + (i + 1) * CH])
        ps = psA if i % 2 == 0 else psB
        for k in range(8):
            P.I("tensor", "matmul", out=ps[:, 0:CH], lhsT=cbc[:, k, :], rhs=wt[:, k, :], start=(k == 0), stop=(k == 7))
        P.I("vector", "tensor_tensor", out=mod[:, i * CH:(i + 1) * CH], in0=mod[:, i * CH:(i + 1) * CH],
            in1=ps[:, 0:CH], op=ALU.add)
    return mod


def sin_of(P, out_ap, ang_ap, tmp_ap, tmpi_ap, shift):
    P.I("vector", "tensor_scalar", out=tmp_ap, in0=ang_ap, scalar1=1.0 / TWO_PI, scalar2=shift / TWO_PI, op0=ALU.mult, op1=ALU.add)
    P.I("vector", "tensor_copy", out=tmpi_ap, in_=tmp_ap)
    P.I("vector", "tensor_tensor", out=tmp_ap, in0=tmp_ap, in1=tmpi_ap, op=ALU.subtract)
    P.I("scalar", "activation", out=out_ap, in_=tmp_ap, func=AF.Sin, scale=TWO_PI)


def build_p1(ntok=TPC):
    nc = new_nc()
    P = Prog(nc)
    io = IO(nc)
    phase1(P, io, ntok)
    P.finish(io.outs)
    P.emit()
    return nc


def phase1(P, io, ntok):
    nc = P.nc
    if hasattr(P, "mla_tiles"):
        del P.mla_tiles
    NST = ntok // 512
    x_in = io.inp("x_in", [ntok, D])
    c_in = io.inp("c_in", [D])
    pos_in = io.inp("pos_in", [ntok], I32)
    adaw = io.inp("ada_w", [D, 6 * D])
    adab = io.inp("ada_b", [6 * D])
    gmix = io.inp("norm_mix_g", [D])
    w_in = io.inp("w_in", [D, IN_COLS])
    qng = io.inp("mla_q_norm_g", [192])
    wuq = io.inp("mla_w_uq", [192, 384])
    kvng = io.inp("mla_kv_norm_g", [128])
    wukv = io.inp("mla_w_ukv", [128, 512])
    qqk = io.inp("mla_q_qk_g", [96])
    kqk = io.inp("mla_k_qk_g", [96])
    inv_ret4 = io.inp("inv_ret4", [128, 1])
    inv_mla = io.inp("inv_mla", [16])
    attq = io.out("attq", [4, 96, ntok], BF16)
    attk = io.out("attk", [4, 96, ntok], BF16)
    attv = io.out("attv", [4, ntok, 64], BF16)
    retq = io.out("retq", [256, ntok], BF16)
    retk = io.out("retk", [256, ntok], BF16)
    retv = io.out("retv", [ntok, 256], BF16)
    lrux = io.out("lrux", [256, ntok], F32)
    cvx = io.out("cvx", [256, ntok], F32)
    bgate = io.out("bgate", [256, ntok], F32)
    sgate = io.out("sgate", [256, ntok], F32)
    ggate = io.out("ggate", [256, ntok], F32)

    psT = [P.ps("psT%d" % i, [128, 1024], BF16) for i in range(2)]
    psF = [P.ps("psF%d" % i, [128, 512], F32) for i in range(3)]
    psU = P.ps("psU", [128, 512], F32)
    psQ = P.ps("psQ", [128, 512], F32)
    psX = P.ps("psX", [128, 1024], BF16)
    fi = [0]

    def nextF():
        p = psF[fi[0] % 3]
        fi[0] += 1
        return p

    idf, idb = make_identities(P)
    P.negpi = P.sb("negpi", [128, 1], F32)
    P.I("vector", "memset", ap=P.negpi[:], constant=-math.pi, _writes=[P.negpi[:]])
    neghalf = P.sb("neghalf", [128, 16], F32)
    P.I("vector", "memset", ap=neghalf[:], constant=-0.5, _writes=[neghalf[:]])
    epsc1 = P.sb("epsc1", [128, 1], F32)
    P.I("vector", "memset", ap=epsc1[:], constant=EPS, _writes=[epsc1[:]])

    mod = compute_mod(P, c_in, adaw, adab, psF[0], psF[1], 0, 2 * D)
    gm = P.sb("gm", [128, D], F32)
    P.dma("sync", gm[:], gmix.partition_broadcast(128))
    P.I("vector", "scalar_tensor_tensor", out=gm[:], in0=mod[:, D:2 * D], scalar=1.0, in1=gm[:], op0=ALU.add, op1=ALU.mult)
    shm = mod[:, 0:D]

    w_bf = P.sb("w_bf", [128, 8, IN_COLS], BF16)
    wv = w_in.rearrange("(k p) n -> p k n", p=128)
    for k in range(8):
        for hh in range(2):
            P.dma("gpsimd", w_bf[:, k, hh * 1328:(hh + 1) * 1328], wv[:, k, hh * 1328:(hh + 1) * 1328])
    w_rot = P.sb("w_rot", [128, 8, 512], BF16)
    for k in range(8):
        src = w_bf[:, k, 1120:1632].rearrange("p (h two i) -> p h two i", two=2, i=32)
        dst = w_rot[:, k, :].rearrange("p (h two i) -> p h two i", two=2, i=32)
        P.I("vector", "tensor_scalar", out=dst[:, :, 0, :], in0=src[:, :, 1, :], scalar1=-1.0, scalar2=None, op0=ALU.mult)
        P.I("vector", "tensor_copy", out=dst[:, :, 1, :], in_=src[:, :, 0, :])
    wuq_bf = P.sb("wuq_bf", [128, 2, 384], BF16)
    P.dma("gpsimd", wuq_bf[:, 0, :], wuq[0:128, :])
    P.dma("gpsimd", wuq_bf[0:64, 1, :], wuq[128:192, :])
    wukv_bf = P.sb("wukv_bf", [128, 512], BF16)
    P.dma("gpsimd", wukv_bf[:], wukv)
    qng_bc = P.sb("qng_bc", [128, 192], F32)
    kvng_bc = P.sb("kvng_bc", [128, 128], F32)
    qqk_bc = P.sb("qqk_bc", [128, 96], F32)
    kqk_bc = P.sb("kqk_bc", [128, 96], F32)
    P.dma("sync", qng_bc[:], qng.partition_broadcast(128))
    P.dma("sync", kvng_bc[:], kvng.partition_broadcast(128))
    P.dma("sync", qqk_bc[:], qqk.partition_broadcast(128))
    P.dma("sync", kqk_bc[:], kqk.partition_broadcast(128))

    invc = P.sb("invc", [128, 1], F32)
    P.dma("sync", invc[:], inv_ret4)
    posi = P.sb("posi", [128, 512], I32)
    angt = P.sb("angt", [128, 512], F32)
    tmpa = P.sb("tmpa", [128, 512], F32)
    tmpai = P.sb("tmpai", [128, 512], I32)
    cos_r = Rot(P, "cosR", [128, 512], F32, 2)
    sin_r = Rot(P, "sinR", [128, 512], F32, 2)
    invm = P.sb("invm", [128, 16], F32)
    P.dma("sync", invm[:], inv_mla.partition_broadcast(128))
    posc_i = P.sb("posc_i", [128, 4], I32)
    posc = P.sb("posc", [128, 4], F32)
    angm = P.sb("angm", [128, 4, 16], F32)
    tmpm = P.sb("tmpm", [128, 4, 16], F32)
    tmpmi = P.sb("tmpmi", [128, 4, 16], I32)
    cosM_r = Rot(P, "cosM", [128, 4, 16], F32, 2)
    sinM_r = Rot(P, "sinM", [128, 4, 16], F32, 2)

    xt_r = Rot(P, "xt", [128, D], F32, 3)
    junk = P.sb("junk", [128, D], BF16)
    ssq = Rot(P, "ssq", [128, 4], F32, 2)
    v4 = Rot(P, "v4", [128, 4], F32, 2)
    rstd4 = Rot(P, "rstd4", [128, 4], F32, 2)
    tmp_r = Rot(P, "tmpx", [128, D], F32, 1)
    hb_r = Rot(P, "hb", [128, D], BF16, 2)
    hT_r = Rot(P, "hT", [128, 8, 512], BF16, 2)
    ev_r = Rot(P, "ev", [128, 512], F32, 4)
    evb_r = Rot(P, "evb", [128, 512], BF16, 4)
    csb_r = Rot(P, "csb", [128, 512], F32, 2)

    for st in range(NST):
        t0 = st * 512
        hT = hT_r.next()
        for j in range(4):
            xt = xt_r.next()
            P.dma("sync", xt[:], x_in[t0 + j * 128:t0 + (j + 1) * 128, :])
            sq = ssq.next()
            P.I("scalar", "activation", out=junk[:], in_=xt[:], func=AF.Square, accum_out=sq[:, 0:1])
            v = v4.next()
            rs = rstd4.next()
            P.I("scalar", "activation", out=v[:, 0:1], in_=sq[:, 0:1], func=AF.Ln, bias=epsc1[:, 0:1], scale=1.0 / D)
            P.I("scalar", "activation", out=rs[:, 0:1], in_=v[:, 0:1], func=AF.Exp, scale=-0.5)
            tm = tmp_r.next()
            hb = hb_r.next()
            P.I("vector", "scalar_tensor_tensor", out=tm[:], in0=xt[:], scalar=rs[:, 0:1], in1=gm[:],
                op0=ALU.mult, op1=ALU.mult)
            P.I("vector", "tensor_tensor", out=hb[:], in0=tm[:], in1=shm, op=ALU.add)
            pt = psT[j % 2]
            for k in range(8):
                P.I("tensor", "transpose", out=pt[:, k * 128:(k + 1) * 128], in_=hb[:, k * 128:(k + 1) * 128], identity=idb[:])
            P.I("scalar", "copy", out=hT[:, :, j * 128:(j + 1) * 128], in_=pt[:].rearrange("p (k t) -> p k t", k=8))
        cosR = cos_r.next()
        sinR = sin_r.next()
        P.dma("scalar", posi[:], pos_in[t0:t0 + 512].partition_broadcast(128))
        P.I("vector", "tensor_copy", out=angt[:], in_=posi[:])
        P.I("vector", "tensor_scalar", out=angt[:], in0=angt[:], scalar1=invc[:, 0:1], scalar2=None, op0=ALU.mult)
        sin_of(P, sinR[:], angt[:], tmpa[:], tmpai[:], 0.0)
        sin_of(P, cosR[:], angt[:], tmpa[:], tmpai[:], 0.5 * math.pi)
        cosM = cosM_r.next()
        sinM = sinM_r.next()
        P.dma("scalar", posc_i[:], pos_in[t0:t0 + 512].rearrange("(n p) -> p n", p=128), allow_slow_non_contiguous=True)
        P.I("vector", "tensor_copy", out=posc[:], in_=posc_i[:])
        P.I("vector", "tensor_tensor", out=angm[:], in0=posc[:].unsqueeze(2).to_broadcast([128, 4, 16]),
            in1=invm[:].unsqueeze(1).to_broadcast([128, 4, 16]), op=ALU.mult)
        sin_of(P, sinM[:], angm[:], tmpm[:], tmpmi[:], 0.0)
        sin_of(P, cosM[:], angm[:], tmpm[:], tmpmi[:], 0.5 * math.pi)

        def fm(wt, c0):
            ps = nextF()
            for k in range(8):
                P.I("tensor", "matmul", out=ps[:], lhsT=wt[:, k, c0:c0 + 128], rhs=hT[:, k, :], start=(k == 0), stop=(k == 7))
            return ps

        for ch in range(2):
            ps = fm(w_bf, ch * 128)
            e = ev_r.next()
            P.I("scalar", "copy", out=e[:], in_=ps[:])
            P.dma("sync", bgate[ch * 128:(ch + 1) * 128, t0:t0 + 512], e[:])
            psc = fm(w_bf, 256 + ch * 128)
            cs = csb_r.next()
            P.I("scalar", "copy", out=cs[:], in_=psc[:])
            psx = fm(w_bf, 512 + ch * 128)
            e = ev_r.next()
            P.I("vector", "tensor_tensor", out=e[:], in0=psx[:], in1=cs[:], op=ALU.mult)
            P.dma("sync", cvx[ch * 128:(ch + 1) * 128, t0:t0 + 512], e[:])
        for qk in range(2):
            for ch in range(2):
                c0 = 1120 + qk * 256 + ch * 128
                ps = fm(w_bf, c0)
                cs = csb_r.next()
                P.I("vector", "scalar_tensor_tensor", out=cs[:], in0=ps[:], scalar=(1.0 if qk == 0 else 0.125),
                    in1=cosR[:], op0=ALU.mult, op1=ALU.mult)
                psr = fm(w_rot, qk * 256 + ch * 128)
                e = ev_r.next()
                P.I("vector", "scalar_tensor_tensor", out=e[:], in0=psr[:], scalar=(1.0 if qk == 0 else 0.125),
                    in1=sinR[:], op0=ALU.mult, op1=ALU.mult)
                eb = evb_r.next()
                P.I("vector", "tensor_tensor", out=eb[:], in0=e[:], in1=cs[:], op=ALU.add)
                P.dma("sync", (retq if qk == 0 else retk)[ch * 128:(ch + 1) * 128, t0:t0 + 512], eb[:])
        for ch in range(2):
            ps = fm(w_bf, 1120 + 768 + ch * 128)
            e = ev_r.next()
            P.I("scalar", "activation", out=e[:], in_=ps[:], func=AF.Silu)
            P.dma("sync", sgate[ch * 128:(ch + 1) * 128, t0:t0 + 512], e[:])
        for ch in range(2):
            ps = fm(w_bf, 2144 + ch * 128)
            e = ev_r.next()
            P.I("scalar", "copy", out=e[:], in_=ps[:])
            P.dma("sync", lrux[ch * 128:(ch + 1) * 128, t0:t0 + 512], e[:])
        for ch in range(2):
            ps = fm(w_bf, 2144 + 256 + ch * 128)
            e = ev_r.next()
            P.I("scalar", "activation", out=e[:], in_=ps[:], func=AF.Gelu)
            P.dma("sync", ggate[ch * 128:(ch + 1) * 128, t0:t0 + 512], e[:])
        for j in range(4):
            tt = t0 + j * 128
            ti = tt // 128
            hTj = hT[:, :, j * 128:(j + 1) * 128]
            ps = nextF()
            for k in range(8):
                P.I("tensor", "matmul", out=ps[:, 0:256], lhsT=hT[:, k, j * 128:(j + 1) * 128], rhs=w_bf[:, k, 1632:1888],
                    start=(k == 0), stop=(k == 7))
            eb = evb_r.next()
            P.I("scalar", "copy", out=eb[:, 0:256], in_=ps[:, 0:256])
            P.dma("sync", retv[tt:tt + 128, :], eb[:, 0:256])
            mla_tile(P, locals(), tt, j, j)


def mla_tile(P, L, tt, ti, j):
    hT, w_bf, psU, psQ, psX = L["hT"], L["w_bf"], L["psU"], L["psQ"], L["psX"]
    idb, neghalf = L["idb"], L["neghalf"]
    qng_bc, kvng_bc, qqk_bc, kqk_bc = L["qng_bc"], L["kvng_bc"], L["qqk_bc"], L["kqk_bc"]
    wuq_bf, wukv_bf, cosM, sinM = L["wuq_bf"], L["wukv_bf"], L["cosM"], L["sinM"]
    attq, attk, attv = L["attq"], L["attk"], L["attv"]
    if not hasattr(P, "mla_tiles"):
        P.mla_tiles = dict(
            junk=P.sb("mjunk", [128, 512], F32),
            st3=Rot(P, "mst3", [128, 16], F32, 2),
            rs3=Rot(P, "mrs3", [128, 16], F32, 2),
            cqn=Rot(P, "mcqn", [128, 320], BF16, 2),
            cT=Rot(P, "mcT", [128, 384], BF16, 2),
            qsb=Rot(P, "mqsb", [128, 384], F32, 2),
            kvsb=Rot(P, "mkvsb", [128, 512], F32, 2),
            sq=Rot(P, "msq", [128, 512], F32, 4),
            Qt=Rot(P, "mQt", [128, 4, 96], BF16, 2),
            Kt=Rot(P, "mKt", [128, 4, 96], BF16, 2),
            Vt=Rot(P, "mVt", [128, 4, 64], BF16, 2),
            r1=Rot(P, "mr1", [128, 4, 32], F32, 2),
            r2=Rot(P, "mr2", [128, 4, 16], F32, 4),
            kr=Rot(P, "mkr", [128, 32], F32, 2),
            kr2=Rot(P, "mkr2", [128, 32], F32, 2),
            QT=Rot(P, "mQT", [96, 4, 128], BF16, 2),
            KT=Rot(P, "mKT", [96, 4, 128], BF16, 2),
            scl=P.sb("mscl", [128, 3], F32),
        )
        sc = P.mla_tiles["scl"]
        P.I("vector", "memset", ap=sc[:, 0:1], constant=1.0 / 192, _writes=[sc[:]])
        P.I("vector", "memset", ap=sc[:, 1:2], constant=1.0 / 128, _writes=[sc[:]])
        P.I("vector", "memset", ap=sc[:, 2:3], constant=1.0 / 32, _writes=[sc[:]])
    M = P.mla_tiles
    for k in range(8):
        P.I("tensor", "matmul", out=psU[:, 0:352], lhsT=hT[:, k, j * 128:(j + 1) * 128], rhs=w_bf[:, k, 768:1120],
            start=(k == 0), stop=(k == 7))
    st3 = M["st3"].next()
    rs3 = M["rs3"].next()
    P.I("scalar", "activation", out=M["junk"][:, 0:192], in_=psU[:, 0:192], func=AF.Square, accum_out=st3[:, 0:1])
    P.I("scalar", "activation", out=M["junk"][:, 0:128], in_=psU[:, 192:320], func=AF.Square, accum_out=st3[:, 1:2])
    P.I("scalar", "activation", out=M["junk"][:, 0:32], in_=psU[:, 320:352], func=AF.Square, accum_out=st3[:, 2:3])
    P.I("vector", "tensor_tensor", out=st3[:, 0:3], in0=st3[:, 0:3], in1=M["scl"][:], op=ALU.mult)
    P.I("scalar", "activation", out=st3[:, 0:3], in_=st3[:, 0:3], func=AF.Ln, bias=L["epsc1"][:, 0:1], scale=1.0)
    P.I("scalar", "activation", out=rs3[:, 0:3], in_=st3[:, 0:3], func=AF.Exp, scale=-0.5)
    cqn = M["cqn"].next()
    P.I("vector", "scalar_tensor_tensor", out=cqn[:, 0:192], in0=psU[:, 0:192], scalar=rs3[:, 0:1], in1=qng_bc[:],
        op0=ALU.mult, op1=ALU.mult)
    P.I("vector", "scalar_tensor_tensor", out=cqn[:, 192:320], in0=psU[:, 192:320], scalar=rs3[:, 1:2], in1=kvng_bc[:],
        op0=ALU.mult, op1=ALU.mult)
    kr = M["kr"].next()
    P.I("vector", "scalar_tensor_tensor", out=kr[:], in0=psU[:, 320:352], scalar=rs3[:, 2:3], in1=kqk_bc[:, 64:96],
        op0=ALU.mult, op1=ALU.mult)
    P.I("tensor", "transpose", out=psX[:, 0:128], in_=cqn[:, 0:128], identity=idb[:])
    P.I("tensor", "transpose", out=psX[0:64, 128:256], in_=cqn[:, 128:192], identity=idb[:])
    P.I("tensor", "transpose", out=psX[:, 256:384], in_=cqn[:, 192:320], identity=idb[:])
    cT = M["cT"].next()
    P.I("scalar", "copy", out=cT[:, 0:128], in_=psX[:, 0:128])
    P.I("scalar", "copy", out=cT[0:64, 128:256], in_=psX[0:64, 128:256])
    P.I("scalar", "copy", out=cT[:, 256:384], in_=psX[:, 256:384])
    P.I("tensor", "matmul", out=psQ[:, 0:384], lhsT=cT[:, 0:128], rhs=wuq_bf[:, 0, :], start=True, stop=False)
    P.I("tensor", "matmul", out=psQ[:, 0:384], lhsT=cT[0:64, 128:256], rhs=wuq_bf[0:64, 1, :], start=False, stop=True)
    qsb = M["qsb"].next()
    sq = M["sq"].next()
    P.I("scalar", "copy", out=qsb[:], in_=psQ[:, 0:384])
    P.I("scalar", "activation", out=sq[:, 0:384], in_=psQ[:, 0:384], func=AF.Square)
    P.I("tensor", "matmul", out=psQ[:, 0:512], lhsT=cT[:, 256:384], rhs=wukv_bf[:], start=True, stop=True)
    kvsb = M["kvsb"].next()
    P.I("scalar", "copy", out=kvsb[:], in_=psQ[:, 0:512])
    st8 = M["st3"].next()
    rs8 = M["rs3"].next()
    sqv = sq[:, 0:384].rearrange("p (h c) -> p h c", c=96)
    P.I("vector", "tensor_reduce", out=st8[:, 0:4], in_=sqv[:, :, 0:64], axis=AX.X, op=ALU.add)
    P.I("vector", "tensor_reduce", out=st8[:, 4:8], in_=sqv[:, :, 64:96], axis=AX.X, op=ALU.add)
    sq2 = M["sq"].next()
    P.I("scalar", "activation", out=sq2[:], in_=kvsb[:], func=AF.Square)
    P.I("vector", "tensor_reduce", out=st8[:, 8:12], in_=sq2[:].rearrange("p (h c) -> p h c", c=128)[:, :, 0:64],
        axis=AX.X, op=ALU.add)
    P.I("vector", "tensor_scalar", out=st8[:, 0:4], in0=st8[:, 0:4], scalar1=1.0 / 64, scalar2=EPS, op0=ALU.mult, op1=ALU.add)
    P.I("vector", "tensor_scalar", out=st8[:, 4:8], in0=st8[:, 4:8], scalar1=1.0 / 32, scalar2=EPS, op0=ALU.mult, op1=ALU.add)
    P.I("vector", "tensor_scalar", out=st8[:, 8:12], in0=st8[:, 8:12], scalar1=1.0 / 64, scalar2=EPS, op0=ALU.mult, op1=ALU.add)
    P.I("scalar", "activation", out=st8[:, 0:12], in_=st8[:, 0:12], func=AF.Ln)
    P.I("scalar", "activation", out=rs8[:, 0:12], in_=st8[:, 0:12], func=AF.Exp, scale=-0.5)
    Qt = M["Qt"].next()
    Kt = M["Kt"].next()
    Vt = M["Vt"].next()
    qv = qsb[:].rearrange("p (h c) -> p h c", c=96)
    kvv = kvsb[:].rearrange("p (h c) -> p h c", c=128)
    r1 = M["r1"].next()
    tq = M["sq"].next()
    tqv = tq[:, 0:256].rearrange("p (h c) -> p h c", c=64)
    P.I("vector", "tensor_tensor", out=tqv, in0=qv[:, :, 0:64], in1=rs8[:, 0:4].unsqueeze(2).to_broadcast([128, 4, 64]), op=ALU.mult)
    P.I("vector", "tensor_tensor", out=Qt[:, :, 0:64], in0=tqv, in1=qqk_bc[:, 0:64].unsqueeze(1).to_broadcast([128, 4, 64]), op=ALU.mult)
    P.I("vector", "tensor_tensor", out=r1[:], in0=qv[:, :, 64:96], in1=rs8[:, 4:8].unsqueeze(2).to_broadcast([128, 4, 32]), op=ALU.mult)
    P.I("vector", "tensor_tensor", out=r1[:], in0=r1[:], in1=qqk_bc[:, 64:96].unsqueeze(1).to_broadcast([128, 4, 32]), op=ALU.mult)
    cb = cosM[:, ti, :].unsqueeze(1).to_broadcast([128, 4, 16])
    sb_ = sinM[:, ti, :].unsqueeze(1).to_broadcast([128, 4, 16])
    a1, a2, a3, a4 = M["r2"].next(), M["r2"].next(), M["r2"].next(), M["r2"].next()
    P.I("vector", "tensor_tensor", out=a1[:], in0=r1[:, :, 0:16], in1=cb, op=ALU.mult)
    P.I("vector", "tensor_tensor", out=a2[:], in0=r1[:, :, 16:32], in1=sb_, op=ALU.mult)
    P.I("vector", "tensor_tensor", out=a3[:], in0=r1[:, :, 16:32], in1=cb, op=ALU.mult)
    P.I("vector", "tensor_tensor", out=a4[:], in0=r1[:, :, 0:16], in1=sb_, op=ALU.mult)
    P.I("vector", "tensor_tensor", out=Qt[:, :, 64:80], in0=a1[:], in1=a2[:], op=ALU.subtract)
    P.I("vector", "tensor_tensor", out=Qt[:, :, 80:96], in0=a3[:], in1=a4[:], op=ALU.add)
    tk = M["sq"].next()
    tkv = tk[:, 0:256].rearrange("p (h c) -> p h c", c=64)
    P.I("vector", "tensor_tensor", out=tkv, in0=kvv[:, :, 0:64], in1=rs8[:, 8:12].unsqueeze(2).to_broadcast([128, 4, 64]), op=ALU.mult)
    P.I("vector", "tensor_tensor", out=Kt[:, :, 0:64], in0=tkv, in1=kqk_bc[:, 0:64].unsqueeze(1).to_broadcast([128, 4, 64]), op=ALU.mult)
    kr2 = M["kr2"].next()
    b1, b2, b3, b4 = M["r2"].next(), M["r2"].next(), M["r2"].next(), M["r2"].next()
    P.I("vector", "tensor_tensor", out=b1[:, 0, :], in0=kr[:, 0:16], in1=cosM[:, ti, :], op=ALU.mult)
    P.I("vector", "tensor_tensor", out=b2[:, 0, :], in0=kr[:, 16:32], in1=sinM[:, ti, :], op=ALU.mult)
    P.I("vector", "tensor_tensor", out=b3[:, 0, :], in0=kr[:, 16:32], in1=cosM[:, ti, :], op=ALU.mult)
    P.I("vector", "tensor_tensor", out=b4[:, 0, :], in0=kr[:, 0:16], in1=sinM[:, ti, :], op=ALU.mult)
    P.I("vector", "tensor_tensor", out=kr2[:, 0:16], in0=b1[:, 0, :], in1=b2[:, 0, :], op=ALU.subtract)
    P.I("vector", "tensor_tensor", out=kr2[:, 16:32], in0=b3[:, 0, :], in1=b4[:, 0, :], op=ALU.add)
    P.I("vector", "tensor_copy", out=Kt[:, :, 64:96], in_=kr2[:].unsqueeze(1).to_broadcast([128, 4, 32]))
    P.I("vector", "tensor_copy", out=Vt[:], in_=kvv[:, :, 64:128])
    P.dma("sync", attv[:, tt:tt + 128, :].rearrange("h t c -> t h c"), Vt[:])
    for h in range(4):
        P.I("tensor", "transpose", out=psX[0:96, 384 + h * 128:384 + (h + 1) * 128], in_=Qt[:, h, :], identity=idb[:])
    QT = M["QT"].next()
    P.I("scalar", "copy", out=QT[:], in_=psX[0:96, 384:896].rearrange("p (h t) -> p h t", h=4))
    P.dma("sync", attq[:, :, tt:tt + 128].rearrange("h c t -> c h t"), QT[:])
    for h in range(4):
        P.I("tensor", "transpose", out=psX[0:96, 384 + h * 128:384 + (h + 1) * 128], in_=Kt[:, h, :], identity=idb[:])
    KT = M["KT"].next()
    P.I("scalar", "copy", out=KT[:], in_=psX[0:96, 384:896].rearrange("p (h t) -> p h t", h=4))
    P.dma("sync", attk[:, :, tt:tt + 128].rearrange("h c t -> c h t"), KT[:])


ATT_SCALE = 96.0 ** -0.5


def build_p2(SS=S):
    nc = new_nc()
    P = Prog(nc)
    io = IO(nc)
    phase2(P, io, SS)
    P.finish(io.outs)
    P.emit()
    return nc


def phase2(P, io, SS, after_setup=None):
    nc = P.nc
    NB = SS // 128
    aq = io.inp("aq", [96, SS], BF16)
    ak = io.inp("ak", [96, SS], BF16)
    av = io.inp("av", [SS, 64], BF16)
    rq = io.inp("rq", [64, SS], BF16)
    rk = io.inp("rk", [64, SS], BF16)
    rv = io.inp("rv", [SS, 64], BF16)
    lx = io.inp("lx", [64, SS])
    cx = io.inp("cx", [64, SS])
    convw = io.inp("convw", [64, 3])
    lcw = io.inp("lcw", [64, 4])
    lcb = io.inp("lcb", [64, 1])
    wa = io.inp("wa", [64, 64])
    ba = io.inp("ba", [64, 1])
    wx = io.inp("wx", [64, 64])
    bx = io.inp("bx", [64, 1])
    lam = io.inp("lam", [64, 1])
    qqk = io.inp("qqk", [96])
    kqk = io.inp("kqk", [96])
    innerT = io.inp("innerT", [128, 128])
    qdecT = io.inp("qdecT", [64, 128])
    kdec = io.inp("kdec", [128, 1])
    cdec = io.inp("cdec", [64, 1])
    ao = io.out("ao", [64, SS])
    ro = io.out("ro", [64, SS])
    lo = io.out("lo", [64, SS])
    co = io.out("co", [64, SS])

    psS = [P.ps("psS%d" % i, [128, 512], F32) for i in range(3)]
    psO = [P.ps("psO%d" % i, [128, 512], F32) for i in range(2)]
    psB = P.ps("psB", [128, 512], F32)
    psR = [P.ps("psR%d" % i, [128, 512], F32) for i in range(2)]
    psKT = psB[:].bitcast(BF16)

    idf, idb = make_identities(P)
    half = P.sb("half", [128, 512], F32)
    P.I("vector", "memset", ap=half[:], constant=0.5, _writes=[half[:]])
    neghalf = P.sb("neghalf", [128, 512], F32)
    P.I("vector", "memset", ap=neghalf[:], constant=-0.5, _writes=[neghalf[:]])
    ones64 = P.sb("ones64", [128, 64], F32)
    P.I("vector", "memset", ap=ones64[:], constant=1.0 / 64, _writes=[ones64[:]])
    ones1 = P.sb("ones1", [128, 64], F32)
    P.I("vector", "memset", ap=ones1[:], constant=1.0, _writes=[ones1[:]])
    epsc = P.sb("epsc", [128, 1], F32)
    P.I("vector", "memset", ap=epsc[:], constant=EPS, _writes=[epsc[:]])

    cw = P.sb("cw", [64, 3], F32)
    P.dma("sync", cw[:], convw, allow_slow_non_contiguous=True)
    CSEG = min(2048, SS)
    cxt = P.sb("cxt", [64, 2 + CSEG], F32)
    cacc = Rot(P, "cacc", [64, CSEG], F32, 2)
    P.I("vector", "memset", ap=cxt[:, 0:2], constant=0.0, _writes=[cxt[:]])
    for sg in range(SS // CSEG):
        c0 = sg * CSEG
        if sg > 0:
            P.I("vector", "tensor_copy", out=cxt[:, 0:2], in_=cxt[:, CSEG:CSEG + 2])
        P.dma("sync", cxt[:, 2:2 + CSEG], cx[:, c0:c0 + CSEG])
        acc = cacc.next()
        P.I("vector", "tensor_scalar", out=acc[:], in0=cxt[:, 2:2 + CSEG], scalar1=cw[:, 2:3], scalar2=None, op0=ALU.mult)
        P.I("vector", "scalar_tensor_tensor", out=acc[:], in0=cxt[:, 1:1 + CSEG], scalar=cw[:, 1:2], in1=acc[:], op0=ALU.mult, op1=ALU.add)
        P.I("vector", "scalar_tensor_tensor", out=acc[:], in0=cxt[:, 0:CSEG], scalar=cw[:, 0:1], in1=acc[:], op0=ALU.mult, op1=ALU.add)
        P.dma("gpsimd", co[:, c0:c0 + CSEG], acc[:])

    lw = P.sb("lw", [64, 4], F32)
    lb = P.sb("lb", [64, 1], F32)
    bat = P.sb("bat", [64, 1], F32)
    bxt = P.sb("bxt", [64, 1], F32)
    lamt = P.sb("lamt", [64, 1], F32)
    nsp = P.sb("nsp", [64, 1], F32)
    wa_bf = P.sb("wa_bf", [64, 64], BF16)
    wx_bf = P.sb("wx_bf", [64, 64], BF16)
    P.dma("sync", lw[:], lcw, allow_slow_non_contiguous=True)
    P.dma("sync", lb[:], lcb, allow_slow_non_contiguous=True)
    P.dma("sync", bat[:], ba, allow_slow_non_contiguous=True)
    P.dma("sync", bxt[:], bx, allow_slow_non_contiguous=True)
    P.dma("sync", lamt[:], lam, allow_slow_non_contiguous=True)
    P.dma("gpsimd", wa_bf[:], wa)
    P.dma("gpsimd", wx_bf[:], wx)
    P.I("scalar", "activation", out=nsp[:], in_=lamt[:], func=AF.Exp, scale=-1.0)
    P.I("scalar", "activation", out=nsp[:], in_=nsp[:], func=AF.Ln, bias=ones1[0:64, 0:1], scale=1.0)
    P.I("vector", "tensor_scalar", out=nsp[:], in0=nsp[:], scalar1=-8.0, scalar2=None, op0=ALU.mult)
    LSEG = min(1024, SS)
    NHF = LSEG // 512
    lxt = P.sb("lxt", [64, 3 + LSEG], F32)
    P.I("vector", "memset", ap=lxt[:, 0:3], constant=0.0, _writes=[lxt[:]])
    xc_r = Rot(P, "xc", [64, LSEG], F32, 1)
    xcb_r = Rot(P, "xcb", [64, LSEG], BF16, 1)
    rg_r = Rot(P, "rg", [64, LSEG], F32, 1)
    ig_r = Rot(P, "ig", [64, LSEG], F32, 1)
    a_r = Rot(P, "la", [64, LSEG], F32, 1)
    a2_r = Rot(P, "la2", [64, LSEG], F32, 1)
    b_r = Rot(P, "lbin", [64, LSEG], F32, 1)
    h_r = Rot(P, "lh", [64, LSEG], F32, 2)
    gbanks = [psS[0], psS[1], psS[2], psO[0]]
    hprev = None
    lru_state = [None]

    def lru_gen():
      for sg in range(SS // LSEG):
        hprev = lru_state[0]
        c0 = sg * LSEG
        if sg > 0:
            P.I("vector", "tensor_copy", out=lxt[:, 0:3], in_=lxt[:, LSEG:LSEG + 3])
        P.dma("sync", lxt[:, 3:3 + LSEG], lx[:, c0:c0 + LSEG])
        xc = xc_r.next()
        P.I("vector", "tensor_scalar", out=xc[:], in0=lxt[:, 3:3 + LSEG], scalar1=lw[:, 3:4], scalar2=lb[:, 0:1], op0=ALU.mult, op1=ALU.add)
        for kk in range(3):
            P.I("vector", "scalar_tensor_tensor", out=xc[:], in0=lxt[:, kk:kk + LSEG], scalar=lw[:, kk:kk + 1], in1=xc[:], op0=ALU.mult, op1=ALU.add)
        xcb = xcb_r.next()
        P.I("vector", "tensor_copy", out=xcb[:], in_=xc[:])
        rg = rg_r.next()
        ig = ig_r.next()
        for hf in range(NHF):
            hs = slice(hf * 512, (hf + 1) * 512)
            P.I("tensor", "matmul", out=psR[0][0:64, :], lhsT=wa_bf[:], rhs=xcb[:, hs], start=True, stop=True)
            P.I("tensor", "matmul", out=psR[1][0:64, :], lhsT=wx_bf[:], rhs=xcb[:, hs], start=True, stop=True)
            P.I("scalar", "activation", out=rg[:, hs], in_=psR[0][0:64, :], func=AF.Sigmoid, bias=bat[:, 0:1], scale=1.0)
            P.I("scalar", "activation", out=ig[:, hs], in_=psR[1][0:64, :], func=AF.Sigmoid, bias=bxt[:, 0:1], scale=1.0)
        a = a_r.next()
        P.I("scalar", "activation", out=a[:], in_=rg[:], func=AF.Exp, scale=nsp[:, 0:1])
        a2 = a2_r.next()
        P.I("scalar", "activation", out=a2[:], in_=a[:], func=AF.Square)
        P.I("scalar", "activation", out=a2[:], in_=a2[:], func=AF.Sqrt, bias=ones1[0:64, 0:1], scale=-1.0)
        bb = b_r.next()
        P.I("vector", "tensor_tensor", out=bb[:], in0=ig[:], in1=xc[:], op=ALU.mult)
        P.I("vector", "tensor_tensor", out=bb[:], in0=bb[:], in1=a2[:], op=ALU.mult)
        h = h_r.next()
        P.I("vector", "tensor_tensor_scan", out=h[:], data0=a[:], data1=bb[:],
            initial=(0.0 if hprev is None else hprev[:, LSEG - 1:LSEG]), op0=ALU.mult, op1=ALU.add)
        lru_state[0] = h
        P.dma("gpsimd", lo[:, c0:c0 + LSEG], h[:])
        yield

    inn = P.sb("inn", [128, 128], F32)
    qd_c = P.sb("qd_c", [64, 512], F32)
    kd_c = P.sb("kd_c", [128, 1], F32)
    cd_c = P.sb("cd_c", [64, 1], F32)
    P.dma("sync", inn[:], innerT)
    for r_ in range(4):
        P.dma("sync", qd_c[:, r_ * 128:(r_ + 1) * 128], qdecT)
    P.dma("sync", kd_c[:], kdec)
    P.dma("sync", cd_c[:], cdec)
    PC = min(32, NB)
    RG = 4
    KVa = P.sb("KVa", [64, PC, 64], F32)
    Sbf = P.sb("Sbf", [64, PC, 64], BF16)
    gam = P.sb("gam", [64, PC], F32)
    carry = P.sb("rcarry", [64, 64], F32)
    P.I("vector", "memset", ap=carry[:], constant=0.0, _writes=[carry[:]])
    P.I("vector", "memset", ap=gam[:], constant=1.0, _writes=[gam[:]])
    P.I("vector", "tensor_scalar", out=gam[:], in0=gam[:], scalar1=cd_c[:, 0:1], scalar2=None, op0=ALU.mult)
    rq_r = Rot(P, "rq_t", [64, RG * 128], BF16, 2)
    rk_r = Rot(P, "rk_t", [64, RG * 128], BF16, 2)
    rv_r = Rot(P, "rv_t", [128, RG, 64], BF16, 2)
    scm_r = Rot(P, "scm", [128, 128], BF16, 3)
    kd_r = Rot(P, "kdt", [128, 64], BF16, 3)
    qd_r = Rot(P, "qdt", [64, RG * 128], BF16, 2)
    oT_r = Rot(P, "oT", [64, RG * 128], F32, 2)
    cen_r = Rot(P, "cen", [64, RG * 128], F32, 2)
    sqr_r = Rot(P, "sqr", [64, RG * 128], F32, 2)
    def ret_gen():
      for pc in range(NB // PC):
        c0 = pc * PC
        for g in range(PC // RG):
            t0 = (c0 + g * RG) * 128
            rkt, rvt = rk_r.next(), rv_r.next()
            P.dma("sync", rkt[:], rk[:, t0:t0 + RG * 128])
            P.dma("sync", rvt[:], rv[t0:t0 + RG * 128, :].rearrange("(n p) c -> p n c", p=128))
            for ci in range(RG):
                i = g * RG + ci
                cs = slice(ci * 128, (ci + 1) * 128)
                pT = psS[i % 3][:].bitcast(BF16)
                P.I("tensor", "transpose", out=pT[:, 0:64], in_=rkt[:, cs], identity=idb[0:64, 0:64])
                kdt = kd_r.next()
                P.I("scalar", "activation", out=kdt[:], in_=pT[:, 0:64], func=AF.Copy, scale=kd_c[:, 0:1])
                pK = psO[i % 2]
                P.I("tensor", "matmul", out=pK[0:64, 0:64], lhsT=kdt[:], rhs=rvt[:, ci, :], start=True, stop=True)
                P.I("vector", "tensor_copy", out=KVa[:, i, :], in_=pK[0:64, 0:64])
            yield
        for dv in range(64):
            P.I("vector", "tensor_tensor_scan", out=KVa[:, :, dv], data0=gam[:], data1=KVa[:, :, dv], initial=carry[:, dv:dv + 1],
                op0=ALU.mult, op1=ALU.add)
        P.I("scalar", "copy", out=Sbf[:, 0, :], in_=carry[:])
        if PC > 1:
            P.I("scalar", "copy", out=Sbf[:, 1:PC, :], in_=KVa[:, 0:PC - 1, :])
        P.I("vector", "tensor_copy", out=carry[:], in_=KVa[:, PC - 1, :])
        for g in range(PC // RG):
            t0 = (c0 + g * RG) * 128
            rqt, rkt, rvt = rq_r.next(), rk_r.next(), rv_r.next()
            P.dma("sync", rqt[:], rq[:, t0:t0 + RG * 128])
            P.dma("sync", rkt[:], rk[:, t0:t0 + RG * 128])
            P.dma("sync", rvt[:], rv[t0:t0 + RG * 128, :].rearrange("(n p) c -> p n c", p=128))
            qdt = qd_r.next()
            P.I("vector", "tensor_tensor", out=qdt[:], in0=rqt[:], in1=qd_c[:], op=ALU.mult)
            oT = oT_r.next()
            for ci in range(RG):
                i = g * RG + ci
                cs = slice(ci * 128, (ci + 1) * 128)
                pA = psS[i % 3]
                P.I("tensor", "matmul", out=pA[:, 0:128], lhsT=rkt[:, cs], rhs=rqt[:, cs], start=True, stop=True)
                scm = scm_r.next()
                P.I("vector", "tensor_tensor", out=scm[:], in0=pA[:, 0:128], in1=inn[:], op=ALU.mult)
                pBk = psO[i % 2]
                P.I("tensor", "matmul", out=pBk[0:64, 0:128], lhsT=rvt[:, ci, :], rhs=scm[:], start=True, stop=False)
                P.I("tensor", "matmul", out=pBk[0:64, 0:128], lhsT=Sbf[:, i, :], rhs=qdt[:, cs], start=False, stop=True)
                P.I("scalar", "copy", out=oT[:, cs], in_=pBk[0:64, 0:128])
            W = RG * 128
            P.I("tensor", "matmul", out=psB[0:64, 0:W], lhsT=ones64[0:64, :], rhs=oT[:], start=True, stop=True)
            cen = cen_r.next()
            P.I("vector", "tensor_tensor", out=cen[:], in0=oT[:], in1=psB[0:64, 0:W], op=ALU.subtract)
            sqr = sqr_r.next()
            P.I("scalar", "activation", out=sqr[:], in_=cen[:], func=AF.Square)
            P.I("tensor", "matmul", out=psB[0:64, 0:W], lhsT=ones64[0:64, :], rhs=sqr[:], start=True, stop=True)
            P.I("scalar", "activation", out=sqr[:], in_=psB[0:64, 0:W], func=AF.Sqrt, bias=epsc[0:64, 0:1], scale=1.0)
            P.I("vector", "reciprocal", out=sqr[:], in_=sqr[:])
            P.I("vector", "tensor_tensor", out=cen[:], in0=cen[:], in1=sqr[:], op=ALU.mult)
            P.dma("gpsimd", ro[:, t0:t0 + W], cen[:])
            yield

    g_l, g_r = lru_gen(), ret_gen()
    n_l = SS // LSEG
    n_r = (NB // PC) * 2 * (PC // RG)
    per = max(1, n_r // max(1, n_l))
    done_l = done_r = False
    while not (done_l and done_r):
        if not done_l:
            try:
                next(g_l)
            except StopIteration:
                done_l = True
        for _ in range(per):
            if not done_r:
                try:
                    next(g_r)
                except StopIteration:
                    done_r = True
    QT = P.sb("QT", [96, SS], BF16)
    KT = P.sb("KT", [96, SS], BF16)
    VA = P.sb("VA", [128, NB, 65], BF16)
    LDC = min(2048, SS)
    for i in range(SS // LDC):
        P.dma("sync", KT[:, i * LDC:(i + 1) * LDC], ak[:, i * LDC:(i + 1) * LDC])
        P.dma("scalar", QT[:, i * LDC:(i + 1) * LDC], aq[:, i * LDC:(i + 1) * LDC])
    P.I("gpsimd", "memset", ap=VA[:, :, 64:65], constant=1.0, _writes=[VA[:]])
    P.dma("sync", VA[:, :, 0:64], av.rearrange("(n p) c -> p n c", p=128))
    tri_f = P.sb("tri_f", [128, 128], F32)
    tri = P.sb("tri", [128, 128], BF16)
    P.I("gpsimd", "memset", ap=tri_f[:], constant=1.0, _writes=[tri_f[:]])
    P.I("gpsimd", "affine_select", out=tri_f[:], in_=tri_f[:], pattern=[[1, 128]], compare_op=ALU.is_ge,
        fill=0.0, base=0, channel_multiplier=-1)
    P.I("vector", "tensor_copy", out=tri[:], in_=tri_f[:])
    gq = P.sb("gq_bc", [128, 96], F32)
    gk = P.sb("gk_bc", [128, 96], F32)
    m2 = P.sb("m2", [128, 2], F32)
    negc = P.sb("negc", [128, 1], F32)
    P.dma("sync", gq[:], qqk.partition_broadcast(128))
    P.dma("sync", gk[:], kqk.partition_broadcast(128))
    P.I("vector", "tensor_tensor", out=gq[:], in0=gq[:], in1=gq[:], op=ALU.mult)
    P.I("vector", "tensor_tensor", out=gk[:], in0=gk[:], in1=gk[:], op=ALU.mult)
    P.I("vector", "tensor_reduce", out=m2[:, 0:1], in_=gq[:], axis=AX.X, op=ALU.max)
    P.I("vector", "tensor_reduce", out=m2[:, 1:2], in_=gk[:], axis=AX.X, op=ALU.max)
    P.I("vector", "tensor_tensor", out=negc[:], in0=m2[:, 0:1], in1=m2[:, 1:2], op=ALU.mult)
    P.I("gpsimd", "tensor_tensor", out=negc[:], in0=negc[:], in1=half[:, 0:1], op=ALU.pow)
    P.I("vector", "tensor_scalar", out=negc[:], in0=negc[:], scalar1=-math.sqrt(96.0), scalar2=None, op0=ALU.mult)
    if after_setup is not None:
        after_setup()
    PT_r = Rot(P, "PT", [128, 512], BF16, 5)
    den_r = Rot(P, "den", [128, 512], F32, 2)
    oa_r = Rot(P, "oa", [64, 512], F32, 2)
    rb_r = Rot(P, "rb", [64, 512], F32, 2)
    si = 0
    for qs in range(SS // 512):
        q0 = qs * 512
        pO = psO[qs % 2]
        nkb = 4 * qs + 4
        pend = []
        for j in range(nkb):
            d = j - 4 * qs
            lo_c = 0 if d <= 0 else d * 128
            pS = psS[si % 3]
            si += 1
            P.I("tensor", "matmul", out=pS[:, lo_c:512], lhsT=KT[:, j * 128:(j + 1) * 128], rhs=QT[:, q0 + lo_c:q0 + 512],
                start=True, stop=True)
            if len(pend) >= 2:
                pj, plo, pPT = pend.pop(0)
                P.I("tensor", "matmul", out=pO[0:65, plo:512], lhsT=VA[:, pj, :], rhs=pPT[:, plo:512], start=(pj == 0), stop=False)
            PT = PT_r.next()
            P.I("scalar", "activation", out=PT[:, lo_c:512], in_=pS[:, lo_c:512], func=AF.Exp, bias=negc[:, 0:1], scale=ATT_SCALE)
            if d >= 0:
                P.I("vector", "tensor_tensor", out=PT[:, lo_c:lo_c + 128], in0=PT[:, lo_c:lo_c + 128], in1=tri[:], op=ALU.mult)
            pend.append((j, lo_c, PT))
        while pend:
            pj, plo, pPT = pend.pop(0)
            P.I("tensor", "matmul", out=pO[0:65, plo:512], lhsT=VA[:, pj, :], rhs=pPT[:, plo:512], start=(pj == 0), stop=(len(pend) == 0))
        den = den_r.next()
        P.I("vector", "tensor_copy", out=den[64:65, :], in_=pO[64:65, :])
        P.I("tensor", "matmul", out=psB[0:64, :], lhsT=ones1[64:65, 0:64], rhs=den[64:65, :], start=True, stop=True)
        rb = rb_r.next()
        P.I("vector", "reciprocal", out=rb[:], in_=psB[0:64, :])
        oa = oa_r.next()
        P.I("vector", "tensor_tensor", out=oa[:], in0=pO[0:64, :], in1=rb[:], op=ALU.mult)
        P.dma("sync", ao[:, q0:q0 + 512], oa[:])


def build_p3(ntok=TPC, TB=1024, NEXP=32, stop=0):
    nc = new_nc()
    P = Prog(nc)
    io = IO(nc)
    phase3(P, io, ntok, TB, NEXP)
    P.finish(io.outs)
    P.emit()
    return nc


def phase3(P, io, ntok, TB=1024, NEXP=32, stop=0):
    nc = P.nc
    x_in = io.inp("x_in", [ntok, D])
    c_in = io.inp("c_in", [D])
    adaw = io.inp("ada_w", [D, 6 * D])
    adab = io.inp("ada_b", [6 * D])
    mixo = [io.inp(n, [256, ntok]) for n in ("co", "ao", "ro", "lo")]
    gates = {0: io.inp("bgate", [256, ntok]), 2: io.inp("sgate", [256, ntok]), 3: io.inp("ggate", [256, ntok])}
    mng = io.inp("mix_norm_g", [D])
    w_out = io.inp("w_out", [D, D])
    gffn = io.inp("norm_ffn_g", [D])
    wgr = io.inp("router_group_w", [D, 4])
    bgr = io.inp("router_group_b", [4])
    wer = io.inp("router_expert_w", [D, 32])
    ber = io.inp("router_expert_b", [32])
    ewg = io.inp("exp_w_gate", [32, D, 256])
    ewu = io.inp("exp_w_up", [32, D, 256])
    ewd = io.inp("exp_w_down", [32, 256, D])
    x_out = io.out("x_out", [ntok, D])
    x1s = io.out("x1s", [ntok, D])

    bank = [P.ps("bank%d" % i, [128, 512], F32) for i in range(8)]
    idf, idb = make_identities(P)
    neghalf = P.sb("neghalf", [128, 256], F32)
    P.I("vector", "memset", ap=neghalf[:], constant=-0.5, _writes=[neghalf[:]])
    onesf = P.sb("onesf", [128, 128], F32)
    P.I("vector", "memset", ap=onesf[:], constant=1.0, _writes=[onesf[:]])
    epsc = P.sb("epsc", [128, 1], F32)
    P.I("vector", "memset", ap=epsc[:], constant=EPS, _writes=[epsc[:]])

    mod = compute_mod(P, c_in, adaw, adab, bank[0], bank[1], 2 * D, 6 * D, CH=128)
    gtm = mod[:, 0:D]
    shf = mod[:, D:2 * D]
    gf = mod[:, 2 * D:3 * D]
    gtf = mod[:, 3 * D:4 * D]
    tmo_r = Rot(P, "tmo", [128, D], F32, 2)
    gtmp = tmo_r.next()
    P.dma("sync", gtmp[:], gffn.partition_broadcast(128))
    P.I("vector", "scalar_tensor_tensor", out=gf, in0=gf, scalar=1.0, in1=gtmp[:], op0=ALU.add, op1=ALU.mult)
    mngc = P.sb("mngc", [128, 8], F32)
    P.dma("sync", mngc[:], mng.rearrange("(k p) -> p k", p=128), allow_slow_non_contiguous=True)
    wout_bf = P.sb("wout_bf", [128, 8, D], BF16)
    wov = w_out.rearrange("(k p) n -> p k n", p=128)
    for k in range(8):
        P.dma("gpsimd", wout_bf[:, k, :], wov[:, k, :])
    wr = P.sb("wr", [128, 8, 36], F32)
    P.dma("sync", wr[:, :, 0:4], wgr.rearrange("(k p) n -> p k n", p=128))
    P.dma("sync", wr[:, :, 4:36], wer.rearrange("(k p) n -> p k n", p=128))
    wr_hi = P.sb("wr_hi", [128, 8, 36], BF16)
    wr_lo = P.sb("wr_lo", [128, 8, 36], BF16)
    P.I("vector", "tensor_copy", out=wr_hi[:], in_=wr[:])
    P.I("vector", "tensor_tensor", out=wr_lo[:], in0=wr[:], in1=wr_hi[:], op=ALU.subtract)
    bg_bc = P.sb("bg_bc", [128, 4], F32)
    be_bc = P.sb("be_bc", [128, 32], F32)
    P.dma("sync", bg_bc[:], bgr.partition_broadcast(128))
    P.dma("sync", be_bc[:], ber.partition_broadcast(128))

    NSUB = TB // 128
    h2T = P.sb("h2T", [128, 8, TB], BF16)
    comb_all = P.sb("comb_all", [128, NSUB, 32], F32)
    yacc = P.sb("yacc", [128, NSUB, D], F32)

    yc_r = Rot(P, "yc", [128, 256], F32, 6)
    gt_r = Rot(P, "gtl", [128, 256], F32, 6)
    ysq_r = Rot(P, "ysq", [128, 256], F32, 8)
    ych = [P.sb("ych%d" % i, [128, 256], F32) for i in range(8)]
    rsb_r = Rot(P, "rsb", [128, 256], F32, 4)
    yT = P.sb("yT", [128, 8, 256], BF16)
    xt_r = Rot(P, "xt", [128, D], F32, 1)
    x1_r = Rot(P, "x1", [128, D], F32, 2)
    junk = P.sb("junk", [128, D], BF16)
    sm_r = Rot(P, "sm", [128, 8], F32, 4)
    h2f_r = Rot(P, "h2f", [128, D], F32, 1)
    h2Tlo = P.sb("h2Tlo", [128, 8, 128], BF16)
    hsp_r = Rot(P, "hsp", [128, D], BF16, 2)
    R8 = Rot(P, "r8", [128, 8], F32, 44)
    R4 = Rot(P, "r4", [128, 4], F32, 20)
    R1 = Rot(P, "r1", [128, 1], F32, 40)
    lg_r = Rot(P, "lgs", [128, 36], F32, 3)
    wgu_r = Rot(P, "wgu", [128, 8, 512], BF16, 2)
    wd_r = Rot(P, "wd", [128, 2, D], BF16, 2)
    sg_r = Rot(P, "sg", [128, 256], BF16, 4)
    hid_r = Rot(P, "hid", [128, 2, 256], BF16, 2)

    if "wgu_s" in io.m:
        wgu_s, wd_s = io.m["wgu_s"], io.m["wd_s"]
    else:
        wgu_s, wd_s = precast_experts(P, ewg, ewu, ewd, NEXP)

    for blk in range(ntok // TB):
        b0 = blk * TB
        for tl in range(TB // 256):
            t0 = b0 + tl * 256
            for m in range(4):
                for c in range(2):
                    ci = m * 2 + c
                    y = ych[ci]
                    if m == 1:
                        P.dma("sync", y[:], mixo[m][c * 128:(c + 1) * 128, t0:t0 + 256])
                    else:
                        yc = yc_r.next()
                        gt = gt_r.next()
                        P.dma("sync", yc[:], mixo[m][c * 128:(c + 1) * 128, t0:t0 + 256])
                        P.dma("scalar", gt[:], gates[m][c * 128:(c + 1) * 128, t0:t0 + 256])
                        P.I("vector", "tensor_tensor", out=y[:], in0=yc[:], in1=gt[:], op=ALU.mult)
            ysqs = []
            for ci in range(8):
                ysq = ysq_r.next()
                ysqs.append(ysq)
                P.I("scalar", "activation", out=ysq[:], in_=ych[ci][:], func=AF.Square)
            psss = []
            for m in range(4):
                pss = bank[m // 2][:, (m % 2) * 256:(m % 2 + 1) * 256]
                psss.append(pss)
                for c in range(2):
                    P.I("tensor", "matmul", out=pss, lhsT=onesf[:], rhs=ysqs[m * 2 + c][:], start=(c == 0), stop=(c == 1))
            rsbs = []
            for m in range(4):
                rsb = rsb_r.next()
                rsbs.append(rsb)
                P.I("scalar", "activation", out=rsb[:], in_=psss[m], func=AF.Ln, bias=epsc[:, 0:1], scale=1.0 / 256)
            for m in range(4):
                P.I("scalar", "activation", out=rsbs[m][:], in_=rsbs[m][:], func=AF.Exp, scale=-0.5)
            for m in range(4):
                for c in range(2):
                    ci = m * 2 + c
                    P.I("vector", "scalar_tensor_tensor", out=yT[:, ci, :], in0=ych[ci][:], scalar=mngc[:, ci:ci + 1], in1=rsbs[m][:],
                        op0=ALU.mult, op1=ALU.mult)
            rgens = []
            for s in range(2):
                tt = t0 + s * 128
                si = tl * 2 + s
                po = [bank[2], bank[3]]
                for hf in range(2):
                    for k in range(8):
                        P.I("tensor", "matmul", out=po[hf][:], lhsT=yT[:, k, s * 128:(s + 1) * 128], rhs=wout_bf[:, k, hf * 512:(hf + 1) * 512],
                            start=(k == 0), stop=(k == 7))
                xt = xt_r.next()
                P.dma("sync", xt[:], x_in[tt:tt + 128, :])
                tmo = tmo_r.next()
                x1 = x1_r.next()
                for hf in range(2):
                    P.I("vector", "tensor_tensor", out=tmo[:, hf * 512:(hf + 1) * 512], in0=po[hf][:], in1=gtm[:, hf * 512:(hf + 1) * 512], op=ALU.mult)
                P.I("vector", "tensor_tensor", out=x1[:], in0=tmo[:], in1=xt[:], op=ALU.add)
                P.dma("sync", x1s[tt:tt + 128, :], x1[:])
                sm = sm_r.next()
                P.I("scalar", "activation", out=junk[:], in_=x1[:], func=AF.Square, accum_out=sm[:, 0:1])
                P.I("scalar", "activation", out=sm[:, 1:2], in_=sm[:, 0:1], func=AF.Ln, bias=epsc[:, 0:1], scale=1.0 / D)
                P.I("scalar", "activation", out=sm[:, 2:3], in_=sm[:, 1:2], func=AF.Exp, scale=-0.5)
                tm2 = tmo_r.next()
                h2f = h2f_r.next()
                P.I("vector", "scalar_tensor_tensor", out=tm2[:], in0=x1[:], scalar=sm[:, 2:3], in1=gf, op0=ALU.mult, op1=ALU.mult)
                P.I("vector", "tensor_tensor", out=h2f[:], in0=tm2[:], in1=shf, op=ALU.add)
                hhi = hsp_r.next()
                hlo = hsp_r.next()
                P.I("scalar", "copy", out=hhi[:], in_=h2f[:])
                P.I("vector", "tensor_tensor", out=hlo[:], in0=h2f[:], in1=hhi[:], op=ALU.subtract)
                b4 = bank[4][:].bitcast(BF16)
                b5 = bank[5][:].bitcast(BF16)
                for k in range(8):
                    P.I("tensor", "transpose", out=b4[:, k * 128:(k + 1) * 128], in_=hhi[:, k * 128:(k + 1) * 128], identity=idb[:])
                for k in range(8):
                    P.I("tensor", "transpose", out=b5[:, k * 128:(k + 1) * 128], in_=hlo[:, k * 128:(k + 1) * 128], identity=idb[:])
                P.I("scalar", "copy", out=h2T[:, :, si * 128:(si + 1) * 128], in_=b4.rearrange("p (k t) -> p k t", k=8))
                P.I("vector", "tensor_copy", out=h2Tlo[:], in_=b5.rearrange("p (k t) -> p k t", k=8))
                pl = bank[6]
                for k in range(8):
                    P.I("tensor", "matmul", out=pl[:, 0:36], lhsT=h2T[:, k, si * 128:(si + 1) * 128], rhs=wr_hi[:, k, :], start=(k == 0), stop=False)
                for k in range(8):
                    P.I("tensor", "matmul", out=pl[:, 0:36], lhsT=h2Tlo[:, k, :], rhs=wr_hi[:, k, :], start=False, stop=False)
                for k in range(8):
                    P.I("tensor", "matmul", out=pl[:, 0:36], lhsT=h2T[:, k, si * 128:(si + 1) * 128], rhs=wr_lo[:, k, :], start=False, stop=(k == 7))
                lg = lg_r.next()
                P.I("vector", "tensor_copy", out=lg[:], in_=pl[:, 0:36])
                rgens.append(router_math(P, lg, comb_all[:, si, :], bg_bc, be_bc, R8, R4, R1))
            live = list(rgens)
            while live:
                nxt = []
                for g_ in live:
                    try:
                        next(g_)
                        nxt.append(g_)
                    except StopIteration:
                        pass
                live = nxt
        NTL = TB // 256
        steps = [(e, tl) for e in range(NEXP) for tl in range(NTL)]
        wts = {}
        bcs = {}
        hids = {}

        def load_expert(e):
            wgu = wgu_r.next()
            wd = wd_r.next()
            P.dma("sync", wgu[:], wgu_s[e])
            P.dma("sync", wd[:], wd_s[e])
            wts[e] = (wgu, wd)

        def gu_mm(i):
            e, tl = steps[i]
            if tl == 0:
                if e not in wts:
                    load_expert(e)
            wgu, wd = wts[e]
            pg = bank[4 + 2 * (i % 2)]
            pu = bank[5 + 2 * (i % 2)]
            for fc in range(2):
                for k in range(8):
                    P.I("tensor", "matmul", out=pg[:, fc * 256:(fc + 1) * 256], lhsT=wgu[:, k, fc * 128:(fc + 1) * 128],
                        rhs=h2T[:, k, tl * 256:(tl + 1) * 256], start=(k == 0), stop=(k == 7))
                for k in range(8):
                    P.I("tensor", "matmul", out=pu[:, fc * 256:(fc + 1) * 256], lhsT=wgu[:, k, 256 + fc * 128:256 + (fc + 1) * 128],
                        rhs=h2T[:, k, tl * 256:(tl + 1) * 256], start=(k == 0), stop=(k == 7))

        def gu_ew(i):
            e, tl = steps[i]
            pg = bank[4 + 2 * (i % 2)]
            pu = bank[5 + 2 * (i % 2)]
            hid = hid_r.next()
            hids[i] = hid
            for fc in range(2):
                sg = sg_r.next()
                P.I("scalar", "activation", out=sg[:], in_=pg[:, fc * 256:(fc + 1) * 256], func=AF.Silu)
                P.I("vector", "tensor_tensor", out=hid[:, fc, :], in0=sg[:], in1=pu[:, fc * 256:(fc + 1) * 256], op=ALU.mult)

        def down(i):
            e, tl = steps[i]
            wgu, wd = wts[e]
            hid = hids.pop(i)
            for s in range(2):
                si = tl * 2 + s
                for hf in range(2):
                    py = bank[s * 2 + hf]
                    for fc in range(2):
                        P.I("tensor", "matmul", out=py[:], lhsT=hid[:, fc, s * 128:(s + 1) * 128], rhs=wd[:, fc, hf * 512:(hf + 1) * 512],
                            start=(fc == 0), stop=(fc == 1))
                    if e == 0:
                        P.I("vector", "tensor_scalar", out=yacc[:, si, hf * 512:(hf + 1) * 512], in0=py[:], scalar1=comb_all[:, si, e:e + 1],
                            scalar2=None, op0=ALU.mult)
                    else:
                        P.I("vector", "scalar_tensor_tensor", out=yacc[:, si, hf * 512:(hf + 1) * 512], in0=py[:], scalar=comb_all[:, si, e:e + 1],
                            in1=yacc[:, si, hf * 512:(hf + 1) * 512], op0=ALU.mult, op1=ALU.add)
            if tl == NTL - 1:
                wts.pop(e, None)
            if tl == 0 and e + 1 < NEXP:
                load_expert(e + 1)

        if steps:
            gu_mm(0)
            gu_ew(0)
        for i in range(len(steps)):
            if i + 1 < len(steps):
                gu_mm(i + 1)
            down(i)
            if i + 1 < len(steps):
                gu_ew(i + 1)
        for si in range(NSUB):
            tt = b0 + si * 128
            x1 = x1_r.next()
            P.dma("sync", x1[:], x1s[tt:tt + 128, :])
            tmo = tmo_r.next()
            P.I("vector", "tensor_tensor", out=tmo[:], in0=yacc[:, si, :], in1=gtf, op=ALU.mult)
            xo = xt_r.next()
            P.I("vector", "tensor_tensor", out=xo[:], in0=tmo[:], in1=x1[:], op=ALU.add)
            P.dma("sync", x_out[tt:tt + 128, :], xo[:])


RSTOP = 0


def precast_experts(P, ewg, ewu, ewd, NEXP=32):
    nc = P.nc
    wgu_s = nc.dram_tensor(P.prefix + "wgu_s", [32, 128, 8, 512], BF16).ap()
    wd_s = nc.dram_tensor(P.prefix + "wd_s", [32, 128, 2, D], BF16).ap()
    for e in range(NEXP):
        gv = ewg[e].rearrange("(k p) f -> p k f", p=128)
        uv = ewu[e].rearrange("(k p) f -> p k f", p=128)
        for k in range(8):
            P.dma("gpsimd", wgu_s[e, :, k, 0:256], gv[:, k, :])
            P.dma("gpsimd", wgu_s[e, :, k, 256:512], uv[:, k, :])
        dv = ewd[e].rearrange("(k p) n -> p k n", p=128)
        for k in range(2):
            P.dma("gpsimd", wd_s[e, :, k, :], dv[:, k, :])
    return wgu_s, wd_s


def router_math(P, lg, comb, bg_bc, be_bc, R8, R4, R1):
    V = lambda *a, **k: P.I("vector", *a, **k)
    yield
    lgg = lg[:, 0:4]
    mx = R1.next()
    V("tensor_reduce", out=mx[:], in_=lgg, axis=AX.X, op=ALU.max)
    yield
    nmx = R1.next()
    V("tensor_scalar", out=nmx[:], in0=mx[:], scalar1=-1.0, scalar2=None, op0=ALU.mult)
    yield
    eg = R4.next()
    sg = R1.next()
    P.I("scalar", "activation", out=eg[:], in_=lgg, func=AF.Exp, bias=nmx[:, 0:1], scale=1.0, accum_out=sg[:, 0:1])
    yield
    if RSTOP == 1:
        return
    rs = R1.next()
    V("reciprocal", out=rs[:], in_=sg[:])
    yield
    gp = R4.next()
    V("tensor_scalar", out=gp[:], in0=eg[:], scalar1=rs[:, 0:1], scalar2=None, op0=ALU.mult)
    yield
    sel = R4.next()
    V("tensor_tensor", out=sel[:], in0=gp[:], in1=bg_bc[:], op=ALU.add)
    yield
    m = R1.next()
    V("tensor_reduce", out=m[:], in_=sel[:], axis=AX.X, op=ALU.max)
    yield
    goh = R4.next()
    V("tensor_scalar", out=goh[:], in0=sel[:], scalar1=m[:, 0:1], scalar2=None, op0=ALU.is_equal)
    yield
    if RSTOP == 2:
        return
    gwj = R4.next()
    gw = R1.next()
    V("tensor_tensor", out=gwj[:], in0=gp[:], in1=goh[:], op=ALU.mult)
    yield
    V("tensor_reduce", out=gw[:], in_=gwj[:], axis=AX.X, op=ALU.add)
    yield
    els = R8.next()
    bes = R8.next()
    V("tensor_scalar", out=els[:], in0=lg[:, 4:12], scalar1=goh[:, 0:1], scalar2=None, op0=ALU.mult)
    yield
    V("tensor_scalar", out=bes[:], in0=be_bc[:, 0:8], scalar1=goh[:, 0:1], scalar2=None, op0=ALU.mult)
    yield
    for g in range(1, 4):
        V("scalar_tensor_tensor", out=els[:], in0=lg[:, 4 + 8 * g:12 + 8 * g], scalar=goh[:, g:g + 1], in1=els[:], op0=ALU.mult, op1=ALU.add)
        yield
        V("scalar_tensor_tensor", out=bes[:], in0=be_bc[:, 8 * g:8 * g + 8], scalar=goh[:, g:g + 1], in1=bes[:], op0=ALU.mult, op1=ALU.add)
        yield
    if RSTOP == 3:
        return
    mx8 = R1.next()
    V("tensor_reduce", out=mx8[:], in_=els[:], axis=AX.X, op=ALU.max)
    yield
    nm8 = R1.next()
    V("tensor_scalar", out=nm8[:], in0=mx8[:], scalar1=-1.0, scalar2=None, op0=ALU.mult)
    yield
    ee = R8.next()
    se = R1.next()
    P.I("scalar", "activation", out=ee[:], in_=els[:], func=AF.Exp, bias=nm8[:, 0:1], scale=1.0, accum_out=se[:, 0:1])
    yield
    rse = R1.next()
    V("reciprocal", out=rse[:], in_=se[:])
    yield
    ep = R8.next()
    V("tensor_scalar", out=ep[:], in0=ee[:], scalar1=rse[:, 0:1], scalar2=None, op0=ALU.mult)
    yield
    if RSTOP == 4:
        return
    sc = R8.next()
    V("tensor_tensor", out=sc[:], in0=ep[:], in1=bes[:], op=ALU.add)
    yield
    m1 = R1.next()
    V("tensor_reduce", out=m1[:], in_=sc[:], axis=AX.X, op=ALU.max)
    yield
    oh1 = R8.next()
    V("tensor_scalar", out=oh1[:], in0=sc[:], scalar1=m1[:, 0:1], scalar2=None, op0=ALU.is_equal)
    yield
    sc2 = R8.next()
    V("scalar_tensor_tensor", out=sc2[:], in0=oh1[:], scalar=-1e9, in1=sc[:], op0=ALU.mult, op1=ALU.add)
    yield
    m2 = R1.next()
    V("tensor_reduce", out=m2[:], in_=sc2[:], axis=AX.X, op=ALU.max)
    yield
    oh2 = R8.next()
    V("tensor_scalar", out=oh2[:], in0=sc2[:], scalar1=m2[:, 0:1], scalar2=None, op0=ALU.is_equal)
    yield
    if RSTOP == 5:
        return
    ohs = R8.next()
    V("tensor_tensor", out=ohs[:], in0=oh1[:], in1=oh2[:], op=ALU.add)
    yield
    tp = R8.next()
    V("tensor_tensor", out=tp[:], in0=ep[:], in1=ohs[:], op=ALU.mult)
    yield
    sp = R1.next()
    V("tensor_reduce", out=sp[:], in_=tp[:], axis=AX.X, op=ALU.add)
    yield
    rsp = R1.next()
    V("reciprocal", out=rsp[:], in_=sp[:])
    yield
    fac = R1.next()
    V("tensor_tensor", out=fac[:], in0=rsp[:], in1=gw[:], op=ALU.mult)
    yield
    ew = R8.next()
    V("tensor_scalar", out=ew[:], in0=tp[:], scalar1=fac[:, 0:1], scalar2=None, op0=ALU.mult)
    yield
    if RSTOP == 6:
        return
    for g in range(4):
        V("tensor_scalar", out=comb[:, g * 8:(g + 1) * 8], in0=ew[:], scalar1=goh[:, g:g + 1], scalar2=None, op0=ALU.mult)
        yield


W_SPECS = dict(
    ada_w=[2, D, 6 * D], ada_b=[2, 6 * D], norm_mix_g=[2, D], w_in=[2, D, IN_COLS], conv_w=[2, 3, 256],
    mla_q_norm_g=[2, 192], mla_w_uq=[2, 192, 384], mla_kv_norm_g=[2, 128], mla_w_ukv=[2, 128, 512],
    mla_q_qk_g=[2, 96], mla_k_qk_g=[2, 96], lru_conv_w=[2, 4, 256], lru_conv_b=[2, 256], lru_w_a=[2, 4, 64, 64],
    lru_b_a=[2, 256], lru_w_x=[2, 4, 64, 64], lru_b_x=[2, 256], lru_lambda=[2, 256], mix_norm_g=[2, D],
    w_out=[2, D, D], norm_ffn_g=[2, D], router_group_w=[2, D, 4], router_group_b=[2, 4], router_expert_w=[2, D, 32],
    router_expert_b=[2, 32], exp_w_gate=[2, 32, D, 256], exp_w_up=[2, 32, D, 256], exp_w_down=[2, 32, 256, D])


def build_fused(SS=S, NL=2, TB=1024):
    nc = new_nc()
    P = Prog(nc)
    x_in = din(nc, "x", [SS, D])
    c_in = din(nc, "c", [D])
    pos = din(nc, "positions", [SS], I32)
    W = {k: din(nc, k, shp) for k, shp in W_SPECS.items()}
    inv_ret4 = din(nc, "inv_ret4", [128, 1])
    inv_mla = din(nc, "inv_mla", [16])
    innerT = din(nc, "innerT", [4, 128, 128])
    qdecT = din(nc, "qdecT", [4, 64, 128])
    kdec = din(nc, "kdec", [4, 128, 1])
    cdec = din(nc, "cdec", [4, 64, 1])
    out = dout(nc, "out", [SS, D])

    def idr(name, shape, dt=F32):
        return nc.dram_tensor(name, list(shape), dt).ap()

    T = dict(attq=idr("i_attq", [4, 96, SS], BF16), attk=idr("i_attk", [4, 96, SS], BF16), attv=idr("i_attv", [4, SS, 64], BF16),
             retq=idr("i_retq", [256, SS], BF16), retk=idr("i_retk", [256, SS], BF16), retv=idr("i_retv", [SS, 256], BF16),
             lrux=idr("i_lrux", [256, SS]), cvx=idr("i_cvx", [256, SS]), bgate=idr("i_bgate", [256, SS]),
             sgate=idr("i_sgate", [256, SS]), ggate=idr("i_ggate", [256, SS]))
    Y = dict(co=idr("i_co", [256, SS]), ao=idr("i_ao", [256, SS]), ro=idr("i_ro", [256, SS]), lo=idr("i_lo", [256, SS]))
    x_mid = idr("i_xmid", [SS, D])
    x1s = idr("i_x1s", [SS, D])
    col = lambda ap: ap.rearrange("(c o) -> c o", o=1)
    cast = {}
    for l in range(NL):
        xs = x_in if l == 0 else x_mid
        xd = out if l == NL - 1 else x_mid
        mk = P.mark()
        P.prefix = "L%dP1_" % l
        m = dict(x_in=xs, c_in=c_in, pos_in=pos, inv_ret4=inv_ret4, inv_mla=inv_mla)
        for k in ("ada_w", "ada_b", "norm_mix_g", "w_in", "mla_q_norm_g", "mla_w_uq", "mla_kv_norm_g", "mla_w_ukv", "mla_q_qk_g", "mla_k_qk_g"):
            m[k] = W[k][l]
        m.update(T)
        phase1(P, IO(nc, m), SS)
        P.release(mk)
        for hd in range(4):
            mk = P.mark()
            P.prefix = "L%dP2h%d_" % (l, hd)
            sl = slice(hd * 64, (hd + 1) * 64)
            m = dict(aq=T["attq"][hd], ak=T["attk"][hd], av=T["attv"][hd], rq=T["retq"][sl], rk=T["retk"][sl], rv=T["retv"][:, sl],
                     lx=T["lrux"][sl], cx=T["cvx"][sl],
                     convw=W["conv_w"][l][:, sl].rearrange("k c -> c k"), lcw=W["lru_conv_w"][l][:, sl].rearrange("k c -> c k"),
                     lcb=col(W["lru_conv_b"][l][sl]), wa=W["lru_w_a"][l][hd], ba=col(W["lru_b_a"][l][sl]),
                     wx=W["lru_w_x"][l][hd], bx=col(W["lru_b_x"][l][sl]), lam=col(W["lru_lambda"][l][sl]),
                     qqk=W["mla_q_qk_g"][l], kqk=W["mla_k_qk_g"][l],
                     innerT=innerT[hd], qdecT=qdecT[hd], kdec=kdec[hd], cdec=cdec[hd],
                     ao=Y["ao"][sl], ro=Y["ro"][sl], lo=Y["lo"][sl], co=Y["co"][sl])
            if hd == 0:
                def _cast(l=l):
                    pfx = P.prefix
                    P.prefix = "L%d_" % l
                    cast[l] = precast_experts(P, W["exp_w_gate"][l], W["exp_w_up"][l], W["exp_w_down"][l])
                    P.prefix = pfx
                phase2(P, IO(nc, m), SS, after_setup=_cast)
            else:
                phase2(P, IO(nc, m), SS)
            P.release(mk)
        mk = P.mark()
        P.prefix = "L%dP3_" % l
        m = dict(x_in=xs, c_in=c_in, x_out=xd, x1s=x1s, bgate=T["bgate"], sgate=T["sgate"], ggate=T["ggate"])
        for k in ("ada_w", "ada_b", "mix_norm_g", "w_out", "norm_ffn_g", "router_group_w", "router_group_b", "router_expert_w",
                  "router_expert_b", "exp_w_gate", "exp_w_up", "exp_w_down"):
            m[k] = W[k][l]
        m.update(Y)
        m["wgu_s"], m["wd_s"] = cast[l]
        phase3(P, IO(nc, m), SS, TB, 32)
        P.release(mk)
    P.finish([out])
    P.emit()
    return nc


_NC_CACHE = {}


def _get(name, fn):
    if name not in _NC_CACHE:
        _NC_CACHE[name] = fn()
    return _NC_CACHE[name]


def _inv_freq(dim):
    return (np.float32(1.0) / (np.float32(10000.0) ** (np.arange(0, dim, 2, dtype=np.float32) / np.float32(dim)))).astype(np.float32)


def _ret_consts(hd):
    gamma = 1.0 - 2.0 ** (-5.0 - hd)
    idx = np.arange(128, dtype=np.float64)
    innerT = np.where(idx[None, :] >= idx[:, None], gamma ** np.maximum(idx[None, :] - idx[:, None], 0.0), 0.0).astype(np.float32)
    qdecT = np.ascontiguousarray(np.tile((gamma ** (idx + 1.0))[None, :], (64, 1))).astype(np.float32)
    kdec = (gamma ** (127.0 - idx)).reshape(128, 1).astype(np.float32)
    cdec = np.full((64, 1), gamma ** 128, np.float32)
    return innerT, qdecT, kdec, cdec


def kernel(**inputs):
    I = {k: np.ascontiguousarray(np.asarray(v)) for k, v in inputs.items()}
    B = I["x"].shape[0]
    nc = _get("fused", build_fused)
    rc = [_ret_consts(h) for h in range(4)]
    consts = dict(inv_ret4=np.ascontiguousarray(np.tile(_inv_freq(64), 4).reshape(128, 1)), inv_mla=_inv_freq(32),
                  innerT=np.stack([r[0] for r in rc]), qdecT=np.stack([r[1] for r in rc]),
                  kdec=np.stack([r[2] for r in rc]), cdec=np.stack([r[3] for r in rc]))
    maps = []
    for b in range(B):
        m = dict(x=np.ascontiguousarray(I["x"][b], dtype=np.float32), c=np.ascontiguousarray(I["c"][b]),
                 positions=np.ascontiguousarray(I["positions"][b]).astype(np.int32))
        for k in W_SPECS:
            m[k] = I[k]
        m.update(consts)
        maps.append(m)
    res = run_bass_kernel_spmd(nc, maps, core_ids=list(range(B))).results
    return np.stack([res[b]["out"] for b in range(B)], axis=0).astype(np.float32)
```
